# Optimizing a Trainium2 kernel written in Bass

```python
import math
import jax
import jax.numpy as jnp
from jax import lax
import numpy as np

D_MODEL = 1024
BATCH = 32
SEQ = 2048
DEPTH = 4

GRID_W = 64
CTX_LEN = 256
HEAD_DIM = 64
ROPE_THETA = 10000.0
BLOCK = 128
BRANCH_W = D_MODEL // 2
N_BRANCH = 4
WA_HEADS = BRANCH_W // HEAD_DIM
WA_KV_HEADS = 2
WA_GROUP = WA_HEADS // WA_KV_HEADS
WINDOW = 128
DIFF_HEADS = BRANCH_W // (2 * HEAD_DIM)
CONV_CH = BRANCH_W
CONV_K = 31
POOL_CH = BRANCH_W
POOL_WINDOWS = (2, 4, 8, 16)
POOL_GROUPS = len(POOL_WINDOWS)
POOL_GC = POOL_CH // POOL_GROUPS
PEER_HEADS = 8
PEER_NKEYS = 128
PEER_EXPERTS = PEER_NKEYS * PEER_NKEYS
PEER_DKEY = 256
PEER_TOPK = 16
PEER_CHUNK = 128
PEER_V_SCALE = 0.5
EPS = 1e-6
NEG_INF = -1e30

SEG_NAMES = ('a_q', 'a_k', 'a_v', 'c_q', 'c_k', 'c_v', 'b_in', 'd_in')
SEG_SIZES = (WA_HEADS * HEAD_DIM, WA_KV_HEADS * HEAD_DIM, WA_KV_HEADS * HEAD_DIM,
             2 * DIFF_HEADS * HEAD_DIM, 2 * DIFF_HEADS * HEAD_DIM, 2 * DIFF_HEADS * HEAD_DIM,
             2 * CONV_CH, POOL_CH)
SEG_START = tuple(int(s) for s in np.cumsum((0,) + SEG_SIZES[:-1]))
IN_COLS = int(sum(SEG_SIZES))
CTX_KV_SEGS = ('a_k', 'a_v', 'c_k', 'c_v')

kernel_name = 'hybrid_parallel_gated_peer_dit'


def rms_norm(x, g):
    xf = x.astype(jnp.float32)
    y = xf * lax.rsqrt(jnp.mean(xf * xf, axis=-1, keepdims=True) + EPS)
    return (y * g.astype(jnp.float32)).astype(x.dtype)


def layer_norm(x, g, b):
    xf = x.astype(jnp.float32)
    mu = jnp.mean(xf, axis=-1, keepdims=True)
    var = jnp.mean(jnp.square(xf - mu), axis=-1, keepdims=True)
    y = (xf - mu) * lax.rsqrt(var + EPS)
    return (y * g.astype(jnp.float32) + b.astype(jnp.float32)).astype(x.dtype)


def ada_modulation(cvec, w, b):
    return jnp.split(jax.nn.silu(cvec) @ w + b, 6, axis=-1)


def modulate(x, g, shift, scale):
    return rms_norm(x, g) * (1.0 + scale) + shift


def axial_rope(length):
    rows = length // GRID_W
    row = jnp.repeat(jnp.arange(rows, dtype=jnp.float32), GRID_W)
    col = jnp.tile(jnp.arange(GRID_W, dtype=jnp.float32), rows)
    n_freq = HEAD_DIM // 4
    inv_freq = ROPE_THETA ** (-jnp.arange(n_freq, dtype=jnp.float32) / n_freq)
    ang = jnp.concatenate([row[:, None] * inv_freq, col[:, None] * inv_freq], axis=-1)
    return jnp.cos(ang), jnp.sin(ang)


def apply_rope(x, cos, sin):
    half = x.shape[-1] // 2
    shape = (x.shape[1],) + (1,) * (x.ndim - 3) + (half,)
    cos = cos.reshape(shape).astype(x.dtype)
    sin = sin.reshape(shape).astype(x.dtype)
    x1, x2 = x[..., :half], x[..., half:]
    return jnp.concatenate([x1 * cos - x2 * sin, x2 * cos + x1 * sin], axis=-1)


def in_proj(h, w_in, names):
    idx = [SEG_NAMES.index(n) for n in names]
    w = jnp.concatenate([w_in[:, SEG_START[j]:SEG_START[j] + SEG_SIZES[j]] for j in idx], axis=1)
    splits = [int(s) for s in np.cumsum([SEG_SIZES[j] for j in idx])[:-1]]
    return dict(zip(names, jnp.split(h @ w, splits, axis=-1)))


def gqa_sink_attend(q, k, v, sink, valid):
    s = jnp.einsum('bqkgd,bjkd->bkgqj', q, k).astype(jnp.float32) * (HEAD_DIM ** -0.5)
    if valid is not None:
        s = jnp.where(valid, s, NEG_INF)
    hkv, grp = q.shape[2], q.shape[3]
    sink_col = jnp.broadcast_to(sink.astype(jnp.float32).reshape(hkv, grp, 1, 1), s.shape[:-1] + (1,))
    p = jax.nn.softmax(jnp.concatenate([s, sink_col], axis=-1), axis=-1)[..., :-1]
    return jnp.einsum('bkgqj,bjkd->bqkgd', p.astype(v.dtype), v)


def window_attention(q, k, v, ck, cv, sink):
    B, L = q.shape[:2]
    Lc = ck.shape[1]
    nb = L // BLOCK
    pad = ((0, 0), (BLOCK, BLOCK), (0, 0), (0, 0))
    kp, vp = jnp.pad(k, pad), jnp.pad(v, pad)
    qblocks = jnp.moveaxis(q.reshape((B, nb, BLOCK) + q.shape[2:]), 1, 0)
    offs = jnp.arange(3 * BLOCK) - BLOCK
    qi = jnp.arange(BLOCK)
    ctx_ok = jnp.ones((BLOCK, Lc), dtype=bool)

    def one_block(args):
        n, qb = args
        start = n * BLOCK
        kb = lax.dynamic_slice_in_dim(kp, start, 3 * BLOCK, axis=1)
        vb = lax.dynamic_slice_in_dim(vp, start, 3 * BLOCK, axis=1)
        kabs = start + offs
        band = ((jnp.abs(kabs[None, :] - (start + qi)[:, None]) <= WINDOW)
                & (kabs >= 0)[None, :] & (kabs < L)[None, :])
        valid = jnp.concatenate([band, ctx_ok], axis=1)
        return gqa_sink_attend(qb, jnp.concatenate([kb, ck], axis=1),
                               jnp.concatenate([vb, cv], axis=1), sink, valid)

    o = lax.map(one_block, (jnp.arange(nb), qblocks))
    return jnp.moveaxis(o, 0, 1).reshape(B, L, WA_HEADS * HEAD_DIM)


def diff_core(q, k, v, lam):
    s = jnp.einsum('bqhcd,bjhcd->bhcqj', q, k).astype(jnp.float32) * (HEAD_DIM ** -0.5)
    p = jax.nn.softmax(s, axis=-1)
    a = p[:, :, 0] - lam * p[:, :, 1]
    return jnp.einsum('bhqj,bjhe->bqhe', a.astype(v.dtype), v)


def dense_diff_attention(q, k, v, ck, cv, lam):
    B, L = q.shape[:2]
    nb = L // BLOCK
    k_all = jnp.concatenate([k, ck], axis=1)
    v_all = jnp.concatenate([v, cv], axis=1)
    qblocks = jnp.moveaxis(q.reshape((B, nb, BLOCK) + q.shape[2:]), 1, 0)
    o = lax.map(lambda qb: diff_core(qb, k_all, v_all, lam), qblocks)
    return jnp.moveaxis(o, 0, 1).reshape(B, L, DIFF_HEADS, 2 * HEAD_DIM)


def diff_output(o, g, lam_init):
    B, L = o.shape[:2]
    return (rms_norm(o, g) * (1.0 - lam_init)).reshape(B, L, DIFF_HEADS * 2 * HEAD_DIM)


def conformer_conv(u, w, b, ln_g, ln_b):
    a, gt = jnp.split(u, 2, axis=-1)
    y = a * jax.nn.sigmoid(gt)
    y = lax.conv_general_dilated(y, w[:, None, :], window_strides=(1,),
                                 padding=((CONV_K // 2, CONV_K // 2),),
                                 dimension_numbers=('NWC', 'WIO', 'NWC'),
                                 feature_group_count=CONV_CH) + b
    return jax.nn.silu(layer_norm(y, ln_g, ln_b))


def pool_mixer(u, w_grp, scale):
    B, L, _ = u.shape
    ug = u.reshape(B, L, POOL_GROUPS, POOL_GC)
    t = jnp.arange(L)
    outs = []
    for g, win in enumerate(POOL_WINDOWS):
        lo, hi = win // 2, win - 1 - win // 2
        xg = ug[:, :, g].astype(jnp.float32)
        cs = lax.cumsum(jnp.pad(xg, ((0, 0), (1 + lo, hi), (0, 0))), axis=1)
        win_sum = cs[:, win:] - cs[:, :L]
        cnt = (jnp.minimum(t + hi, L - 1) - jnp.maximum(t - lo, 0) + 1).astype(jnp.float32)
        outs.append(win_sum / cnt[:, None] - xg)
    y = jnp.stack(outs, axis=2).astype(u.dtype)
    y = jnp.einsum('blgc,gce->blge', y, w_grp)
    return y.reshape(B, L, POOL_CH) * scale


def merge_branches(h, ys, w_gate, b_gate, w_branch, w_out):
    acc = jax.nn.sigmoid(h @ w_gate[0] + b_gate[0]) * (ys[0] @ w_branch[0])
    for n in range(1, N_BRANCH):
        acc = acc + jax.nn.sigmoid(h @ w_gate[n] + b_gate[n]) * (ys[n] @ w_branch[n])
    return acc @ w_out


def token_mixer(h, hc, w_in, w_gate, b_gate, sink, lam, lam_init, diff_g, conv_w, conv_b,
                ln_g, ln_b, pool_w, pool_scale, w_branch, w_out, ctx_out):
    B, L, _ = h.shape
    Lc = hc.shape[1]
    cos, sin = axial_rope(L)
    s = in_proj(h, w_in, SEG_NAMES)
    sc = in_proj(hc, w_in, SEG_NAMES if ctx_out else CTX_KV_SEGS)
    cka = sc['a_k'].reshape(B, Lc, WA_KV_HEADS, HEAD_DIM)
    cva = sc['a_v'].reshape(B, Lc, WA_KV_HEADS, HEAD_DIM)
    ckc = sc['c_k'].reshape(B, Lc, DIFF_HEADS, 2, HEAD_DIM)
    cvc = sc['c_v'].reshape(B, Lc, DIFF_HEADS, 2 * HEAD_DIM)
    qa = apply_rope(s['a_q'].reshape(B, L, WA_KV_HEADS, WA_GROUP, HEAD_DIM), cos, sin)
    ka = apply_rope(s['a_k'].reshape(B, L, WA_KV_HEADS, HEAD_DIM), cos, sin)
    va = s['a_v'].reshape(B, L, WA_KV_HEADS, HEAD_DIM)
    y_a = window_attention(qa, ka, va, cka, cva, sink)
    qc = apply_rope(s['c_q'].reshape(B, L, DIFF_HEADS, 2, HEAD_DIM), cos, sin)
    kc = apply_rope(s['c_k'].reshape(B, L, DIFF_HEADS, 2, HEAD_DIM), cos, sin)
    vc = s['c_v'].reshape(B, L, DIFF_HEADS, 2 * HEAD_DIM)
    y_c = diff_output(dense_diff_attention(qc, kc, vc, ckc, cvc, lam), diff_g, lam_init)
    y_b = conformer_conv(s['b_in'], conv_w, conv_b, ln_g, ln_b)
    y_d = pool_mixer(s['d_in'], pool_w, pool_scale)
    out = merge_branches(h, (y_a, y_c, y_b, y_d), w_gate, b_gate, w_branch, w_out)
    if not ctx_out:
        return out, None
    cqa = sc['a_q'].reshape(B, Lc, WA_KV_HEADS, WA_GROUP, HEAD_DIM)
    yc_a = gqa_sink_attend(cqa, cka, cva, sink, None).reshape(B, Lc, WA_HEADS * HEAD_DIM)
    cqc = sc['c_q'].reshape(B, Lc, DIFF_HEADS, 2, HEAD_DIM)
    yc_c = diff_output(diff_core(cqc, ckc, cvc, lam), diff_g, lam_init)
    yc_b = conformer_conv(sc['b_in'], conv_w, conv_b, ln_g, ln_b)
    yc_d = pool_mixer(sc['d_in'], pool_w, pool_scale)
    out_c = merge_branches(hc, (yc_a, yc_c, yc_b, yc_d), w_gate, b_gate, w_branch, w_out)
    return out, out_c


def peer_ffn(h, wq, k1, k2, u_tab, v_tab):
    B, L, D = h.shape
    hc = h.reshape(B * L // PEER_CHUNK, PEER_CHUNK, D)

    def chunk(xc):
        q = (xc @ wq).reshape(PEER_CHUNK, PEER_HEADS, 2, PEER_DKEY // 2)
        s1 = jnp.einsum('thd,hnd->thn', q[:, :, 0], k1)
        s2 = jnp.einsum('thd,hnd->thn', q[:, :, 1], k2)
        v1, i1 = lax.top_k(s1, PEER_TOPK)
        v2, i2 = lax.top_k(s2, PEER_TOPK)
        cand = (v1[..., :, None] + v2[..., None, :]).reshape(PEER_CHUNK, PEER_HEADS, PEER_TOPK * PEER_TOPK)
        sc, ci = lax.top_k(cand, PEER_TOPK)
        idx = (jnp.take_along_axis(i1, ci // PEER_TOPK, axis=-1) * PEER_NKEYS
               + jnp.take_along_axis(i2, ci % PEER_TOPK, axis=-1))
        gate = jax.nn.softmax(sc.astype(jnp.float32), axis=-1).astype(xc.dtype)
        u = jnp.take(u_tab, idx, axis=0)
        act = jax.nn.gelu(jnp.einsum('td,thkd->thk', xc, u), approximate=False)
        v = jnp.take(v_tab, idx, axis=0)
        return jnp.einsum('thk,thkd->td', gate * act, v)

    return lax.map(chunk, hc).reshape(B, L, D)


def setup_inputs(seed: int = 0) -> dict:
    key = jax.random.key(seed)
    ks = jax.random.split(key, 40)
    D = D_MODEL

    def nrm(k, shape, scale):
        return jax.random.normal(k, shape, dtype=jnp.float32) * scale

    return {
        'x': nrm(ks[0], (BATCH, SEQ, D), 1.0),
        'c': nrm(ks[1], (BATCH, D), 1.0),
        'ctx': nrm(ks[2], (BATCH, CTX_LEN, D), 1.0),
        'c_ctx': nrm(ks[3], (D,), 1.0),
        'w_mod': nrm(ks[4], (DEPTH, D, 6 * D), 0.5 * D ** -0.5),
        'b_mod': nrm(ks[5], (DEPTH, 6 * D), 0.02),
        'norm1_g': 1.0 + nrm(ks[6], (DEPTH, D), 0.02),
        'norm2_g': 1.0 + nrm(ks[7], (DEPTH, D), 0.02),
        'w_in': nrm(ks[8], (DEPTH, D, IN_COLS), D ** -0.5),
        'w_gate': nrm(ks[9], (DEPTH, N_BRANCH, D, D), D ** -0.5),
        'b_gate': nrm(ks[10], (DEPTH, N_BRANCH, D), 0.02),
        'attn_sink': nrm(ks[11], (DEPTH, WA_HEADS), 0.5),
        'lam_q1': nrm(ks[12], (DEPTH, HEAD_DIM), 0.1),
        'lam_k1': nrm(ks[13], (DEPTH, HEAD_DIM), 0.1),
        'lam_q2': nrm(ks[14], (DEPTH, HEAD_DIM), 0.1),
        'lam_k2': nrm(ks[15], (DEPTH, HEAD_DIM), 0.1),
        'diff_norm_g': 1.0 + nrm(ks[16], (DEPTH, 2 * HEAD_DIM), 0.02),
        'conv_w': nrm(ks[17], (DEPTH, CONV_K, CONV_CH), CONV_K ** -0.5),
        'conv_b': nrm(ks[18], (DEPTH, CONV_CH), 0.02),
        'conv_ln_g': 1.0 + nrm(ks[19], (DEPTH, CONV_CH), 0.02),
        'conv_ln_b': nrm(ks[20], (DEPTH, CONV_CH), 0.02),
        'pool_w': nrm(ks[21], (DEPTH, POOL_GROUPS, POOL_GC, POOL_GC), POOL_GC ** -0.5),
        'pool_scale': 1.0 + nrm(ks[22], (DEPTH, POOL_CH), 0.1),
        'w_branch': nrm(ks[23], (DEPTH, N_BRANCH, BRANCH_W, D), BRANCH_W ** -0.5),
        'w_out': nrm(ks[24], (DEPTH, D, D), D ** -0.5),
        'peer_wq': nrm(ks[25], (DEPTH, D, PEER_HEADS * PEER_DKEY), D ** -0.5),
        'peer_k1': nrm(ks[26], (DEPTH, PEER_HEADS, PEER_NKEYS, PEER_DKEY // 2), (PEER_DKEY // 2) ** -0.5),
        'peer_k2': nrm(ks[27], (DEPTH, PEER_HEADS, PEER_NKEYS, PEER_DKEY // 2), (PEER_DKEY // 2) ** -0.5),
        'peer_u': nrm(ks[28], (DEPTH, PEER_EXPERTS, D), D ** -0.5),
        'peer_v': nrm(ks[29], (DEPTH, PEER_EXPERTS, D), PEER_V_SCALE),
        'final_g': 1.0 + nrm(ks[30], (D,), 0.02),
    }


def reference(x, c, ctx, c_ctx, w_mod, b_mod, norm1_g, norm2_g, w_in, w_gate, b_gate, attn_sink,
              lam_q1, lam_k1, lam_q2, lam_k2, diff_norm_g, conv_w, conv_b, conv_ln_g, conv_ln_b,
              pool_w, pool_scale, w_branch, w_out, peer_wq, peer_k1, peer_k2, peer_u, peer_v, final_g):
    xc = ctx
    c_lat = c[:, None, :]
    c_con = c_ctx[None, None, :]
    for i in range(DEPTH):
        ctx_out = i < DEPTH - 1
        lam_init = 0.8 - 0.6 * math.exp(-0.3 * i)
        lam = (jnp.exp(jnp.sum(lam_q1[i].astype(jnp.float32) * lam_k1[i].astype(jnp.float32)))
               - jnp.exp(jnp.sum(lam_q2[i].astype(jnp.float32) * lam_k2[i].astype(jnp.float32)))
               + lam_init)
        sh1, sc1, g1, sh2, sc2, g2 = ada_modulation(c_lat, w_mod[i], b_mod[i])
        csh1, csc1, cg1, csh2, csc2, cg2 = ada_modulation(c_con, w_mod[i], b_mod[i])
        h = modulate(x, norm1_g[i], sh1, sc1)
        hc = modulate(xc, norm1_g[i], csh1, csc1)
        y, yc = token_mixer(h, hc, w_in[i], w_gate[i], b_gate[i], attn_sink[i], lam, lam_init,
                            diff_norm_g[i], conv_w[i], conv_b[i], conv_ln_g[i], conv_ln_b[i],
                            pool_w[i], pool_scale[i], w_branch[i], w_out[i], ctx_out)
        x = x + g1 * y
        h2 = modulate(x, norm2_g[i], sh2, sc2)
        x = x + g2 * peer_ffn(h2, peer_wq[i], peer_k1[i], peer_k2[i], peer_u[i], peer_v[i])
        if ctx_out:
            xc = xc + cg1 * yc
            hc2 = modulate(xc, norm2_g[i], csh2, csc2)
            xc = xc + cg2 * peer_ffn(hc2, peer_wq[i], peer_k1[i], peer_k2[i], peer_u[i], peer_v[i])
    return rms_norm(x, final_g)
```

```python
import math
import contextlib
import numpy as np
import concourse.bass as bass
import concourse.mybir as mybir
from concourse.bass_utils import run_bass_kernel_spmd

F32 = mybir.dt.float32
BF16 = mybir.dt.bfloat16
I32 = mybir.dt.int32
U32 = mybir.dt.uint32
AF = mybir.ActivationFunctionType
ALU = mybir.AluOpType
AX = mybir.AxisListType

D = 1024
L = 2048
LC = 256
T = L + LC
NL = 4
NBC = 4
EPS = 1e-6
NE = 16384
CHUNKS = [(0, 512), (512, 512), (1024, 512), (1536, 512), (2048, 256)]
SEG = dict(a_q=0, a_k=512, a_v=640, c_q=768, c_k=1280, c_v=1792, b_in=2304, d_in=3328)
POOLW = (2, 4, 8, 16)


class Res:
    __slots__ = ("w", "r")

    def __init__(self):
        self.w = None
        self.r = {}


class SY:
    def __init__(self, nc, es):
        self.nc = nc
        self.E = dict(pe=nc.tensor, act=nc.scalar, dve=nc.vector, pool=nc.gpsimd, sp=nc.sync)
        self.csem = {e: es.enter_context(nc.semaphore("c_" + e)) for e in ("pe", "act", "dve", "pool")}
        self.ccnt = {e: 0 for e in self.csem}
        self.Q = dict(sp="sp", pq="pool", aq="act")
        self.R = dict(sp=8, pq=8, aq=4)
        self.dsem = {q: [es.enter_context(nc.semaphore(f"d_{q}{i}")) for i in range(self.R[q])] for q in self.Q}
        self.dval = {q: [0] * self.R[q] for q in self.Q}
        self.dn = {q: 0 for q in self.Q}
        self.seen = {e: {} for e in self.E}
        self.ninst = 0

    def _wait(self, eng, t):
        sem, val = t[0], t[1]
        k = id(sem)
        if self.seen[eng].get(k, 0) >= val:
            return
        self.seen[eng][k] = val
        self.E[eng].wait_ge(sem, val)

    def _deps(self, eng, is_dma, reads, writes):
        for r in reads:
            if r.w is not None:
                self._wait(eng, r.w)
        for w in writes:
            if w.w is not None and (is_dma or w.w[3] or w.w[2] != eng):
                self._wait(eng, w.w)
            for t in w.r.values():
                if is_dma or t[3] or t[2] != eng:
                    self._wait(eng, t)

    def _commit(self, t, reads, writes):
        for r in reads:
            r.r[id(t[0])] = t
        for w in writes:
            w.w = t
            w.r = {}

    def op(self, eng, fn, reads=(), writes=()):
        self._deps(eng, False, reads, writes)
        ins = fn()
        self.ccnt[eng] += 1
        ins.then_inc(self.csem[eng], 1)
        self._commit((self.csem[eng], self.ccnt[eng], eng, False), reads, writes)
        self.ninst += 1

    def dma(self, q, out, in_, reads=(), writes=(), **kw):
        eng = self.Q[q]
        self._deps(eng, True, reads, writes)
        i = self.dn[q] % self.R[q]
        self.dn[q] += 1
        sem = self.dsem[q][i]
        if self.dval[q][i] > 0:
            self._wait(eng, (sem, self.dval[q][i]))
        self.dval[q][i] += 16
        self.E[eng].dma_start(out=out, in_=in_, **kw).then_inc(sem, 16)
        self._commit((sem, self.dval[q][i], eng, True), reads, writes)
        self.ninst += 1

    def all_tickets(self):
        ts = [(self.csem[e], self.ccnt[e]) for e in self.csem if self.ccnt[e] > 0]
        for q in self.Q:
            for i in range(self.R[q]):
                if self.dval[q][i] > 0:
                    ts.append((self.dsem[q][i], self.dval[q][i]))
        return ts

    def barrier(self, engs=("pe", "act", "dve", "pool", "sp")):
        ts = self.all_tickets()
        for e in engs:
            for t in ts:
                self._wait(e, t)


class Tile:
    def __init__(self, t, nres=1):
        self.t = t
        self.res = [Res() for _ in range(nres)]

    @property
    def r(self):
        return self.res[0]


def build_program(cfg):
    nb_run = cfg.get("nb", NBC)
    nl_run = cfg.get("nl", NL)
    taps = cfg.get("taps", ())
    do_peer = cfg.get("peer", True)

    NLA = cfg.get("nl_alloc", NL)
    nc = bass.Bass("TRN2", target_bir_lowering=False)
    es = contextlib.ExitStack()
    es.__enter__()
    sy = SY(nc, es)

    def din(name, shape):
        return nc.dram_tensor(name, list(shape), F32, kind="ExternalInput").ap()

    x_in = din("x", [NBC, L, D])
    c_in = din("c", [NBC, D])
    ctx_in = din("ctx", [NBC, LC, D])
    c_ctx = din("c_ctx", [D])
    w_mod = din("w_mod", [NLA, D, 6 * D])
    b_mod = din("b_mod", [NLA, 6 * D])
    norm1_g = din("norm1_g", [NLA, D])
    norm2_g = din("norm2_g", [NLA, D])
    w_in = din("w_in", [NLA, D, 3840])
    w_gate = din("w_gate", [NLA, 4, D, D])
    b_gate = din("b_gate", [NLA, 4, D])
    attn_sink = din("attn_sink", [NLA, 8])
    lam_q1 = din("lam_q1", [NLA, 64])
    lam_k1 = din("lam_k1", [NLA, 64])
    lam_q2 = din("lam_q2", [NLA, 64])
    lam_k2 = din("lam_k2", [NLA, 64])
    diff_norm_g = din("diff_norm_g", [NLA, 128])
    conv_w = din("conv_w", [NLA, 31, 512])
    conv_b = din("conv_b", [NLA, 512])
    conv_ln_g = din("conv_ln_g", [NLA, 512])
    conv_ln_b = din("conv_ln_b", [NLA, 512])
    pool_w = din("pool_w", [NLA, 4, 128, 128])
    pool_scale = din("pool_scale", [NLA, 512])
    w_branch = din("w_branch", [NLA, 4, 512, D])
    w_out = din("w_out", [NLA, D, D])
    peer_wq = din("peer_wq", [NLA, D, 2048])
    peer_k1 = din("peer_k1", [NLA, 8, 128, 128])
    peer_k2 = din("peer_k2", [NLA, 8, 128, 128])
    peer_u = din("peer_u", [NLA, NE, D])
    peer_v = din("peer_v", [NLA, NE, D])
    final_g = din("final_g", [D])
    out_d = nc.dram_tensor("out", [NBC, L, D], F32, kind="ExternalOutput").ap()

    xT_d = nc.dram_tensor("xT_scr", [NBC, 128, 8, T], F32, kind="Internal").ap()
    xT_res = [[Res() for _ in CHUNKS] for _ in range(NBC)]
    yT_d = nc.dram_tensor("yT_scr", [4, 128, 4, T], BF16, kind="Internal").ap()
    yT_res = [[Res() for _ in CHUNKS] for _ in range(4)]
    tap_out = {}

    def sb(name, shape, dt, nres=1):
        return Tile(es.enter_context(nc.sbuf_tensor(name, list(shape), dt)), nres)

    def tap(name, ap, shape, dt, reads):
        if name not in taps:
            return
        d = nc.dram_tensor("tap_" + name, list(shape), dt, kind="ExternalOutput").ap()
        tap_out[name] = d
        r = Res()
        sy.dma("sp", d, ap, reads=reads, writes=[r])
        tap_res.append(r)

    tap_res = []

    PS = [Tile(es.enter_context(nc.psum_tensor(f"ps{i}", [128, 512], F32))) for i in range(8)]

    ident = sb("ident", [128, 128], F32)
    identb = sb("identb", [128, 128], BF16)
    maskGE = sb("maskGE", [128, 128], BF16)
    maskLE = sb("maskLE", [128, 128], BF16)
    onesD = sb("onesD", [128, 128], F32)
    ones5 = sb("ones5", [128, 128], F32)
    ones1 = sb("ones1", [128, 128], F32)
    onesb = sb("onesb", [128, 128], BF16)
    iof = sb("iof", [128, 128], F32)
    itmp = sb("itmp", [128, 128], I32)
    sy.op("pool", lambda: nc.gpsimd.iota(itmp.t[:], pattern=[[1, 128]], base=0, channel_multiplier=-1), writes=[itmp.r])
    sy.op("dve", lambda: nc.vector.tensor_single_scalar(out=ident.t[:], in_=itmp.t[:], scalar=0, op=ALU.is_equal), reads=[itmp.r], writes=[ident.r])
    sy.op("dve", lambda: nc.vector.tensor_single_scalar(out=identb.t[:], in_=itmp.t[:], scalar=0, op=ALU.is_equal), reads=[itmp.r], writes=[identb.r])
    sy.op("dve", lambda: nc.vector.tensor_single_scalar(out=maskGE.t[:], in_=itmp.t[:], scalar=0, op=ALU.is_le), reads=[itmp.r], writes=[maskGE.r])
    sy.op("dve", lambda: nc.vector.tensor_single_scalar(out=maskLE.t[:], in_=itmp.t[:], scalar=0, op=ALU.is_ge), reads=[itmp.r], writes=[maskLE.r])
    itmp2 = sb("itmp2", [128, 128], I32)
    sy.op("pool", lambda: nc.gpsimd.iota(itmp2.t[:], pattern=[[1, 128]], base=0, channel_multiplier=0), writes=[itmp2.r])
    sy.op("dve", lambda: nc.vector.tensor_copy(out=iof.t[:], in_=itmp2.t[:]), reads=[itmp2.r], writes=[iof.r])
    sy.op("pool", lambda: nc.gpsimd.memset(onesD.t[:], 1.0 / 1024), writes=[onesD.r])
    sy.op("pool", lambda: nc.gpsimd.memset(ones5.t[:], 1.0 / 512), writes=[ones5.r])
    sy.op("pool", lambda: nc.gpsimd.memset(ones1.t[:], 1.0 / 128), writes=[ones1.r])
    sy.op("pool", lambda: nc.gpsimd.memset(onesb.t[:], 1.0), writes=[onesb.r])

    NVT = NL * 240 + 48
    VT = sb("VT", [128, NVT], F32)
    vt_off = {}

    def vt(name, l=0, i=0):
        return VT.t[:, vt_off[(name, l)] + i: vt_off[(name, l)] + i + 1]

    def vtr(name, l, i0, n):
        return VT.t[:, vt_off[(name, l)] + i0: vt_off[(name, l)] + i0 + n]

    stg = [sb(f"stg{i}", [128, 128], F32) for i in range(2)]
    col = 0
    nstage = 0

    def stage_transpose(items):
        nonlocal col, nstage
        st = stg[nstage % 2]
        ps = PS[nstage % 2]
        nstage += 1
        row = 0
        for (name, l, ap) in items:
            R_ = ap.shape[0]
            sy.dma("sp", st.t[row:row + R_, :], ap, writes=[st.r])
            vt_off[(name, l)] = col + row
            row += R_
        sy.op("pe", lambda: nc.tensor.transpose(out=ps.t[:, 0:row], in_=st.t[0:row, :], identity=ident.t[0:row, 0:row]),
              reads=[st.r, ident.r], writes=[ps.r])
        c0 = col
        sy.op("act", lambda: nc.scalar.copy(out=VT.t[:, c0:c0 + row], in_=ps.t[:, 0:row]), reads=[ps.r], writes=[VT.r])
        col += row

    def v2(ap1d):
        return ap1d.rearrange("(r p) -> r p", p=128)

    for l in range(NLA):
        stage_transpose([
            ("n1g", l, v2(norm1_g[l])), ("n2g", l, v2(norm2_g[l])),
            ("bg", l, b_gate[l].rearrange("n (r p) -> (n r) p", p=128)),
            ("cb", l, v2(conv_b[l])), ("lng", l, v2(conv_ln_g[l])), ("lnb", l, v2(conv_ln_b[l])),
            ("psc", l, v2(pool_scale[l])), ("dg", l, v2(diff_norm_g[l])), ("bmod", l, v2(b_mod[l])),
        ])
        stage_transpose([("cw", l, conv_w[l].rearrange("k (r p) -> (k r) p", p=128))])
    stage_transpose([("fg", 0, v2(final_g)), ("cctx", 0, v2(c_ctx)), ("cb4", 0, c_in.rearrange("b (r p) -> (b r) p", p=128))])
    assert col <= NVT, col

    scT = sb("scT", [128, 8, 5], F32)
    sy.op("act", lambda: nc.scalar.activation(out=scT.t[:, :, 0:4].rearrange("p k j -> p j k"), in_=vtr("cb4", 0, 0, 32).rearrange("p (j k) -> p j k", k=8), func=AF.Silu),
          reads=[VT.r], writes=[scT.r])
    sy.op("act", lambda: nc.scalar.activation(out=scT.t[:, :, 4], in_=vtr("cctx", 0, 0, 8), func=AF.Silu), reads=[VT.r], writes=[scT.r])

    MOD = sb("MOD", [128, NL, 6, 8, 5], F32)
    AM = sb("AM", [128, NL, 2, 8, 5], F32)
    with contextlib.ExitStack() as es0:
        wm = [Tile(es0.enter_context(nc.sbuf_tensor(f"wm{i}", [128, 8, 1024], F32))) for i in range(2)]
        k = 0
        for l in range(nl_run):
            for v in range(6):
                w = wm[k % 2]
                ps = PS[2 + k % 2]
                k += 1
                sy.dma("sp", w.t[:], w_mod[l].rearrange("(kc p) n -> p kc n", p=128)[:, :, v * 1024:(v + 1) * 1024], writes=[w.r])
                for oc in range(8):
                    for kc in range(8):
                        sy.op("pe", lambda: nc.tensor.matmul(ps.t[:, oc * 5:(oc + 1) * 5], w.t[:, kc, oc * 128:(oc + 1) * 128], scT.t[:, kc, :], start=(kc == 0), stop=(kc == 7)),
                              reads=[w.r, scT.r], writes=[ps.r])
                sy.op("dve", lambda: nc.vector.tensor_tensor(out=MOD.t[:, l, v, :, :], in0=ps.t[:, 0:40].rearrange("p (o j) -> p o j", j=5),
                                                             in1=vtr("bmod", l, v * 8, 8).unsqueeze(2).to_broadcast([128, 8, 5]), op=ALU.add),
                      reads=[ps.r, VT.r], writes=[MOD.r])
            for wi, (v, gname) in enumerate(((1, "n1g"), (4, "n2g"))):
                sy.op("dve", lambda: nc.vector.tensor_scalar(out=AM.t[:, l, wi, :, :], in0=MOD.t[:, l, v, :, :], scalar1=1.0, scalar2=None, op0=ALU.add),
                      reads=[MOD.r], writes=[AM.r])
                sy.op("dve", lambda: nc.vector.tensor_tensor(out=AM.t[:, l, wi, :, :], in0=AM.t[:, l, wi, :, :],
                                                             in1=vtr(gname, l, 0, 8).unsqueeze(2).to_broadcast([128, 8, 5]), op=ALU.mult),
                      reads=[AM.r, VT.r], writes=[AM.r])
        sy.barrier()
    tap("MOD", MOD.t[:], [128, NL, 6, 8, 5], F32, [MOD.r])
    tap("AM", AM.t[:], [128, NL, 2, 8, 5], F32, [AM.r])

    with contextlib.ExitStack() as es0:
        xin = [Tile(es0.enter_context(nc.sbuf_tensor(f"xin{i}", [128, D], F32))) for i in range(2)]
        xo = [Tile(es0.enter_context(nc.sbuf_tensor(f"xo{i}", [128, 8, 512], F32))) for i in range(2)]
        k = 0
        for b in range(nb_run):
            for ci, (t0, n) in enumerate(CHUNKS):
                o = xo[ci % 2]
                for tt in range(n // 128):
                    xi = xin[k % 2]
                    src = x_in[b, t0 + tt * 128:t0 + (tt + 1) * 128, :] if t0 < L else ctx_in[b, tt * 128:(tt + 1) * 128, :]
                    sy.dma("sp", xi.t[:], src, writes=[xi.r])
                    for half in range(2):
                        ps = PS[(2 * k + half) % 4]
                        for c4 in range(4):
                            c = half * 4 + c4
                            sy.op("pe", lambda: nc.tensor.transpose(out=ps.t[:, c4 * 128:(c4 + 1) * 128], in_=xi.t[:, c * 128:(c + 1) * 128], identity=ident.t[:]),
                                  reads=[xi.r, ident.r], writes=[ps.r])
                        eng = "act" if half == 0 else "dve"
                        dst = o.t[:, half * 4:(half + 1) * 4, tt * 128:(tt + 1) * 128]
                        srcp = ps.t[:, :].rearrange("p (c t) -> p c t", t=128)
                        if eng == "act":
                            sy.op("act", lambda: nc.scalar.copy(out=dst, in_=srcp), reads=[ps.r], writes=[o.r])
                        else:
                            sy.op("dve", lambda: nc.vector.tensor_copy(out=dst, in_=srcp), reads=[ps.r], writes=[o.r])
                    k += 1
                sy.dma("sp", xT_d[b, :, :, t0:t0 + n], o.t[:, :, 0:n], reads=[o.r], writes=[xT_res[b][ci]])
        sy.barrier()

    cs_d = nc.dram_tensor("cs_scr", [2, 128, T], F32, kind="Internal").ap()
    cs_res = Res()
    with contextlib.ExitStack() as es0:
        cosF = Tile(es0.enter_context(nc.sbuf_tensor("cosF", [128, T], F32)))
        sinS = Tile(es0.enter_context(nc.sbuf_tensor("sinS", [128, T], F32)))
        pidx = Tile(es0.enter_context(nc.sbuf_tensor("pidx", [128, 1], I32)))
        pf = Tile(es0.enter_context(nc.sbuf_tensor("pf", [128, 8], F32)))
        rowt = Tile(es0.enter_context(nc.sbuf_tensor("rowt", [128, L], I32)))
        colt = Tile(es0.enter_context(nc.sbuf_tensor("colt", [128, L], I32)))
        rowf = Tile(es0.enter_context(nc.sbuf_tensor("rowf", [128, L], F32)))
        colf = Tile(es0.enter_context(nc.sbuf_tensor("colf", [128, L], F32)))
        sy.op("pool", lambda: nc.gpsimd.iota(pidx.t[:], pattern=[[0, 1]], base=0, channel_multiplier=1), writes=[pidx.r])
        sy.op("pool", lambda: nc.gpsimd.iota(rowt.t[:], pattern=[[1, 32], [0, 64]], base=0, channel_multiplier=0), writes=[rowt.r])
        sy.op("pool", lambda: nc.gpsimd.iota(colt.t[:], pattern=[[0, 32], [1, 64]], base=0, channel_multiplier=0), writes=[colt.r])
        sy.op("dve", lambda: nc.vector.tensor_copy(out=rowf.t[:], in_=rowt.t[:]), reads=[rowt.r], writes=[rowf.r])
        sy.op("dve", lambda: nc.vector.tensor_copy(out=colf.t[:], in_=colt.t[:]), reads=[colt.r], writes=[colf.r])
        sy.op("dve", lambda: nc.vector.tensor_copy(out=pf.t[:, 0:1], in_=pidx.t[:]), reads=[pidx.r], writes=[pf.r])
        def dv(fn, reads, writes):
            sy.op("dve", fn, reads=reads, writes=writes)
        dv(lambda: nc.vector.tensor_single_scalar(out=pf.t[:, 7:8], in_=pf.t[:, 0:1], scalar=32.0, op=ALU.is_ge), [pf.r], [pf.r])
        dv(lambda: nc.vector.tensor_single_scalar(out=pf.t[:, 6:7], in_=pf.t[:, 0:1], scalar=64.0, op=ALU.is_ge), [pf.r], [pf.r])
        dv(lambda: nc.vector.tensor_tensor(out=pf.t[:, 7:8], in0=pf.t[:, 7:8], in1=pf.t[:, 6:7], op=ALU.add), [pf.r], [pf.r])
        dv(lambda: nc.vector.tensor_single_scalar(out=pf.t[:, 1:2], in_=pf.t[:, 0:1], scalar=96.0, op=ALU.is_ge), [pf.r], [pf.r])
        dv(lambda: nc.vector.tensor_tensor(out=pf.t[:, 7:8], in0=pf.t[:, 7:8], in1=pf.t[:, 1:2], op=ALU.add), [pf.r], [pf.r])
        dv(lambda: nc.vector.scalar_tensor_tensor(out=pf.t[:, 1:2], in0=pf.t[:, 7:8], scalar=-32.0, in1=pf.t[:, 0:1], op0=ALU.mult, op1=ALU.add), [pf.r], [pf.r])
        dv(lambda: nc.vector.tensor_single_scalar(out=pf.t[:, 3:4], in_=pf.t[:, 1:2], scalar=16.0, op=ALU.is_lt), [pf.r], [pf.r])
        dv(lambda: nc.vector.tensor_single_scalar(out=pf.t[:, 7:8], in_=pf.t[:, 1:2], scalar=16.0, op=ALU.is_ge), [pf.r], [pf.r])
        dv(lambda: nc.vector.scalar_tensor_tensor(out=pf.t[:, 2:3], in0=pf.t[:, 7:8], scalar=-16.0, in1=pf.t[:, 1:2], op0=ALU.mult, op1=ALU.add), [pf.r], [pf.r])
        sy.op("act", lambda: nc.scalar.activation(out=pf.t[:, 4:5], in_=pf.t[:, 2:3], func=AF.Exp, scale=-math.log(10000.0) / 16.0), reads=[pf.r], writes=[pf.r])
        dv(lambda: nc.vector.tensor_single_scalar(out=pf.t[:, 5:6], in_=pf.t[:, 0:1], scalar=32.0, op=ALU.is_ge), [pf.r], [pf.r])
        dv(lambda: nc.vector.tensor_single_scalar(out=pf.t[:, 7:8], in_=pf.t[:, 0:1], scalar=64.0, op=ALU.is_ge), [pf.r], [pf.r])
        dv(lambda: nc.vector.tensor_tensor(out=pf.t[:, 5:6], in0=pf.t[:, 5:6], in1=pf.t[:, 7:8], op=ALU.subtract), [pf.r], [pf.r])
        dv(lambda: nc.vector.tensor_single_scalar(out=pf.t[:, 7:8], in_=pf.t[:, 0:1], scalar=96.0, op=ALU.is_ge), [pf.r], [pf.r])
        dv(lambda: nc.vector.tensor_tensor(out=pf.t[:, 5:6], in0=pf.t[:, 5:6], in1=pf.t[:, 7:8], op=ALU.add), [pf.r], [pf.r])
        dv(lambda: nc.vector.tensor_scalar(out=pf.t[:, 5:6], in0=pf.t[:, 5:6], scalar1=2.0, scalar2=-1.0, op0=ALU.mult, op1=ALU.add), [pf.r], [pf.r])
        dv(lambda: nc.vector.tensor_tensor(out=rowf.t[:], in0=rowf.t[:], in1=colf.t[:], op=ALU.subtract), [rowf.r, colf.r], [rowf.r])
        dv(lambda: nc.vector.scalar_tensor_tensor(out=colf.t[:], in0=rowf.t[:], scalar=pf.t[:, 3:4], in1=colf.t[:], op0=ALU.mult, op1=ALU.add), [rowf.r, colf.r, pf.r], [colf.r])
        dv(lambda: nc.vector.tensor_scalar(out=colf.t[:], in0=colf.t[:], scalar1=pf.t[:, 4:5], scalar2=None, op0=ALU.mult), [colf.r, pf.r], [colf.r])

        def reduce_sin(dst, shift):
            dv(lambda: nc.vector.tensor_scalar(out=rowf.t[:], in0=colf.t[:], scalar1=shift, scalar2=1.0 / (2 * math.pi), op0=ALU.add, op1=ALU.mult), [colf.r], [rowf.r])
            dv(lambda: nc.vector.tensor_copy(out=rowt.t[:], in_=rowf.t[:]), [rowf.r], [rowt.r])
            dv(lambda: nc.vector.tensor_copy(out=rowf.t[:], in_=rowt.t[:]), [rowt.r], [rowf.r])
            dv(lambda: nc.vector.scalar_tensor_tensor(out=rowf.t[:], in0=rowf.t[:], scalar=-2 * math.pi, in1=colf.t[:], op0=ALU.mult, op1=ALU.add), [rowf.r, colf.r], [rowf.r])
            dv(lambda: nc.vector.tensor_single_scalar(out=rowf.t[:], in_=rowf.t[:], scalar=shift, op=ALU.add), [rowf.r], [rowf.r])
            dv(lambda: nc.vector.tensor_single_scalar(out=colt.t[:].bitcast(F32), in_=rowf.t[:], scalar=math.pi, op=ALU.is_gt), [rowf.r], [colt.r])
            dv(lambda: nc.vector.scalar_tensor_tensor(out=rowf.t[:], in0=colt.t[:].bitcast(F32), scalar=-2 * math.pi, in1=rowf.t[:], op0=ALU.mult, op1=ALU.add), [rowf.r, colt.r], [rowf.r])
            dv(lambda: nc.vector.tensor_scalar(out=rowf.t[:], in0=rowf.t[:], scalar1=math.pi, scalar2=-math.pi, op0=ALU.min, op1=ALU.max), [rowf.r], [rowf.r])
            sy.op("act", lambda: nc.scalar.activation(out=dst, in_=rowf.t[:], func=AF.Sin), reads=[rowf.r], writes=[sinS.r, cosF.r])

        reduce_sin(sinS.t[:, 0:L], 0.0)
        dv(lambda: nc.vector.tensor_scalar(out=sinS.t[:, 0:L], in0=sinS.t[:, 0:L], scalar1=pf.t[:, 5:6], scalar2=None, op0=ALU.mult), [sinS.r, pf.r], [sinS.r])
        reduce_sin(cosF.t[:, 0:L], 0.5 * math.pi)
        sy.op("pool", lambda: nc.gpsimd.memset(cosF.t[:, L:T], 1.0), writes=[cosF.r])
        sy.op("pool", lambda: nc.gpsimd.memset(sinS.t[:, L:T], 0.0), writes=[sinS.r])
        sy.dma("sp", cs_d[0], cosF.t[:], reads=[cosF.r], writes=[cs_res])
        sy.dma("sp", cs_d[1], sinS.t[:], reads=[sinS.r], writes=[cs_res])
        tap("cosF", cosF.t[:], [128, T], F32, [cosF.r])
        tap("sinS", sinS.t[:], [128, T], F32, [sinS.r])
        sy.barrier()

    skc = sb("skc", [1, NLA * 8], F32)
    sinkl = sb("sinkl", [1, 2, 128], BF16)
    nlam = sb("nlam", [128, NLA], F32)
    gsc = sb("gsc", [128, NLA], F32)
    with contextlib.ExitStack() as es0:
        sk = Tile(es0.enter_context(nc.sbuf_tensor("sk", [1, NLA * 8], F32)))
        lq = Tile(es0.enter_context(nc.sbuf_tensor("lq", [128, 4, NLA, 64], F32)))
        ls = Tile(es0.enter_context(nc.sbuf_tensor("ls", [128, 2, NLA], F32)))
        sy.dma("sp", sk.t[:], attn_sink.rearrange("l h -> (l h)").unsqueeze(0), writes=[sk.r])
        sy.op("act", lambda: nc.scalar.activation(out=skc.t[:], in_=sk.t[:], func=AF.Exp), reads=[sk.r], writes=[skc.r])
        sy.op("pool", lambda: nc.gpsimd.memset(sinkl.t[:, 0, 0:64], 0.0), writes=[sinkl.r])
        sy.op("pool", lambda: nc.gpsimd.memset(sinkl.t[:, 0, 64:128], 1.0), writes=[sinkl.r])
        sy.op("pool", lambda: nc.gpsimd.memset(sinkl.t[:, 1, 0:64], 1.0), writes=[sinkl.r])
        sy.op("pool", lambda: nc.gpsimd.memset(sinkl.t[:, 1, 64:128], 0.0), writes=[sinkl.r])
        for i, a in enumerate((lam_q1, lam_k1, lam_q2, lam_k2)):
            sy.dma("sp", lq.t[:, i, :, :], a.unsqueeze(0).to_broadcast([128, NLA, 64]), writes=[lq.r])
        for i in range(2):
            sy.op("dve", lambda: nc.vector.tensor_tensor(out=lq.t[:, 2 * i, :, :], in0=lq.t[:, 2 * i, :, :], in1=lq.t[:, 2 * i + 1, :, :], op=ALU.mult), reads=[lq.r], writes=[lq.r])
            sy.op("dve", lambda: nc.vector.tensor_reduce(out=ls.t[:, i, :], in_=lq.t[:, 2 * i, :, :], axis=AX.X, op=ALU.add), reads=[lq.r], writes=[ls.r])
        sy.op("act", lambda: nc.scalar.activation(out=ls.t[:], in_=ls.t[:], func=AF.Exp), reads=[ls.r], writes=[ls.r])
        sy.op("dve", lambda: nc.vector.tensor_tensor(out=nlam.t[:], in0=ls.t[:, 1, :], in1=ls.t[:, 0, :], op=ALU.subtract), reads=[ls.r], writes=[nlam.r])
        for l in range(NLA):
            li = 0.8 - 0.6 * math.exp(-0.3 * l)
            sy.op("dve", lambda: nc.vector.tensor_single_scalar(out=nlam.t[:, l:l + 1], in_=nlam.t[:, l:l + 1], scalar=-li, op=ALU.add), reads=[nlam.r], writes=[nlam.r])
            sy.op("dve", lambda: nc.vector.tensor_single_scalar(out=gsc.t[:, l:l + 1], in_=vt("dg", l), scalar=1.0 - li, op=ALU.mult), reads=[VT.r], writes=[gsc.r])
        sy.barrier()
    tap("nlam", nlam.t[:], [128, NLA], F32, [nlam.r])

    UTs_d = nc.dram_tensor("UTs_scr", [NLA, 128, 128, 8, 128], BF16, kind="Internal").ap()
    Vs_d = nc.dram_tensor("Vs_scr", [NLA, 128, 128, 1024], BF16, kind="Internal").ap()
    h2_d = nc.dram_tensor("h2_scr", [128, 8, T], BF16, kind="Internal").ap()
    rt_d = nc.dram_tensor("rt_scr", [128, 3, T], F32, kind="Internal").ap()
    tab_res = [Res() for _ in range(NLA)]
    if do_peer:
        with contextlib.ExitStack() as es0:
            ub = [Tile(es0.enter_context(nc.sbuf_tensor(f"ub{i}", [128, 1024], BF16))) for i in range(3)]
            uo = [Tile(es0.enter_context(nc.sbuf_tensor(f"uo{i}", [128, 8, 128], BF16))) for i in range(3)]
            vb = [Tile(es0.enter_context(nc.sbuf_tensor(f"vb{i}", [128, 8, 1024], BF16))) for i in range(2)]
            for l in range(nl_run):
                uv = peer_u[l].rearrange("(i1 i2) d -> i2 i1 d", i2=128)
                vv = peer_v[l].rearrange("(i1 i2) d -> i1 i2 d", i2=128)
                for i2 in range(128):
                    u_, o_ = ub[i2 % 3], uo[i2 % 3]
                    ps = PS[i2 % 4]
                    psb = ps.t[:].bitcast(BF16)
                    sy.dma("pq", u_.t[:], uv[i2], writes=[u_.r])
                    for kc in range(8):
                        sy.op("pe", lambda: nc.tensor.transpose(out=psb[:, kc * 128:(kc + 1) * 128], in_=u_.t[:, kc * 128:(kc + 1) * 128], identity=identb.t[:]),
                              reads=[u_.r, identb.r], writes=[ps.r])
                    if i2 % 2 == 0:
                        sy.op("act", lambda: nc.scalar.copy(out=o_.t[:].rearrange("p k i -> p (k i)"), in_=psb[:, 0:1024]), reads=[ps.r], writes=[o_.r])
                    else:
                        sy.op("dve", lambda: nc.vector.tensor_copy(out=o_.t[:].rearrange("p k i -> p (k i)"), in_=psb[:, 0:1024]), reads=[ps.r], writes=[o_.r])
                    sy.dma("sp", UTs_d[l, :, i2, :, :], o_.t[:], reads=[o_.r], writes=[tab_res[l]])
                    if i2 % 8 == 7:
                        g = i2 // 8
                        v_ = vb[g % 2]
                        sy.dma("pq", v_.t[:], vv[:, g * 8:(g + 1) * 8, :], writes=[v_.r])
                        sy.dma("sp", Vs_d[l, :, g * 8:(g + 1) * 8, :], v_.t[:], reads=[v_.r], writes=[tab_res[l]])
            sy.barrier()

    wq_toggle = [0]

    def wload(dst_ap, src_ap, tile):
        sy.dma("pq", dst_ap, src_ap, writes=[tile.r])

    def norm_chunk(b, l, ci, which, x_t, sq_t, dst_fn, dst_res, ps, rs_t, tmp_t):
        t0, n = CHUNKS[ci]
        j = b if t0 < L else 4
        sy.op("act", lambda: nc.scalar.activation(out=sq_t.t[:, :, 0:n], in_=x_t.t[:, :, 0:n], func=AF.Square), reads=[x_t.r], writes=[sq_t.r])
        for c in range(8):
            sy.op("pe", lambda: nc.tensor.matmul(ps.t[:, 0:n], onesD.t[:], sq_t.t[:, c, 0:n], start=(c == 0), stop=(c == 7)), reads=[sq_t.r, onesD.r], writes=[ps.r])
        sy.op("act", lambda: nc.scalar.activation(out=rs_t.t[:, 0:n], in_=ps.t[:, 0:n], func=AF.Sqrt, bias=EPS, scale=1.0), reads=[ps.r], writes=[rs_t.r])
        sy.op("dve", lambda: nc.vector.reciprocal(out=rs_t.t[:, 0:n], in_=rs_t.t[:, 0:n]), reads=[rs_t.r], writes=[rs_t.r])
        for c in range(8):
            if which < 2:
                a_ap = AM.t[:, l, which, c, j:j + 1]
                s_ap = MOD.t[:, l, 0 if which == 0 else 3, c, j:j + 1]
            else:
                a_ap = vt("fg", 0, c)
                s_ap = None
            sy.op("dve", lambda: nc.vector.scalar_tensor_tensor(out=tmp_t.t[:, c, 0:n], in0=x_t.t[:, c, 0:n], scalar=a_ap, in1=rs_t.t[:, 0:n], op0=ALU.mult, op1=ALU.mult),
                  reads=[x_t.r, rs_t.r, AM.r, VT.r], writes=[tmp_t.r])
            if s_ap is not None:
                sy.op("act", lambda: nc.scalar.activation(out=dst_fn(c), in_=tmp_t.t[:, c, 0:n], func=AF.Identity, bias=s_ap, scale=1.0), reads=[tmp_t.r, MOD.r], writes=[dst_res])

    state = dict(nc=nc, sy=sy, es=es, PS=PS, sb=sb, tap=tap, tap_out=tap_out, tap_res=tap_res)
    consts = dict(ident=ident, identb=identb, maskGE=maskGE, maskLE=maskLE, onesD=onesD, ones5=ones5, ones1=ones1, onesb=onesb, iof=iof,
                  VT=VT, vt=vt, vtr=vtr, MOD=MOD, AM=AM, cs_d=cs_d, cs_res=cs_res, skc=skc, sinkl=sinkl, nlam=nlam, gsc=gsc)
    dram = dict(w_in=w_in, w_gate=w_gate, w_branch=w_branch, w_out=w_out, pool_w=pool_w, peer_wq=peer_wq, peer_k1=peer_k1, peer_k2=peer_k2,
                peer_u=peer_u, peer_v=peer_v, xT_d=xT_d, xT_res=xT_res, out_d=out_d, yT_d=yT_d, yT_res=yT_res,
                UTs_d=UTs_d, Vs_d=Vs_d, h2_d=h2_d, rt_d=rt_d, tab_res=tab_res)

    from_mixer = mixer_phase
    for l in range(nl_run):
        for b in range(nb_run):
            from_mixer(state, consts, dram, cfg, b, l, norm_chunk, wload)
            if do_peer and not cfg.get("skip_peer_phase"):
                peer_phase(state, consts, dram, cfg, b, l, norm_chunk, wload)

    final_phase(state, consts, dram, cfg, nb_run, norm_chunk)
    sy.barrier(engs=("sp",))
    es.close()
    return nc, tap_out


_uid = [0]


def mixer_phase(state, consts, dram, cfg, b, l, norm_chunk, wload):
    if cfg.get("skip_mixer"):
        return
    nc, sy, PS, tap = state["nc"], state["sy"], state["PS"], state["tap"]
    C = consts
    vt, vtr, MOD = C["vt"], C["vtr"], C["MOD"]
    xT_d, xT_res = dram["xT_d"], dram["xT_res"]
    ctx_out = l < NL - 1
    qchunks = list(range(5)) if ctx_out else list(range(4))
    wv = dram["w_in"][l].rearrange("(kc p) n -> p kc n", p=128)
    branches = cfg.get("branches", "ACBD")

    def mk(scope, name, shape, dt, nres=1):
        _uid[0] += 1
        return Tile(scope.enter_context(nc.sbuf_tensor(f"{name}_{_uid[0]}", list(shape), dt)), nres)

    def dve(fn, reads, writes):
        sy.op("dve", fn, reads=reads, writes=writes)

    def act(fn, reads, writes):
        sy.op("act", fn, reads=reads, writes=writes)

    def pool(fn, reads, writes):
        sy.op("pool", fn, reads=reads, writes=writes)

    def pe(fn, reads, writes):
        sy.op("pe", fn, reads=reads, writes=writes)

    with contextlib.ExitStack() as ms:
        hT = mk(ms, "hT", [128, 8, T], BF16, nres=5)
        with contextlib.ExitStack() as s1:
            xt = [mk(s1, "xt", [128, 8, 512], F32) for _ in range(2)]
            sq = mk(s1, "sq", [128, 8, 512], F32)
            tmp = mk(s1, "tmp", [128, 8, 512], F32)
            rs = [mk(s1, "rs", [128, 512], F32) for _ in range(2)]
            for ci, (t0, n) in enumerate(CHUNKS):
                x_t = xt[ci % 2]
                sy.dma("sp", x_t.t[:, :, 0:n], xT_d[b, :, :, t0:t0 + n], reads=[xT_res[b][ci]], writes=[x_t.r])
                norm_chunk(b, l, ci, 0, x_t, sq, (lambda c, t0=t0, n=n: hT.t[:, c, t0:t0 + n]), hT.res[ci], PS[ci % 2], rs[ci % 2], tmp)
            sy.barrier()
        tap(f"hT{l}", hT.t[:], [128, 8, T], BF16, hT.res)

        yT_d, yT_res = dram["yT_d"], dram["yT_res"]
        cosF = mk(ms, "cosF", [128, T], F32)
        sinS = mk(ms, "sinS", [128, T], F32)
        sy.dma("sp", cosF.t[:], C["cs_d"][0], reads=[C["cs_res"]], writes=[cosF.r])
        sy.dma("sp", sinS.t[:], C["cs_d"][1], reads=[C["cs_res"]], writes=[sinS.r])
        ystg = [mk(ms, "ystg", [128, 512], BF16) for _ in range(3)]
        ycnt = [0]

        def y_out(nbr, kc, ci, p0, p1, fn_write):
            t0, n = CHUNKS[ci]
            st = ystg[ycnt[0] % 3]
            ycnt[0] += 1
            fn_write(st.t[p0:p1, 0:n], st.r)
            sy.dma("sp", yT_d[nbr, p0:p1, kc, t0:t0 + n], st.t[p0:p1, 0:n], reads=[st.r], writes=[yT_res[nbr][ci]])

        def proj(ps, Wt, c0, ci, ncols=128):
            t0, n = CHUNKS[ci]
            for kc in range(8):
                pe(lambda: nc.tensor.matmul(ps.t[0:ncols, 0:n], Wt.t[:, kc, c0:c0 + ncols], hT.t[:, kc, t0:t0 + n], start=(kc == 0), stop=(kc == 7)),
                   [Wt.r, hT.res[ci]], [ps.r])

        def load_rot(Wr, seg0, ncols):
            dv_ = Wr.t[:, :, 0:ncols].rearrange("p k (h two j) -> p k h two j", two=2, j=32)
            sv_ = wv[:, :, seg0:seg0 + ncols].rearrange("p k (h two j) -> p k h two j", two=2, j=32)
            if cfg.get("fake_rot"):
                sy.dma("pq", Wr.t[:, :, 0:ncols], wv[:, :, seg0:seg0 + ncols], writes=[Wr.r])
                return
            for kc in range(8):
                sy.dma("pq", dv_[:, kc, :, 0, :], sv_[:, kc, :, 1, :], writes=[Wr.r])
                sy.dma("pq", dv_[:, kc, :, 1, :], sv_[:, kc, :, 0, :], writes=[Wr.r])

        def rope_evac(psA, psB, ci, dst_ap, dst_res, t1, t2):
            t0, n = CHUNKS[ci]
            dve(lambda: nc.vector.tensor_tensor(out=t1.t[:, 0:n], in0=psA.t[:, 0:n], in1=cosF.t[:, t0:t0 + n], op=ALU.mult), [psA.r, cosF.r], [t1.r])
            dve(lambda: nc.vector.tensor_tensor(out=t2.t[:, 0:n], in0=psB.t[:, 0:n], in1=sinS.t[:, t0:t0 + n], op=ALU.mult), [psB.r, sinS.r], [t2.r])
            dve(lambda: nc.vector.tensor_tensor(out=dst_ap, in0=t1.t[:, 0:n], in1=t2.t[:, 0:n], op=ALU.add), [t1.r, t2.r], [dst_res])

        if "A" in branches:
            with contextlib.ExitStack() as sa:
                Waq = mk(sa, "Waq", [128, 8, 512], BF16)
                WaqR = mk(sa, "WaqR", [128, 8, 512], BF16)
                Wak = mk(sa, "Wak", [128, 8, 256], BF16)
                WakR = mk(sa, "WakR", [128, 8, 256], BF16)
                Wav = mk(sa, "Wav", [128, 8, 128], BF16)
                qaT = mk(sa, "qaT", [128, 4, T], BF16)
                kaT = mk(sa, "kaT", [128, 2, T], BF16)
                va = mk(sa, "va", [128, 18, 2, 2, 128], BF16)
                t1 = [mk(sa, "t1", [128, 512], F32) for _ in range(2)]
                t2 = [mk(sa, "t2", [128, 512], F32) for _ in range(2)]
                wload(Waq.t[:], wv[:, :, 0:512], Waq)
                load_rot(WaqR, 0, 512)
                for kvh in range(2):
                    for dup in range(2):
                        c0 = (kvh * 2 + dup) * 64
                        sy.dma("pq", Wak.t[:, :, c0:c0 + 64], wv[:, :, 512 + kvh * 64:512 + (kvh + 1) * 64], writes=[Wak.r])
                        for hf in range(2):
                            sy.dma("pq", WakR.t[:, :, c0 + hf * 32:c0 + (hf + 1) * 32],
                                   wv[:, :, 512 + kvh * 64 + (1 - hf) * 32:512 + kvh * 64 + (2 - hf) * 32], writes=[WakR.r])
                wload(Wav.t[:], wv[:, :, 640:768], Wav)
                pool(lambda: nc.gpsimd.memset(va.t[:, :, :, 0, 64:128], 1.0), [], [va.r])
                pool(lambda: nc.gpsimd.memset(va.t[:, :, :, 1, 0:64], 1.0), [], [va.r])
                k = 0
                for ci in range(5):
                    t0, n = CHUNKS[ci]
                    for hc in range(4):
                        if ci == 4 and not ctx_out:
                            continue
                        pa, pb = PS[(2 * k) % 4], PS[(2 * k + 1) % 4]
                        proj(pa, Waq, hc * 128, ci)
                        proj(pb, WaqR, hc * 128, ci)
                        rope_evac(pa, pb, ci, qaT.t[:, hc, t0:t0 + n], qaT.r, t1[k % 2], t2[k % 2])
                        k += 1
                    for kc2 in range(2):
                        pa, pb = PS[(2 * k) % 4], PS[(2 * k + 1) % 4]
                        proj(pa, Wak, kc2 * 128, ci)
                        proj(pb, WakR, kc2 * 128, ci)
                        rope_evac(pa, pb, ci, kaT.t[:, kc2, t0:t0 + n], kaT.r, t1[k % 2], t2[k % 2])
                        k += 1
                for tt in range(18):
                    ps = PS[4 + tt % 2]
                    ci = min(tt // 4, 4)
                    for kc in range(8):
                        pe(lambda: nc.tensor.matmul(ps.t[:, 0:128], hT.t[:, kc, tt * 128:(tt + 1) * 128], Wav.t[:, kc, :], start=(kc == 0), stop=(kc == 7)),
                           [Wav.r, hT.res[ci]], [ps.r])
                    src = ps.t[:, 0:128].rearrange("p (k d) -> p k d", d=64)
                    act(lambda: nc.scalar.copy(out=va.t[:, tt, :, 0, 0:64], in_=src), [ps.r], [va.r])
                    dve(lambda: nc.vector.tensor_copy(out=va.t[:, tt, :, 1, 64:128], in_=src), [ps.r], [va.r])
                tap(f"qaT{l}", qaT.t[:], [128, 4, T], BF16, [qaT.r])
                tap(f"kaT{l}", kaT.t[:], [128, 2, T], BF16, [kaT.r])
                E = [mk(sa, "E", [128, 16, 384], BF16) for _ in range(2)]
                Ec = [mk(sa, "Ec", [128, 2, T], BF16) for _ in range(2)]
                rden = [mk(sa, "rden", [128, 512], F32) for _ in range(2)]
                maskGE, maskLE = C["maskGE"], C["maskLE"]
                sinkl, skc = C["sinkl"], C["skc"]
                esrow = mk(sa, "esrow", [1, 8, 512], BF16)
                dve(lambda: nc.vector.tensor_copy(out=esrow.t[:], in_=skc.t[:, l * 8:(l + 1) * 8].unsqueeze(2).to_broadcast([1, 8, 512])), [skc.r], [esrow.r])
                kk = 0
                for h in range(8):
                    kvh, par, hc = h // 4, h % 2, h // 2
                    p0 = 64 * par
                    q0 = 64 - p0
                    Eh, Ech = E[h % 2], Ec[h % 2]
                    for j in range(16):
                        qlo, qhi = max(0, j - 1) * 128, min(16, j + 2) * 128
                        n = qhi - qlo
                        ps = PS[kk % 2]
                        kk += 1
                        pe(lambda: nc.tensor.matmul(ps.t[:, 0:n], kaT.t[p0:p0 + 64, kvh, j * 128:(j + 1) * 128], qaT.t[p0:p0 + 64, hc, qlo:qhi], start=True, stop=True),
                           [kaT.r, qaT.r], [ps.r])
                        act(lambda: nc.scalar.activation(out=Eh.t[:, j, 0:n], in_=ps.t[:, 0:n], func=AF.Exp, scale=0.125), [ps.r], [Eh.r])
                        if j >= 1:
                            dve(lambda: nc.vector.tensor_tensor(out=Eh.t[:, j, 0:128], in0=Eh.t[:, j, 0:128], in1=maskLE.t[:], op=ALU.mult), [Eh.r, maskLE.r], [Eh.r])
                        if j <= 14:
                            off = (j + 1) * 128 - qlo
                            dve(lambda: nc.vector.tensor_tensor(out=Eh.t[:, j, off:off + 128], in0=Eh.t[:, j, off:off + 128], in1=maskGE.t[:], op=ALU.mult), [Eh.r, maskGE.r], [Eh.r])
                    for jc in range(2):
                        for ci in qchunks:
                            t0, n = CHUNKS[ci]
                            ps = PS[kk % 2]
                            kk += 1
                            pe(lambda: nc.tensor.matmul(ps.t[:, 0:n], kaT.t[p0:p0 + 64, kvh, L + jc * 128:L + (jc + 1) * 128], qaT.t[p0:p0 + 64, hc, t0:t0 + n], start=True, stop=True),
                               [kaT.r, qaT.r], [ps.r])
                            act(lambda: nc.scalar.activation(out=Ech.t[:, jc, t0:t0 + n], in_=ps.t[:, 0:n], func=AF.Exp, scale=0.125), [ps.r], [Ech.r])
                    for ci in qchunks:
                        t0, n = CHUNKS[ci]
                        pso = PS[2 + kk % 2]
                        rd = rden[kk % 2]
                        kk += 1
                        mm = [(slice(0, n), sinkl.t[0:1, par, :], esrow.t[0:1, h, 0:n], [sinkl.r, esrow.r])]
                        for jc in range(2):
                            mm.append((slice(0, n), va.t[:, 16 + jc, kvh, par, :], Ech.t[:, jc, t0:t0 + n], [va.r, Ech.r]))
                        if ci < 4:
                            for j in range(4 * ci - 1, 4 * ci + 5):
                                if 0 <= j < 16:
                                    bl_lo, bl_hi = max(4 * ci, j - 1), min(4 * ci + 3, j + 1)
                                    sub0, nsub = (bl_lo - 4 * ci) * 128, (bl_hi - bl_lo + 1) * 128
                                    eoff = bl_lo * 128 - max(0, j - 1) * 128
                                    mm.append((slice(sub0, sub0 + nsub), va.t[:, j, kvh, par, :], Eh.t[:, j, eoff:eoff + nsub], [va.r, Eh.r]))
                        for i, (sl, lt, rh, rd_) in enumerate(mm):
                            pe(lambda: nc.tensor.matmul(pso.t[:, sl], lt, rh, start=(i == 0), stop=(i == len(mm) - 1)), rd_, [pso.r])
                        dve(lambda: nc.vector.reciprocal(out=rd.t[p0:p0 + 64, 0:n], in_=pso.t[q0:q0 + 64, 0:n]), [pso.r], [rd.r])
                        y_out(0, hc, ci, p0, p0 + 64, lambda ap, r_: dve(lambda: nc.vector.tensor_tensor(out=ap, in0=pso.t[p0:p0 + 64, 0:n], in1=rd.t[p0:p0 + 64, 0:n], op=ALU.mult),
                                                                        [pso.r, rd.r], [r_]))
                sy.barrier()
            tap(f"yA{l}", yT_d[0], [128, 4, T], BF16, yT_res[0])

        if "C" in branches:
            with contextlib.ExitStack() as sc:
                Wcq = mk(sc, "Wcq", [128, 8, 512], BF16)
                WcqR = mk(sc, "WcqR", [128, 8, 512], BF16)
                Wck = mk(sc, "Wck", [128, 8, 512], BF16)
                WckR = mk(sc, "WckR", [128, 8, 512], BF16)
                Wcv = mk(sc, "Wcv", [128, 8, 512], BF16)
                qcT = mk(sc, "qcT", [128, 4, T], BF16)
                kcT = mk(sc, "kcT", [128, 4, T], BF16)
                vc = mk(sc, "vc", [128, 18, 512], BF16)
                t1 = [mk(sc, "t1", [128, 512], F32) for _ in range(2)]
                t2 = [mk(sc, "t2", [128, 512], F32) for _ in range(2)]
                wload(Wcq.t[:], wv[:, :, 768:1280], Wcq)
                load_rot(WcqR, 768, 512)
                wload(Wck.t[:], wv[:, :, 1280:1792], Wck)
                load_rot(WckR, 1280, 512)
                wload(Wcv.t[:], wv[:, :, 1792:2304], Wcv)
                k = 0
                for ci in range(5):
                    t0, n = CHUNKS[ci]
                    for hc in range(4):
                        for (W_, WR_, dstT) in ((Wcq, WcqR, qcT), (Wck, WckR, kcT)):
                            if dstT is qcT and ci == 4 and not ctx_out:
                                continue
                            pa, pb = PS[(2 * k) % 4], PS[(2 * k + 1) % 4]
                            proj(pa, W_, hc * 128, ci)
                            proj(pb, WR_, hc * 128, ci)
                            rope_evac(pa, pb, ci, dstT.t[:, hc, t0:t0 + n], dstT.r, t1[k % 2], t2[k % 2])
                            k += 1
                for tt in range(18):
                    ps = PS[4 + tt % 2]
                    ci = min(tt // 4, 4)
                    for kc in range(8):
                        pe(lambda: nc.tensor.matmul(ps.t[:, 0:512], hT.t[:, kc, tt * 128:(tt + 1) * 128], Wcv.t[:, kc, :], start=(kc == 0), stop=(kc == 7)),
                           [Wcv.r, hT.res[ci]], [ps.r])
                    if tt % 2 == 0:
                        act(lambda: nc.scalar.copy(out=vc.t[:, tt, :], in_=ps.t[:, 0:512]), [ps.r], [vc.r])
                    else:
                        dve(lambda: nc.vector.tensor_copy(out=vc.t[:, tt, :], in_=ps.t[:, 0:512]), [ps.r], [vc.r])
                Et = [mk(sc, "Et", [128, 512], BF16) for _ in range(3)]
                rd = [mk(sc, "rdc", [128, 512], F32) for _ in range(2)]
                tu = [mk(sc, "tu", [128, 512], F32) for _ in range(2)]
                ot = mk(sc, "ot", [128, 512], F32)
                sqo = mk(sc, "sqo", [128, 512], F32)
                rso = mk(sc, "rso", [128, 512], F32)
                onesb, ones1, nlam, gsc = C["onesb"], C["ones1"], C["nlam"], C["gsc"]
                kk = 0
                for h in range(4):
                    for ci in qchunks:
                        t0, n = CHUNKS[ci]
                        keyt = list(range(18)) if ci < 4 else [16, 17]
                        items = [(c, ji, j) for c in range(2) for ji, j in enumerate(keyt)]
                        SK = 2
                        scb = (PS[0], PS[1], PS[6])
                        pend = []

                        def emit_score(idx_):
                            c, ji, j = items[idx_]
                            p0 = 64 * c
                            ps = scb[(kk0 + idx_) % 3]
                            et = Et[(kk0 + idx_) % 3]
                            pe(lambda: nc.tensor.matmul(ps.t[:, 0:n], kcT.t[p0:p0 + 64, h, j * 128:(j + 1) * 128], qcT.t[p0:p0 + 64, h, t0:t0 + n], start=True, stop=True),
                               [kcT.r, qcT.r], [ps.r])
                            act(lambda: nc.scalar.activation(out=et.t[:, 0:n], in_=ps.t[:, 0:n], func=AF.Exp, scale=0.125), [ps.r], [et.r])

                        def emit_pv(idx_):
                            c, ji, j = items[idx_]
                            et = Et[(kk0 + idx_) % 3]
                            psU, psD = PS[2 + c], PS[4 + c]
                            pe(lambda: nc.tensor.matmul(psU.t[:, 0:n], vc.t[:, j, h * 128:(h + 1) * 128], et.t[:, 0:n], start=(ji == 0), stop=(ji == len(keyt) - 1)),
                               [vc.r, et.r], [psU.r])
                            pe(lambda: nc.tensor.matmul(psD.t[:, 0:n], onesb.t[:], et.t[:, 0:n], start=(ji == 0), stop=(ji == len(keyt) - 1)),
                               [onesb.r, et.r], [psD.r])

                        kk0 = kk
                        for idx_ in range(len(items) + SK):
                            if idx_ < len(items):
                                emit_score(idx_)
                            if idx_ >= SK:
                                emit_pv(idx_ - SK)
                        kk += len(items)
                        for c in range(2):
                            dve(lambda: nc.vector.reciprocal(out=rd[c].t[:, 0:n], in_=PS[4 + c].t[:, 0:n]), [PS[4 + c].r], [rd[c].r])
                            dve(lambda: nc.vector.tensor_tensor(out=tu[c].t[:, 0:n], in0=PS[2 + c].t[:, 0:n], in1=rd[c].t[:, 0:n], op=ALU.mult), [PS[2 + c].r, rd[c].r], [tu[c].r])
                        dve(lambda: nc.vector.scalar_tensor_tensor(out=ot.t[:, 0:n], in0=tu[1].t[:, 0:n], scalar=nlam.t[:, l:l + 1], in1=tu[0].t[:, 0:n], op0=ALU.mult, op1=ALU.add),
                            [tu[0].r, tu[1].r, nlam.r], [ot.r])
                        act(lambda: nc.scalar.activation(out=sqo.t[:, 0:n], in_=ot.t[:, 0:n], func=AF.Square), [ot.r], [sqo.r])
                        psn = PS[7]
                        pe(lambda: nc.tensor.matmul(psn.t[:, 0:n], ones1.t[:], sqo.t[:, 0:n], start=True, stop=True), [ones1.r, sqo.r], [psn.r])
                        act(lambda: nc.scalar.activation(out=rso.t[:, 0:n], in_=psn.t[:, 0:n], func=AF.Sqrt, bias=EPS, scale=1.0), [psn.r], [rso.r])
                        dve(lambda: nc.vector.reciprocal(out=rso.t[:, 0:n], in_=rso.t[:, 0:n]), [rso.r], [rso.r])
                        y_out(1, h, ci, 0, 128, lambda ap, r_: dve(lambda: nc.vector.scalar_tensor_tensor(out=ap, in0=ot.t[:, 0:n], scalar=gsc.t[:, l:l + 1], in1=rso.t[:, 0:n], op0=ALU.mult, op1=ALU.mult),
                                                                  [ot.r, rso.r, gsc.r], [r_]))
                sy.barrier()
            tap(f"yC{l}", yT_d[1], [128, 4, T], BF16, yT_res[1])

        if "B" in branches:
            with contextlib.ExitStack() as sb_:
                YW = 15 + L + 15 + 15 + LC + 15
                offs = (15, 15 + L + 15 + 15)
                Wb = mk(sb_, "Wb", [128, 8, 1024], BF16)
                ypad = mk(sb_, "ypad", [128, 4, YW], BF16)
                Dm = mk(sb_, "Dm", [128, 124, 128], BF16)
                z = mk(sb_, "z", [128, 4, T], F32)
                sg = [mk(sb_, "sg", [128, 512], F32) for _ in range(2)]
                wload(Wb.t[:], wv[:, :, 2304:3328], Wb)
                pool(lambda: nc.gpsimd.memset(ypad.t[:], 0.0), [], [ypad.r])
                identb = C["identb"]
                for i in range(124):
                    fn = (lambda: nc.vector.tensor_scalar(out=Dm.t[:, i, :], in0=identb.t[:], scalar1=vt("cw", l, i), scalar2=None, op0=ALU.mult))
                    dve(fn, [identb.r, C["VT"].r], [Dm.r])
                bchunks = qchunks
                k = 0
                for cc in range(4):
                    for ci in bchunks:
                        t0, n = CHUNKS[ci]
                        pa, pg = PS[(2 * k) % 4], PS[(2 * k + 1) % 4]
                        s_ = sg[k % 2]
                        k += 1
                        proj(pa, Wb, cc * 128, ci)
                        proj(pg, Wb, 512 + cc * 128, ci)
                        act(lambda: nc.scalar.activation(out=s_.t[:, 0:n], in_=pg.t[:, 0:n], func=AF.Sigmoid), [pg.r], [s_.r])
                        yo = offs[0] + t0 if ci < 4 else offs[1]
                        dve(lambda: nc.vector.tensor_tensor(out=ypad.t[:, cc, yo:yo + n], in0=pa.t[:, 0:n], in1=s_.t[:, 0:n], op=ALU.mult), [pa.r, s_.r], [ypad.r])
                k = 0
                for cc in range(4):
                    for ci in bchunks:
                        t0, n = CHUNKS[ci]
                        ps = PS[4 + k % 2]
                        k += 1
                        base = (t0 if ci < 4 else offs[1] - 15)
                        for kk_ in range(31):
                            pe(lambda: nc.tensor.matmul(ps.t[:, 0:n], Dm.t[:, kk_ * 4 + cc, :], ypad.t[:, cc, base + kk_:base + kk_ + n], start=(kk_ == 0), stop=(kk_ == 30)),
                               [Dm.r, ypad.r], [ps.r])
                        act(lambda: nc.scalar.activation(out=z.t[:, cc, t0:t0 + n], in_=ps.t[:, 0:n], func=AF.Identity, bias=vt("cb", l, cc), scale=1.0), [ps.r, C["VT"].r], [z.r])
                ones5 = C["ones5"]
                zsq = mk(sb_, "zsq", [128, 4, 512], F32)
                m2 = mk(sb_, "m2", [128, 512], F32)
                var = mk(sb_, "var", [128, 512], F32)
                tz = [mk(sb_, "tz", [128, 512], F32) for _ in range(2)]
                for ci in bchunks:
                    t0, n = CHUNKS[ci]
                    psm, psq = PS[6], PS[7]
                    act(lambda: nc.scalar.activation(out=zsq.t[:, :, 0:n], in_=z.t[:, :, t0:t0 + n], func=AF.Square), [z.r], [zsq.r])
                    for cc in range(4):
                        pe(lambda: nc.tensor.matmul(psm.t[:, 0:n], ones5.t[:], z.t[:, cc, t0:t0 + n], start=(cc == 0), stop=(cc == 3)), [ones5.r, z.r], [psm.r])
                    for cc in range(4):
                        pe(lambda: nc.tensor.matmul(psq.t[:, 0:n], ones5.t[:], zsq.t[:, cc, 0:n], start=(cc == 0), stop=(cc == 3)), [ones5.r, zsq.r], [psq.r])
                    act(lambda: nc.scalar.activation(out=m2.t[:, 0:n], in_=psm.t[:, 0:n], func=AF.Square), [psm.r], [m2.r])
                    dve(lambda: nc.vector.tensor_tensor(out=var.t[:, 0:n], in0=psq.t[:, 0:n], in1=m2.t[:, 0:n], op=ALU.subtract), [psq.r, m2.r], [var.r])
                    dve(lambda: nc.vector.tensor_scalar_max(out=var.t[:, 0:n], in0=var.t[:, 0:n], scalar1=0.0), [var.r], [var.r])
                    act(lambda: nc.scalar.activation(out=var.t[:, 0:n], in_=var.t[:, 0:n], func=AF.Sqrt, bias=EPS, scale=1.0), [var.r], [var.r])
                    dve(lambda: nc.vector.reciprocal(out=var.t[:, 0:n], in_=var.t[:, 0:n]), [var.r], [var.r])
                    for cc in range(4):
                        tz_ = tz[cc % 2]
                        dve(lambda: nc.vector.tensor_tensor(out=tz_.t[:, 0:n], in0=z.t[:, cc, t0:t0 + n], in1=psm.t[:, 0:n], op=ALU.subtract), [z.r, psm.r], [tz_.r])
                        dve(lambda: nc.vector.tensor_tensor(out=tz_.t[:, 0:n], in0=tz_.t[:, 0:n], in1=var.t[:, 0:n], op=ALU.mult), [tz_.r, var.r], [tz_.r])
                        y_out(2, cc, ci, 0, 128, lambda ap, r_: act(lambda: nc.scalar.activation(out=ap, in_=tz_.t[:, 0:n], func=AF.Silu, bias=vt("lnb", l, cc), scale=vt("lng", l, cc)),
                                                                   [tz_.r, C["VT"].r], [r_]))
                sy.barrier()
            tap(f"yB{l}", yT_d[2], [128, 4, T], BF16, yT_res[2])

        if "D" in branches:
            with contextlib.ExitStack() as sd:
                PW = 24 + L + 24 + LC + 24
                poff = (24, 24 + L + 24)
                Wd = mk(sd, "Wd", [128, 8, 512], BF16)
                Wp = mk(sd, "Wp", [128, 4, 128], BF16)
                bufs = [mk(sd, f"pb{i}", [128, PW], F32) for i in range(5)]
                yq = mk(sd, "yq", [128, T], BF16)
                edge = mk(sd, "edge", [128, 64], F32)
                etmp = mk(sd, "etmp", [128, 16], F32)
                wload(Wd.t[:], wv[:, :, 3328:3840], Wd)
                wload(Wp.t[:], dram["pool_w"][l].rearrange("g c e -> c g e"), Wp)
                for bf in bufs:
                    pool(lambda: nc.gpsimd.memset(bf.t[:], 0.0), [], [bf.r])
                bchunks = qchunks
                k = 0
                for g in range(4):
                    win = POOLW[g]
                    lo, hi = win // 2, win - 1 - win // 2
                    xb = bufs[0]
                    for ci in bchunks:
                        t0, n = CHUNKS[ci]
                        ps = PS[k % 2]
                        k += 1
                        proj(ps, Wd, g * 128, ci)
                        xo_ = poff[0] + t0 if ci < 4 else poff[1]
                        act(lambda: nc.scalar.copy(out=xb.t[:, xo_:xo_ + n], in_=ps.t[:, 0:n]), [ps.r], [xb.r])
                    nlev = g + 1
                    for lv in range(nlev):
                        w_ = 1 << lv
                        src, dst = bufs[lv], bufs[lv + 1]
                        pool(lambda: nc.gpsimd.tensor_tensor(out=dst.t[:, w_:PW], in0=src.t[:, w_:PW], in1=src.t[:, 0:PW - w_], op=ALU.add), [src.r], [dst.r])
                    ws = bufs[nlev]
                    segs = [(poff[0], L, 0)] + ([(poff[1], LC, L)] if ctx_out else [])
                    for (so, sl, yo) in segs:
                        dve(lambda: nc.vector.scalar_tensor_tensor(out=yq.t[:, yo:yo + sl], in0=ws.t[:, so + hi:so + hi + sl], scalar=1.0 / win, in1=xb.t[:, so:so + sl], op0=ALU.mult, op1=ALU.subtract),
                            [ws.r, xb.r], [yq.r])
                        ecols = [(t, t + hi + 1) for t in range(lo)] + [(sl - 1 - i, lo + 1 + i) for i in range(hi)]
                        for (t, cnt) in ecols:
                            dve(lambda: nc.vector.scalar_tensor_tensor(out=yq.t[:, yo + t:yo + t + 1], in0=ws.t[:, so + hi + t:so + hi + t + 1], scalar=1.0 / cnt, in1=xb.t[:, so + t:so + t + 1], op0=ALU.mult, op1=ALU.subtract),
                                [ws.r, xb.r], [yq.r])
                    for ci in bchunks:
                        t0, n = CHUNKS[ci]
                        ps = PS[2 + k % 2]
                        k += 1
                        pe(lambda: nc.tensor.matmul(ps.t[:, 0:n], Wp.t[:, g, :], yq.t[:, t0:t0 + n], start=True, stop=True), [Wp.r, yq.r], [ps.r])
                        y_out(3, g, ci, 0, 128, lambda ap, r_: act(lambda: nc.scalar.activation(out=ap, in_=ps.t[:, 0:n], func=AF.Copy, scale=vt("psc", l, g)), [ps.r, C["VT"].r], [r_]))
                sy.barrier()
            tap(f"yD{l}", yT_d[3], [128, 4, T], BF16, yT_res[3])

        if cfg.get("merge", True):
            with contextlib.ExitStack() as sm:
                accT = mk(sm, "accT", [128, 8, T], BF16)
                with contextlib.ExitStack() as sm1:
                    Wg = [mk(sm1, "Wg", [128, 4, 8, 128], BF16) for _ in range(2)]
                    Wbr = [mk(sm1, "Wbr", [128, 4, 4, 128], BF16) for _ in range(2)]
                    sgm = [mk(sm1, "sgm", [128, 512], F32) for _ in range(2)]
                    tm = [mk(sm1, "tm", [128, 512], F32) for _ in range(2)]
                    acc = mk(sm1, "acc", [128, 512], F32)
                    ych = [mk(sm1, "ych", [128, 4, 4, 512], BF16) for _ in range(2)]
                    wgv = dram["w_gate"][l].rearrange("n (kc p) e -> p n kc e", p=128)
                    wbv = dram["w_branch"][l].rearrange("n (kc p) e -> p n kc e", p=128)
                    border = (0, 2, 1, 3)
                    border = (0, 1, 2, 3)
                    k = 0
                    for ec in range(8):
                        wg_, wb_ = Wg[ec % 2], Wbr[ec % 2]
                        for n_ in range(4):
                            sy.dma("pq", wg_.t[:, n_, :, :], wgv[:, n_, :, ec * 128:(ec + 1) * 128], writes=[wg_.r])
                            sy.dma("pq", wb_.t[:, n_, :, :], wbv[:, n_, :, ec * 128:(ec + 1) * 128], writes=[wb_.r])
                        for ci in qchunks:
                            t0, n = CHUNKS[ci]
                            yc_ = ych[(ec * 5 + ci) % 2]
                            for i_ in range(4):
                                sy.dma("sp", yc_.t[:, i_, :, 0:n], yT_d[i_, :, :, t0:t0 + n], reads=[yT_res[i_][ci]], writes=[yc_.r])
                            for n_ in range(4):
                                pg, pb_ = PS[(2 * k) % 4], PS[(2 * k + 1) % 4]
                                s_ = sgm[k % 2]
                                t_ = tm[k % 2]
                                k += 1
                                for kc in range(8):
                                    pe(lambda: nc.tensor.matmul(pg.t[:, 0:n], wg_.t[:, n_, kc, :], hT.t[:, kc, t0:t0 + n], start=(kc == 0), stop=(kc == 7)), [wg_.r, hT.res[ci]], [pg.r])
                                for kc in range(4):
                                    pe(lambda: nc.tensor.matmul(pb_.t[:, 0:n], wb_.t[:, n_, kc, :], yc_.t[:, n_, kc, 0:n], start=(kc == 0), stop=(kc == 3)), [wb_.r, yc_.r], [pb_.r])
                                act(lambda: nc.scalar.activation(out=s_.t[:, 0:n], in_=pg.t[:, 0:n], func=AF.Sigmoid, bias=vt("bg", l, n_ * 8 + ec), scale=1.0), [pg.r, C["VT"].r], [s_.r])
                                if n_ == 0:
                                    dve(lambda: nc.vector.tensor_tensor(out=acc.t[:, 0:n], in0=pb_.t[:, 0:n], in1=s_.t[:, 0:n], op=ALU.mult), [pb_.r, s_.r], [acc.r])
                                else:
                                    dve(lambda: nc.vector.tensor_tensor(out=t_.t[:, 0:n], in0=pb_.t[:, 0:n], in1=s_.t[:, 0:n], op=ALU.mult), [pb_.r, s_.r], [t_.r])
                                    if n_ < 3:
                                        dve(lambda: nc.vector.tensor_tensor(out=acc.t[:, 0:n], in0=acc.t[:, 0:n], in1=t_.t[:, 0:n], op=ALU.add), [acc.r, t_.r], [acc.r])
                                    else:
                                        dve(lambda: nc.vector.tensor_tensor(out=accT.t[:, ec, t0:t0 + n], in0=acc.t[:, 0:n], in1=t_.t[:, 0:n], op=ALU.add), [acc.r, t_.r], [accT.r])
                    sy.barrier()
                tap(f"accT{l}", accT.t[:], [128, 8, T], BF16, [accT.r])
                Wo = mk(sm, "Wo", [128, 8, 8, 128], BF16)
                xt = [mk(sm, "xtm", [128, 8, 512], F32) for _ in range(2)]
                wov = dram["w_out"][l].rearrange("(kc p) (oc e) -> p oc kc e", p=128, e=128)
                for oc in range(8):
                    sy.dma("pq", Wo.t[:, oc, :, :], wov[:, oc, :, :], writes=[Wo.r])
                for ci in qchunks:
                    t0, n = CHUNKS[ci]
                    j = b if ci < 4 else 4
                    x_t = xt[ci % 2]
                    sy.dma("sp", x_t.t[:, :, 0:n], xT_d[b, :, :, t0:t0 + n], reads=[xT_res[b][ci]], writes=[x_t.r])
                    for oc in range(8):
                        ps = PS[4 + oc % 4]
                        for kc in range(8):
                            pe(lambda: nc.tensor.matmul(ps.t[:, 0:n], Wo.t[:, oc, kc, :], accT.t[:, kc, t0:t0 + n], start=(kc == 0), stop=(kc == 7)), [Wo.r, accT.r], [ps.r])
                        dve(lambda: nc.vector.scalar_tensor_tensor(out=x_t.t[:, oc, 0:n], in0=ps.t[:, 0:n], scalar=MOD.t[:, l, 2, oc, j:j + 1], in1=x_t.t[:, oc, 0:n], op0=ALU.mult, op1=ALU.add),
                            [ps.r, x_t.r, MOD.r], [x_t.r])
                    sy.dma("sp", xT_d[b, :, :, t0:t0 + n], x_t.t[:, :, 0:n], reads=[x_t.r], writes=[xT_res[b][ci]])
                sy.barrier()
        tap(f"xmix{l}", xT_d[b], [128, 8, T], F32, xT_res[b])
        sy.barrier()


def _overlap_res(xT_res_b, c0, n):
    out = []
    for ci, (t0, nn) in enumerate(CHUNKS):
        if t0 < c0 + n and c0 < t0 + nn:
            out.append(xT_res_b[ci])
    return out


def peer_phase(state, consts, dram, cfg, b, l, norm_chunk, wload):
    nc, sy, PS, tap = state["nc"], state["sy"], state["PS"], state["tap"]
    C = consts
    vt, MOD, iof, ident, identb = C["vt"], C["MOD"], C["iof"], C["ident"], C["identb"]
    xT_d, xT_res = dram["xT_d"], dram["xT_res"]
    h2_d, rt_d = dram["h2_d"], dram["rt_d"]
    ctx_out = l < NL - 1
    chunks = list(range(5)) if ctx_out else list(range(4))
    h2_res, rt_res = Res(), Res()

    def mk(scope, name, shape, dt, nres=1):
        _uid[0] += 1
        return Tile(scope.enter_context(nc.sbuf_tensor(f"{name}_{_uid[0]}", list(shape), dt)), nres)

    def dve(fn, reads, writes):
        sy.op("dve", fn, reads=reads, writes=writes)

    def act(fn, reads, writes):
        sy.op("act", fn, reads=reads, writes=writes)

    def pool(fn, reads, writes):
        sy.op("pool", fn, reads=reads, writes=writes)

    def pe(fn, reads, writes):
        sy.op("pe", fn, reads=reads, writes=writes)

    with contextlib.ExitStack() as s1:
        Wq = mk(s1, "Wq", [128, 8, 2048], BF16)
        kst = mk(s1, "kst", [128, 16, 128], BF16)
        kT = mk(s1, "kT", [128, 16, 128], BF16)
        xt = [mk(s1, "xt", [128, 8, 512], F32) for _ in range(2)]
        sq = mk(s1, "sq", [128, 8, 512], F32)
        tmp = mk(s1, "tmp", [128, 8, 512], F32)
        rs = [mk(s1, "rs", [128, 512], F32) for _ in range(2)]
        h2c = [mk(s1, "h2c", [128, 8, 512], BF16) for _ in range(2)]
        qT = mk(s1, "qT", [128, 16, 512], BF16)
        sc = mk(s1, "sc", [128, 16, 128], F32)
        scw = mk(s1, "scw", [128, 16, 128], F32, nres=16)
        v16 = mk(s1, "v16", [128, 16, 16], F32, nres=16)
        i16 = mk(s1, "i16", [128, 16, 16], U32, nres=16)
        i16f = mk(s1, "i16f", [128, 16, 16], F32)
        cand = mk(s1, "cand", [128, 8, 16, 16], F32)
        candw = mk(s1, "candw", [128, 8, 256], F32, nres=8)
        c16 = mk(s1, "c16", [128, 8, 16], F32, nres=8)
        ci16 = mk(s1, "ci16", [128, 8, 16], U32, nres=8)
        iab = mk(s1, "iab", [128, 2, 8, 16], U32)
        fab = mk(s1, "fab", [128, 2, 8, 16], F32)
        oh = mk(s1, "oh", [128, 8, 16, 16], F32)
        sel = mk(s1, "sel", [128, 3, 128], F32)
        gs = mk(s1, "gs", [128, 8], F32)
        rtile = [mk(s1, "rtile", [128, 3, 128], F32) for _ in range(2)]
        wload(Wq.t[:], dram["peer_wq"][l].rearrange("(kc p) n -> p kc n", p=128), Wq)
        for half, kd in enumerate((dram["peer_k1"], dram["peer_k2"])):
            sy.dma("pq", kst.t[:].rearrange("n (h two) d -> n h two d", two=2)[:, :, half, :], kd[l].rearrange("h n d -> n h d"), writes=[kst.r])
        for blk in range(16):
            ps = PS[blk % 2]
            psb = ps.t[:].bitcast(BF16)
            pe(lambda: nc.tensor.transpose(out=psb[:, 0:128], in_=kst.t[:, blk, :], identity=identb.t[:]), [kst.r, identb.r], [ps.r])
            act(lambda: nc.scalar.copy(out=kT.t[:, blk, :], in_=psb[:, 0:128]), [ps.r], [kT.r])
        ntile = 0
        for ci in chunks:
            t0, n = CHUNKS[ci]
            x_t = xt[ci % 2]
            h2 = h2c[ci % 2]
            sy.dma("sp", x_t.t[:, :, 0:n], xT_d[b, :, :, t0:t0 + n], reads=[xT_res[b][ci]], writes=[x_t.r])
            norm_chunk(b, l, ci, 1, x_t, sq, (lambda c, h2=h2, n=n: h2.t[:, c, 0:n]), h2.r, PS[ci % 2], rs[ci % 2], tmp)
            sy.dma("sp", h2_d[:, :, t0:t0 + n], h2.t[:, :, 0:n], reads=[h2.r], writes=[h2_res])
            for blk in range(16):
                ps = PS[2 + blk % 2]
                for kc in range(8):
                    pe(lambda: nc.tensor.matmul(ps.t[:, 0:n], Wq.t[:, kc, blk * 128:(blk + 1) * 128], h2.t[:, kc, 0:n], start=(kc == 0), stop=(kc == 7)), [Wq.r, h2.r], [ps.r])
                if blk % 2 == 0:
                    act(lambda: nc.scalar.copy(out=qT.t[:, blk, 0:n], in_=ps.t[:, 0:n]), [ps.r], [qT.r])
                else:
                    dve(lambda: nc.vector.tensor_copy(out=qT.t[:, blk, 0:n], in_=ps.t[:, 0:n]), [ps.r], [qT.r])
            for tt in range(n // 128):
                for q4 in range(4):
                    ps = PS[4 + q4]
                    for bi in range(4):
                        blk = q4 * 4 + bi
                        pe(lambda: nc.tensor.matmul(ps.t[:, bi * 128:(bi + 1) * 128], qT.t[:, blk, tt * 128:(tt + 1) * 128], kT.t[:, blk, :], start=True, stop=True), [qT.r, kT.r], [ps.r])
                    src = ps.t[:, :].rearrange("p (k n) -> p k n", n=128)
                    if q4 % 2 == 0:
                        act(lambda: nc.scalar.copy(out=sc.t[:, q4 * 4:(q4 + 1) * 4, :], in_=src), [ps.r], [sc.r])
                    else:
                        dve(lambda: nc.vector.tensor_copy(out=sc.t[:, q4 * 4:(q4 + 1) * 4, :], in_=src), [ps.r], [sc.r])
                R16 = range(16)
                for blk in R16:
                    dve(lambda: nc.vector.max(out=v16.t[:, blk, 0:8], in_=sc.t[:, blk, :]), [sc.r], [v16.res[blk]])
                for blk in R16:
                    dve(lambda: nc.vector.max_index(out=i16.t[:, blk, 0:8], in_max=v16.t[:, blk, 0:8], in_values=sc.t[:, blk, :]), [sc.r, v16.res[blk]], [i16.res[blk]])
                for blk in R16:
                    dve(lambda: nc.vector.match_replace(out=scw.t[:, blk, :], in_to_replace=v16.t[:, blk, 0:8], in_values=sc.t[:, blk, :], imm_value=-1e30), [sc.r, v16.res[blk]], [scw.res[blk]])
                for blk in R16:
                    dve(lambda: nc.vector.max(out=v16.t[:, blk, 8:16], in_=scw.t[:, blk, :]), [scw.res[blk]], [v16.res[blk]])
                for blk in R16:
                    dve(lambda: nc.vector.max_index(out=i16.t[:, blk, 8:16], in_max=v16.t[:, blk, 8:16], in_values=scw.t[:, blk, :]), [scw.res[blk], v16.res[blk]], [i16.res[blk]])
                dve(lambda: nc.vector.tensor_copy(out=i16f.t[:], in_=i16.t[:]), i16.res, [i16f.r])
                v16v = v16.t[:].rearrange("p (h two) k -> p h two k", two=2)
                i16v = i16f.t[:].rearrange("p (h two) k -> p h two k", two=2)
                dve(lambda: nc.vector.tensor_tensor(out=cand.t[:], in0=v16v[:, :, 0, :].unsqueeze(3).to_broadcast([128, 8, 16, 16]),
                                                    in1=v16v[:, :, 1, :].unsqueeze(2).to_broadcast([128, 8, 16, 16]), op=ALU.add), v16.res, [cand.r])
                candf = cand.t[:].rearrange("p h a b -> p h (a b)")
                R8 = range(8)
                for h in R8:
                    dve(lambda: nc.vector.max(out=c16.t[:, h, 0:8], in_=candf[:, h, :]), [cand.r], [c16.res[h]])
                for h in R8:
                    dve(lambda: nc.vector.max_index(out=ci16.t[:, h, 0:8], in_max=c16.t[:, h, 0:8], in_values=candf[:, h, :]), [cand.r, c16.res[h]], [ci16.res[h]])
                for h in R8:
                    dve(lambda: nc.vector.match_replace(out=candw.t[:, h, :], in_to_replace=c16.t[:, h, 0:8], in_values=candf[:, h, :], imm_value=-1e30), [cand.r, c16.res[h]], [candw.res[h]])
                for h in R8:
                    dve(lambda: nc.vector.max(out=c16.t[:, h, 8:16], in_=candw.t[:, h, :]), [candw.res[h]], [c16.res[h]])
                for h in R8:
                    dve(lambda: nc.vector.max_index(out=ci16.t[:, h, 8:16], in_max=c16.t[:, h, 8:16], in_values=candw.t[:, h, :]), [candw.res[h], c16.res[h]], [ci16.res[h]])
                dve(lambda: nc.vector.tensor_single_scalar(out=iab.t[:, 0, :, :], in_=ci16.t[:], scalar=4, op=ALU.arith_shift_right), ci16.res, [iab.r])
                dve(lambda: nc.vector.tensor_single_scalar(out=iab.t[:, 1, :, :], in_=ci16.t[:], scalar=15, op=ALU.bitwise_and), ci16.res, [iab.r])
                dve(lambda: nc.vector.tensor_copy(out=fab.t[:], in_=iab.t[:]), [iab.r], [fab.r])
                io16 = iof.t[:, 0:16].unsqueeze(1).unsqueeze(1).to_broadcast([128, 8, 16, 16])
                for w_ in range(2):
                    dve(lambda: nc.vector.tensor_tensor(out=oh.t[:], in0=io16, in1=fab.t[:, w_, :, :].unsqueeze(3).to_broadcast([128, 8, 16, 16]), op=ALU.is_equal), [fab.r, iof.r], [oh.r])
                    dve(lambda: nc.vector.tensor_tensor(out=oh.t[:], in0=oh.t[:], in1=i16v[:, :, w_, :].unsqueeze(2).to_broadcast([128, 8, 16, 16]), op=ALU.mult), [oh.r, i16f.r], [oh.r])
                    dve(lambda: nc.vector.tensor_reduce(out=sel.t[:, w_, :].rearrange("p (h k) -> p h k", k=16), in_=oh.t[:], axis=AX.X, op=ALU.add), [oh.r], [sel.r])
                g3 = sel.t[:, 2, :].rearrange("p (h k) -> p h k", k=16)
                dve(lambda: nc.vector.tensor_tensor(out=g3, in0=c16.t[:], in1=c16.t[:, :, 0:1].to_broadcast([128, 8, 16]), op=ALU.subtract), c16.res, [sel.r])
                act(lambda: nc.scalar.activation(out=g3, in_=g3, func=AF.Exp), [sel.r], [sel.r])
                dve(lambda: nc.vector.tensor_reduce(out=gs.t[:], in_=g3, axis=AX.X, op=ALU.add), [sel.r], [gs.r])
                dve(lambda: nc.vector.reciprocal(out=gs.t[:], in_=gs.t[:]), [gs.r], [gs.r])
                dve(lambda: nc.vector.tensor_tensor(out=g3, in0=g3, in1=gs.t[:].unsqueeze(2).to_broadcast([128, 8, 16]), op=ALU.mult), [sel.r, gs.r], [sel.r])
                ps = PS[ntile % 2]
                rt_ = rtile[ntile % 2]
                ntile += 1
                for w_ in range(3):
                    pe(lambda: nc.tensor.transpose(out=ps.t[:, w_ * 128:(w_ + 1) * 128], in_=sel.t[:, w_, :], identity=ident.t[:]), [sel.r, ident.r], [ps.r])
                act(lambda: nc.scalar.copy(out=rt_.t[:], in_=ps.t[:, 0:384].rearrange("p (w t) -> p w t", t=128)), [ps.r], [rt_.r])
                c0 = t0 + tt * 128
                sy.dma("sp", rt_d[:, :, c0:c0 + 128], rt_.t[:], reads=[rt_.r], writes=[rt_res])
        sy.barrier()
    tap(f"rt{l}", rt_d, [128, 3, T], F32, [rt_res])
    tap(f"h2T{l}", h2_d, [128, 8, T], BF16, [h2_res])
    if cfg.get("peer_pass1_only"):
        return

    UTs, Vs, tab = dram["UTs_d"][l], dram["Vs_d"][l], dram["tab_res"][l]
    if ctx_out:
        stiles = [(s_ * 384, 384) for s_ in range(6)]
    else:
        stiles = [(s_ * 384, 384) for s_ in range(5)] + [(1920, 128)]
    with contextlib.ExitStack() as s2:
        GW = mk(s2, "GW", [128, 128, 384], BF16)
        PH = mk(s2, "PH", [128, 32768], BF16)
        accO = mk(s2, "accO", [128, 8, 384], F32)
        h2s = mk(s2, "h2s", [128, 8, 384], BF16)
        xp = [mk(s2, "xp", [128, 384], F32) for _ in range(2)]
        UTc = [(PH.t[:, i * 4096:(i + 1) * 4096].rearrange("p (a k i) -> p a k i", a=4, k=8), Res()) for i in range(2)]
        Ab = [(PH.t[:, 8192 + i * 8192:8192 + i * 8192 + 4096].rearrange("p (i t) -> p i t", t=32), Res()) for i in range(3)]
        Bb = [(PH.t[:, 12288 + i * 8192:12288 + i * 8192 + 4096].rearrange("p (i t) -> p i t", t=32), Res()) for i in range(3)]
        Vc = [(PH.t[:, i * 16384:(i + 1) * 16384].rearrange("p (a d) -> p a d", d=1024), Res()) for i in range(2)]
        iotaT = Tile.__new__(Tile)
        iotaT.t = None
        iotaT_ap = PH.t[:, 0:4096].rearrange("p (i t) -> p i t", t=32)
        iotaT_r = UTc[0][1]
        RTb = mk(s2, "RTb", [128, 3, 384], BF16)
        tmpg = [mk(s2, "tmpg", [128, 384], BF16) for _ in range(2)]
        p2 = cfg.get("p2", "SWVR")
        wbanks = (PS[2], PS[3], PS[6], PS[7])
        for (c0, n) in stiles[:cfg.get("p2_ntiles", 99)]:
            sy.dma("sp", h2s.t[:, :, 0:n], h2_d[:, :, c0:c0 + n], reads=[h2_res], writes=[h2s.r])
            sy.dma("pq", RTb.t[:, :, 0:n], rt_d[:, :, c0:c0 + n], reads=[rt_res], writes=[RTb.r])
            dve(lambda: nc.vector.tensor_copy(out=iotaT_ap, in_=iof.t[:, :].unsqueeze(2).to_broadcast([128, 128, 32])), [iof.r], [iotaT_r])
            ngrp = n // 32 if "W" in p2 else 0
            wk = 0
            for gi in range(ngrp):
                (A_, ar), (B_, br) = Ab[gi % 3], Bb[gi % 3]
                tg = gi * 32
                dve(lambda: nc.vector.tensor_tensor(out=A_, in0=iotaT_ap, in1=RTb.t[:, 0, tg:tg + 32].unsqueeze(1).to_broadcast([128, 128, 32]), op=ALU.is_equal), [RTb.r, iotaT_r], [ar])
                dve(lambda: nc.vector.tensor_tensor(out=A_, in0=A_, in1=RTb.t[:, 2, tg:tg + 32].unsqueeze(1).to_broadcast([128, 128, 32]), op=ALU.mult), [RTb.r, ar], [ar])
                dve(lambda: nc.vector.tensor_tensor(out=B_, in0=iotaT_ap, in1=RTb.t[:, 1, tg:tg + 32].unsqueeze(1).to_broadcast([128, 128, 32]), op=ALU.is_equal), [RTb.r, iotaT_r], [br])
                for q_ in range(8):
                    ps = wbanks[wk % 4]
                    wk += 1
                    for tl in range(4):
                        ti = q_ * 4 + tl
                        pe(lambda: nc.tensor.matmul(ps.t[:, tl * 128:(tl + 1) * 128], A_[:, :, ti], B_[:, :, ti], start=True, stop=True), [ar, br], [ps.r])
                    ta = tg + q_ * 4
                    gwv = GW.t[:, :, ta:ta + 4].rearrange("p i t -> p t i")
                    act(lambda: nc.scalar.copy(out=gwv, in_=ps.t[:, 0:512].rearrange("p (t i) -> p t i", i=128)), [ps.r], [GW.r])
            for g in (range(32) if "S" in p2 else ()):
                ut, ur = UTc[g % 2]
                sy.dma("sp", ut, UTs[:, g * 4:(g + 1) * 4, :, :], reads=[tab], writes=[ur])
                for a_ in range(4):
                    i2 = g * 4 + a_
                    ps = PS[i2 % 2]
                    tg_ = tmpg[i2 % 2]
                    for kc in range(8):
                        pe(lambda: nc.tensor.matmul(ps.t[:, 0:n], ut[:, a_, kc, :], h2s.t[:, kc, 0:n], start=(kc == 0), stop=(kc == 7)), [ur, h2s.r], [ps.r])
                    act(lambda: nc.scalar.activation(out=tg_.t[:, 0:n], in_=ps.t[:, 0:n], func=AF.Gelu), [ps.r], [tg_.r])
                    dve(lambda: nc.vector.tensor_tensor(out=GW.t[:, i2, 0:n], in0=GW.t[:, i2, 0:n], in1=tg_.t[:, 0:n], op=ALU.mult), [GW.r, tg_.r], [GW.r])
            sy.barrier()
            for blk in (range(8) if "V" in p2 else ()):
                vt_, vr = Vc[blk % 2]
                for hv in range(2):
                    sy.dma("sp", vt_[:, hv * 8:(hv + 1) * 8, :], Vs[:, blk * 16 + hv * 8:blk * 16 + (hv + 1) * 8, :], reads=[tab], writes=[vr])
                for dc in range(8):
                    ps = PS[4 + dc % 2]
                    for ii in range(16):
                        pe(lambda: nc.tensor.matmul(ps.t[:, 0:n], vt_[:, ii, dc * 128:(dc + 1) * 128], GW.t[:, blk * 16 + ii, 0:n], start=(ii == 0), stop=(ii == 15)), [vr, GW.r], [ps.r])
                    if blk == 0:
                        act(lambda: nc.scalar.copy(out=accO.t[:, dc, 0:n], in_=ps.t[:, 0:n]), [ps.r], [accO.r])
                    else:
                        dve(lambda: nc.vector.tensor_tensor(out=accO.t[:, dc, 0:n], in0=ps.t[:, 0:n], in1=accO.t[:, dc, 0:n], op=ALU.add), [ps.r, accO.r], [accO.r])
            xres = _overlap_res(xT_res[b], c0, n)
            for dc in (range(8) if "R" in p2 else ()):
                xp_ = xp[dc % 2]
                sy.dma("sp", xp_.t[:, 0:n], xT_d[b, :, dc, c0:c0 + n], reads=xres, writes=[xp_.r])
                segs = []
                if c0 < L:
                    segs.append((0, min(n, L - c0), b))
                if c0 + n > L:
                    segs.append((max(0, L - c0), n, 4))
                for (a0, a1, j) in segs:
                    dve(lambda: nc.vector.scalar_tensor_tensor(out=xp_.t[:, a0:a1], in0=accO.t[:, dc, a0:a1], scalar=MOD.t[:, l, 5, dc, j:j + 1], in1=xp_.t[:, a0:a1], op0=ALU.mult, op1=ALU.add),
                        [accO.r, xp_.r, MOD.r], [xp_.r])
                sy.dma("sp", xT_d[b, :, dc, c0:c0 + n], xp_.t[:, 0:n], reads=[xp_.r], writes=xres)
            sy.barrier()
    tap(f"x{l}", xT_d[b], [128, 8, T], F32, xT_res[b])


def final_phase(state, consts, dram, cfg, nb_run, norm_chunk):
    if cfg.get("skip_final"):
        return
    nc, sy, PS = state["nc"], state["sy"], state["PS"]
    ident = consts["ident"]
    xT_d, xT_res, out_d = dram["xT_d"], dram["xT_res"], dram["out_d"]
    with contextlib.ExitStack() as sf:
        def mk(name, shape, dt):
            _uid[0] += 1
            return Tile(sf.enter_context(nc.sbuf_tensor(f"{name}_{_uid[0]}", list(shape), dt)))
        xt = [mk("xt", [128, 8, 512], F32) for _ in range(2)]
        sq = mk("sq", [128, 8, 512], F32)
        tmp = [mk("tmp", [128, 8, 512], F32) for _ in range(2)]
        rs = [mk("rs", [128, 512], F32) for _ in range(2)]
        ot = [mk("ot", [128, 1024], F32) for _ in range(2)]
        k = 0
        for b in range(nb_run):
            for ci in range(4):
                t0, n = CHUNKS[ci]
                x_t, tm_ = xt[ci % 2], tmp[ci % 2]
                sy.dma("sp", x_t.t[:, :, 0:n], xT_d[b, :, :, t0:t0 + n], reads=[xT_res[b][ci]], writes=[x_t.r])
                norm_chunk(b, 0, ci, 2, x_t, sq, None, None, PS[ci % 2], rs[ci % 2], tm_)
                for tt in range(4):
                    o_ = ot[k % 2]
                    for half in range(2):
                        ps = PS[2 + (2 * k + half) % 4]
                        for c4 in range(4):
                            c = half * 4 + c4
                            sy.op("pe", lambda: nc.tensor.transpose(out=ps.t[:, c4 * 128:(c4 + 1) * 128], in_=tm_.t[:, c, tt * 128:(tt + 1) * 128], identity=ident.t[:]),
                                  reads=[tm_.r, ident.r], writes=[ps.r])
                        if half == 0:
                            sy.op("act", lambda: nc.scalar.copy(out=o_.t[:, 0:512], in_=ps.t[:, 0:512]), reads=[ps.r], writes=[o_.r])
                        else:
                            sy.op("dve", lambda: nc.vector.tensor_copy(out=o_.t[:, 512:1024], in_=ps.t[:, 0:512]), reads=[ps.r], writes=[o_.r])
                    k += 1
                    sy.dma("sp", out_d[b, t0 + tt * 128:t0 + (tt + 1) * 128, :], o_.t[:], reads=[o_.r], writes=[Res()])
        sy.barrier()


_W_NAMES = ["c_ctx", "w_mod", "b_mod", "norm1_g", "norm2_g", "w_in", "w_gate", "b_gate", "attn_sink", "lam_q1", "lam_k1", "lam_q2", "lam_k2",
            "diff_norm_g", "conv_w", "conv_b", "conv_ln_g", "conv_ln_b", "pool_w", "pool_scale", "w_branch", "w_out", "peer_wq", "peer_k1",
            "peer_k2", "peer_u", "peer_v", "final_g"]


def kernel(**inputs):
    n_cores = 8
    nc, _ = build_program({})
    shared = {k: np.ascontiguousarray(np.asarray(inputs[k], dtype=np.float32)) for k in _W_NAMES}
    in_maps = []
    for i in range(n_cores):
        m = dict(shared)
        for k in ("x", "c", "ctx"):
            m[k] = np.ascontiguousarray(np.asarray(inputs[k], dtype=np.float32)[i * NBC:(i + 1) * NBC])
        in_maps.append(m)
    res = run_bass_kernel_spmd(nc, in_maps, core_ids=list(range(n_cores)))
    return np.concatenate([np.asarray(r["out"], dtype=np.float32) for r in res.results], axis=0)
```

```python
import math
import contextlib
import numpy as np
import concourse.bass as bass
import concourse.mybir as mybir
from concourse.bass_utils import run_bass_kernel_spmd

F32 = mybir.dt.float32
BF16 = mybir.dt.bfloat16
I32 = mybir.dt.int32
U32 = mybir.dt.uint32
AF = mybir.ActivationFunctionType
ALU = mybir.AluOpType
AX = mybir.AxisListType

D = 1024
L = 2048
LC = 256
T = L + LC
NL = 4
NBC = 4
EPS = 1e-6
NE = 16384
CHUNKS = [(0, 512), (512, 512), (1024, 512), (1536, 512), (2048, 256)]
SEG = dict(a_q=0, a_k=512, a_v=640, c_q=768, c_k=1280, c_v=1792, b_in=2304, d_in=3328)
POOLW = (2, 4, 8, 16)


class Res:
    __slots__ = ("w", "r")

    def __init__(self):
        self.w = None
        self.r = {}


class SY:
    def __init__(self, nc, es):
        self.nc = nc
        self.E = dict(pe=nc.tensor, act=nc.scalar, dve=nc.vector, pool=nc.gpsimd, sp=nc.sync)
        self.csem = {e: es.enter_context(nc.semaphore("c_" + e)) for e in ("pe", "act", "dve", "pool")}
        self.ccnt = {e: 0 for e in self.csem}
        self.Q = dict(sp="sp", pq="pool", aq="act")
        self.R = dict(sp=8, pq=8, aq=4)
        self.dsem = {q: [es.enter_context(nc.semaphore(f"d_{q}{i}")) for i in range(self.R[q])] for q in self.Q}
        self.dval = {q: [0] * self.R[q] for q in self.Q}
        self.dn = {q: 0 for q in self.Q}
        self.seen = {e: {} for e in self.E}
        self.ninst = 0

    def _wait(self, eng, t):
        sem, val = t[0], t[1]
        k = id(sem)
        if self.seen[eng].get(k, 0) >= val:
            return
        self.seen[eng][k] = val
        self.E[eng].wait_ge(sem, val)

    def _deps(self, eng, is_dma, reads, writes):
        for r in reads:
            if r.w is not None:
                self._wait(eng, r.w)
        for w in writes:
            if w.w is not None and (is_dma or w.w[3] or w.w[2] != eng):
                self._wait(eng, w.w)
            for t in w.r.values():
                if is_dma or t[3] or t[2] != eng:
                    self._wait(eng, t)

    def _commit(self, t, reads, writes):
        for r in reads:
            r.r[id(t[0])] = t
        for w in writes:
            w.w = t
            w.r = {}

    def op(self, eng, fn, reads=(), writes=(), final=True):
        self._deps(eng, False, reads, writes)
        ins = fn()
        if final:
            self.ccnt[eng] += 1
            ins.then_inc(self.csem[eng], 1)
            t = (self.csem[eng], self.ccnt[eng], eng, False)
        else:
            t = (self.csem[eng], self.ccnt[eng] + 1, eng, False)
        self._commit(t, reads, writes)
        self.ninst += 1

    def dma(self, q, out, in_, reads=(), writes=(), **kw):
        eng = self.Q[q]
        self._deps(eng, True, reads, writes)
        i = self.dn[q] % self.R[q]
        self.dn[q] += 1
        sem = self.dsem[q][i]
        if self.dval[q][i] > 0:
            self._wait(eng, (sem, self.dval[q][i]))
        self.dval[q][i] += 16
        self.E[eng].dma_start(out=out, in_=in_, **kw).then_inc(sem, 16)
        self._commit((sem, self.dval[q][i], eng, True), reads, writes)
        self.ninst += 1

    def all_tickets(self):
        ts = [(self.csem[e], self.ccnt[e]) for e in self.csem if self.ccnt[e] > 0]
        for q in self.Q:
            for i in range(self.R[q]):
                if self.dval[q][i] > 0:
                    ts.append((self.dsem[q][i], self.dval[q][i]))
        return ts

    def barrier(self, engs=("pe", "act", "dve", "pool", "sp")):
        ts = self.all_tickets()
        for e in engs:
            for t in ts:
                self._wait(e, t)


class Tile:
    def __init__(self, t, nres=1):
        self.t = t
        self.res = [Res() for _ in range(nres)]

    @property
    def r(self):
        return self.res[0]


def build_program(cfg):
    nb_run = cfg.get("nb", NBC)
    nl_run = cfg.get("nl", NL)
    taps = cfg.get("taps", ())
    do_peer = cfg.get("peer", True)

    NLA = cfg.get("nl_alloc", NL)
    nc = bass.Bass("TRN2", target_bir_lowering=False)
    es = contextlib.ExitStack()
    es.__enter__()
    sy = SY(nc, es)

    def din(name, shape):
        return nc.dram_tensor(name, list(shape), F32, kind="ExternalInput").ap()

    x_in = din("x", [NBC, L, D])
    c_in = din("c", [NBC, D])
    ctx_in = din("ctx", [NBC, LC, D])
    c_ctx = din("c_ctx", [D])
    w_mod = din("w_mod", [NLA, D, 6 * D])
    b_mod = din("b_mod", [NLA, 6 * D])
    norm1_g = din("norm1_g", [NLA, D])
    norm2_g = din("norm2_g", [NLA, D])
    w_in = din("w_in", [NLA, D, 3840])
    w_gate = din("w_gate", [NLA, 4, D, D])
    b_gate = din("b_gate", [NLA, 4, D])
    attn_sink = din("attn_sink", [NLA, 8])
    lam_q1 = din("lam_q1", [NLA, 64])
    lam_k1 = din("lam_k1", [NLA, 64])
    lam_q2 = din("lam_q2", [NLA, 64])
    lam_k2 = din("lam_k2", [NLA, 64])
    diff_norm_g = din("diff_norm_g", [NLA, 128])
    conv_w = din("conv_w", [NLA, 31, 512])
    conv_b = din("conv_b", [NLA, 512])
    conv_ln_g = din("conv_ln_g", [NLA, 512])
    conv_ln_b = din("conv_ln_b", [NLA, 512])
    pool_w = din("pool_w", [NLA, 4, 128, 128])
    pool_scale = din("pool_scale", [NLA, 512])
    w_branch = din("w_branch", [NLA, 4, 512, D])
    w_out = din("w_out", [NLA, D, D])
    peer_wq = din("peer_wq", [NLA, D, 2048])
    peer_k1 = din("peer_k1", [NLA, 8, 128, 128])
    peer_k2 = din("peer_k2", [NLA, 8, 128, 128])
    peer_u = din("peer_u", [NLA, NE, D])
    peer_v = din("peer_v", [NLA, NE, D])
    final_g = din("final_g", [D])
    out_d = nc.dram_tensor("out", [NBC, L, D], F32, kind="ExternalOutput").ap()

    xT_d = nc.dram_tensor("xT_scr", [NBC, 128, 8, T], F32, kind="Internal").ap()
    xT_res = [[Res() for _ in CHUNKS] for _ in range(NBC)]
    yT_d = nc.dram_tensor("yT_scr", [4, 128, 4, T], BF16, kind="Internal").ap()
    yT_res = [[Res() for _ in CHUNKS] for _ in range(4)]
    tap_out = {}

    def sb(name, shape, dt, nres=1):
        return Tile(es.enter_context(nc.sbuf_tensor(name, list(shape), dt)), nres)

    def tap(name, ap, shape, dt, reads):
        if name not in taps:
            return
        d = nc.dram_tensor("tap_" + name, list(shape), dt, kind="ExternalOutput").ap()
        tap_out[name] = d
        r = Res()
        sy.dma("sp", d, ap, reads=reads, writes=[r])
        tap_res.append(r)

    tap_res = []

    PS = [Tile(es.enter_context(nc.psum_tensor(f"ps{i}", [128, 512], F32))) for i in range(8)]

    ident = sb("ident", [128, 128], F32)
    identb = sb("identb", [128, 128], BF16)
    maskGE = sb("maskGE", [128, 128], BF16)
    maskLE = sb("maskLE", [128, 128], BF16)
    onesD = sb("onesD", [128, 128], F32)
    ones5 = sb("ones5", [128, 128], F32)
    ones1 = sb("ones1", [128, 128], F32)
    onesb = sb("onesb", [128, 128], BF16)
    iof = sb("iof", [128, 128], F32)
    itmp = sb("itmp", [128, 128], I32)
    sy.op("pool", lambda: nc.gpsimd.iota(itmp.t[:], pattern=[[1, 128]], base=0, channel_multiplier=-1), writes=[itmp.r])
    sy.op("dve", lambda: nc.vector.tensor_single_scalar(out=ident.t[:], in_=itmp.t[:], scalar=0, op=ALU.is_equal), reads=[itmp.r], writes=[ident.r])
    sy.op("dve", lambda: nc.vector.tensor_single_scalar(out=identb.t[:], in_=itmp.t[:], scalar=0, op=ALU.is_equal), reads=[itmp.r], writes=[identb.r])
    sy.op("dve", lambda: nc.vector.tensor_single_scalar(out=maskGE.t[:], in_=itmp.t[:], scalar=0, op=ALU.is_le), reads=[itmp.r], writes=[maskGE.r])
    sy.op("dve", lambda: nc.vector.tensor_single_scalar(out=maskLE.t[:], in_=itmp.t[:], scalar=0, op=ALU.is_ge), reads=[itmp.r], writes=[maskLE.r])
    itmp2 = sb("itmp2", [128, 128], I32)
    sy.op("pool", lambda: nc.gpsimd.iota(itmp2.t[:], pattern=[[1, 128]], base=0, channel_multiplier=0), writes=[itmp2.r])
    sy.op("dve", lambda: nc.vector.tensor_copy(out=iof.t[:], in_=itmp2.t[:]), reads=[itmp2.r], writes=[iof.r])
    sy.op("pool", lambda: nc.gpsimd.memset(onesD.t[:], 1.0 / 1024), writes=[onesD.r])
    sy.op("pool", lambda: nc.gpsimd.memset(ones5.t[:], 1.0 / 512), writes=[ones5.r])
    sy.op("pool", lambda: nc.gpsimd.memset(ones1.t[:], 1.0 / 128), writes=[ones1.r])
    sy.op("pool", lambda: nc.gpsimd.memset(onesb.t[:], 1.0), writes=[onesb.r])

    NVT = NL * 240 + 48
    VT = sb("VT", [128, NVT], F32)
    vt_off = {}

    def vt(name, l=0, i=0):
        return VT.t[:, vt_off[(name, l)] + i: vt_off[(name, l)] + i + 1]

    def vtr(name, l, i0, n):
        return VT.t[:, vt_off[(name, l)] + i0: vt_off[(name, l)] + i0 + n]

    stg = [sb(f"stg{i}", [128, 128], F32) for i in range(2)]
    col = 0
    nstage = 0

    def stage_transpose(items):
        nonlocal col, nstage
        st = stg[nstage % 2]
        ps = PS[nstage % 2]
        nstage += 1
        row = 0
        for (name, l, ap) in items:
            R_ = ap.shape[0]
            sy.dma("sp", st.t[row:row + R_, :], ap, writes=[st.r])
            vt_off[(name, l)] = col + row
            row += R_
        sy.op("pe", lambda: nc.tensor.transpose(out=ps.t[:, 0:row], in_=st.t[0:row, :], identity=ident.t[0:row, 0:row]),
              reads=[st.r, ident.r], writes=[ps.r])
        c0 = col
        sy.op("act", lambda: nc.scalar.copy(out=VT.t[:, c0:c0 + row], in_=ps.t[:, 0:row]), reads=[ps.r], writes=[VT.r])
        col += row

    def v2(ap1d):
        return ap1d.rearrange("(r p) -> r p", p=128)

    for l in range(NLA):
        stage_transpose([
            ("n1g", l, v2(norm1_g[l])), ("n2g", l, v2(norm2_g[l])),
            ("bg", l, b_gate[l].rearrange("n (r p) -> (n r) p", p=128)),
            ("cb", l, v2(conv_b[l])), ("lng", l, v2(conv_ln_g[l])), ("lnb", l, v2(conv_ln_b[l])),
            ("psc", l, v2(pool_scale[l])), ("dg", l, v2(diff_norm_g[l])), ("bmod", l, v2(b_mod[l])),
        ])
        stage_transpose([("cw", l, conv_w[l].rearrange("k (r p) -> (k r) p", p=128))])
    stage_transpose([("fg", 0, v2(final_g)), ("cctx", 0, v2(c_ctx)), ("cb4", 0, c_in.rearrange("b (r p) -> (b r) p", p=128))])
    assert col <= NVT, col

    scT = sb("scT", [128, 8, 5], F32)
    sy.op("act", lambda: nc.scalar.activation(out=scT.t[:, :, 0:4].rearrange("p k j -> p j k"), in_=vtr("cb4", 0, 0, 32).rearrange("p (j k) -> p j k", k=8), func=AF.Silu),
          reads=[VT.r], writes=[scT.r])
    sy.op("act", lambda: nc.scalar.activation(out=scT.t[:, :, 4], in_=vtr("cctx", 0, 0, 8), func=AF.Silu), reads=[VT.r], writes=[scT.r])

    MOD = sb("MOD", [128, NL, 6, 8, 5], F32)
    AM = sb("AM", [128, NL, 2, 8, 5], F32)
    with contextlib.ExitStack() as es0:
        wm = [Tile(es0.enter_context(nc.sbuf_tensor(f"wm{i}", [128, 8, 1024], F32))) for i in range(2)]
        k = 0
        for l in range(nl_run):
            for v in range(6):
                w = wm[k % 2]
                ps = PS[2 + k % 2]
                k += 1
                sy.dma("sp", w.t[:], w_mod[l].rearrange("(kc p) n -> p kc n", p=128)[:, :, v * 1024:(v + 1) * 1024], writes=[w.r])
                for oc in range(8):
                    for kc in range(8):
                        sy.op("pe", lambda: nc.tensor.matmul(ps.t[:, oc * 5:(oc + 1) * 5], w.t[:, kc, oc * 128:(oc + 1) * 128], scT.t[:, kc, :], start=(kc == 0), stop=(kc == 7)),
                              reads=[w.r, scT.r], writes=[ps.r])
                sy.op("dve", lambda: nc.vector.tensor_tensor(out=MOD.t[:, l, v, :, :], in0=ps.t[:, 0:40].rearrange("p (o j) -> p o j", j=5),
                                                             in1=vtr("bmod", l, v * 8, 8).unsqueeze(2).to_broadcast([128, 8, 5]), op=ALU.add),
                      reads=[ps.r, VT.r], writes=[MOD.r])
            for wi, (v, gname) in enumerate(((1, "n1g"), (4, "n2g"))):
                sy.op("dve", lambda: nc.vector.tensor_scalar(out=AM.t[:, l, wi, :, :], in0=MOD.t[:, l, v, :, :], scalar1=1.0, scalar2=None, op0=ALU.add),
                      reads=[MOD.r], writes=[AM.r])
                sy.op("dve", lambda: nc.vector.tensor_tensor(out=AM.t[:, l, wi, :, :], in0=AM.t[:, l, wi, :, :],
                                                             in1=vtr(gname, l, 0, 8).unsqueeze(2).to_broadcast([128, 8, 5]), op=ALU.mult),
                      reads=[AM.r, VT.r], writes=[AM.r])
        sy.barrier()
    tap("MOD", MOD.t[:], [128, NL, 6, 8, 5], F32, [MOD.r])
    tap("AM", AM.t[:], [128, NL, 2, 8, 5], F32, [AM.r])

    with contextlib.ExitStack() as es0:
        xin = [Tile(es0.enter_context(nc.sbuf_tensor(f"xin{i}", [128, D], F32))) for i in range(2)]
        xo = [Tile(es0.enter_context(nc.sbuf_tensor(f"xo{i}", [128, 8, 512], F32))) for i in range(2)]
        k = 0
        for b in range(nb_run):
            for ci, (t0, n) in enumerate(CHUNKS):
                o = xo[ci % 2]
                for tt in range(n // 128):
                    xi = xin[k % 2]
                    src = x_in[b, t0 + tt * 128:t0 + (tt + 1) * 128, :] if t0 < L else ctx_in[b, tt * 128:(tt + 1) * 128, :]
                    sy.dma("sp", xi.t[:], src, writes=[xi.r])
                    for half in range(2):
                        ps = PS[(2 * k + half) % 4]
                        for c4 in range(4):
                            c = half * 4 + c4
                            sy.op("pe", lambda: nc.tensor.transpose(out=ps.t[:, c4 * 128:(c4 + 1) * 128], in_=xi.t[:, c * 128:(c + 1) * 128], identity=ident.t[:]),
                                  reads=[xi.r, ident.r], writes=[ps.r])
                        eng = "act" if half == 0 else "dve"
                        dst = o.t[:, half * 4:(half + 1) * 4, tt * 128:(tt + 1) * 128]
                        srcp = ps.t[:, :].rearrange("p (c t) -> p c t", t=128)
                        if eng == "act":
                            sy.op("act", lambda: nc.scalar.copy(out=dst, in_=srcp), reads=[ps.r], writes=[o.r])
                        else:
                            sy.op("dve", lambda: nc.vector.tensor_copy(out=dst, in_=srcp), reads=[ps.r], writes=[o.r])
                    k += 1
                sy.dma("sp", xT_d[b, :, :, t0:t0 + n], o.t[:, :, 0:n], reads=[o.r], writes=[xT_res[b][ci]])
        sy.barrier()

    cs_d = nc.dram_tensor("cs_scr", [2, 128, T], F32, kind="Internal").ap()
    cs_res = Res()
    with contextlib.ExitStack() as es0:
        cosF = Tile(es0.enter_context(nc.sbuf_tensor("cosF", [128, T], F32)))
        sinS = Tile(es0.enter_context(nc.sbuf_tensor("sinS", [128, T], F32)))
        pidx = Tile(es0.enter_context(nc.sbuf_tensor("pidx", [128, 1], I32)))
        pf = Tile(es0.enter_context(nc.sbuf_tensor("pf", [128, 8], F32)))
        rowt = Tile(es0.enter_context(nc.sbuf_tensor("rowt", [128, L], I32)))
        colt = Tile(es0.enter_context(nc.sbuf_tensor("colt", [128, L], I32)))
        rowf = Tile(es0.enter_context(nc.sbuf_tensor("rowf", [128, L], F32)))
        colf = Tile(es0.enter_context(nc.sbuf_tensor("colf", [128, L], F32)))
        sy.op("pool", lambda: nc.gpsimd.iota(pidx.t[:], pattern=[[0, 1]], base=0, channel_multiplier=1), writes=[pidx.r])
        sy.op("pool", lambda: nc.gpsimd.iota(rowt.t[:], pattern=[[1, 32], [0, 64]], base=0, channel_multiplier=0), writes=[rowt.r])
        sy.op("pool", lambda: nc.gpsimd.iota(colt.t[:], pattern=[[0, 32], [1, 64]], base=0, channel_multiplier=0), writes=[colt.r])
        sy.op("dve", lambda: nc.vector.tensor_copy(out=rowf.t[:], in_=rowt.t[:]), reads=[rowt.r], writes=[rowf.r])
        sy.op("dve", lambda: nc.vector.tensor_copy(out=colf.t[:], in_=colt.t[:]), reads=[colt.r], writes=[colf.r])
        sy.op("dve", lambda: nc.vector.tensor_copy(out=pf.t[:, 0:1], in_=pidx.t[:]), reads=[pidx.r], writes=[pf.r])
        def dv(fn, reads, writes):
            sy.op("dve", fn, reads=reads, writes=writes)
        dv(lambda: nc.vector.tensor_single_scalar(out=pf.t[:, 7:8], in_=pf.t[:, 0:1], scalar=32.0, op=ALU.is_ge), [pf.r], [pf.r])
        dv(lambda: nc.vector.tensor_single_scalar(out=pf.t[:, 6:7], in_=pf.t[:, 0:1], scalar=64.0, op=ALU.is_ge), [pf.r], [pf.r])
        dv(lambda: nc.vector.tensor_tensor(out=pf.t[:, 7:8], in0=pf.t[:, 7:8], in1=pf.t[:, 6:7], op=ALU.add), [pf.r], [pf.r])
        dv(lambda: nc.vector.tensor_single_scalar(out=pf.t[:, 1:2], in_=pf.t[:, 0:1], scalar=96.0, op=ALU.is_ge), [pf.r], [pf.r])
        dv(lambda: nc.vector.tensor_tensor(out=pf.t[:, 7:8], in0=pf.t[:, 7:8], in1=pf.t[:, 1:2], op=ALU.add), [pf.r], [pf.r])
        dv(lambda: nc.vector.scalar_tensor_tensor(out=pf.t[:, 1:2], in0=pf.t[:, 7:8], scalar=-32.0, in1=pf.t[:, 0:1], op0=ALU.mult, op1=ALU.add), [pf.r], [pf.r])
        dv(lambda: nc.vector.tensor_single_scalar(out=pf.t[:, 3:4], in_=pf.t[:, 1:2], scalar=16.0, op=ALU.is_lt), [pf.r], [pf.r])
        dv(lambda: nc.vector.tensor_single_scalar(out=pf.t[:, 7:8], in_=pf.t[:, 1:2], scalar=16.0, op=ALU.is_ge), [pf.r], [pf.r])
        dv(lambda: nc.vector.scalar_tensor_tensor(out=pf.t[:, 2:3], in0=pf.t[:, 7:8], scalar=-16.0, in1=pf.t[:, 1:2], op0=ALU.mult, op1=ALU.add), [pf.r], [pf.r])
        sy.op("act", lambda: nc.scalar.activation(out=pf.t[:, 4:5], in_=pf.t[:, 2:3], func=AF.Exp, scale=-math.log(10000.0) / 16.0), reads=[pf.r], writes=[pf.r])
        dv(lambda: nc.vector.tensor_single_scalar(out=pf.t[:, 5:6], in_=pf.t[:, 0:1], scalar=32.0, op=ALU.is_ge), [pf.r], [pf.r])
        dv(lambda: nc.vector.tensor_single_scalar(out=pf.t[:, 7:8], in_=pf.t[:, 0:1], scalar=64.0, op=ALU.is_ge), [pf.r], [pf.r])
        dv(lambda: nc.vector.tensor_tensor(out=pf.t[:, 5:6], in0=pf.t[:, 5:6], in1=pf.t[:, 7:8], op=ALU.subtract), [pf.r], [pf.r])
        dv(lambda: nc.vector.tensor_single_scalar(out=pf.t[:, 7:8], in_=pf.t[:, 0:1], scalar=96.0, op=ALU.is_ge), [pf.r], [pf.r])
        dv(lambda: nc.vector.tensor_tensor(out=pf.t[:, 5:6], in0=pf.t[:, 5:6], in1=pf.t[:, 7:8], op=ALU.add), [pf.r], [pf.r])
        dv(lambda: nc.vector.tensor_scalar(out=pf.t[:, 5:6], in0=pf.t[:, 5:6], scalar1=2.0, scalar2=-1.0, op0=ALU.mult, op1=ALU.add), [pf.r], [pf.r])
        dv(lambda: nc.vector.tensor_tensor(out=rowf.t[:], in0=rowf.t[:], in1=colf.t[:], op=ALU.subtract), [rowf.r, colf.r], [rowf.r])
        dv(lambda: nc.vector.scalar_tensor_tensor(out=colf.t[:], in0=rowf.t[:], scalar=pf.t[:, 3:4], in1=colf.t[:], op0=ALU.mult, op1=ALU.add), [rowf.r, colf.r, pf.r], [colf.r])
        dv(lambda: nc.vector.tensor_scalar(out=colf.t[:], in0=colf.t[:], scalar1=pf.t[:, 4:5], scalar2=None, op0=ALU.mult), [colf.r, pf.r], [colf.r])

        def reduce_sin(dst, shift):
            dv(lambda: nc.vector.tensor_scalar(out=rowf.t[:], in0=colf.t[:], scalar1=shift, scalar2=1.0 / (2 * math.pi), op0=ALU.add, op1=ALU.mult), [colf.r], [rowf.r])
            dv(lambda: nc.vector.tensor_copy(out=rowt.t[:], in_=rowf.t[:]), [rowf.r], [rowt.r])
            dv(lambda: nc.vector.tensor_copy(out=rowf.t[:], in_=rowt.t[:]), [rowt.r], [rowf.r])
            dv(lambda: nc.vector.scalar_tensor_tensor(out=rowf.t[:], in0=rowf.t[:], scalar=-2 * math.pi, in1=colf.t[:], op0=ALU.mult, op1=ALU.add), [rowf.r, colf.r], [rowf.r])
            dv(lambda: nc.vector.tensor_single_scalar(out=rowf.t[:], in_=rowf.t[:], scalar=shift, op=ALU.add), [rowf.r], [rowf.r])
            dv(lambda: nc.vector.tensor_single_scalar(out=colt.t[:].bitcast(F32), in_=rowf.t[:], scalar=math.pi, op=ALU.is_gt), [rowf.r], [colt.r])
            dv(lambda: nc.vector.scalar_tensor_tensor(out=rowf.t[:], in0=colt.t[:].bitcast(F32), scalar=-2 * math.pi, in1=rowf.t[:], op0=ALU.mult, op1=ALU.add), [rowf.r, colt.r], [rowf.r])
            dv(lambda: nc.vector.tensor_scalar(out=rowf.t[:], in0=rowf.t[:], scalar1=math.pi, scalar2=-math.pi, op0=ALU.min, op1=ALU.max), [rowf.r], [rowf.r])
            sy.op("act", lambda: nc.scalar.activation(out=dst, in_=rowf.t[:], func=AF.Sin), reads=[rowf.r], writes=[sinS.r, cosF.r])

        reduce_sin(sinS.t[:, 0:L], 0.0)
        dv(lambda: nc.vector.tensor_scalar(out=sinS.t[:, 0:L], in0=sinS.t[:, 0:L], scalar1=pf.t[:, 5:6], scalar2=None, op0=ALU.mult), [sinS.r, pf.r], [sinS.r])
        reduce_sin(cosF.t[:, 0:L], 0.5 * math.pi)
        sy.op("pool", lambda: nc.gpsimd.memset(cosF.t[:, L:T], 1.0), writes=[cosF.r])
        sy.op("pool", lambda: nc.gpsimd.memset(sinS.t[:, L:T], 0.0), writes=[sinS.r])
        sy.dma("sp", cs_d[0], cosF.t[:], reads=[cosF.r], writes=[cs_res])
        sy.dma("sp", cs_d[1], sinS.t[:], reads=[sinS.r], writes=[cs_res])
        tap("cosF", cosF.t[:], [128, T], F32, [cosF.r])
        tap("sinS", sinS.t[:], [128, T], F32, [sinS.r])
        sy.barrier()

    skc = sb("skc", [1, NLA * 8], F32)
    sinkl = sb("sinkl", [128, 2, 128], BF16)
    nlam = sb("nlam", [128, NLA], F32)
    gsc = sb("gsc", [128, NLA], F32)
    with contextlib.ExitStack() as es0:
        sk = Tile(es0.enter_context(nc.sbuf_tensor("sk", [1, NLA * 8], F32)))
        lq = Tile(es0.enter_context(nc.sbuf_tensor("lq", [128, 4, NLA, 64], F32)))
        ls = Tile(es0.enter_context(nc.sbuf_tensor("ls", [128, 2, NLA], F32)))
        sy.dma("sp", sk.t[:], attn_sink.rearrange("l h -> (l h)").unsqueeze(0), writes=[sk.r])
        sy.op("act", lambda: nc.scalar.activation(out=skc.t[:], in_=sk.t[:], func=AF.Exp), reads=[sk.r], writes=[skc.r])
        sy.op("pool", lambda: nc.gpsimd.memset(sinkl.t[:], 0.0), writes=[sinkl.r])
        sy.op("pool", lambda: nc.gpsimd.memset(sinkl.t[0:1, 0, 64:128], 1.0), writes=[sinkl.r])
        sy.op("pool", lambda: nc.gpsimd.memset(sinkl.t[0:1, 1, 0:64], 1.0), writes=[sinkl.r])
        for i, a in enumerate((lam_q1, lam_k1, lam_q2, lam_k2)):
            sy.dma("sp", lq.t[:, i, :, :], a.unsqueeze(0).to_broadcast([128, NLA, 64]), writes=[lq.r])
        for i in range(2):
            sy.op("dve", lambda: nc.vector.tensor_tensor(out=lq.t[:, 2 * i, :, :], in0=lq.t[:, 2 * i, :, :], in1=lq.t[:, 2 * i + 1, :, :], op=ALU.mult), reads=[lq.r], writes=[lq.r])
            sy.op("dve", lambda: nc.vector.tensor_reduce(out=ls.t[:, i, :], in_=lq.t[:, 2 * i, :, :], axis=AX.X, op=ALU.add), reads=[lq.r], writes=[ls.r])
        sy.op("act", lambda: nc.scalar.activation(out=ls.t[:], in_=ls.t[:], func=AF.Exp), reads=[ls.r], writes=[ls.r])
        sy.op("dve", lambda: nc.vector.tensor_tensor(out=nlam.t[:], in0=ls.t[:, 1, :], in1=ls.t[:, 0, :], op=ALU.subtract), reads=[ls.r], writes=[nlam.r])
        for l in range(NLA):
            li = 0.8 - 0.6 * math.exp(-0.3 * l)
            sy.op("dve", lambda: nc.vector.tensor_single_scalar(out=nlam.t[:, l:l + 1], in_=nlam.t[:, l:l + 1], scalar=-li, op=ALU.add), reads=[nlam.r], writes=[nlam.r])
            sy.op("dve", lambda: nc.vector.tensor_single_scalar(out=gsc.t[:, l:l + 1], in_=vt("dg", l), scalar=1.0 - li, op=ALU.mult), reads=[VT.r], writes=[gsc.r])
        sy.barrier()
    tap("nlam", nlam.t[:], [128, NLA], F32, [nlam.r])

    UTs_d = nc.dram_tensor("UTs_scr", [NLA, 128, 128, 8, 128], BF16, kind="Internal").ap()
    Vs_d = nc.dram_tensor("Vs_scr", [NLA, 128, 128, 1024], BF16, kind="Internal").ap()
    h2_d = nc.dram_tensor("h2_scr", [128, 8, T], BF16, kind="Internal").ap()
    rt_d = nc.dram_tensor("rt_scr", [128, 3, T], F32, kind="Internal").ap()
    tab_res = [Res() for _ in range(NLA)]
    if do_peer:
        with contextlib.ExitStack() as es0:
            ub = [Tile(es0.enter_context(nc.sbuf_tensor(f"ub{i}", [128, 1024], BF16))) for i in range(3)]
            uo = [Tile(es0.enter_context(nc.sbuf_tensor(f"uo{i}", [128, 8, 128], BF16))) for i in range(3)]
            vb = [Tile(es0.enter_context(nc.sbuf_tensor(f"vb{i}", [128, 8, 1024], BF16))) for i in range(2)]
            for l in range(nl_run):
                uv = peer_u[l].rearrange("(i1 i2) d -> i2 i1 d", i2=128)
                vv = peer_v[l].rearrange("(i1 i2) d -> i1 i2 d", i2=128)
                for i2 in range(128):
                    u_, o_ = ub[i2 % 3], uo[i2 % 3]
                    ps = PS[i2 % 4]
                    psb = ps.t[:].bitcast(BF16)
                    sy.dma("pq", u_.t[:], uv[i2], writes=[u_.r])
                    for kc in range(8):
                        sy.op("pe", lambda: nc.tensor.transpose(out=psb[:, kc * 128:(kc + 1) * 128], in_=u_.t[:, kc * 128:(kc + 1) * 128], identity=identb.t[:]),
                              reads=[u_.r, identb.r], writes=[ps.r])
                    if i2 % 2 == 0:
                        sy.op("act", lambda: nc.scalar.copy(out=o_.t[:].rearrange("p k i -> p (k i)"), in_=psb[:, 0:1024]), reads=[ps.r], writes=[o_.r])
                    else:
                        sy.op("dve", lambda: nc.vector.tensor_copy(out=o_.t[:].rearrange("p k i -> p (k i)"), in_=psb[:, 0:1024]), reads=[ps.r], writes=[o_.r])
                    sy.dma("sp", UTs_d[l, :, i2, :, :], o_.t[:], reads=[o_.r], writes=[tab_res[l]])
                    if i2 % 8 == 7:
                        g = i2 // 8
                        v_ = vb[g % 2]
                        sy.dma("pq", v_.t[:], vv[:, g * 8:(g + 1) * 8, :], writes=[v_.r])
                        sy.dma("sp", Vs_d[l, :, g * 8:(g + 1) * 8, :], v_.t[:], reads=[v_.r], writes=[tab_res[l]])
            sy.barrier()

    wq_toggle = [0]

    def wload(dst_ap, src_ap, tile):
        sy.dma("pq", dst_ap, src_ap, writes=[tile.r])

    def norm_chunk(b, l, ci, which, x_t, sq_t, dst_fn, dst_res, ps, rs_t, tmp_t):
        t0, n = CHUNKS[ci]
        j = b if t0 < L else 4
        sy.op("act", lambda: nc.scalar.activation(out=sq_t.t[:, :, 0:n], in_=x_t.t[:, :, 0:n], func=AF.Square), reads=[x_t.r], writes=[sq_t.r])
        for c in range(8):
            sy.op("pe", lambda: nc.tensor.matmul(ps.t[:, 0:n], onesD.t[:], sq_t.t[:, c, 0:n], start=(c == 0), stop=(c == 7)), reads=[sq_t.r, onesD.r], writes=[ps.r], final=(c == 7))
        sy.op("act", lambda: nc.scalar.activation(out=rs_t.t[:, 0:n], in_=ps.t[:, 0:n], func=AF.Sqrt, bias=EPS, scale=1.0), reads=[ps.r], writes=[rs_t.r])
        sy.op("dve", lambda: nc.vector.reciprocal(out=rs_t.t[:, 0:n], in_=rs_t.t[:, 0:n]), reads=[rs_t.r], writes=[rs_t.r])
        for c in range(8):
            if which < 2:
                a_ap = AM.t[:, l, which, c, j:j + 1]
                s_ap = MOD.t[:, l, 0 if which == 0 else 3, c, j:j + 1]
            else:
                a_ap = vt("fg", 0, c)
                s_ap = None
            sy.op("dve", lambda: nc.vector.scalar_tensor_tensor(out=tmp_t.t[:, c, 0:n], in0=x_t.t[:, c, 0:n], scalar=a_ap, in1=rs_t.t[:, 0:n], op0=ALU.mult, op1=ALU.mult),
                  reads=[x_t.r, rs_t.r, AM.r, VT.r], writes=[tmp_t.r])
            if s_ap is not None:
                sy.op("act", lambda: nc.scalar.activation(out=dst_fn(c), in_=tmp_t.t[:, c, 0:n], func=AF.Identity, bias=s_ap, scale=1.0), reads=[tmp_t.r, MOD.r], writes=[dst_res])

    state = dict(nc=nc, sy=sy, es=es, PS=PS, sb=sb, tap=tap, tap_out=tap_out, tap_res=tap_res)
    consts = dict(ident=ident, identb=identb, maskGE=maskGE, maskLE=maskLE, onesD=onesD, ones5=ones5, ones1=ones1, onesb=onesb, iof=iof,
                  VT=VT, vt=vt, vtr=vtr, MOD=MOD, AM=AM, cs_d=cs_d, cs_res=cs_res, skc=skc, sinkl=sinkl, nlam=nlam, gsc=gsc)
    dram = dict(w_in=w_in, w_gate=w_gate, w_branch=w_branch, w_out=w_out, pool_w=pool_w, peer_wq=peer_wq, peer_k1=peer_k1, peer_k2=peer_k2,
                peer_u=peer_u, peer_v=peer_v, xT_d=xT_d, xT_res=xT_res, out_d=out_d, yT_d=yT_d, yT_res=yT_res,
                UTs_d=UTs_d, Vs_d=Vs_d, h2_d=h2_d, rt_d=rt_d, tab_res=tab_res)

    from_mixer = mixer_phase
    for l in range(nl_run):
        for b in range(nb_run):
            from_mixer(state, consts, dram, cfg, b, l, norm_chunk, wload)
            if do_peer and not cfg.get("skip_peer_phase"):
                peer_phase(state, consts, dram, cfg, b, l, norm_chunk, wload)

    final_phase(state, consts, dram, cfg, nb_run, norm_chunk)
    sy.barrier(engs=("sp",))
    es.close()
    return nc, tap_out


_uid = [0]


def mixer_phase(state, consts, dram, cfg, b, l, norm_chunk, wload):
    if cfg.get("skip_mixer"):
        return
    nc, sy, PS, tap = state["nc"], state["sy"], state["PS"], state["tap"]
    C = consts
    vt, vtr, MOD = C["vt"], C["vtr"], C["MOD"]
    xT_d, xT_res = dram["xT_d"], dram["xT_res"]
    ctx_out = l < NL - 1
    qchunks = list(range(5)) if ctx_out else list(range(4))
    wv = dram["w_in"][l].rearrange("(kc p) n -> p kc n", p=128)
    branches = cfg.get("branches", "ACBD")

    def mk(scope, name, shape, dt, nres=1):
        _uid[0] += 1
        return Tile(scope.enter_context(nc.sbuf_tensor(f"{name}_{_uid[0]}", list(shape), dt)), nres)

    def dve(fn, reads, writes):
        sy.op("dve", fn, reads=reads, writes=writes)

    def act(fn, reads, writes):
        sy.op("act", fn, reads=reads, writes=writes)

    def pool(fn, reads, writes):
        sy.op("pool", fn, reads=reads, writes=writes)

    def pe(fn, reads, writes, final=True):
        sy.op("pe", fn, reads=reads, writes=writes, final=final)

    with contextlib.ExitStack() as ms:
        hT = mk(ms, "hT", [128, 8, T], BF16, nres=5)
        with contextlib.ExitStack() as s1:
            xt = [mk(s1, "xt", [128, 8, 512], F32) for _ in range(2)]
            sq = mk(s1, "sq", [128, 8, 512], F32)
            tmp = mk(s1, "tmp", [128, 8, 512], F32)
            rs = [mk(s1, "rs", [128, 512], F32) for _ in range(2)]
            for ci, (t0, n) in enumerate(CHUNKS):
                x_t = xt[ci % 2]
                sy.dma("sp", x_t.t[:, :, 0:n], xT_d[b, :, :, t0:t0 + n], reads=[xT_res[b][ci]], writes=[x_t.r])
                norm_chunk(b, l, ci, 0, x_t, sq, (lambda c, t0=t0, n=n: hT.t[:, c, t0:t0 + n]), hT.res[ci], PS[ci % 2], rs[ci % 2], tmp)
            sy.barrier()
        tap(f"hT{l}", hT.t[:], [128, 8, T], BF16, hT.res)

        yT_d, yT_res = dram["yT_d"], dram["yT_res"]
        cosF = mk(ms, "cosF", [128, T], F32)
        sinS = mk(ms, "sinS", [128, T], F32)
        sy.dma("sp", cosF.t[:], C["cs_d"][0], reads=[C["cs_res"]], writes=[cosF.r])
        sy.dma("sp", sinS.t[:], C["cs_d"][1], reads=[C["cs_res"]], writes=[sinS.r])
        ystg = [mk(ms, "ystg", [128, 512], BF16) for _ in range(3)]
        ycnt = [0]

        def y_out(nbr, kc, ci, p0, p1, fn_write):
            t0, n = CHUNKS[ci]
            st = ystg[ycnt[0] % 3]
            ycnt[0] += 1
            fn_write(st.t[p0:p1, 0:n], st.r)
            sy.dma("sp", yT_d[nbr, p0:p1, kc, t0:t0 + n], st.t[p0:p1, 0:n], reads=[st.r], writes=[yT_res[nbr][ci]])

        def proj(ps, Wt, c0, ci, ncols=128):
            t0, n = CHUNKS[ci]
            for kc in range(8):
                pe(lambda: nc.tensor.matmul(ps.t[0:ncols, 0:n], Wt.t[:, kc, c0:c0 + ncols], hT.t[:, kc, t0:t0 + n], start=(kc == 0), stop=(kc == 7)),
                   [Wt.r, hT.res[ci]], [ps.r], final=(kc == 7))

        def load_rot(Wr, seg0, ncols):
            dv_ = Wr.t[:, :, 0:ncols].rearrange("p k (h two j) -> p k h two j", two=2, j=32)
            sv_ = wv[:, :, seg0:seg0 + ncols].rearrange("p k (h two j) -> p k h two j", two=2, j=32)
            if cfg.get("fake_rot"):
                sy.dma("pq", Wr.t[:, :, 0:ncols], wv[:, :, seg0:seg0 + ncols], writes=[Wr.r])
                return
            for kc in range(8):
                sy.dma("pq", dv_[:, kc, :, 0, :], sv_[:, kc, :, 1, :], writes=[Wr.r])
                sy.dma("pq", dv_[:, kc, :, 1, :], sv_[:, kc, :, 0, :], writes=[Wr.r])

        def rope_evac(psA, psB, ci, dst_ap, dst_res, t1, t2, splits=None):
            t0, n = CHUNKS[ci]
            dve(lambda: nc.vector.tensor_tensor(out=t1.t[:, 0:n], in0=psA.t[:, 0:n], in1=cosF.t[:, t0:t0 + n], op=ALU.mult), [psA.r, cosF.r], [t1.r])
            dve(lambda: nc.vector.tensor_tensor(out=t2.t[:, 0:n], in0=psB.t[:, 0:n], in1=sinS.t[:, t0:t0 + n], op=ALU.mult), [psB.r, sinS.r], [t2.r])
            if splits is None:
                dve(lambda: nc.vector.tensor_tensor(out=dst_ap, in0=t1.t[:, 0:n], in1=t2.t[:, 0:n], op=ALU.add), [t1.r, t2.r], [dst_res])
            else:
                for (q0_, q1_, dap) in splits:
                    dve(lambda: nc.vector.tensor_tensor(out=dap, in0=t1.t[q0_:q1_, 0:n], in1=t2.t[q0_:q1_, 0:n], op=ALU.add), [t1.r, t2.r], [dst_res])

        if "A" in branches:
            with contextlib.ExitStack() as sa:
                qaT = mk(sa, "qaT", [128, 4, T], BF16)
                kaz = mk(sa, "kaz", [128, 2, 2, T], BF16)
                va = mk(sa, "va", [128, 18, 2, 2, 128], BF16)
                sap = contextlib.ExitStack()
                Waq = mk(sap, "Waq", [128, 8, 512], BF16)
                WaqR = mk(sap, "WaqR", [128, 8, 512], BF16)
                Wak = mk(sap, "Wak", [128, 8, 256], BF16)
                WakR = mk(sap, "WakR", [128, 8, 256], BF16)
                Wav = mk(sap, "Wav", [128, 8, 128], BF16)
                t1 = [mk(sap, "t1", [128, 512], F32) for _ in range(2)]
                t2 = [mk(sap, "t2", [128, 512], F32) for _ in range(2)]
                pool(lambda: nc.gpsimd.memset(kaz.t[64:128, :, 0, :], 0.0), [], [kaz.r])
                pool(lambda: nc.gpsimd.memset(kaz.t[0:64, :, 1, :], 0.0), [], [kaz.r])
                wload(Waq.t[:], wv[:, :, 0:512], Waq)
                load_rot(WaqR, 0, 512)
                for kvh in range(2):
                    for dup in range(2):
                        c0 = (kvh * 2 + dup) * 64
                        sy.dma("pq", Wak.t[:, :, c0:c0 + 64], wv[:, :, 512 + kvh * 64:512 + (kvh + 1) * 64], writes=[Wak.r])
                        for hf in range(2):
                            sy.dma("pq", WakR.t[:, :, c0 + hf * 32:c0 + (hf + 1) * 32],
                                   wv[:, :, 512 + kvh * 64 + (1 - hf) * 32:512 + kvh * 64 + (2 - hf) * 32], writes=[WakR.r])
                wload(Wav.t[:], wv[:, :, 640:768], Wav)
                pool(lambda: nc.gpsimd.memset(va.t[:, :, :, 0, 64:128], 1.0), [], [va.r])
                pool(lambda: nc.gpsimd.memset(va.t[:, :, :, 1, 0:64], 1.0), [], [va.r])
                k = 0
                for ci in range(5):
                    t0, n = CHUNKS[ci]
                    for hc in range(4):
                        if ci == 4 and not ctx_out:
                            continue
                        pa, pb = PS[(2 * k) % 4], PS[(2 * k + 1) % 4]
                        proj(pa, Waq, hc * 128, ci)
                        proj(pb, WaqR, hc * 128, ci)
                        rope_evac(pa, pb, ci, qaT.t[:, hc, t0:t0 + n], qaT.r, t1[k % 2], t2[k % 2])
                        k += 1
                    for kc2 in range(2):
                        pa, pb = PS[(2 * k) % 4], PS[(2 * k + 1) % 4]
                        proj(pa, Wak, kc2 * 128, ci)
                        proj(pb, WakR, kc2 * 128, ci)
                        rope_evac(pa, pb, ci, None, kaz.r, t1[k % 2], t2[k % 2],
                                  splits=[(0, 64, kaz.t[0:64, kc2, 0, t0:t0 + n]), (64, 128, kaz.t[64:128, kc2, 1, t0:t0 + n])])
                        k += 1
                for tt in range(18):
                    ps = PS[4 + tt % 2]
                    ci = min(tt // 4, 4)
                    for kc in range(8):
                        pe(lambda: nc.tensor.matmul(ps.t[:, 0:128], hT.t[:, kc, tt * 128:(tt + 1) * 128], Wav.t[:, kc, :], start=(kc == 0), stop=(kc == 7)),
                           [Wav.r, hT.res[ci]], [ps.r], final=(kc == 7))
                    src = ps.t[:, 0:128].rearrange("p (k d) -> p k d", d=64)
                    act(lambda: nc.scalar.copy(out=va.t[:, tt, :, 0, 0:64], in_=src), [ps.r], [va.r])
                    dve(lambda: nc.vector.tensor_copy(out=va.t[:, tt, :, 1, 64:128], in_=src), [ps.r], [va.r])
                sy.barrier()
                sap.close()
                E = [mk(sa, "E", [128, 16, 384], BF16) for _ in range(2)]
                Ec = [mk(sa, "Ec", [128, 2, T], BF16) for _ in range(2)]
                rden = [mk(sa, "rden", [128, 512], F32) for _ in range(2)]
                maskGE, maskLE = C["maskGE"], C["maskLE"]
                sinkl, skc = C["sinkl"], C["skc"]
                esrow = mk(sa, "esrow", [128, 8, 512], BF16)
                pool(lambda: nc.gpsimd.memset(esrow.t[:], 0.0), [], [esrow.r])
                dve(lambda: nc.vector.tensor_copy(out=esrow.t[0:1, :, :], in_=skc.t[:, l * 8:(l + 1) * 8].unsqueeze(2).to_broadcast([1, 8, 512])), [skc.r], [esrow.r])
                kk = 0
                for h in range(8):
                    kvh, par, hc = h // 4, h % 2, h // 2
                    p0 = 64 * par
                    q0 = 64 - p0
                    Eh, Ech = E[h % 2], Ec[h % 2]
                    for j in range(16):
                        qlo, qhi = max(0, j - 1) * 128, min(16, j + 2) * 128
                        n = qhi - qlo
                        ps = PS[kk % 2]
                        kk += 1
                        pe(lambda: nc.tensor.matmul(ps.t[:, 0:n], kaz.t[:, kvh, par, j * 128:(j + 1) * 128], qaT.t[:, hc, qlo:qhi], start=True, stop=True),
                           [kaz.r, qaT.r], [ps.r])
                        act(lambda: nc.scalar.activation(out=Eh.t[:, j, 0:n], in_=ps.t[:, 0:n], func=AF.Exp, scale=0.125), [ps.r], [Eh.r])
                        if j >= 1:
                            dve(lambda: nc.vector.tensor_tensor(out=Eh.t[:, j, 0:128], in0=Eh.t[:, j, 0:128], in1=maskLE.t[:], op=ALU.mult), [Eh.r, maskLE.r], [Eh.r])
                        if j <= 14:
                            off = (j + 1) * 128 - qlo
                            dve(lambda: nc.vector.tensor_tensor(out=Eh.t[:, j, off:off + 128], in0=Eh.t[:, j, off:off + 128], in1=maskGE.t[:], op=ALU.mult), [Eh.r, maskGE.r], [Eh.r])
                    for jc in range(2):
                        for ci in qchunks:
                            t0, n = CHUNKS[ci]
                            ps = PS[kk % 2]
                            kk += 1
                            pe(lambda: nc.tensor.matmul(ps.t[:, 0:n], kaz.t[:, kvh, par, L + jc * 128:L + (jc + 1) * 128], qaT.t[:, hc, t0:t0 + n], start=True, stop=True),
                               [kaz.r, qaT.r], [ps.r])
                            act(lambda: nc.scalar.activation(out=Ech.t[:, jc, t0:t0 + n], in_=ps.t[:, 0:n], func=AF.Exp, scale=0.125), [ps.r], [Ech.r])
                    for ci in qchunks:
                        t0, n = CHUNKS[ci]
                        pso = PS[2 + kk % 2]
                        rd = rden[kk % 2]
                        kk += 1
                        mm = [(slice(0, n), sinkl.t[:, par, :], esrow.t[:, h, 0:n], [sinkl.r, esrow.r])]
                        for jc in range(2):
                            mm.append((slice(0, n), va.t[:, 16 + jc, kvh, par, :], Ech.t[:, jc, t0:t0 + n], [va.r, Ech.r]))
                        if ci < 4:
                            for j in range(4 * ci - 1, 4 * ci + 5):
                                if 0 <= j < 16:
                                    bl_lo, bl_hi = max(4 * ci, j - 1), min(4 * ci + 3, j + 1)
                                    sub0, nsub = (bl_lo - 4 * ci) * 128, (bl_hi - bl_lo + 1) * 128
                                    eoff = bl_lo * 128 - max(0, j - 1) * 128
                                    mm.append((slice(sub0, sub0 + nsub), va.t[:, j, kvh, par, :], Eh.t[:, j, eoff:eoff + nsub], [va.r, Eh.r]))
                        for i, (sl, lt, rh, rd_) in enumerate(mm):
                            pe(lambda: nc.tensor.matmul(pso.t[:, sl], lt, rh, start=(i == 0), stop=(i == len(mm) - 1)), rd_, [pso.r], final=(i == len(mm) - 1))
                        dve(lambda: nc.vector.reciprocal(out=rd.t[p0:p0 + 64, 0:n], in_=pso.t[q0:q0 + 64, 0:n]), [pso.r], [rd.r])
                        y_out(0, hc, ci, p0, p0 + 64, lambda ap, r_: dve(lambda: nc.vector.tensor_tensor(out=ap, in0=pso.t[p0:p0 + 64, 0:n], in1=rd.t[p0:p0 + 64, 0:n], op=ALU.mult),
                                                                        [pso.r, rd.r], [r_]))
                sy.barrier()
            tap(f"yA{l}", yT_d[0], [128, 4, T], BF16, yT_res[0])

        if "C" in branches:
            with contextlib.ExitStack() as sc:
                qcT = mk(sc, "qcT", [128, 4, T], BF16)
                kcz = mk(sc, "kcz", [128, 2, 4, T], BF16)
                vc = mk(sc, "vc", [128, 18, 512], BF16)
                scp = contextlib.ExitStack()
                Wcq = mk(scp, "Wcq", [128, 8, 512], BF16)
                WcqR = mk(scp, "WcqR", [128, 8, 512], BF16)
                Wck = mk(scp, "Wck", [128, 8, 512], BF16)
                WckR = mk(scp, "WckR", [128, 8, 512], BF16)
                Wcv = mk(scp, "Wcv", [128, 8, 512], BF16)
                t1 = [mk(scp, "t1", [128, 512], F32) for _ in range(2)]
                t2 = [mk(scp, "t2", [128, 512], F32) for _ in range(2)]
                pool(lambda: nc.gpsimd.memset(kcz.t[64:128, 0, :, :], 0.0), [], [kcz.r])
                pool(lambda: nc.gpsimd.memset(kcz.t[0:64, 1, :, :], 0.0), [], [kcz.r])
                wload(Wcq.t[:], wv[:, :, 768:1280], Wcq)
                load_rot(WcqR, 768, 512)
                wload(Wck.t[:], wv[:, :, 1280:1792], Wck)
                load_rot(WckR, 1280, 512)
                wload(Wcv.t[:], wv[:, :, 1792:2304], Wcv)
                k = 0
                for ci in range(5):
                    t0, n = CHUNKS[ci]
                    for hc in range(4):
                        for (W_, WR_, dstT) in ((Wcq, WcqR, qcT), (Wck, WckR, kcz)):
                            if dstT is qcT and ci == 4 and not ctx_out:
                                continue
                            pa, pb = PS[(2 * k) % 4], PS[(2 * k + 1) % 4]
                            proj(pa, W_, hc * 128, ci)
                            proj(pb, WR_, hc * 128, ci)
                            if dstT is qcT:
                                rope_evac(pa, pb, ci, qcT.t[:, hc, t0:t0 + n], qcT.r, t1[k % 2], t2[k % 2])
                            else:
                                rope_evac(pa, pb, ci, None, kcz.r, t1[k % 2], t2[k % 2],
                                          splits=[(0, 64, kcz.t[0:64, 0, hc, t0:t0 + n]), (64, 128, kcz.t[64:128, 1, hc, t0:t0 + n])])
                            k += 1
                for tt in range(18):
                    ps = PS[4 + tt % 2]
                    ci = min(tt // 4, 4)
                    for kc in range(8):
                        pe(lambda: nc.tensor.matmul(ps.t[:, 0:512], hT.t[:, kc, tt * 128:(tt + 1) * 128], Wcv.t[:, kc, :], start=(kc == 0), stop=(kc == 7)),
                           [Wcv.r, hT.res[ci]], [ps.r], final=(kc == 7))
                    if tt % 2 == 0:
                        act(lambda: nc.scalar.copy(out=vc.t[:, tt, :], in_=ps.t[:, 0:512]), [ps.r], [vc.r])
                    else:
                        dve(lambda: nc.vector.tensor_copy(out=vc.t[:, tt, :], in_=ps.t[:, 0:512]), [ps.r], [vc.r])
                sy.barrier()
                scp.close()
                Et = [mk(sc, "Et", [128, 512], BF16) for _ in range(3)]
                rd = [mk(sc, "rdc", [128, 512], F32) for _ in range(2)]
                tu = [mk(sc, "tu", [128, 512], F32) for _ in range(2)]
                ot = mk(sc, "ot", [128, 512], F32)
                sqo = mk(sc, "sqo", [128, 512], F32)
                rso = mk(sc, "rso", [128, 512], F32)
                onesb, ones1, nlam, gsc = C["onesb"], C["ones1"], C["nlam"], C["gsc"]
                kk = 0
                for h in range(4):
                    for ci in qchunks:
                        t0, n = CHUNKS[ci]
                        keyt = list(range(18)) if ci < 4 else [16, 17]
                        items = [(c, ji, j) for c in range(2) for ji, j in enumerate(keyt)]
                        SK = 2
                        scb = (PS[0], PS[1], PS[6])
                        pend = []

                        def emit_score(idx_):
                            c, ji, j = items[idx_]
                            p0 = 64 * c
                            ps = scb[(kk0 + idx_) % 3]
                            et = Et[(kk0 + idx_) % 3]
                            pe(lambda: nc.tensor.matmul(ps.t[:, 0:n], kcz.t[:, c, h, j * 128:(j + 1) * 128], qcT.t[:, h, t0:t0 + n], start=True, stop=True),
                               [kcz.r, qcT.r], [ps.r])
                            act(lambda: nc.scalar.activation(out=et.t[:, 0:n], in_=ps.t[:, 0:n], func=AF.Exp, scale=0.125), [ps.r], [et.r])

                        def emit_pv(idx_):
                            c, ji, j = items[idx_]
                            et = Et[(kk0 + idx_) % 3]
                            psU, psD = PS[2 + c], PS[4 + c]
                            pe(lambda: nc.tensor.matmul(psU.t[:, 0:n], vc.t[:, j, h * 128:(h + 1) * 128], et.t[:, 0:n], start=(ji == 0), stop=(ji == len(keyt) - 1)),
                               [vc.r, et.r], [psU.r], final=(ji == len(keyt) - 1))
                            pe(lambda: nc.tensor.matmul(psD.t[:, 0:n], onesb.t[:], et.t[:, 0:n], start=(ji == 0), stop=(ji == len(keyt) - 1)),
                               [onesb.r, et.r], [psD.r], final=(ji == len(keyt) - 1))

                        kk0 = kk
                        for idx_ in range(len(items) + SK):
                            if idx_ < len(items):
                                emit_score(idx_)
                            if idx_ >= SK:
                                emit_pv(idx_ - SK)
                        kk += len(items)
                        for c in range(2):
                            dve(lambda: nc.vector.reciprocal(out=rd[c].t[:, 0:n], in_=PS[4 + c].t[:, 0:n]), [PS[4 + c].r], [rd[c].r])
                            dve(lambda: nc.vector.tensor_tensor(out=tu[c].t[:, 0:n], in0=PS[2 + c].t[:, 0:n], in1=rd[c].t[:, 0:n], op=ALU.mult), [PS[2 + c].r, rd[c].r], [tu[c].r])
                        dve(lambda: nc.vector.scalar_tensor_tensor(out=ot.t[:, 0:n], in0=tu[1].t[:, 0:n], scalar=nlam.t[:, l:l + 1], in1=tu[0].t[:, 0:n], op0=ALU.mult, op1=ALU.add),
                            [tu[0].r, tu[1].r, nlam.r], [ot.r])
                        act(lambda: nc.scalar.activation(out=sqo.t[:, 0:n], in_=ot.t[:, 0:n], func=AF.Square), [ot.r], [sqo.r])
                        psn = PS[7]
                        pe(lambda: nc.tensor.matmul(psn.t[:, 0:n], ones1.t[:], sqo.t[:, 0:n], start=True, stop=True), [ones1.r, sqo.r], [psn.r])
                        act(lambda: nc.scalar.activation(out=rso.t[:, 0:n], in_=psn.t[:, 0:n], func=AF.Sqrt, bias=EPS, scale=1.0), [psn.r], [rso.r])
                        dve(lambda: nc.vector.reciprocal(out=rso.t[:, 0:n], in_=rso.t[:, 0:n]), [rso.r], [rso.r])
                        y_out(1, h, ci, 0, 128, lambda ap, r_: dve(lambda: nc.vector.scalar_tensor_tensor(out=ap, in0=ot.t[:, 0:n], scalar=gsc.t[:, l:l + 1], in1=rso.t[:, 0:n], op0=ALU.mult, op1=ALU.mult),
                                                                  [ot.r, rso.r, gsc.r], [r_]))
                sy.barrier()
            tap(f"yC{l}", yT_d[1], [128, 4, T], BF16, yT_res[1])

        if "B" in branches:
            with contextlib.ExitStack() as sb_:
                YW = 15 + L + 15 + 15 + LC + 15
                offs = (15, 15 + L + 15 + 15)
                Wb = mk(sb_, "Wb", [128, 8, 1024], BF16)
                ypad = mk(sb_, "ypad", [128, 4, YW], BF16)
                Dm = mk(sb_, "Dm", [128, 124, 128], BF16)
                z = mk(sb_, "z", [128, 4, T], F32)
                sg = [mk(sb_, "sg", [128, 512], F32) for _ in range(2)]
                wload(Wb.t[:], wv[:, :, 2304:3328], Wb)
                pool(lambda: nc.gpsimd.memset(ypad.t[:], 0.0), [], [ypad.r])
                identb = C["identb"]
                for i in range(124):
                    fn = (lambda: nc.vector.tensor_scalar(out=Dm.t[:, i, :], in0=identb.t[:], scalar1=vt("cw", l, i), scalar2=None, op0=ALU.mult))
                    dve(fn, [identb.r, C["VT"].r], [Dm.r])
                bchunks = qchunks
                k = 0
                for cc in range(4):
                    for ci in bchunks:
                        t0, n = CHUNKS[ci]
                        pa, pg = PS[(2 * k) % 4], PS[(2 * k + 1) % 4]
                        s_ = sg[k % 2]
                        k += 1
                        proj(pa, Wb, cc * 128, ci)
                        proj(pg, Wb, 512 + cc * 128, ci)
                        act(lambda: nc.scalar.activation(out=s_.t[:, 0:n], in_=pg.t[:, 0:n], func=AF.Sigmoid), [pg.r], [s_.r])
                        yo = offs[0] + t0 if ci < 4 else offs[1]
                        dve(lambda: nc.vector.tensor_tensor(out=ypad.t[:, cc, yo:yo + n], in0=pa.t[:, 0:n], in1=s_.t[:, 0:n], op=ALU.mult), [pa.r, s_.r], [ypad.r])
                k = 0
                for cc in range(4):
                    for ci in bchunks:
                        t0, n = CHUNKS[ci]
                        ps = PS[4 + k % 2]
                        k += 1
                        base = (t0 if ci < 4 else offs[1] - 15)
                        for kk_ in range(31):
                            pe(lambda: nc.tensor.matmul(ps.t[:, 0:n], Dm.t[:, kk_ * 4 + cc, :], ypad.t[:, cc, base + kk_:base + kk_ + n], start=(kk_ == 0), stop=(kk_ == 30)),
                               [Dm.r, ypad.r], [ps.r], final=(kk_ == 30))
                        act(lambda: nc.scalar.activation(out=z.t[:, cc, t0:t0 + n], in_=ps.t[:, 0:n], func=AF.Identity, bias=vt("cb", l, cc), scale=1.0), [ps.r, C["VT"].r], [z.r])
                ones5 = C["ones5"]
                zsq = mk(sb_, "zsq", [128, 4, 512], F32)
                m2 = mk(sb_, "m2", [128, 512], F32)
                var = mk(sb_, "var", [128, 512], F32)
                tz = [mk(sb_, "tz", [128, 512], F32) for _ in range(2)]
                for ci in bchunks:
                    t0, n = CHUNKS[ci]
                    psm, psq = PS[6], PS[7]
                    act(lambda: nc.scalar.activation(out=zsq.t[:, :, 0:n], in_=z.t[:, :, t0:t0 + n], func=AF.Square), [z.r], [zsq.r])
                    for cc in range(4):
                        pe(lambda: nc.tensor.matmul(psm.t[:, 0:n], ones5.t[:], z.t[:, cc, t0:t0 + n], start=(cc == 0), stop=(cc == 3)), [ones5.r, z.r], [psm.r], final=(cc == 3))
                    for cc in range(4):
                        pe(lambda: nc.tensor.matmul(psq.t[:, 0:n], ones5.t[:], zsq.t[:, cc, 0:n], start=(cc == 0), stop=(cc == 3)), [ones5.r, zsq.r], [psq.r], final=(cc == 3))
                    act(lambda: nc.scalar.activation(out=m2.t[:, 0:n], in_=psm.t[:, 0:n], func=AF.Square), [psm.r], [m2.r])
                    dve(lambda: nc.vector.tensor_tensor(out=var.t[:, 0:n], in0=psq.t[:, 0:n], in1=m2.t[:, 0:n], op=ALU.subtract), [psq.r, m2.r], [var.r])
                    dve(lambda: nc.vector.tensor_scalar_max(out=var.t[:, 0:n], in0=var.t[:, 0:n], scalar1=0.0), [var.r], [var.r])
                    act(lambda: nc.scalar.activation(out=var.t[:, 0:n], in_=var.t[:, 0:n], func=AF.Sqrt, bias=EPS, scale=1.0), [var.r], [var.r])
                    dve(lambda: nc.vector.reciprocal(out=var.t[:, 0:n], in_=var.t[:, 0:n]), [var.r], [var.r])
                    for cc in range(4):
                        tz_ = tz[cc % 2]
                        dve(lambda: nc.vector.tensor_tensor(out=tz_.t[:, 0:n], in0=z.t[:, cc, t0:t0 + n], in1=psm.t[:, 0:n], op=ALU.subtract), [z.r, psm.r], [tz_.r])
                        dve(lambda: nc.vector.tensor_tensor(out=tz_.t[:, 0:n], in0=tz_.t[:, 0:n], in1=var.t[:, 0:n], op=ALU.mult), [tz_.r, var.r], [tz_.r])
                        y_out(2, cc, ci, 0, 128, lambda ap, r_: act(lambda: nc.scalar.activation(out=ap, in_=tz_.t[:, 0:n], func=AF.Silu, bias=vt("lnb", l, cc), scale=vt("lng", l, cc)),
                                                                   [tz_.r, C["VT"].r], [r_]))
                sy.barrier()
            tap(f"yB{l}", yT_d[2], [128, 4, T], BF16, yT_res[2])

        if "D" in branches:
            with contextlib.ExitStack() as sd:
                PW = 24 + L + 24 + LC + 24
                poff = (24, 24 + L + 24)
                Wd = mk(sd, "Wd", [128, 8, 512], BF16)
                Wp = mk(sd, "Wp", [128, 4, 128], BF16)
                bufs = [mk(sd, f"pb{i}", [128, PW], F32) for i in range(5)]
                yq = mk(sd, "yq", [128, T], BF16)
                edge = mk(sd, "edge", [128, 64], F32)
                etmp = mk(sd, "etmp", [128, 16], F32)
                wload(Wd.t[:], wv[:, :, 3328:3840], Wd)
                wload(Wp.t[:], dram["pool_w"][l].rearrange("g c e -> c g e"), Wp)
                for bf in bufs:
                    pool(lambda: nc.gpsimd.memset(bf.t[:], 0.0), [], [bf.r])
                bchunks = qchunks
                k = 0
                for g in range(4):
                    win = POOLW[g]
                    lo, hi = win // 2, win - 1 - win // 2
                    xb = bufs[0]
                    for ci in bchunks:
                        t0, n = CHUNKS[ci]
                        ps = PS[k % 2]
                        k += 1
                        proj(ps, Wd, g * 128, ci)
                        xo_ = poff[0] + t0 if ci < 4 else poff[1]
                        act(lambda: nc.scalar.copy(out=xb.t[:, xo_:xo_ + n], in_=ps.t[:, 0:n]), [ps.r], [xb.r])
                    nlev = g + 1
                    for lv in range(nlev):
                        w_ = 1 << lv
                        src, dst = bufs[lv], bufs[lv + 1]
                        pool(lambda: nc.gpsimd.tensor_tensor(out=dst.t[:, w_:PW], in0=src.t[:, w_:PW], in1=src.t[:, 0:PW - w_], op=ALU.add), [src.r], [dst.r])
                    ws = bufs[nlev]
                    segs = [(poff[0], L, 0)] + ([(poff[1], LC, L)] if ctx_out else [])
                    for (so, sl, yo) in segs:
                        dve(lambda: nc.vector.scalar_tensor_tensor(out=yq.t[:, yo:yo + sl], in0=ws.t[:, so + hi:so + hi + sl], scalar=1.0 / win, in1=xb.t[:, so:so + sl], op0=ALU.mult, op1=ALU.subtract),
                            [ws.r, xb.r], [yq.r])
                        ecols = [(t, t + hi + 1) for t in range(lo)] + [(sl - 1 - i, lo + 1 + i) for i in range(hi)]
                        for (t, cnt) in ecols:
                            dve(lambda: nc.vector.scalar_tensor_tensor(out=yq.t[:, yo + t:yo + t + 1], in0=ws.t[:, so + hi + t:so + hi + t + 1], scalar=1.0 / cnt, in1=xb.t[:, so + t:so + t + 1], op0=ALU.mult, op1=ALU.subtract),
                                [ws.r, xb.r], [yq.r])
                    for ci in bchunks:
                        t0, n = CHUNKS[ci]
                        ps = PS[2 + k % 2]
                        k += 1
                        pe(lambda: nc.tensor.matmul(ps.t[:, 0:n], Wp.t[:, g, :], yq.t[:, t0:t0 + n], start=True, stop=True), [Wp.r, yq.r], [ps.r])
                        y_out(3, g, ci, 0, 128, lambda ap, r_: act(lambda: nc.scalar.activation(out=ap, in_=ps.t[:, 0:n], func=AF.Copy, scale=vt("psc", l, g)), [ps.r, C["VT"].r], [r_]))
                sy.barrier()
            tap(f"yD{l}", yT_d[3], [128, 4, T], BF16, yT_res[3])

        if cfg.get("merge", True):
            with contextlib.ExitStack() as sm:
                accT = mk(sm, "accT", [128, 8, T], BF16)
                with contextlib.ExitStack() as sm1:
                    Wg = [mk(sm1, "Wg", [128, 4, 8, 128], BF16) for _ in range(2)]
                    Wbr = [mk(sm1, "Wbr", [128, 4, 4, 128], BF16) for _ in range(2)]
                    sgm = [mk(sm1, "sgm", [128, 512], F32) for _ in range(2)]
                    tm = [mk(sm1, "tm", [128, 512], F32) for _ in range(2)]
                    acc = mk(sm1, "acc", [128, 512], F32)
                    ych = [mk(sm1, "ych", [128, 4, 4, 512], BF16) for _ in range(2)]
                    wgv = dram["w_gate"][l].rearrange("n (kc p) e -> p n kc e", p=128)
                    wbv = dram["w_branch"][l].rearrange("n (kc p) e -> p n kc e", p=128)
                    border = (0, 2, 1, 3)
                    border = (0, 1, 2, 3)
                    k = 0
                    for ec in range(8):
                        wg_, wb_ = Wg[ec % 2], Wbr[ec % 2]
                        for n_ in range(4):
                            sy.dma("pq", wg_.t[:, n_, :, :], wgv[:, n_, :, ec * 128:(ec + 1) * 128], writes=[wg_.r])
                            sy.dma("pq", wb_.t[:, n_, :, :], wbv[:, n_, :, ec * 128:(ec + 1) * 128], writes=[wb_.r])
                        for ci in qchunks:
                            t0, n = CHUNKS[ci]
                            yc_ = ych[(ec * 5 + ci) % 2]
                            for i_ in range(4):
                                sy.dma("sp", yc_.t[:, i_, :, 0:n], yT_d[i_, :, :, t0:t0 + n], reads=[yT_res[i_][ci]], writes=[yc_.r])
                            for n_ in range(4):
                                pg, pb_ = PS[(2 * k) % 4], PS[(2 * k + 1) % 4]
                                s_ = sgm[k % 2]
                                t_ = tm[k % 2]
                                k += 1
                                for kc in range(8):
                                    pe(lambda: nc.tensor.matmul(pg.t[:, 0:n], wg_.t[:, n_, kc, :], hT.t[:, kc, t0:t0 + n], start=(kc == 0), stop=(kc == 7)), [wg_.r, hT.res[ci]], [pg.r], final=(kc == 7))
                                for kc in range(4):
                                    pe(lambda: nc.tensor.matmul(pb_.t[:, 0:n], wb_.t[:, n_, kc, :], yc_.t[:, n_, kc, 0:n], start=(kc == 0), stop=(kc == 3)), [wb_.r, yc_.r], [pb_.r], final=(kc == 3))
                                act(lambda: nc.scalar.activation(out=s_.t[:, 0:n], in_=pg.t[:, 0:n], func=AF.Sigmoid, bias=vt("bg", l, n_ * 8 + ec), scale=1.0), [pg.r, C["VT"].r], [s_.r])
                                if n_ == 0:
                                    dve(lambda: nc.vector.tensor_tensor(out=acc.t[:, 0:n], in0=pb_.t[:, 0:n], in1=s_.t[:, 0:n], op=ALU.mult), [pb_.r, s_.r], [acc.r])
                                else:
                                    dve(lambda: nc.vector.tensor_tensor(out=t_.t[:, 0:n], in0=pb_.t[:, 0:n], in1=s_.t[:, 0:n], op=ALU.mult), [pb_.r, s_.r], [t_.r])
                                    if n_ < 3:
                                        dve(lambda: nc.vector.tensor_tensor(out=acc.t[:, 0:n], in0=acc.t[:, 0:n], in1=t_.t[:, 0:n], op=ALU.add), [acc.r, t_.r], [acc.r])
                                    else:
                                        dve(lambda: nc.vector.tensor_tensor(out=accT.t[:, ec, t0:t0 + n], in0=acc.t[:, 0:n], in1=t_.t[:, 0:n], op=ALU.add), [acc.r, t_.r], [accT.r])
                    sy.barrier()
                tap(f"accT{l}", accT.t[:], [128, 8, T], BF16, [accT.r])
                Wo = mk(sm, "Wo", [128, 8, 8, 128], BF16)
                xt = [mk(sm, "xtm", [128, 8, 512], F32) for _ in range(2)]
                wov = dram["w_out"][l].rearrange("(kc p) (oc e) -> p oc kc e", p=128, e=128)
                for oc in range(8):
                    sy.dma("pq", Wo.t[:, oc, :, :], wov[:, oc, :, :], writes=[Wo.r])
                for ci in qchunks:
                    t0, n = CHUNKS[ci]
                    j = b if ci < 4 else 4
                    x_t = xt[ci % 2]
                    sy.dma("sp", x_t.t[:, :, 0:n], xT_d[b, :, :, t0:t0 + n], reads=[xT_res[b][ci]], writes=[x_t.r])
                    for oc in range(8):
                        ps = PS[4 + oc % 4]
                        for kc in range(8):
                            pe(lambda: nc.tensor.matmul(ps.t[:, 0:n], Wo.t[:, oc, kc, :], accT.t[:, kc, t0:t0 + n], start=(kc == 0), stop=(kc == 7)), [Wo.r, accT.r], [ps.r], final=(kc == 7))
                        dve(lambda: nc.vector.scalar_tensor_tensor(out=x_t.t[:, oc, 0:n], in0=ps.t[:, 0:n], scalar=MOD.t[:, l, 2, oc, j:j + 1], in1=x_t.t[:, oc, 0:n], op0=ALU.mult, op1=ALU.add),
                            [ps.r, x_t.r, MOD.r], [x_t.r])
                    sy.dma("sp", xT_d[b, :, :, t0:t0 + n], x_t.t[:, :, 0:n], reads=[x_t.r], writes=[xT_res[b][ci]])
                sy.barrier()
        tap(f"xmix{l}", xT_d[b], [128, 8, T], F32, xT_res[b])
        sy.barrier()


def _overlap_res(xT_res_b, c0, n):
    out = []
    for ci, (t0, nn) in enumerate(CHUNKS):
        if t0 < c0 + n and c0 < t0 + nn:
            out.append(xT_res_b[ci])
    return out


def peer_phase(state, consts, dram, cfg, b, l, norm_chunk, wload):
    nc, sy, PS, tap = state["nc"], state["sy"], state["PS"], state["tap"]
    C = consts
    vt, MOD, iof, ident, identb = C["vt"], C["MOD"], C["iof"], C["ident"], C["identb"]
    xT_d, xT_res = dram["xT_d"], dram["xT_res"]
    h2_d, rt_d = dram["h2_d"], dram["rt_d"]
    ctx_out = l < NL - 1
    chunks = list(range(5)) if ctx_out else list(range(4))
    h2_res, rt_res = Res(), Res()

    def mk(scope, name, shape, dt, nres=1):
        _uid[0] += 1
        return Tile(scope.enter_context(nc.sbuf_tensor(f"{name}_{_uid[0]}", list(shape), dt)), nres)

    def dve(fn, reads, writes):
        sy.op("dve", fn, reads=reads, writes=writes)

    def act(fn, reads, writes):
        sy.op("act", fn, reads=reads, writes=writes)

    def pool(fn, reads, writes):
        sy.op("pool", fn, reads=reads, writes=writes)

    def pe(fn, reads, writes, final=True):
        sy.op("pe", fn, reads=reads, writes=writes, final=final)

    with contextlib.ExitStack() as s1:
        Wq = mk(s1, "Wq", [128, 8, 2048], BF16)
        kst = mk(s1, "kst", [128, 16, 128], BF16)
        kT = mk(s1, "kT", [128, 16, 128], BF16)
        xt = [mk(s1, "xt", [128, 8, 512], F32) for _ in range(2)]
        sq = mk(s1, "sq", [128, 8, 512], F32)
        tmp = mk(s1, "tmp", [128, 8, 512], F32)
        rs = [mk(s1, "rs", [128, 512], F32) for _ in range(2)]
        h2c = [mk(s1, "h2c", [128, 8, 512], BF16) for _ in range(2)]
        qT = mk(s1, "qT", [128, 16, 512], BF16)
        sc = mk(s1, "sc", [128, 16, 128], F32)
        scw = mk(s1, "scw", [128, 16, 128], F32, nres=16)
        v16 = mk(s1, "v16", [128, 16, 16], F32, nres=16)
        i16 = mk(s1, "i16", [128, 16, 16], U32, nres=16)
        i16f = mk(s1, "i16f", [128, 16, 16], F32)
        cand = mk(s1, "cand", [128, 8, 16, 16], F32)
        candw = mk(s1, "candw", [128, 8, 256], F32, nres=8)
        c16 = mk(s1, "c16", [128, 8, 16], F32, nres=8)
        ci16 = mk(s1, "ci16", [128, 8, 16], U32, nres=8)
        iab = mk(s1, "iab", [128, 2, 8, 16], U32)
        fab = mk(s1, "fab", [128, 2, 8, 16], F32)
        oh = mk(s1, "oh", [128, 8, 16, 16], F32)
        sel = mk(s1, "sel", [128, 3, 128], F32)
        gs = mk(s1, "gs", [128, 8], F32)
        rtile = [mk(s1, "rtile", [128, 3, 128], F32) for _ in range(2)]
        wload(Wq.t[:], dram["peer_wq"][l].rearrange("(kc p) n -> p kc n", p=128), Wq)
        for half, kd in enumerate((dram["peer_k1"], dram["peer_k2"])):
            sy.dma("pq", kst.t[:].rearrange("n (h two) d -> n h two d", two=2)[:, :, half, :], kd[l].rearrange("h n d -> n h d"), writes=[kst.r])
        for blk in range(16):
            ps = PS[blk % 2]
            psb = ps.t[:].bitcast(BF16)
            pe(lambda: nc.tensor.transpose(out=psb[:, 0:128], in_=kst.t[:, blk, :], identity=identb.t[:]), [kst.r, identb.r], [ps.r])
            act(lambda: nc.scalar.copy(out=kT.t[:, blk, :], in_=psb[:, 0:128]), [ps.r], [kT.r])
        ntile = 0
        for ci in chunks:
            t0, n = CHUNKS[ci]
            x_t = xt[ci % 2]
            h2 = h2c[ci % 2]
            sy.dma("sp", x_t.t[:, :, 0:n], xT_d[b, :, :, t0:t0 + n], reads=[xT_res[b][ci]], writes=[x_t.r])
            norm_chunk(b, l, ci, 1, x_t, sq, (lambda c, h2=h2, n=n: h2.t[:, c, 0:n]), h2.r, PS[ci % 2], rs[ci % 2], tmp)
            sy.dma("sp", h2_d[:, :, t0:t0 + n], h2.t[:, :, 0:n], reads=[h2.r], writes=[h2_res])
            for blk in range(16):
                ps = PS[2 + blk % 2]
                for kc in range(8):
                    pe(lambda: nc.tensor.matmul(ps.t[:, 0:n], Wq.t[:, kc, blk * 128:(blk + 1) * 128], h2.t[:, kc, 0:n], start=(kc == 0), stop=(kc == 7)), [Wq.r, h2.r], [ps.r], final=(kc == 7))
                if blk % 2 == 0:
                    act(lambda: nc.scalar.copy(out=qT.t[:, blk, 0:n], in_=ps.t[:, 0:n]), [ps.r], [qT.r])
                else:
                    dve(lambda: nc.vector.tensor_copy(out=qT.t[:, blk, 0:n], in_=ps.t[:, 0:n]), [ps.r], [qT.r])
            for tt in range(n // 128):
                for q4 in range(4):
                    ps = PS[4 + q4]
                    for bi in range(4):
                        blk = q4 * 4 + bi
                        pe(lambda: nc.tensor.matmul(ps.t[:, bi * 128:(bi + 1) * 128], qT.t[:, blk, tt * 128:(tt + 1) * 128], kT.t[:, blk, :], start=True, stop=True), [qT.r, kT.r], [ps.r])
                    src = ps.t[:, :].rearrange("p (k n) -> p k n", n=128)
                    if q4 % 2 == 0:
                        act(lambda: nc.scalar.copy(out=sc.t[:, q4 * 4:(q4 + 1) * 4, :], in_=src), [ps.r], [sc.r])
                    else:
                        dve(lambda: nc.vector.tensor_copy(out=sc.t[:, q4 * 4:(q4 + 1) * 4, :], in_=src), [ps.r], [sc.r])
                R16 = range(16)
                for blk in R16:
                    dve(lambda: nc.vector.max(out=v16.t[:, blk, 0:8], in_=sc.t[:, blk, :]), [sc.r], [v16.res[blk]])
                for blk in R16:
                    dve(lambda: nc.vector.max_index(out=i16.t[:, blk, 0:8], in_max=v16.t[:, blk, 0:8], in_values=sc.t[:, blk, :]), [sc.r, v16.res[blk]], [i16.res[blk]])
                for blk in R16:
                    dve(lambda: nc.vector.match_replace(out=scw.t[:, blk, :], in_to_replace=v16.t[:, blk, 0:8], in_values=sc.t[:, blk, :], imm_value=-1e30), [sc.r, v16.res[blk]], [scw.res[blk]])
                for blk in R16:
                    dve(lambda: nc.vector.max(out=v16.t[:, blk, 8:16], in_=scw.t[:, blk, :]), [scw.res[blk]], [v16.res[blk]])
                for blk in R16:
                    dve(lambda: nc.vector.max_index(out=i16.t[:, blk, 8:16], in_max=v16.t[:, blk, 8:16], in_values=scw.t[:, blk, :]), [scw.res[blk], v16.res[blk]], [i16.res[blk]])
                dve(lambda: nc.vector.tensor_copy(out=i16f.t[:], in_=i16.t[:]), i16.res, [i16f.r])
                v16v = v16.t[:].rearrange("p (h two) k -> p h two k", two=2)
                i16v = i16f.t[:].rearrange("p (h two) k -> p h two k", two=2)
                dve(lambda: nc.vector.tensor_tensor(out=cand.t[:], in0=v16v[:, :, 0, :].unsqueeze(3).to_broadcast([128, 8, 16, 16]),
                                                    in1=v16v[:, :, 1, :].unsqueeze(2).to_broadcast([128, 8, 16, 16]), op=ALU.add), v16.res, [cand.r])
                candf = cand.t[:].rearrange("p h a b -> p h (a b)")
                R8 = range(8)
                for h in R8:
                    dve(lambda: nc.vector.max(out=c16.t[:, h, 0:8], in_=candf[:, h, :]), [cand.r], [c16.res[h]])
                for h in R8:
                    dve(lambda: nc.vector.max_index(out=ci16.t[:, h, 0:8], in_max=c16.t[:, h, 0:8], in_values=candf[:, h, :]), [cand.r, c16.res[h]], [ci16.res[h]])
                for h in R8:
                    dve(lambda: nc.vector.match_replace(out=candw.t[:, h, :], in_to_replace=c16.t[:, h, 0:8], in_values=candf[:, h, :], imm_value=-1e30), [cand.r, c16.res[h]], [candw.res[h]])
                for h in R8:
                    dve(lambda: nc.vector.max(out=c16.t[:, h, 8:16], in_=candw.t[:, h, :]), [candw.res[h]], [c16.res[h]])
                for h in R8:
                    dve(lambda: nc.vector.max_index(out=ci16.t[:, h, 8:16], in_max=c16.t[:, h, 8:16], in_values=candw.t[:, h, :]), [candw.res[h], c16.res[h]], [ci16.res[h]])
                dve(lambda: nc.vector.tensor_single_scalar(out=iab.t[:, 0, :, :], in_=ci16.t[:], scalar=4, op=ALU.arith_shift_right), ci16.res, [iab.r])
                dve(lambda: nc.vector.tensor_single_scalar(out=iab.t[:, 1, :, :], in_=ci16.t[:], scalar=15, op=ALU.bitwise_and), ci16.res, [iab.r])
                dve(lambda: nc.vector.tensor_copy(out=fab.t[:], in_=iab.t[:]), [iab.r], [fab.r])
                io16 = iof.t[:, 0:16].unsqueeze(1).unsqueeze(1).to_broadcast([128, 8, 16, 16])
                for w_ in range(2):
                    dve(lambda: nc.vector.tensor_tensor(out=oh.t[:], in0=io16, in1=fab.t[:, w_, :, :].unsqueeze(3).to_broadcast([128, 8, 16, 16]), op=ALU.is_equal), [fab.r, iof.r], [oh.r])
                    dve(lambda: nc.vector.tensor_tensor(out=oh.t[:], in0=oh.t[:], in1=i16v[:, :, w_, :].unsqueeze(2).to_broadcast([128, 8, 16, 16]), op=ALU.mult), [oh.r, i16f.r], [oh.r])
                    dve(lambda: nc.vector.tensor_reduce(out=sel.t[:, w_, :].rearrange("p (h k) -> p h k", k=16), in_=oh.t[:], axis=AX.X, op=ALU.add), [oh.r], [sel.r])
                g3 = sel.t[:, 2, :].rearrange("p (h k) -> p h k", k=16)
                dve(lambda: nc.vector.tensor_tensor(out=g3, in0=c16.t[:], in1=c16.t[:, :, 0:1].to_broadcast([128, 8, 16]), op=ALU.subtract), c16.res, [sel.r])
                act(lambda: nc.scalar.activation(out=g3, in_=g3, func=AF.Exp), [sel.r], [sel.r])
                dve(lambda: nc.vector.tensor_reduce(out=gs.t[:], in_=g3, axis=AX.X, op=ALU.add), [sel.r], [gs.r])
                dve(lambda: nc.vector.reciprocal(out=gs.t[:], in_=gs.t[:]), [gs.r], [gs.r])
                dve(lambda: nc.vector.tensor_tensor(out=g3, in0=g3, in1=gs.t[:].unsqueeze(2).to_broadcast([128, 8, 16]), op=ALU.mult), [sel.r, gs.r], [sel.r])
                ps = PS[ntile % 2]
                rt_ = rtile[ntile % 2]
                ntile += 1
                for w_ in range(3):
                    pe(lambda: nc.tensor.transpose(out=ps.t[:, w_ * 128:(w_ + 1) * 128], in_=sel.t[:, w_, :], identity=ident.t[:]), [sel.r, ident.r], [ps.r])
                act(lambda: nc.scalar.copy(out=rt_.t[:], in_=ps.t[:, 0:384].rearrange("p (w t) -> p w t", t=128)), [ps.r], [rt_.r])
                c0 = t0 + tt * 128
                sy.dma("sp", rt_d[:, :, c0:c0 + 128], rt_.t[:], reads=[rt_.r], writes=[rt_res])
        sy.barrier()
    tap(f"rt{l}", rt_d, [128, 3, T], F32, [rt_res])
    tap(f"h2T{l}", h2_d, [128, 8, T], BF16, [h2_res])
    if cfg.get("peer_pass1_only"):
        return

    UTs, Vs, tab = dram["UTs_d"][l], dram["Vs_d"][l], dram["tab_res"][l]
    if ctx_out:
        stiles = [(s_ * 384, 384) for s_ in range(6)]
    else:
        stiles = [(s_ * 384, 384) for s_ in range(5)] + [(1920, 128)]
    with contextlib.ExitStack() as s2:
        GW = mk(s2, "GW", [128, 128, 384], BF16)
        PH = mk(s2, "PH", [128, 32768], BF16)
        accO = mk(s2, "accO", [128, 8, 384], F32)
        h2s = mk(s2, "h2s", [128, 8, 384], BF16)
        xp = [mk(s2, "xp", [128, 384], F32) for _ in range(2)]
        UTc = [(PH.t[:, i * 4096:(i + 1) * 4096].rearrange("p (a k i) -> p a k i", a=4, k=8), Res()) for i in range(2)]
        Ab = [(PH.t[:, 8192 + i * 8192:8192 + i * 8192 + 4096].rearrange("p (i t) -> p i t", t=32), Res()) for i in range(3)]
        Bb = [(PH.t[:, 12288 + i * 8192:12288 + i * 8192 + 4096].rearrange("p (i t) -> p i t", t=32), Res()) for i in range(3)]
        Vc = [(PH.t[:, i * 16384:(i + 1) * 16384].rearrange("p (a d) -> p a d", d=1024), Res()) for i in range(2)]
        iotaT = Tile.__new__(Tile)
        iotaT.t = None
        iotaT_ap = PH.t[:, 0:4096].rearrange("p (i t) -> p i t", t=32)
        iotaT_r = UTc[0][1]
        RTb = mk(s2, "RTb", [128, 3, 384], BF16)
        tmpg = [mk(s2, "tmpg", [128, 384], BF16) for _ in range(2)]
        p2 = cfg.get("p2", "SWVR")
        wbanks = (PS[2], PS[3], PS[6], PS[7])
        for (c0, n) in stiles[:cfg.get("p2_ntiles", 99)]:
            sy.dma("sp", h2s.t[:, :, 0:n], h2_d[:, :, c0:c0 + n], reads=[h2_res], writes=[h2s.r])
            sy.dma("pq", RTb.t[:, :, 0:n], rt_d[:, :, c0:c0 + n], reads=[rt_res], writes=[RTb.r])
            dve(lambda: nc.vector.tensor_copy(out=iotaT_ap, in_=iof.t[:, :].unsqueeze(2).to_broadcast([128, 128, 32])), [iof.r], [iotaT_r])
            ngrp = n // 32 if "W" in p2 else 0
            wk = 0
            for gi in range(ngrp):
                (A_, ar), (B_, br) = Ab[gi % 3], Bb[gi % 3]
                tg = gi * 32
                dve(lambda: nc.vector.tensor_tensor(out=A_, in0=iotaT_ap, in1=RTb.t[:, 0, tg:tg + 32].unsqueeze(1).to_broadcast([128, 128, 32]), op=ALU.is_equal), [RTb.r, iotaT_r], [ar])
                dve(lambda: nc.vector.tensor_tensor(out=A_, in0=A_, in1=RTb.t[:, 2, tg:tg + 32].unsqueeze(1).to_broadcast([128, 128, 32]), op=ALU.mult), [RTb.r, ar], [ar])
                dve(lambda: nc.vector.tensor_tensor(out=B_, in0=iotaT_ap, in1=RTb.t[:, 1, tg:tg + 32].unsqueeze(1).to_broadcast([128, 128, 32]), op=ALU.is_equal), [RTb.r, iotaT_r], [br])
                for q_ in range(8):
                    ps = wbanks[wk % 4]
                    wk += 1
                    wdbg = cfg.get("wdbg", "")
                    for tl in range(4):
                        ti = q_ * 4 + tl
                        if "nope" not in wdbg:
                            pe(lambda: nc.tensor.matmul(ps.t[:, 0:512].rearrange("p (i t) -> p t i", t=4)[:, tl, :], A_[:, :, ti], B_[:, :, ti], start=True, stop=True), [ar, br], [ps.r])
                    ta = tg + q_ * 4
                    gwv = GW.t[:, :, ta:ta + 4]
                    if "noact" not in wdbg:
                        act(lambda: nc.scalar.copy(out=gwv, in_=ps.t[:, 0:512].rearrange("p (i t) -> p i t", t=4)), [ps.r], [GW.r])
            for g in (range(32) if "S" in p2 else ()):
                ut, ur = UTc[g % 2]
                sy.dma("sp", ut, UTs[:, g * 4:(g + 1) * 4, :, :], reads=[tab], writes=[ur])
                for a_ in range(4):
                    i2 = g * 4 + a_
                    ps = PS[i2 % 2]
                    tg_ = tmpg[i2 % 2]
                    for kc in range(8):
                        pe(lambda: nc.tensor.matmul(ps.t[:, 0:n], ut[:, a_, kc, :], h2s.t[:, kc, 0:n], start=(kc == 0), stop=(kc == 7)), [ur, h2s.r], [ps.r], final=(kc == 7))
                    act(lambda: nc.scalar.activation(out=tg_.t[:, 0:n], in_=ps.t[:, 0:n], func=AF.Gelu), [ps.r], [tg_.r])
                    dve(lambda: nc.vector.tensor_tensor(out=GW.t[:, i2, 0:n], in0=GW.t[:, i2, 0:n], in1=tg_.t[:, 0:n], op=ALU.mult), [GW.r, tg_.r], [GW.r])
            sy.barrier()
            for blk in (range(8) if "V" in p2 else ()):
                vt_, vr = Vc[blk % 2]
                for hv in range(2):
                    sy.dma("sp", vt_[:, hv * 8:(hv + 1) * 8, :], Vs[:, blk * 16 + hv * 8:blk * 16 + (hv + 1) * 8, :], reads=[tab], writes=[vr])
                for dc in range(8):
                    ps = PS[4 + dc % 2]
                    for ii in range(16):
                        pe(lambda: nc.tensor.matmul(ps.t[:, 0:n], vt_[:, ii, dc * 128:(dc + 1) * 128], GW.t[:, blk * 16 + ii, 0:n], start=(ii == 0), stop=(ii == 15)), [vr, GW.r], [ps.r], final=(ii == 15))
                    if blk == 0:
                        act(lambda: nc.scalar.copy(out=accO.t[:, dc, 0:n], in_=ps.t[:, 0:n]), [ps.r], [accO.r])
                    else:
                        dve(lambda: nc.vector.tensor_tensor(out=accO.t[:, dc, 0:n], in0=ps.t[:, 0:n], in1=accO.t[:, dc, 0:n], op=ALU.add), [ps.r, accO.r], [accO.r])
            xres = _overlap_res(xT_res[b], c0, n)
            if "R" in p2:
                segs = []
                if c0 < L:
                    segs.append((0, min(n, L - c0), b))
                if c0 + n > L:
                    segs.append((max(0, L - c0), n, 4))
                for (a0, a1, j) in segs:
                    dve(lambda: nc.vector.tensor_tensor(out=accO.t[:, :, a0:a1], in0=accO.t[:, :, a0:a1], in1=MOD.t[:, l, 5, :, j:j + 1].to_broadcast([128, 8, a1 - a0]), op=ALU.mult),
                        [accO.r, MOD.r], [accO.r])
                sy.dma("pq", xT_d[b, :, :, c0:c0 + n], accO.t[:, :, 0:n], reads=[accO.r], writes=xres, accum_op=ALU.add)
            sy.barrier()
    tap(f"x{l}", xT_d[b], [128, 8, T], F32, xT_res[b])


def final_phase(state, consts, dram, cfg, nb_run, norm_chunk):
    if cfg.get("skip_final"):
        return
    nc, sy, PS = state["nc"], state["sy"], state["PS"]
    ident = consts["ident"]
    xT_d, xT_res, out_d = dram["xT_d"], dram["xT_res"], dram["out_d"]
    with contextlib.ExitStack() as sf:
        def mk(name, shape, dt):
            _uid[0] += 1
            return Tile(sf.enter_context(nc.sbuf_tensor(f"{name}_{_uid[0]}", list(shape), dt)))
        xt = [mk("xt", [128, 8, 512], F32) for _ in range(2)]
        sq = mk("sq", [128, 8, 512], F32)
        tmp = [mk("tmp", [128, 8, 512], F32) for _ in range(2)]
        rs = [mk("rs", [128, 512], F32) for _ in range(2)]
        ot = [mk("ot", [128, 1024], F32) for _ in range(2)]
        k = 0
        for b in range(nb_run):
            for ci in range(4):
                t0, n = CHUNKS[ci]
                x_t, tm_ = xt[ci % 2], tmp[ci % 2]
                sy.dma("sp", x_t.t[:, :, 0:n], xT_d[b, :, :, t0:t0 + n], reads=[xT_res[b][ci]], writes=[x_t.r])
                norm_chunk(b, 0, ci, 2, x_t, sq, None, None, PS[ci % 2], rs[ci % 2], tm_)
                for tt in range(4):
                    o_ = ot[k % 2]
                    for half in range(2):
                        ps = PS[2 + (2 * k + half) % 4]
                        for c4 in range(4):
                            c = half * 4 + c4
                            sy.op("pe", lambda: nc.tensor.transpose(out=ps.t[:, c4 * 128:(c4 + 1) * 128], in_=tm_.t[:, c, tt * 128:(tt + 1) * 128], identity=ident.t[:]),
                                  reads=[tm_.r, ident.r], writes=[ps.r])
                        if half == 0:
                            sy.op("act", lambda: nc.scalar.copy(out=o_.t[:, 0:512], in_=ps.t[:, 0:512]), reads=[ps.r], writes=[o_.r])
                        else:
                            sy.op("dve", lambda: nc.vector.tensor_copy(out=o_.t[:, 512:1024], in_=ps.t[:, 0:512]), reads=[ps.r], writes=[o_.r])
                    k += 1
                    sy.dma("sp", out_d[b, t0 + tt * 128:t0 + (tt + 1) * 128, :], o_.t[:], reads=[o_.r], writes=[Res()])
        sy.barrier()


_W_NAMES = ["c_ctx", "w_mod", "b_mod", "norm1_g", "norm2_g", "w_in", "w_gate", "b_gate", "attn_sink", "lam_q1", "lam_k1", "lam_q2", "lam_k2",
            "diff_norm_g", "conv_w", "conv_b", "conv_ln_g", "conv_ln_b", "pool_w", "pool_scale", "w_branch", "w_out", "peer_wq", "peer_k1",
            "peer_k2", "peer_u", "peer_v", "final_g"]


def kernel(**inputs):
    n_cores = 8
    nc, _ = build_program({})
    shared = {k: np.ascontiguousarray(np.asarray(inputs[k], dtype=np.float32)) for k in _W_NAMES}
    in_maps = []
    for i in range(n_cores):
        m = dict(shared)
        for k in ("x", "c", "ctx"):
            m[k] = np.ascontiguousarray(np.asarray(inputs[k], dtype=np.float32)[i * NBC:(i + 1) * NBC])
        in_maps.append(m)
    res = run_bass_kernel_spmd(nc, in_maps, core_ids=list(range(n_cores)))
    return np.concatenate([np.asarray(r["out"], dtype=np.float32) for r in res.results], axis=0)
```

```python
import math
import contextlib
import numpy as np
import concourse.bass as bass
import concourse.mybir as mybir
from concourse.bass_utils import run_bass_kernel_spmd

F32 = mybir.dt.float32
BF16 = mybir.dt.bfloat16
I32 = mybir.dt.int32
U32 = mybir.dt.uint32
AF = mybir.ActivationFunctionType
ALU = mybir.AluOpType
AX = mybir.AxisListType

D = 1024
L = 2048
LC = 256
T = L + LC
NL = 4
NBC = 4
EPS = 1e-6
NE = 16384
CHUNKS = [(0, 512), (512, 512), (1024, 512), (1536, 512), (2048, 256)]
SEG = dict(a_q=0, a_k=512, a_v=640, c_q=768, c_k=1280, c_v=1792, b_in=2304, d_in=3328)
POOLW = (2, 4, 8, 16)


class Res:
    __slots__ = ("w", "r")

    def __init__(self):
        self.w = None
        self.r = {}


class SY:
    def __init__(self, nc, es):
        self.nc = nc
        self.E = dict(pe=nc.tensor, act=nc.scalar, dve=nc.vector, pool=nc.gpsimd, sp=nc.sync)
        self.csem = {e: es.enter_context(nc.semaphore("c_" + e)) for e in ("pe", "act", "dve", "pool")}
        self.ccnt = {e: 0 for e in self.csem}
        self.Q = dict(sp="sp", pq="pool", aq="act")
        self.R = dict(sp=8, pq=4, aq=2)
        self.dsem = {q: [es.enter_context(nc.semaphore(f"d_{q}{i}")) for i in range(self.R[q])] for q in self.Q}
        self.dval = {q: [0] * self.R[q] for q in self.Q}
        self.dn = {q: 0 for q in self.Q}
        self.seen = {e: {} for e in self.E}
        self.ninst = 0

    def _wait(self, eng, t):
        sem, val = t[0], t[1]
        k = id(sem)
        if self.seen[eng].get(k, 0) >= val:
            return
        self.seen[eng][k] = val
        self.E[eng].wait_ge(sem, val)

    def _deps(self, eng, is_dma, reads, writes):
        for r in reads:
            if r.w is not None:
                self._wait(eng, r.w)
        for w in writes:
            if w.w is not None and (is_dma or w.w[3] or w.w[2] != eng):
                self._wait(eng, w.w)
            for t in w.r.values():
                if is_dma or t[3] or t[2] != eng:
                    self._wait(eng, t)

    def _commit(self, t, reads, writes):
        for r in reads:
            r.r[id(t[0])] = t
        for w in writes:
            w.w = t
            w.r = {}

    def op(self, eng, fn, reads=(), writes=(), final=True):
        self._deps(eng, False, reads, writes)
        ins = fn()
        if final:
            self.ccnt[eng] += 1
            ins.then_inc(self.csem[eng], 1)
            t = (self.csem[eng], self.ccnt[eng], eng, False)
        else:
            t = (self.csem[eng], self.ccnt[eng] + 1, eng, False)
        self._commit(t, reads, writes)
        self.ninst += 1

    def dma(self, q, out, in_, reads=(), writes=(), **kw):
        eng = self.Q[q]
        self._deps(eng, True, reads, writes)
        i = self.dn[q] % self.R[q]
        self.dn[q] += 1
        sem = self.dsem[q][i]
        if self.dval[q][i] > 0:
            self._wait(eng, (sem, self.dval[q][i]))
        self.dval[q][i] += 16
        self.E[eng].dma_start(out=out, in_=in_, **kw).then_inc(sem, 16)
        self._commit((sem, self.dval[q][i], eng, True), reads, writes)
        self.ninst += 1

    def all_tickets(self):
        ts = [(self.csem[e], self.ccnt[e]) for e in self.csem if self.ccnt[e] > 0]
        for q in self.Q:
            for i in range(self.R[q]):
                if self.dval[q][i] > 0:
                    ts.append((self.dsem[q][i], self.dval[q][i]))
        return ts

    def barrier(self, engs=("pe", "act", "dve", "pool", "sp")):
        ts = self.all_tickets()
        for e in engs:
            for t in ts:
                self._wait(e, t)


class Tile:
    def __init__(self, t, nres=1):
        self.t = t
        self.res = [Res() for _ in range(nres)]

    @property
    def r(self):
        return self.res[0]


def build_program(cfg):
    nb_run = cfg.get("nb", NBC)
    nl_run = cfg.get("nl", NL)
    taps = cfg.get("taps", ())
    do_peer = cfg.get("peer", True)

    NLA = cfg.get("nl_alloc", NL)
    nc = bass.Bass("TRN2", target_bir_lowering=False)
    es = contextlib.ExitStack()
    es.__enter__()
    sy = SY(nc, es)

    def din(name, shape):
        return nc.dram_tensor(name, list(shape), F32, kind="ExternalInput").ap()

    x_in = din("x", [NBC, L, D])
    c_in = din("c", [NBC, D])
    ctx_in = din("ctx", [NBC, LC, D])
    c_ctx = din("c_ctx", [D])
    w_mod = din("w_mod", [NLA, D, 6 * D])
    b_mod = din("b_mod", [NLA, 6 * D])
    norm1_g = din("norm1_g", [NLA, D])
    norm2_g = din("norm2_g", [NLA, D])
    w_in = din("w_in", [NLA, D, 3840])
    w_gate = din("w_gate", [NLA, 4, D, D])
    b_gate = din("b_gate", [NLA, 4, D])
    attn_sink = din("attn_sink", [NLA, 8])
    lam_q1 = din("lam_q1", [NLA, 64])
    lam_k1 = din("lam_k1", [NLA, 64])
    lam_q2 = din("lam_q2", [NLA, 64])
    lam_k2 = din("lam_k2", [NLA, 64])
    diff_norm_g = din("diff_norm_g", [NLA, 128])
    conv_w = din("conv_w", [NLA, 31, 512])
    conv_b = din("conv_b", [NLA, 512])
    conv_ln_g = din("conv_ln_g", [NLA, 512])
    conv_ln_b = din("conv_ln_b", [NLA, 512])
    pool_w = din("pool_w", [NLA, 4, 128, 128])
    pool_scale = din("pool_scale", [NLA, 512])
    w_branch = din("w_branch", [NLA, 4, 512, D])
    w_out = din("w_out", [NLA, D, D])
    peer_wq = din("peer_wq", [NLA, D, 2048])
    peer_k1 = din("peer_k1", [NLA, 8, 128, 128])
    peer_k2 = din("peer_k2", [NLA, 8, 128, 128])
    peer_u = din("peer_u", [NLA, NE, D])
    peer_v = din("peer_v", [NLA, NE, D])
    final_g = din("final_g", [D])
    out_d = nc.dram_tensor("out", [NBC, L, D], F32, kind="ExternalOutput").ap()

    xT_d = nc.dram_tensor("xT_scr", [NBC, 128, 8, T], F32, kind="Internal").ap()
    xT_res = [[Res() for _ in CHUNKS] for _ in range(NBC)]
    yT_d = nc.dram_tensor("yT_scr", [4, 128, 4, T], BF16, kind="Internal").ap()
    yT_res = [[Res() for _ in CHUNKS] for _ in range(4)]
    tap_out = {}

    def sb(name, shape, dt, nres=1):
        return Tile(es.enter_context(nc.sbuf_tensor(name, list(shape), dt)), nres)

    def tap(name, ap, shape, dt, reads):
        if name not in taps:
            return
        d = nc.dram_tensor("tap_" + name, list(shape), dt, kind="ExternalOutput").ap()
        tap_out[name] = d
        r = Res()
        sy.dma("sp", d, ap, reads=reads, writes=[r])
        tap_res.append(r)

    tap_res = []

    PS = [Tile(es.enter_context(nc.psum_tensor(f"ps{i}", [128, 512], F32))) for i in range(8)]

    ident = sb("ident", [128, 128], F32)
    identb = sb("identb", [128, 128], BF16)
    maskGE = sb("maskGE", [128, 128], BF16)
    maskLE = sb("maskLE", [128, 128], BF16)
    onesD = sb("onesD", [128, 128], F32)
    ones5 = sb("ones5", [128, 128], F32)
    ones1 = sb("ones1", [128, 128], F32)
    onesb = sb("onesb", [128, 128], BF16)
    iof = sb("iof", [128, 128], F32)
    itmp = sb("itmp", [128, 128], I32)
    sy.op("pool", lambda: nc.gpsimd.iota(itmp.t[:], pattern=[[1, 128]], base=0, channel_multiplier=-1), writes=[itmp.r])
    sy.op("dve", lambda: nc.vector.tensor_single_scalar(out=ident.t[:], in_=itmp.t[:], scalar=0, op=ALU.is_equal), reads=[itmp.r], writes=[ident.r])
    sy.op("dve", lambda: nc.vector.tensor_single_scalar(out=identb.t[:], in_=itmp.t[:], scalar=0, op=ALU.is_equal), reads=[itmp.r], writes=[identb.r])
    sy.op("dve", lambda: nc.vector.tensor_single_scalar(out=maskGE.t[:], in_=itmp.t[:], scalar=0, op=ALU.is_le), reads=[itmp.r], writes=[maskGE.r])
    sy.op("dve", lambda: nc.vector.tensor_single_scalar(out=maskLE.t[:], in_=itmp.t[:], scalar=0, op=ALU.is_ge), reads=[itmp.r], writes=[maskLE.r])
    itmp2 = sb("itmp2", [128, 128], I32)
    sy.op("pool", lambda: nc.gpsimd.iota(itmp2.t[:], pattern=[[1, 128]], base=0, channel_multiplier=0), writes=[itmp2.r])
    sy.op("dve", lambda: nc.vector.tensor_copy(out=iof.t[:], in_=itmp2.t[:]), reads=[itmp2.r], writes=[iof.r])
    sy.op("pool", lambda: nc.gpsimd.memset(onesD.t[:], 1.0 / 1024), writes=[onesD.r])
    sy.op("pool", lambda: nc.gpsimd.memset(ones5.t[:], 1.0 / 512), writes=[ones5.r])
    sy.op("pool", lambda: nc.gpsimd.memset(ones1.t[:], 1.0 / 128), writes=[ones1.r])
    sy.op("pool", lambda: nc.gpsimd.memset(onesb.t[:], 1.0), writes=[onesb.r])

    NVT = NL * 240 + 48
    VT = sb("VT", [128, NVT], F32)
    vt_off = {}

    def vt(name, l=0, i=0):
        return VT.t[:, vt_off[(name, l)] + i: vt_off[(name, l)] + i + 1]

    def vtr(name, l, i0, n):
        return VT.t[:, vt_off[(name, l)] + i0: vt_off[(name, l)] + i0 + n]

    stg = [sb(f"stg{i}", [128, 128], F32) for i in range(2)]
    col = 0
    nstage = 0

    def stage_transpose(items):
        nonlocal col, nstage
        st = stg[nstage % 2]
        ps = PS[nstage % 2]
        nstage += 1
        row = 0
        for (name, l, ap) in items:
            R_ = ap.shape[0]
            sy.dma("sp", st.t[row:row + R_, :], ap, writes=[st.r])
            vt_off[(name, l)] = col + row
            row += R_
        sy.op("pe", lambda: nc.tensor.transpose(out=ps.t[:, 0:row], in_=st.t[0:row, :], identity=ident.t[0:row, 0:row]),
              reads=[st.r, ident.r], writes=[ps.r])
        c0 = col
        sy.op("act", lambda: nc.scalar.copy(out=VT.t[:, c0:c0 + row], in_=ps.t[:, 0:row]), reads=[ps.r], writes=[VT.r])
        col += row

    def v2(ap1d):
        return ap1d.rearrange("(r p) -> r p", p=128)

    for l in range(NLA):
        stage_transpose([
            ("n1g", l, v2(norm1_g[l])), ("n2g", l, v2(norm2_g[l])),
            ("bg", l, b_gate[l].rearrange("n (r p) -> (n r) p", p=128)),
            ("cb", l, v2(conv_b[l])), ("lng", l, v2(conv_ln_g[l])), ("lnb", l, v2(conv_ln_b[l])),
            ("psc", l, v2(pool_scale[l])), ("dg", l, v2(diff_norm_g[l])), ("bmod", l, v2(b_mod[l])),
        ])
        stage_transpose([("cw", l, conv_w[l].rearrange("k (r p) -> (k r) p", p=128))])
    stage_transpose([("fg", 0, v2(final_g)), ("cctx", 0, v2(c_ctx)), ("cb4", 0, c_in.rearrange("b (r p) -> (b r) p", p=128))])
    assert col <= NVT, col

    scT = sb("scT", [128, 8, 5], F32)
    sy.op("act", lambda: nc.scalar.activation(out=scT.t[:, :, 0:4].rearrange("p k j -> p j k"), in_=vtr("cb4", 0, 0, 32).rearrange("p (j k) -> p j k", k=8), func=AF.Silu),
          reads=[VT.r], writes=[scT.r])
    sy.op("act", lambda: nc.scalar.activation(out=scT.t[:, :, 4], in_=vtr("cctx", 0, 0, 8), func=AF.Silu), reads=[VT.r], writes=[scT.r])

    MOD = sb("MOD", [128, NL, 6, 8, 5], F32)
    AM = sb("AM", [128, NL, 2, 8, 5], F32)
    with contextlib.ExitStack() as es0:
        wm = [Tile(es0.enter_context(nc.sbuf_tensor(f"wm{i}", [128, 8, 1024], F32))) for i in range(2)]
        k = 0
        for l in range(nl_run):
            for v in range(6):
                w = wm[k % 2]
                ps = PS[2 + k % 2]
                k += 1
                sy.dma("sp", w.t[:], w_mod[l].rearrange("(kc p) n -> p kc n", p=128)[:, :, v * 1024:(v + 1) * 1024], writes=[w.r])
                for oc in range(8):
                    for kc in range(8):
                        sy.op("pe", lambda: nc.tensor.matmul(ps.t[:, oc * 5:(oc + 1) * 5], w.t[:, kc, oc * 128:(oc + 1) * 128], scT.t[:, kc, :], start=(kc == 0), stop=(kc == 7)),
                              reads=[w.r, scT.r], writes=[ps.r])
                sy.op("dve", lambda: nc.vector.tensor_tensor(out=MOD.t[:, l, v, :, :], in0=ps.t[:, 0:40].rearrange("p (o j) -> p o j", j=5),
                                                             in1=vtr("bmod", l, v * 8, 8).unsqueeze(2).to_broadcast([128, 8, 5]), op=ALU.add),
                      reads=[ps.r, VT.r], writes=[MOD.r])
            for wi, (v, gname) in enumerate(((1, "n1g"), (4, "n2g"))):
                sy.op("dve", lambda: nc.vector.tensor_scalar(out=AM.t[:, l, wi, :, :], in0=MOD.t[:, l, v, :, :], scalar1=1.0, scalar2=None, op0=ALU.add),
                      reads=[MOD.r], writes=[AM.r])
                sy.op("dve", lambda: nc.vector.tensor_tensor(out=AM.t[:, l, wi, :, :], in0=AM.t[:, l, wi, :, :],
                                                             in1=vtr(gname, l, 0, 8).unsqueeze(2).to_broadcast([128, 8, 5]), op=ALU.mult),
                      reads=[AM.r, VT.r], writes=[AM.r])
        sy.barrier()
    tap("MOD", MOD.t[:], [128, NL, 6, 8, 5], F32, [MOD.r])
    tap("AM", AM.t[:], [128, NL, 2, 8, 5], F32, [AM.r])

    with contextlib.ExitStack() as es0:
        xin = [Tile(es0.enter_context(nc.sbuf_tensor(f"xin{i}", [128, D], F32))) for i in range(2)]
        xo = [Tile(es0.enter_context(nc.sbuf_tensor(f"xo{i}", [128, 8, 512], F32))) for i in range(2)]
        k = 0
        for b in range(nb_run):
            for ci, (t0, n) in enumerate(CHUNKS):
                o = xo[ci % 2]
                for tt in range(n // 128):
                    xi = xin[k % 2]
                    src = x_in[b, t0 + tt * 128:t0 + (tt + 1) * 128, :] if t0 < L else ctx_in[b, tt * 128:(tt + 1) * 128, :]
                    sy.dma("sp", xi.t[:], src, writes=[xi.r])
                    for half in range(2):
                        ps = PS[(2 * k + half) % 4]
                        for c4 in range(4):
                            c = half * 4 + c4
                            sy.op("pe", lambda: nc.tensor.transpose(out=ps.t[:, c4 * 128:(c4 + 1) * 128], in_=xi.t[:, c * 128:(c + 1) * 128], identity=ident.t[:]),
                                  reads=[xi.r, ident.r], writes=[ps.r])
                        eng = "act" if half == 0 else "dve"
                        dst = o.t[:, half * 4:(half + 1) * 4, tt * 128:(tt + 1) * 128]
                        srcp = ps.t[:, :].rearrange("p (c t) -> p c t", t=128)
                        if eng == "act":
                            sy.op("act", lambda: nc.scalar.copy(out=dst, in_=srcp), reads=[ps.r], writes=[o.r])
                        else:
                            sy.op("dve", lambda: nc.vector.tensor_copy(out=dst, in_=srcp), reads=[ps.r], writes=[o.r])
                    k += 1
                sy.dma("sp", xT_d[b, :, :, t0:t0 + n], o.t[:, :, 0:n], reads=[o.r], writes=[xT_res[b][ci]])
        sy.barrier()

    cs_d = nc.dram_tensor("cs_scr", [2, 128, T], F32, kind="Internal").ap()
    cs_res = Res()
    with contextlib.ExitStack() as es0:
        cosF = Tile(es0.enter_context(nc.sbuf_tensor("cosF", [128, T], F32)))
        sinS = Tile(es0.enter_context(nc.sbuf_tensor("sinS", [128, T], F32)))
        pidx = Tile(es0.enter_context(nc.sbuf_tensor("pidx", [128, 1], I32)))
        pf = Tile(es0.enter_context(nc.sbuf_tensor("pf", [128, 8], F32)))
        rowt = Tile(es0.enter_context(nc.sbuf_tensor("rowt", [128, L], I32)))
        colt = Tile(es0.enter_context(nc.sbuf_tensor("colt", [128, L], I32)))
        rowf = Tile(es0.enter_context(nc.sbuf_tensor("rowf", [128, L], F32)))
        colf = Tile(es0.enter_context(nc.sbuf_tensor("colf", [128, L], F32)))
        sy.op("pool", lambda: nc.gpsimd.iota(pidx.t[:], pattern=[[0, 1]], base=0, channel_multiplier=1), writes=[pidx.r])
        sy.op("pool", lambda: nc.gpsimd.iota(rowt.t[:], pattern=[[1, 32], [0, 64]], base=0, channel_multiplier=0), writes=[rowt.r])
        sy.op("pool", lambda: nc.gpsimd.iota(colt.t[:], pattern=[[0, 32], [1, 64]], base=0, channel_multiplier=0), writes=[colt.r])
        sy.op("dve", lambda: nc.vector.tensor_copy(out=rowf.t[:], in_=rowt.t[:]), reads=[rowt.r], writes=[rowf.r])
        sy.op("dve", lambda: nc.vector.tensor_copy(out=colf.t[:], in_=colt.t[:]), reads=[colt.r], writes=[colf.r])
        sy.op("dve", lambda: nc.vector.tensor_copy(out=pf.t[:, 0:1], in_=pidx.t[:]), reads=[pidx.r], writes=[pf.r])
        def dv(fn, reads, writes):
            sy.op("dve", fn, reads=reads, writes=writes)
        dv(lambda: nc.vector.tensor_single_scalar(out=pf.t[:, 7:8], in_=pf.t[:, 0:1], scalar=32.0, op=ALU.is_ge), [pf.r], [pf.r])
        dv(lambda: nc.vector.tensor_single_scalar(out=pf.t[:, 6:7], in_=pf.t[:, 0:1], scalar=64.0, op=ALU.is_ge), [pf.r], [pf.r])
        dv(lambda: nc.vector.tensor_tensor(out=pf.t[:, 7:8], in0=pf.t[:, 7:8], in1=pf.t[:, 6:7], op=ALU.add), [pf.r], [pf.r])
        dv(lambda: nc.vector.tensor_single_scalar(out=pf.t[:, 1:2], in_=pf.t[:, 0:1], scalar=96.0, op=ALU.is_ge), [pf.r], [pf.r])
        dv(lambda: nc.vector.tensor_tensor(out=pf.t[:, 7:8], in0=pf.t[:, 7:8], in1=pf.t[:, 1:2], op=ALU.add), [pf.r], [pf.r])
        dv(lambda: nc.vector.scalar_tensor_tensor(out=pf.t[:, 1:2], in0=pf.t[:, 7:8], scalar=-32.0, in1=pf.t[:, 0:1], op0=ALU.mult, op1=ALU.add), [pf.r], [pf.r])
        dv(lambda: nc.vector.tensor_single_scalar(out=pf.t[:, 3:4], in_=pf.t[:, 1:2], scalar=16.0, op=ALU.is_lt), [pf.r], [pf.r])
        dv(lambda: nc.vector.tensor_single_scalar(out=pf.t[:, 7:8], in_=pf.t[:, 1:2], scalar=16.0, op=ALU.is_ge), [pf.r], [pf.r])
        dv(lambda: nc.vector.scalar_tensor_tensor(out=pf.t[:, 2:3], in0=pf.t[:, 7:8], scalar=-16.0, in1=pf.t[:, 1:2], op0=ALU.mult, op1=ALU.add), [pf.r], [pf.r])
        sy.op("act", lambda: nc.scalar.activation(out=pf.t[:, 4:5], in_=pf.t[:, 2:3], func=AF.Exp, scale=-math.log(10000.0) / 16.0), reads=[pf.r], writes=[pf.r])
        dv(lambda: nc.vector.tensor_single_scalar(out=pf.t[:, 5:6], in_=pf.t[:, 0:1], scalar=32.0, op=ALU.is_ge), [pf.r], [pf.r])
        dv(lambda: nc.vector.tensor_single_scalar(out=pf.t[:, 7:8], in_=pf.t[:, 0:1], scalar=64.0, op=ALU.is_ge), [pf.r], [pf.r])
        dv(lambda: nc.vector.tensor_tensor(out=pf.t[:, 5:6], in0=pf.t[:, 5:6], in1=pf.t[:, 7:8], op=ALU.subtract), [pf.r], [pf.r])
        dv(lambda: nc.vector.tensor_single_scalar(out=pf.t[:, 7:8], in_=pf.t[:, 0:1], scalar=96.0, op=ALU.is_ge), [pf.r], [pf.r])
        dv(lambda: nc.vector.tensor_tensor(out=pf.t[:, 5:6], in0=pf.t[:, 5:6], in1=pf.t[:, 7:8], op=ALU.add), [pf.r], [pf.r])
        dv(lambda: nc.vector.tensor_scalar(out=pf.t[:, 5:6], in0=pf.t[:, 5:6], scalar1=2.0, scalar2=-1.0, op0=ALU.mult, op1=ALU.add), [pf.r], [pf.r])
        dv(lambda: nc.vector.tensor_tensor(out=rowf.t[:], in0=rowf.t[:], in1=colf.t[:], op=ALU.subtract), [rowf.r, colf.r], [rowf.r])
        dv(lambda: nc.vector.scalar_tensor_tensor(out=colf.t[:], in0=rowf.t[:], scalar=pf.t[:, 3:4], in1=colf.t[:], op0=ALU.mult, op1=ALU.add), [rowf.r, colf.r, pf.r], [colf.r])
        dv(lambda: nc.vector.tensor_scalar(out=colf.t[:], in0=colf.t[:], scalar1=pf.t[:, 4:5], scalar2=None, op0=ALU.mult), [colf.r, pf.r], [colf.r])

        def reduce_sin(dst, shift):
            dv(lambda: nc.vector.tensor_scalar(out=rowf.t[:], in0=colf.t[:], scalar1=shift, scalar2=1.0 / (2 * math.pi), op0=ALU.add, op1=ALU.mult), [colf.r], [rowf.r])
            dv(lambda: nc.vector.tensor_copy(out=rowt.t[:], in_=rowf.t[:]), [rowf.r], [rowt.r])
            dv(lambda: nc.vector.tensor_copy(out=rowf.t[:], in_=rowt.t[:]), [rowt.r], [rowf.r])
            dv(lambda: nc.vector.scalar_tensor_tensor(out=rowf.t[:], in0=rowf.t[:], scalar=-2 * math.pi, in1=colf.t[:], op0=ALU.mult, op1=ALU.add), [rowf.r, colf.r], [rowf.r])
            dv(lambda: nc.vector.tensor_single_scalar(out=rowf.t[:], in_=rowf.t[:], scalar=shift, op=ALU.add), [rowf.r], [rowf.r])
            dv(lambda: nc.vector.tensor_single_scalar(out=colt.t[:].bitcast(F32), in_=rowf.t[:], scalar=math.pi, op=ALU.is_gt), [rowf.r], [colt.r])
            dv(lambda: nc.vector.scalar_tensor_tensor(out=rowf.t[:], in0=colt.t[:].bitcast(F32), scalar=-2 * math.pi, in1=rowf.t[:], op0=ALU.mult, op1=ALU.add), [rowf.r, colt.r], [rowf.r])
            dv(lambda: nc.vector.tensor_scalar(out=rowf.t[:], in0=rowf.t[:], scalar1=math.pi, scalar2=-math.pi, op0=ALU.min, op1=ALU.max), [rowf.r], [rowf.r])
            sy.op("act", lambda: nc.scalar.activation(out=dst, in_=rowf.t[:], func=AF.Sin), reads=[rowf.r], writes=[sinS.r, cosF.r])

        reduce_sin(sinS.t[:, 0:L], 0.0)
        dv(lambda: nc.vector.tensor_scalar(out=sinS.t[:, 0:L], in0=sinS.t[:, 0:L], scalar1=pf.t[:, 5:6], scalar2=None, op0=ALU.mult), [sinS.r, pf.r], [sinS.r])
        reduce_sin(cosF.t[:, 0:L], 0.5 * math.pi)
        sy.op("pool", lambda: nc.gpsimd.memset(cosF.t[:, L:T], 1.0), writes=[cosF.r])
        sy.op("pool", lambda: nc.gpsimd.memset(sinS.t[:, L:T], 0.0), writes=[sinS.r])
        sy.dma("sp", cs_d[0], cosF.t[:], reads=[cosF.r], writes=[cs_res])
        sy.dma("sp", cs_d[1], sinS.t[:], reads=[sinS.r], writes=[cs_res])
        tap("cosF", cosF.t[:], [128, T], F32, [cosF.r])
        tap("sinS", sinS.t[:], [128, T], F32, [sinS.r])
        sy.barrier()

    skc = sb("skc", [1, NLA * 8], F32)
    sinkl = sb("sinkl", [128, 2, 128], BF16)
    nlam = sb("nlam", [128, NLA], F32)
    gsc = sb("gsc", [128, NLA], F32)
    with contextlib.ExitStack() as es0:
        sk = Tile(es0.enter_context(nc.sbuf_tensor("sk", [1, NLA * 8], F32)))
        lq = Tile(es0.enter_context(nc.sbuf_tensor("lq", [128, 4, NLA, 64], F32)))
        ls = Tile(es0.enter_context(nc.sbuf_tensor("ls", [128, 2, NLA], F32)))
        sy.dma("sp", sk.t[:], attn_sink.rearrange("l h -> (l h)").unsqueeze(0), writes=[sk.r])
        sy.op("act", lambda: nc.scalar.activation(out=skc.t[:], in_=sk.t[:], func=AF.Exp), reads=[sk.r], writes=[skc.r])
        sy.op("pool", lambda: nc.gpsimd.memset(sinkl.t[:], 0.0), writes=[sinkl.r])
        sy.op("pool", lambda: nc.gpsimd.memset(sinkl.t[0:1, 0, 64:128], 1.0), writes=[sinkl.r])
        sy.op("pool", lambda: nc.gpsimd.memset(sinkl.t[0:1, 1, 0:64], 1.0), writes=[sinkl.r])
        for i, a in enumerate((lam_q1, lam_k1, lam_q2, lam_k2)):
            sy.dma("sp", lq.t[:, i, :, :], a.unsqueeze(0).to_broadcast([128, NLA, 64]), writes=[lq.r])
        for i in range(2):
            sy.op("dve", lambda: nc.vector.tensor_tensor(out=lq.t[:, 2 * i, :, :], in0=lq.t[:, 2 * i, :, :], in1=lq.t[:, 2 * i + 1, :, :], op=ALU.mult), reads=[lq.r], writes=[lq.r])
            sy.op("dve", lambda: nc.vector.tensor_reduce(out=ls.t[:, i, :], in_=lq.t[:, 2 * i, :, :], axis=AX.X, op=ALU.add), reads=[lq.r], writes=[ls.r])
        sy.op("act", lambda: nc.scalar.activation(out=ls.t[:], in_=ls.t[:], func=AF.Exp), reads=[ls.r], writes=[ls.r])
        sy.op("dve", lambda: nc.vector.tensor_tensor(out=nlam.t[:], in0=ls.t[:, 1, :], in1=ls.t[:, 0, :], op=ALU.subtract), reads=[ls.r], writes=[nlam.r])
        for l in range(NLA):
            li = 0.8 - 0.6 * math.exp(-0.3 * l)
            sy.op("dve", lambda: nc.vector.tensor_single_scalar(out=nlam.t[:, l:l + 1], in_=nlam.t[:, l:l + 1], scalar=-li, op=ALU.add), reads=[nlam.r], writes=[nlam.r])
            sy.op("dve", lambda: nc.vector.tensor_single_scalar(out=gsc.t[:, l:l + 1], in_=vt("dg", l), scalar=1.0 - li, op=ALU.mult), reads=[VT.r], writes=[gsc.r])
        sy.barrier()
    tap("nlam", nlam.t[:], [128, NLA], F32, [nlam.r])

    UTs_d = nc.dram_tensor("UTs_scr", [NLA, 128, 128, 8, 128], BF16, kind="Internal").ap()
    Vs_d = nc.dram_tensor("Vs_scr", [NLA, 128, 128, 1024], BF16, kind="Internal").ap()
    h2_d = nc.dram_tensor("h2_scr", [128, 8, T], BF16, kind="Internal").ap()
    rt_d = nc.dram_tensor("rt_scr", [128, 3, T], F32, kind="Internal").ap()
    tab_res = [Res() for _ in range(NLA)]
    if do_peer:
        with contextlib.ExitStack() as es0:
            ub = [Tile(es0.enter_context(nc.sbuf_tensor(f"ub{i}", [128, 1024], BF16))) for i in range(3)]
            uo = [Tile(es0.enter_context(nc.sbuf_tensor(f"uo{i}", [128, 8, 128], BF16))) for i in range(3)]
            vb = [Tile(es0.enter_context(nc.sbuf_tensor(f"vb{i}", [128, 8, 1024], BF16))) for i in range(2)]
            for l in range(nl_run):
                uv = peer_u[l].rearrange("(i1 i2) d -> i2 i1 d", i2=128)
                vv = peer_v[l].rearrange("(i1 i2) d -> i1 i2 d", i2=128)
                for i2 in range(128):
                    u_, o_ = ub[i2 % 3], uo[i2 % 3]
                    ps = PS[i2 % 4]
                    psb = ps.t[:].bitcast(BF16)
                    sy.dma("pq", u_.t[:], uv[i2], writes=[u_.r])
                    for kc in range(8):
                        sy.op("pe", lambda: nc.tensor.transpose(out=psb[:, kc * 128:(kc + 1) * 128], in_=u_.t[:, kc * 128:(kc + 1) * 128], identity=identb.t[:]),
                              reads=[u_.r, identb.r], writes=[ps.r])
                    if i2 % 2 == 0:
                        sy.op("act", lambda: nc.scalar.copy(out=o_.t[:].rearrange("p k i -> p (k i)"), in_=psb[:, 0:1024]), reads=[ps.r], writes=[o_.r])
                    else:
                        sy.op("dve", lambda: nc.vector.tensor_copy(out=o_.t[:].rearrange("p k i -> p (k i)"), in_=psb[:, 0:1024]), reads=[ps.r], writes=[o_.r])
                    sy.dma("sp", UTs_d[l, :, i2, :, :], o_.t[:], reads=[o_.r], writes=[tab_res[l]])
                    if i2 % 8 == 7:
                        g = i2 // 8
                        v_ = vb[g % 2]
                        sy.dma("pq", v_.t[:], vv[:, g * 8:(g + 1) * 8, :], writes=[v_.r])
                        sy.dma("sp", Vs_d[l, :, g * 8:(g + 1) * 8, :], v_.t[:], reads=[v_.r], writes=[tab_res[l]])
            sy.barrier()

    wq_toggle = [0]

    def wload(dst_ap, src_ap, tile):
        sy.dma("pq", dst_ap, src_ap, writes=[tile.r])

    def norm_chunk(b, l, ci, which, x_t, sq_t, dst_fn, dst_res, ps, rs_t, tmp_t):
        t0, n = CHUNKS[ci]
        j = b if t0 < L else 4
        sy.op("act", lambda: nc.scalar.activation(out=sq_t.t[:, :, 0:n], in_=x_t.t[:, :, 0:n], func=AF.Square), reads=[x_t.r], writes=[sq_t.r])
        for c in range(8):
            sy.op("pe", lambda: nc.tensor.matmul(ps.t[:, 0:n], onesD.t[:], sq_t.t[:, c, 0:n], start=(c == 0), stop=(c == 7)), reads=[sq_t.r, onesD.r], writes=[ps.r], final=(c == 7))
        sy.op("act", lambda: nc.scalar.activation(out=rs_t.t[:, 0:n], in_=ps.t[:, 0:n], func=AF.Sqrt, bias=EPS, scale=1.0), reads=[ps.r], writes=[rs_t.r])
        sy.op("dve", lambda: nc.vector.reciprocal(out=rs_t.t[:, 0:n], in_=rs_t.t[:, 0:n]), reads=[rs_t.r], writes=[rs_t.r])
        for c in range(8):
            if which < 2:
                a_ap = AM.t[:, l, which, c, j:j + 1]
                s_ap = MOD.t[:, l, 0 if which == 0 else 3, c, j:j + 1]
            else:
                a_ap = vt("fg", 0, c)
                s_ap = None
            sy.op("dve", lambda: nc.vector.scalar_tensor_tensor(out=tmp_t.t[:, c, 0:n], in0=x_t.t[:, c, 0:n], scalar=a_ap, in1=rs_t.t[:, 0:n], op0=ALU.mult, op1=ALU.mult),
                  reads=[x_t.r, rs_t.r, AM.r, VT.r], writes=[tmp_t.r])
            if s_ap is not None:
                sy.op("act", lambda: nc.scalar.activation(out=dst_fn(c), in_=tmp_t.t[:, c, 0:n], func=AF.Identity, bias=s_ap, scale=1.0), reads=[tmp_t.r, MOD.r], writes=[dst_res])

    state = dict(nc=nc, sy=sy, es=es, PS=PS, sb=sb, tap=tap, tap_out=tap_out, tap_res=tap_res)
    consts = dict(ident=ident, identb=identb, maskGE=maskGE, maskLE=maskLE, onesD=onesD, ones5=ones5, ones1=ones1, onesb=onesb, iof=iof,
                  VT=VT, vt=vt, vtr=vtr, MOD=MOD, AM=AM, cs_d=cs_d, cs_res=cs_res, skc=skc, sinkl=sinkl, nlam=nlam, gsc=gsc)
    dram = dict(w_in=w_in, w_gate=w_gate, w_branch=w_branch, w_out=w_out, pool_w=pool_w, peer_wq=peer_wq, peer_k1=peer_k1, peer_k2=peer_k2,
                peer_u=peer_u, peer_v=peer_v, xT_d=xT_d, xT_res=xT_res, out_d=out_d, yT_d=yT_d, yT_res=yT_res,
                UTs_d=UTs_d, Vs_d=Vs_d, h2_d=h2_d, rt_d=rt_d, tab_res=tab_res)

    from_mixer = mixer_phase
    for l in range(nl_run):
        for b in range(nb_run):
            from_mixer(state, consts, dram, cfg, b, l, norm_chunk, wload)
            if do_peer and not cfg.get("skip_peer_phase"):
                peer_phase(state, consts, dram, cfg, b, l, norm_chunk, wload)

    final_phase(state, consts, dram, cfg, nb_run, norm_chunk)
    sy.barrier(engs=("sp",))
    es.close()
    return nc, tap_out


_uid = [0]


def mixer_phase(state, consts, dram, cfg, b, l, norm_chunk, wload):
    if cfg.get("skip_mixer"):
        return
    nc, sy, PS, tap = state["nc"], state["sy"], state["PS"], state["tap"]
    C = consts
    vt, vtr, MOD = C["vt"], C["vtr"], C["MOD"]
    xT_d, xT_res = dram["xT_d"], dram["xT_res"]
    ctx_out = l < NL - 1
    qchunks = list(range(5)) if ctx_out else list(range(4))
    wv = dram["w_in"][l].rearrange("(kc p) n -> p kc n", p=128)
    branches = cfg.get("branches", "ACBD")

    def mk(scope, name, shape, dt, nres=1):
        _uid[0] += 1
        return Tile(scope.enter_context(nc.sbuf_tensor(f"{name}_{_uid[0]}", list(shape), dt)), nres)

    def dve(fn, reads, writes):
        sy.op("dve", fn, reads=reads, writes=writes)

    def act(fn, reads, writes):
        sy.op("act", fn, reads=reads, writes=writes)

    def pool(fn, reads, writes):
        sy.op("pool", fn, reads=reads, writes=writes)

    def pe(fn, reads, writes, final=True):
        sy.op("pe", fn, reads=reads, writes=writes, final=final)

    with contextlib.ExitStack() as ms:
        hT = mk(ms, "hT", [128, 8, T], BF16, nres=5)
        with contextlib.ExitStack() as s1:
            xt = [mk(s1, "xt", [128, 8, 512], F32) for _ in range(2)]
            sq = mk(s1, "sq", [128, 8, 512], F32)
            tmp = mk(s1, "tmp", [128, 8, 512], F32)
            rs = [mk(s1, "rs", [128, 512], F32) for _ in range(2)]
            for ci, (t0, n) in enumerate(CHUNKS):
                x_t = xt[ci % 2]
                sy.dma("sp", x_t.t[:, :, 0:n], xT_d[b, :, :, t0:t0 + n], reads=[xT_res[b][ci]], writes=[x_t.r])
                norm_chunk(b, l, ci, 0, x_t, sq, (lambda c, t0=t0, n=n: hT.t[:, c, t0:t0 + n]), hT.res[ci], PS[ci % 2], rs[ci % 2], tmp)
            sy.barrier()
        tap(f"hT{l}", hT.t[:], [128, 8, T], BF16, hT.res)

        yT_d, yT_res = dram["yT_d"], dram["yT_res"]
        ystg = [mk(ms, "ystg", [128, 512], BF16) for _ in range(3)]
        acs = contextlib.ExitStack()
        cosF = mk(acs, "cosF", [128, T], F32)
        sinS = mk(acs, "sinS", [128, T], F32)
        sy.dma("sp", cosF.t[:], C["cs_d"][0], reads=[C["cs_res"]], writes=[cosF.r])
        sy.dma("sp", sinS.t[:], C["cs_d"][1], reads=[C["cs_res"]], writes=[sinS.r])
        ycnt = [0]

        def y_out(nbr, kc, ci, p0, p1, fn_write):
            t0, n = CHUNKS[ci]
            st = ystg[ycnt[0] % 3]
            ycnt[0] += 1
            fn_write(st.t[p0:p1, 0:n], st.r)
            sy.dma("sp", yT_d[nbr, p0:p1, kc, t0:t0 + n], st.t[p0:p1, 0:n], reads=[st.r], writes=[yT_res[nbr][ci]])

        def proj(ps, Wt, c0, ci, ncols=128):
            t0, n = CHUNKS[ci]
            for kc in range(8):
                pe(lambda: nc.tensor.matmul(ps.t[0:ncols, 0:n], Wt.t[:, kc, c0:c0 + ncols], hT.t[:, kc, t0:t0 + n], start=(kc == 0), stop=(kc == 7)),
                   [Wt.r, hT.res[ci]], [ps.r], final=(kc == 7))

        def load_rot(Wr, seg0, ncols):
            dv_ = Wr.t[:, :, 0:ncols].rearrange("p k (h two j) -> p k h two j", two=2, j=32)
            sv_ = wv[:, :, seg0:seg0 + ncols].rearrange("p k (h two j) -> p k h two j", two=2, j=32)
            if cfg.get("fake_rot"):
                sy.dma("pq", Wr.t[:, :, 0:ncols], wv[:, :, seg0:seg0 + ncols], writes=[Wr.r])
                return
            for kc in range(8):
                sy.dma("pq", dv_[:, kc, :, 0, :], sv_[:, kc, :, 1, :], writes=[Wr.r])
                sy.dma("pq", dv_[:, kc, :, 1, :], sv_[:, kc, :, 0, :], writes=[Wr.r])

        def rope_evac(psA, psB, ci, dst_ap, dst_res, t1, t2, splits=None):
            t0, n = CHUNKS[ci]
            dve(lambda: nc.vector.tensor_tensor(out=t1.t[:, 0:n], in0=psA.t[:, 0:n], in1=cosF.t[:, t0:t0 + n], op=ALU.mult), [psA.r, cosF.r], [t1.r])
            dve(lambda: nc.vector.tensor_tensor(out=t2.t[:, 0:n], in0=psB.t[:, 0:n], in1=sinS.t[:, t0:t0 + n], op=ALU.mult), [psB.r, sinS.r], [t2.r])
            if splits is None:
                dve(lambda: nc.vector.tensor_tensor(out=dst_ap, in0=t1.t[:, 0:n], in1=t2.t[:, 0:n], op=ALU.add), [t1.r, t2.r], [dst_res])
            else:
                for (q0_, q1_, dap) in splits:
                    dve(lambda: nc.vector.tensor_tensor(out=dap, in0=t1.t[q0_:q1_, 0:n], in1=t2.t[q0_:q1_, 0:n], op=ALU.add), [t1.r, t2.r], [dst_res])

        if "A" in branches:
            with contextlib.ExitStack() as sa:
                qaT = mk(sa, "qaT", [128, 4, T], BF16)
                kaz = mk(sa, "kaz", [128, 2, 2, T], BF16)
                va = mk(sa, "va", [128, 18, 2, 2, 128], BF16)
                sap = contextlib.ExitStack()
                Waq = mk(sap, "Waq", [128, 8, 512], BF16)
                WaqR = mk(sap, "WaqR", [128, 8, 512], BF16)
                Wak = mk(sap, "Wak", [128, 8, 256], BF16)
                WakR = mk(sap, "WakR", [128, 8, 256], BF16)
                Wav = mk(sap, "Wav", [128, 8, 128], BF16)
                t1 = [mk(sap, "t1", [128, 512], F32) for _ in range(2)]
                t2 = [mk(sap, "t2", [128, 512], F32) for _ in range(2)]
                pool(lambda: nc.gpsimd.memset(kaz.t[64:128, :, 0, :], 0.0), [], [kaz.r])
                pool(lambda: nc.gpsimd.memset(kaz.t[0:64, :, 1, :], 0.0), [], [kaz.r])
                wload(Waq.t[:], wv[:, :, 0:512], Waq)
                load_rot(WaqR, 0, 512)
                for kvh in range(2):
                    for dup in range(2):
                        c0 = (kvh * 2 + dup) * 64
                        sy.dma("pq", Wak.t[:, :, c0:c0 + 64], wv[:, :, 512 + kvh * 64:512 + (kvh + 1) * 64], writes=[Wak.r])
                        for hf in range(2):
                            sy.dma("pq", WakR.t[:, :, c0 + hf * 32:c0 + (hf + 1) * 32],
                                   wv[:, :, 512 + kvh * 64 + (1 - hf) * 32:512 + kvh * 64 + (2 - hf) * 32], writes=[WakR.r])
                wload(Wav.t[:], wv[:, :, 640:768], Wav)
                pool(lambda: nc.gpsimd.memset(va.t[:, :, :, 0, 64:128], 1.0), [], [va.r])
                pool(lambda: nc.gpsimd.memset(va.t[:, :, :, 1, 0:64], 1.0), [], [va.r])
                k = 0
                for ci in range(5):
                    t0, n = CHUNKS[ci]
                    for hc in range(4):
                        if ci == 4 and not ctx_out:
                            continue
                        pa, pb = PS[(2 * k) % 4], PS[(2 * k + 1) % 4]
                        proj(pa, Waq, hc * 128, ci)
                        proj(pb, WaqR, hc * 128, ci)
                        rope_evac(pa, pb, ci, qaT.t[:, hc, t0:t0 + n], qaT.r, t1[k % 2], t2[k % 2])
                        k += 1
                    for kc2 in range(2):
                        pa, pb = PS[(2 * k) % 4], PS[(2 * k + 1) % 4]
                        proj(pa, Wak, kc2 * 128, ci)
                        proj(pb, WakR, kc2 * 128, ci)
                        rope_evac(pa, pb, ci, None, kaz.r, t1[k % 2], t2[k % 2],
                                  splits=[(0, 64, kaz.t[0:64, kc2, 0, t0:t0 + n]), (64, 128, kaz.t[64:128, kc2, 1, t0:t0 + n])])
                        k += 1
                for tt in range(18):
                    ps = PS[4 + tt % 2]
                    ci = min(tt // 4, 4)
                    for kc in range(8):
                        pe(lambda: nc.tensor.matmul(ps.t[:, 0:128], hT.t[:, kc, tt * 128:(tt + 1) * 128], Wav.t[:, kc, :], start=(kc == 0), stop=(kc == 7)),
                           [Wav.r, hT.res[ci]], [ps.r], final=(kc == 7))
                    src = ps.t[:, 0:128].rearrange("p (k d) -> p k d", d=64)
                    act(lambda: nc.scalar.copy(out=va.t[:, tt, :, 0, 0:64], in_=src), [ps.r], [va.r])
                    dve(lambda: nc.vector.tensor_copy(out=va.t[:, tt, :, 1, 64:128], in_=src), [ps.r], [va.r])
                sy.barrier()
                sap.close()
                E = [mk(sa, "E", [128, 16, 384], BF16) for _ in range(2)]
                Ec = [mk(sa, "Ec", [128, 2, T], BF16) for _ in range(2)]
                rden = [mk(sa, "rden", [128, 512], F32) for _ in range(2)]
                maskGE, maskLE = C["maskGE"], C["maskLE"]
                sinkl, skc = C["sinkl"], C["skc"]
                esrow = mk(sa, "esrow", [128, 8, 512], BF16)
                pool(lambda: nc.gpsimd.memset(esrow.t[:], 0.0), [], [esrow.r])
                dve(lambda: nc.vector.tensor_copy(out=esrow.t[0:1, :, :], in_=skc.t[:, l * 8:(l + 1) * 8].unsqueeze(2).to_broadcast([1, 8, 512])), [skc.r], [esrow.r])
                kk = 0
                for h in range(8):
                    kvh, par, hc = h // 4, h % 2, h // 2
                    p0 = 64 * par
                    q0 = 64 - p0
                    Eh, Ech = E[h % 2], Ec[h % 2]
                    for j in range(16):
                        qlo, qhi = max(0, j - 1) * 128, min(16, j + 2) * 128
                        n = qhi - qlo
                        ps = PS[kk % 2]
                        kk += 1
                        pe(lambda: nc.tensor.matmul(ps.t[:, 0:n], kaz.t[:, kvh, par, j * 128:(j + 1) * 128], qaT.t[:, hc, qlo:qhi], start=True, stop=True),
                           [kaz.r, qaT.r], [ps.r])
                        act(lambda: nc.scalar.activation(out=Eh.t[:, j, 0:n], in_=ps.t[:, 0:n], func=AF.Exp, scale=0.125), [ps.r], [Eh.r])
                        if j >= 1:
                            dve(lambda: nc.vector.tensor_tensor(out=Eh.t[:, j, 0:128], in0=Eh.t[:, j, 0:128], in1=maskLE.t[:], op=ALU.mult), [Eh.r, maskLE.r], [Eh.r])
                        if j <= 14:
                            off = (j + 1) * 128 - qlo
                            dve(lambda: nc.vector.tensor_tensor(out=Eh.t[:, j, off:off + 128], in0=Eh.t[:, j, off:off + 128], in1=maskGE.t[:], op=ALU.mult), [Eh.r, maskGE.r], [Eh.r])
                    for jc in range(2):
                        for ci in qchunks:
                            t0, n = CHUNKS[ci]
                            ps = PS[kk % 2]
                            kk += 1
                            pe(lambda: nc.tensor.matmul(ps.t[:, 0:n], kaz.t[:, kvh, par, L + jc * 128:L + (jc + 1) * 128], qaT.t[:, hc, t0:t0 + n], start=True, stop=True),
                               [kaz.r, qaT.r], [ps.r])
                            act(lambda: nc.scalar.activation(out=Ech.t[:, jc, t0:t0 + n], in_=ps.t[:, 0:n], func=AF.Exp, scale=0.125), [ps.r], [Ech.r])
                    for ci in qchunks:
                        t0, n = CHUNKS[ci]
                        pso = PS[2 + kk % 2]
                        rd = rden[kk % 2]
                        kk += 1
                        mm = [(slice(0, n), sinkl.t[:, par, :], esrow.t[:, h, 0:n], [sinkl.r, esrow.r])]
                        for jc in range(2):
                            mm.append((slice(0, n), va.t[:, 16 + jc, kvh, par, :], Ech.t[:, jc, t0:t0 + n], [va.r, Ech.r]))
                        if ci < 4:
                            for j in range(4 * ci - 1, 4 * ci + 5):
                                if 0 <= j < 16:
                                    bl_lo, bl_hi = max(4 * ci, j - 1), min(4 * ci + 3, j + 1)
                                    sub0, nsub = (bl_lo - 4 * ci) * 128, (bl_hi - bl_lo + 1) * 128
                                    eoff = bl_lo * 128 - max(0, j - 1) * 128
                                    mm.append((slice(sub0, sub0 + nsub), va.t[:, j, kvh, par, :], Eh.t[:, j, eoff:eoff + nsub], [va.r, Eh.r]))
                        for i, (sl, lt, rh, rd_) in enumerate(mm):
                            pe(lambda: nc.tensor.matmul(pso.t[:, sl], lt, rh, start=(i == 0), stop=(i == len(mm) - 1)), rd_, [pso.r], final=(i == len(mm) - 1))
                        dve(lambda: nc.vector.reciprocal(out=rd.t[p0:p0 + 64, 0:n], in_=pso.t[q0:q0 + 64, 0:n]), [pso.r], [rd.r])
                        y_out(0, hc, ci, p0, p0 + 64, lambda ap, r_: dve(lambda: nc.vector.tensor_tensor(out=ap, in0=pso.t[p0:p0 + 64, 0:n], in1=rd.t[p0:p0 + 64, 0:n], op=ALU.mult),
                                                                        [pso.r, rd.r], [r_]))
                sy.barrier()
            tap(f"yA{l}", yT_d[0], [128, 4, T], BF16, yT_res[0])

        if "C" in branches:
            with contextlib.ExitStack() as sc:
                qcT = mk(sc, "qcT", [128, 4, T], BF16)
                kcz = mk(sc, "kcz", [128, 2, 4, T], BF16)
                vc = mk(sc, "vc", [128, 18, 512], BF16)
                scp = contextlib.ExitStack()
                Wcq = mk(scp, "Wcq", [128, 8, 512], BF16)
                WcqR = mk(scp, "WcqR", [128, 8, 512], BF16)
                Wck = mk(scp, "Wck", [128, 8, 512], BF16)
                WckR = mk(scp, "WckR", [128, 8, 512], BF16)
                Wcv = mk(scp, "Wcv", [128, 8, 512], BF16)
                t1 = [mk(scp, "t1", [128, 512], F32) for _ in range(2)]
                t2 = [mk(scp, "t2", [128, 512], F32) for _ in range(2)]
                pool(lambda: nc.gpsimd.memset(kcz.t[64:128, 0, :, :], 0.0), [], [kcz.r])
                pool(lambda: nc.gpsimd.memset(kcz.t[0:64, 1, :, :], 0.0), [], [kcz.r])
                wload(Wcq.t[:], wv[:, :, 768:1280], Wcq)
                load_rot(WcqR, 768, 512)
                wload(Wck.t[:], wv[:, :, 1280:1792], Wck)
                load_rot(WckR, 1280, 512)
                wload(Wcv.t[:], wv[:, :, 1792:2304], Wcv)
                k = 0
                for ci in range(5):
                    t0, n = CHUNKS[ci]
                    for hc in range(4):
                        for (W_, WR_, dstT) in ((Wcq, WcqR, qcT), (Wck, WckR, kcz)):
                            if dstT is qcT and ci == 4 and not ctx_out:
                                continue
                            pa, pb = PS[(2 * k) % 4], PS[(2 * k + 1) % 4]
                            proj(pa, W_, hc * 128, ci)
                            proj(pb, WR_, hc * 128, ci)
                            if dstT is qcT:
                                rope_evac(pa, pb, ci, qcT.t[:, hc, t0:t0 + n], qcT.r, t1[k % 2], t2[k % 2])
                            else:
                                rope_evac(pa, pb, ci, None, kcz.r, t1[k % 2], t2[k % 2],
                                          splits=[(0, 64, kcz.t[0:64, 0, hc, t0:t0 + n]), (64, 128, kcz.t[64:128, 1, hc, t0:t0 + n])])
                            k += 1
                for tt in range(18):
                    ps = PS[4 + tt % 2]
                    ci = min(tt // 4, 4)
                    for kc in range(8):
                        pe(lambda: nc.tensor.matmul(ps.t[:, 0:512], hT.t[:, kc, tt * 128:(tt + 1) * 128], Wcv.t[:, kc, :], start=(kc == 0), stop=(kc == 7)),
                           [Wcv.r, hT.res[ci]], [ps.r], final=(kc == 7))
                    if tt % 2 == 0:
                        act(lambda: nc.scalar.copy(out=vc.t[:, tt, :], in_=ps.t[:, 0:512]), [ps.r], [vc.r])
                    else:
                        dve(lambda: nc.vector.tensor_copy(out=vc.t[:, tt, :], in_=ps.t[:, 0:512]), [ps.r], [vc.r])
                sy.barrier()
                scp.close()
                Et = [mk(sc, "Et", [128, 512], BF16) for _ in range(3)]
                rd = [mk(sc, "rdc", [128, 512], F32) for _ in range(2)]
                tu = [mk(sc, "tu", [128, 512], F32) for _ in range(2)]
                ot = mk(sc, "ot", [128, 512], F32)
                sqo = mk(sc, "sqo", [128, 512], F32)
                rso = mk(sc, "rso", [128, 512], F32)
                onesb, ones1, nlam, gsc = C["onesb"], C["ones1"], C["nlam"], C["gsc"]
                kk = 0
                for h in range(4):
                    for ci in qchunks:
                        t0, n = CHUNKS[ci]
                        keyt = list(range(18)) if ci < 4 else [16, 17]
                        items = [(c, ji, j) for c in range(2) for ji, j in enumerate(keyt)]
                        SK = 2
                        scb = (PS[0], PS[1], PS[6])
                        pend = []

                        def emit_score(idx_):
                            c, ji, j = items[idx_]
                            p0 = 64 * c
                            ps = scb[(kk0 + idx_) % 3]
                            et = Et[(kk0 + idx_) % 3]
                            pe(lambda: nc.tensor.matmul(ps.t[:, 0:n], kcz.t[:, c, h, j * 128:(j + 1) * 128], qcT.t[:, h, t0:t0 + n], start=True, stop=True),
                               [kcz.r, qcT.r], [ps.r])
                            act(lambda: nc.scalar.activation(out=et.t[:, 0:n], in_=ps.t[:, 0:n], func=AF.Exp, scale=0.125), [ps.r], [et.r])

                        def emit_pv(idx_):
                            c, ji, j = items[idx_]
                            et = Et[(kk0 + idx_) % 3]
                            psU, psD = PS[2 + c], PS[4 + c]
                            pe(lambda: nc.tensor.matmul(psU.t[:, 0:n], vc.t[:, j, h * 128:(h + 1) * 128], et.t[:, 0:n], start=(ji == 0), stop=(ji == len(keyt) - 1)),
                               [vc.r, et.r], [psU.r], final=(ji == len(keyt) - 1))
                            pe(lambda: nc.tensor.matmul(psD.t[:, 0:n], onesb.t[:], et.t[:, 0:n], start=(ji == 0), stop=(ji == len(keyt) - 1)),
                               [onesb.r, et.r], [psD.r], final=(ji == len(keyt) - 1))

                        kk0 = kk
                        for idx_ in range(len(items) + SK):
                            if idx_ < len(items):
                                emit_score(idx_)
                            if idx_ >= SK:
                                emit_pv(idx_ - SK)
                        kk += len(items)
                        for c in range(2):
                            dve(lambda: nc.vector.reciprocal(out=rd[c].t[:, 0:n], in_=PS[4 + c].t[:, 0:n]), [PS[4 + c].r], [rd[c].r])
                            dve(lambda: nc.vector.tensor_tensor(out=tu[c].t[:, 0:n], in0=PS[2 + c].t[:, 0:n], in1=rd[c].t[:, 0:n], op=ALU.mult), [PS[2 + c].r, rd[c].r], [tu[c].r])
                        dve(lambda: nc.vector.scalar_tensor_tensor(out=ot.t[:, 0:n], in0=tu[1].t[:, 0:n], scalar=nlam.t[:, l:l + 1], in1=tu[0].t[:, 0:n], op0=ALU.mult, op1=ALU.add),
                            [tu[0].r, tu[1].r, nlam.r], [ot.r])
                        act(lambda: nc.scalar.activation(out=sqo.t[:, 0:n], in_=ot.t[:, 0:n], func=AF.Square), [ot.r], [sqo.r])
                        psn = PS[7]
                        pe(lambda: nc.tensor.matmul(psn.t[:, 0:n], ones1.t[:], sqo.t[:, 0:n], start=True, stop=True), [ones1.r, sqo.r], [psn.r])
                        act(lambda: nc.scalar.activation(out=rso.t[:, 0:n], in_=psn.t[:, 0:n], func=AF.Sqrt, bias=EPS, scale=1.0), [psn.r], [rso.r])
                        dve(lambda: nc.vector.reciprocal(out=rso.t[:, 0:n], in_=rso.t[:, 0:n]), [rso.r], [rso.r])
                        y_out(1, h, ci, 0, 128, lambda ap, r_: dve(lambda: nc.vector.scalar_tensor_tensor(out=ap, in0=ot.t[:, 0:n], scalar=gsc.t[:, l:l + 1], in1=rso.t[:, 0:n], op0=ALU.mult, op1=ALU.mult),
                                                                  [ot.r, rso.r, gsc.r], [r_]))
                sy.barrier()
            tap(f"yC{l}", yT_d[1], [128, 4, T], BF16, yT_res[1])

        sy.barrier()
        acs.close()
        if "B" in branches:
            with contextlib.ExitStack() as sb_:
                YW = 15 + L + 15 + 15 + LC + 15
                offs = (15, 15 + L + 15 + 15)
                Wb = mk(sb_, "Wb", [128, 8, 1024], BF16)
                ypad = mk(sb_, "ypad", [128, 4, YW], BF16)
                Dm = mk(sb_, "Dm", [128, 124, 128], BF16)
                z = mk(sb_, "z", [128, 4, T], F32)
                sg = [mk(sb_, "sg", [128, 512], F32) for _ in range(2)]
                wload(Wb.t[:], wv[:, :, 2304:3328], Wb)
                pool(lambda: nc.gpsimd.memset(ypad.t[:], 0.0), [], [ypad.r])
                identb = C["identb"]
                for i in range(124):
                    fn = (lambda: nc.vector.tensor_scalar(out=Dm.t[:, i, :], in0=identb.t[:], scalar1=vt("cw", l, i), scalar2=None, op0=ALU.mult))
                    dve(fn, [identb.r, C["VT"].r], [Dm.r])
                bchunks = qchunks
                k = 0
                for cc in range(4):
                    for ci in bchunks:
                        t0, n = CHUNKS[ci]
                        pa, pg = PS[(2 * k) % 4], PS[(2 * k + 1) % 4]
                        s_ = sg[k % 2]
                        k += 1
                        proj(pa, Wb, cc * 128, ci)
                        proj(pg, Wb, 512 + cc * 128, ci)
                        act(lambda: nc.scalar.activation(out=s_.t[:, 0:n], in_=pg.t[:, 0:n], func=AF.Sigmoid), [pg.r], [s_.r])
                        yo = offs[0] + t0 if ci < 4 else offs[1]
                        dve(lambda: nc.vector.tensor_tensor(out=ypad.t[:, cc, yo:yo + n], in0=pa.t[:, 0:n], in1=s_.t[:, 0:n], op=ALU.mult), [pa.r, s_.r], [ypad.r])
                k = 0
                for cc in range(4):
                    for ci in bchunks:
                        t0, n = CHUNKS[ci]
                        ps = PS[4 + k % 2]
                        k += 1
                        base = (t0 if ci < 4 else offs[1] - 15)
                        for kk_ in range(31):
                            pe(lambda: nc.tensor.matmul(ps.t[:, 0:n], Dm.t[:, kk_ * 4 + cc, :], ypad.t[:, cc, base + kk_:base + kk_ + n], start=(kk_ == 0), stop=(kk_ == 30)),
                               [Dm.r, ypad.r], [ps.r], final=(kk_ == 30))
                        act(lambda: nc.scalar.activation(out=z.t[:, cc, t0:t0 + n], in_=ps.t[:, 0:n], func=AF.Identity, bias=vt("cb", l, cc), scale=1.0), [ps.r, C["VT"].r], [z.r])
                ones5 = C["ones5"]
                zsq = mk(sb_, "zsq", [128, 4, 512], F32)
                m2 = mk(sb_, "m2", [128, 512], F32)
                var = mk(sb_, "var", [128, 512], F32)
                tz = [mk(sb_, "tz", [128, 512], F32) for _ in range(2)]
                for ci in bchunks:
                    t0, n = CHUNKS[ci]
                    psm, psq = PS[6], PS[7]
                    act(lambda: nc.scalar.activation(out=zsq.t[:, :, 0:n], in_=z.t[:, :, t0:t0 + n], func=AF.Square), [z.r], [zsq.r])
                    for cc in range(4):
                        pe(lambda: nc.tensor.matmul(psm.t[:, 0:n], ones5.t[:], z.t[:, cc, t0:t0 + n], start=(cc == 0), stop=(cc == 3)), [ones5.r, z.r], [psm.r], final=(cc == 3))
                    for cc in range(4):
                        pe(lambda: nc.tensor.matmul(psq.t[:, 0:n], ones5.t[:], zsq.t[:, cc, 0:n], start=(cc == 0), stop=(cc == 3)), [ones5.r, zsq.r], [psq.r], final=(cc == 3))
                    act(lambda: nc.scalar.activation(out=m2.t[:, 0:n], in_=psm.t[:, 0:n], func=AF.Square), [psm.r], [m2.r])
                    dve(lambda: nc.vector.tensor_tensor(out=var.t[:, 0:n], in0=psq.t[:, 0:n], in1=m2.t[:, 0:n], op=ALU.subtract), [psq.r, m2.r], [var.r])
                    dve(lambda: nc.vector.tensor_scalar_max(out=var.t[:, 0:n], in0=var.t[:, 0:n], scalar1=0.0), [var.r], [var.r])
                    act(lambda: nc.scalar.activation(out=var.t[:, 0:n], in_=var.t[:, 0:n], func=AF.Sqrt, bias=EPS, scale=1.0), [var.r], [var.r])
                    dve(lambda: nc.vector.reciprocal(out=var.t[:, 0:n], in_=var.t[:, 0:n]), [var.r], [var.r])
                    for cc in range(4):
                        tz_ = tz[cc % 2]
                        dve(lambda: nc.vector.tensor_tensor(out=tz_.t[:, 0:n], in0=z.t[:, cc, t0:t0 + n], in1=psm.t[:, 0:n], op=ALU.subtract), [z.r, psm.r], [tz_.r])
                        dve(lambda: nc.vector.tensor_tensor(out=tz_.t[:, 0:n], in0=tz_.t[:, 0:n], in1=var.t[:, 0:n], op=ALU.mult), [tz_.r, var.r], [tz_.r])
                        y_out(2, cc, ci, 0, 128, lambda ap, r_: act(lambda: nc.scalar.activation(out=ap, in_=tz_.t[:, 0:n], func=AF.Silu, bias=vt("lnb", l, cc), scale=vt("lng", l, cc)),
                                                                   [tz_.r, C["VT"].r], [r_]))
                sy.barrier()
            tap(f"yB{l}", yT_d[2], [128, 4, T], BF16, yT_res[2])

        if "D" in branches:
            with contextlib.ExitStack() as sd:
                PW = 24 + L + 24 + LC + 24
                poff = (24, 24 + L + 24)
                Wd = mk(sd, "Wd", [128, 8, 512], BF16)
                Wp = mk(sd, "Wp", [128, 4, 128], BF16)
                bufs = [mk(sd, f"pb{i}", [128, PW], F32) for i in range(5)]
                yq = mk(sd, "yq", [128, T], BF16)
                edge = mk(sd, "edge", [128, 64], F32)
                etmp = mk(sd, "etmp", [128, 16], F32)
                wload(Wd.t[:], wv[:, :, 3328:3840], Wd)
                wload(Wp.t[:], dram["pool_w"][l].rearrange("g c e -> c g e"), Wp)
                for bf in bufs:
                    pool(lambda: nc.gpsimd.memset(bf.t[:], 0.0), [], [bf.r])
                bchunks = qchunks
                k = 0
                for g in range(4):
                    win = POOLW[g]
                    lo, hi = win // 2, win - 1 - win // 2
                    xb = bufs[0]
                    for ci in bchunks:
                        t0, n = CHUNKS[ci]
                        ps = PS[k % 2]
                        k += 1
                        proj(ps, Wd, g * 128, ci)
                        xo_ = poff[0] + t0 if ci < 4 else poff[1]
                        act(lambda: nc.scalar.copy(out=xb.t[:, xo_:xo_ + n], in_=ps.t[:, 0:n]), [ps.r], [xb.r])
                    nlev = g + 1
                    for lv in range(nlev):
                        w_ = 1 << lv
                        src, dst = bufs[lv], bufs[lv + 1]
                        pool(lambda: nc.gpsimd.tensor_tensor(out=dst.t[:, w_:PW], in0=src.t[:, w_:PW], in1=src.t[:, 0:PW - w_], op=ALU.add), [src.r], [dst.r])
                    ws = bufs[nlev]
                    segs = [(poff[0], L, 0)] + ([(poff[1], LC, L)] if ctx_out else [])
                    for (so, sl, yo) in segs:
                        dve(lambda: nc.vector.scalar_tensor_tensor(out=yq.t[:, yo:yo + sl], in0=ws.t[:, so + hi:so + hi + sl], scalar=1.0 / win, in1=xb.t[:, so:so + sl], op0=ALU.mult, op1=ALU.subtract),
                            [ws.r, xb.r], [yq.r])
                        ecols = [(t, t + hi + 1) for t in range(lo)] + [(sl - 1 - i, lo + 1 + i) for i in range(hi)]
                        for (t, cnt) in ecols:
                            dve(lambda: nc.vector.scalar_tensor_tensor(out=yq.t[:, yo + t:yo + t + 1], in0=ws.t[:, so + hi + t:so + hi + t + 1], scalar=1.0 / cnt, in1=xb.t[:, so + t:so + t + 1], op0=ALU.mult, op1=ALU.subtract),
                                [ws.r, xb.r], [yq.r])
                    for ci in bchunks:
                        t0, n = CHUNKS[ci]
                        ps = PS[2 + k % 2]
                        k += 1
                        pe(lambda: nc.tensor.matmul(ps.t[:, 0:n], Wp.t[:, g, :], yq.t[:, t0:t0 + n], start=True, stop=True), [Wp.r, yq.r], [ps.r])
                        y_out(3, g, ci, 0, 128, lambda ap, r_: act(lambda: nc.scalar.activation(out=ap, in_=ps.t[:, 0:n], func=AF.Copy, scale=vt("psc", l, g)), [ps.r, C["VT"].r], [r_]))
                sy.barrier()
            tap(f"yD{l}", yT_d[3], [128, 4, T], BF16, yT_res[3])

        if cfg.get("merge", True):
            with contextlib.ExitStack() as sm:
                accT = mk(sm, "accT", [128, 8, T], BF16)
                with contextlib.ExitStack() as sm1:
                    Wg = [mk(sm1, "Wg", [128, 4, 8, 256], BF16) for _ in range(2)]
                    Wbr = [mk(sm1, "Wbr", [128, 4, 4, 256], BF16) for _ in range(2)]
                    sgm = [mk(sm1, "sgm", [128, 512], F32) for _ in range(2)]
                    tm = [mk(sm1, "tm", [128, 512], F32) for _ in range(2)]
                    acc = mk(sm1, "acc", [128, 512], F32)
                    ych = [mk(sm1, "ych", [128, 4, 4, 512], BF16) for _ in range(2)]
                    wgv = dram["w_gate"][l].rearrange("n (kc p) e -> p n kc e", p=128)
                    wbv = dram["w_branch"][l].rearrange("n (kc p) e -> p n kc e", p=128)
                    border = (0, 2, 1, 3)
                    border = (0, 1, 2, 3)
                    k = 0
                    for ecp in range(4):
                        wg_, wb_ = Wg[ecp % 2], Wbr[ecp % 2]
                        for n_ in range(4):
                            sy.dma("pq", wg_.t[:, n_, :, :], wgv[:, n_, :, ecp * 256:(ecp + 1) * 256], writes=[wg_.r])
                            sy.dma("pq", wb_.t[:, n_, :, :], wbv[:, n_, :, ecp * 256:(ecp + 1) * 256], writes=[wb_.r])
                        for ci in qchunks:
                            t0, n = CHUNKS[ci]
                            yc_ = ych[(ecp * 5 + ci) % 2]
                            for i_ in range(4):
                                sy.dma("sp", yc_.t[:, i_, :, 0:n], yT_d[i_, :, :, t0:t0 + n], reads=[yT_res[i_][ci]], writes=[yc_.r])
                            for e2 in range(2):
                                ec = ecp * 2 + e2
                                es_ = slice(e2 * 128, (e2 + 1) * 128)
                                for n_ in range(4):
                                    pg, pb_ = PS[(2 * k) % 4], PS[(2 * k + 1) % 4]
                                    s_ = sgm[k % 2]
                                    t_ = tm[k % 2]
                                    k += 1
                                    for kc in range(8):
                                        pe(lambda: nc.tensor.matmul(pg.t[:, 0:n], wg_.t[:, n_, kc, es_], hT.t[:, kc, t0:t0 + n], start=(kc == 0), stop=(kc == 7)), [wg_.r, hT.res[ci]], [pg.r], final=(kc == 7))
                                    for kc in range(4):
                                        pe(lambda: nc.tensor.matmul(pb_.t[:, 0:n], wb_.t[:, n_, kc, es_], yc_.t[:, n_, kc, 0:n], start=(kc == 0), stop=(kc == 3)), [wb_.r, yc_.r], [pb_.r], final=(kc == 3))
                                    act(lambda: nc.scalar.activation(out=s_.t[:, 0:n], in_=pg.t[:, 0:n], func=AF.Sigmoid, bias=vt("bg", l, n_ * 8 + ec), scale=1.0), [pg.r, C["VT"].r], [s_.r])
                                    if n_ == 0:
                                        dve(lambda: nc.vector.tensor_tensor(out=acc.t[:, 0:n], in0=pb_.t[:, 0:n], in1=s_.t[:, 0:n], op=ALU.mult), [pb_.r, s_.r], [acc.r])
                                    else:
                                        dve(lambda: nc.vector.tensor_tensor(out=t_.t[:, 0:n], in0=pb_.t[:, 0:n], in1=s_.t[:, 0:n], op=ALU.mult), [pb_.r, s_.r], [t_.r])
                                        if n_ < 3:
                                            dve(lambda: nc.vector.tensor_tensor(out=acc.t[:, 0:n], in0=acc.t[:, 0:n], in1=t_.t[:, 0:n], op=ALU.add), [acc.r, t_.r], [acc.r])
                                        else:
                                            dve(lambda: nc.vector.tensor_tensor(out=accT.t[:, ec, t0:t0 + n], in0=acc.t[:, 0:n], in1=t_.t[:, 0:n], op=ALU.add), [acc.r, t_.r], [accT.r])
                    sy.barrier()
                tap(f"accT{l}", accT.t[:], [128, 8, T], BF16, [accT.r])
                Wo = mk(sm, "Wo", [128, 8, 8, 128], BF16)
                xt = [mk(sm, "xtm", [128, 8, 512], F32) for _ in range(2)]
                wov = dram["w_out"][l].rearrange("(kc p) (oc e) -> p oc kc e", p=128, e=128)
                for oc in range(8):
                    sy.dma("pq", Wo.t[:, oc, :, :], wov[:, oc, :, :], writes=[Wo.r])
                for ci in qchunks:
                    t0, n = CHUNKS[ci]
                    j = b if ci < 4 else 4
                    x_t = xt[ci % 2]
                    sy.dma("sp", x_t.t[:, :, 0:n], xT_d[b, :, :, t0:t0 + n], reads=[xT_res[b][ci]], writes=[x_t.r])
                    for oc in range(8):
                        ps = PS[4 + oc % 4]
                        for kc in range(8):
                            pe(lambda: nc.tensor.matmul(ps.t[:, 0:n], Wo.t[:, oc, kc, :], accT.t[:, kc, t0:t0 + n], start=(kc == 0), stop=(kc == 7)), [Wo.r, accT.r], [ps.r], final=(kc == 7))
                        dve(lambda: nc.vector.scalar_tensor_tensor(out=x_t.t[:, oc, 0:n], in0=ps.t[:, 0:n], scalar=MOD.t[:, l, 2, oc, j:j + 1], in1=x_t.t[:, oc, 0:n], op0=ALU.mult, op1=ALU.add),
                            [ps.r, x_t.r, MOD.r], [x_t.r])
                    sy.dma("sp", xT_d[b, :, :, t0:t0 + n], x_t.t[:, :, 0:n], reads=[x_t.r], writes=[xT_res[b][ci]])
                sy.barrier()
        tap(f"xmix{l}", xT_d[b], [128, 8, T], F32, xT_res[b])
        sy.barrier()


def _overlap_res(xT_res_b, c0, n):
    out = []
    for ci, (t0, nn) in enumerate(CHUNKS):
        if t0 < c0 + n and c0 < t0 + nn:
            out.append(xT_res_b[ci])
    return out


def peer_phase(state, consts, dram, cfg, b, l, norm_chunk, wload):
    nc, sy, PS, tap = state["nc"], state["sy"], state["PS"], state["tap"]
    C = consts
    vt, MOD, iof, ident, identb = C["vt"], C["MOD"], C["iof"], C["ident"], C["identb"]
    xT_d, xT_res = dram["xT_d"], dram["xT_res"]
    h2_d, rt_d = dram["h2_d"], dram["rt_d"]
    ctx_out = l < NL - 1
    chunks = list(range(5)) if ctx_out else list(range(4))
    h2_res, rt_res = Res(), Res()

    def mk(scope, name, shape, dt, nres=1):
        _uid[0] += 1
        return Tile(scope.enter_context(nc.sbuf_tensor(f"{name}_{_uid[0]}", list(shape), dt)), nres)

    def dve(fn, reads, writes):
        sy.op("dve", fn, reads=reads, writes=writes)

    def act(fn, reads, writes):
        sy.op("act", fn, reads=reads, writes=writes)

    def pool(fn, reads, writes):
        sy.op("pool", fn, reads=reads, writes=writes)

    def pe(fn, reads, writes, final=True):
        sy.op("pe", fn, reads=reads, writes=writes, final=final)

    with contextlib.ExitStack() as s1:
        Wq = mk(s1, "Wq", [128, 8, 2048], BF16)
        kst = mk(s1, "kst", [128, 16, 128], BF16)
        kT = mk(s1, "kT", [128, 16, 128], BF16)
        xt = [mk(s1, "xt", [128, 8, 512], F32) for _ in range(2)]
        sq = mk(s1, "sq", [128, 8, 512], F32)
        tmp = mk(s1, "tmp", [128, 8, 512], F32)
        rs = [mk(s1, "rs", [128, 512], F32) for _ in range(2)]
        h2c = [mk(s1, "h2c", [128, 8, 512], BF16) for _ in range(2)]
        qT = mk(s1, "qT", [128, 16, 512], BF16)
        sc = mk(s1, "sc", [128, 16, 128], F32)
        scw = mk(s1, "scw", [128, 16, 128], F32, nres=16)
        v16 = mk(s1, "v16", [128, 16, 16], F32, nres=16)
        i16 = mk(s1, "i16", [128, 16, 16], U32, nres=16)
        i16f = mk(s1, "i16f", [128, 16, 16], F32)
        cand = mk(s1, "cand", [128, 8, 16, 16], F32)
        candw = mk(s1, "candw", [128, 8, 256], F32, nres=8)
        c16 = mk(s1, "c16", [128, 8, 16], F32, nres=8)
        ci16 = mk(s1, "ci16", [128, 8, 16], U32, nres=8)
        iab = mk(s1, "iab", [128, 2, 8, 16], U32)
        fab = mk(s1, "fab", [128, 2, 8, 16], F32)
        oh = mk(s1, "oh", [128, 8, 16, 16], F32)
        sel = mk(s1, "sel", [128, 3, 128], F32)
        gs = mk(s1, "gs", [128, 8], F32)
        rtile = [mk(s1, "rtile", [128, 3, 128], F32) for _ in range(2)]
        wload(Wq.t[:], dram["peer_wq"][l].rearrange("(kc p) n -> p kc n", p=128), Wq)
        for half, kd in enumerate((dram["peer_k1"], dram["peer_k2"])):
            sy.dma("pq", kst.t[:].rearrange("n (h two) d -> n h two d", two=2)[:, :, half, :], kd[l].rearrange("h n d -> n h d"), writes=[kst.r])
        for blk in range(16):
            ps = PS[blk % 2]
            psb = ps.t[:].bitcast(BF16)
            pe(lambda: nc.tensor.transpose(out=psb[:, 0:128], in_=kst.t[:, blk, :], identity=identb.t[:]), [kst.r, identb.r], [ps.r])
            act(lambda: nc.scalar.copy(out=kT.t[:, blk, :], in_=psb[:, 0:128]), [ps.r], [kT.r])
        ntile = 0
        for ci in chunks:
            t0, n = CHUNKS[ci]
            x_t = xt[ci % 2]
            h2 = h2c[ci % 2]
            sy.dma("sp", x_t.t[:, :, 0:n], xT_d[b, :, :, t0:t0 + n], reads=[xT_res[b][ci]], writes=[x_t.r])
            norm_chunk(b, l, ci, 1, x_t, sq, (lambda c, h2=h2, n=n: h2.t[:, c, 0:n]), h2.r, PS[ci % 2], rs[ci % 2], tmp)
            sy.dma("sp", h2_d[:, :, t0:t0 + n], h2.t[:, :, 0:n], reads=[h2.r], writes=[h2_res])
            for blk in range(16):
                ps = PS[2 + blk % 2]
                for kc in range(8):
                    pe(lambda: nc.tensor.matmul(ps.t[:, 0:n], Wq.t[:, kc, blk * 128:(blk + 1) * 128], h2.t[:, kc, 0:n], start=(kc == 0), stop=(kc == 7)), [Wq.r, h2.r], [ps.r], final=(kc == 7))
                if blk % 2 == 0:
                    act(lambda: nc.scalar.copy(out=qT.t[:, blk, 0:n], in_=ps.t[:, 0:n]), [ps.r], [qT.r])
                else:
                    dve(lambda: nc.vector.tensor_copy(out=qT.t[:, blk, 0:n], in_=ps.t[:, 0:n]), [ps.r], [qT.r])
            for tt in range(n // 128):
                for q4 in range(4):
                    ps = PS[4 + q4]
                    for bi in range(4):
                        blk = q4 * 4 + bi
                        pe(lambda: nc.tensor.matmul(ps.t[:, bi * 128:(bi + 1) * 128], qT.t[:, blk, tt * 128:(tt + 1) * 128], kT.t[:, blk, :], start=True, stop=True), [qT.r, kT.r], [ps.r])
                    src = ps.t[:, :].rearrange("p (k n) -> p k n", n=128)
                    if q4 % 2 == 0:
                        act(lambda: nc.scalar.copy(out=sc.t[:, q4 * 4:(q4 + 1) * 4, :], in_=src), [ps.r], [sc.r])
                    else:
                        dve(lambda: nc.vector.tensor_copy(out=sc.t[:, q4 * 4:(q4 + 1) * 4, :], in_=src), [ps.r], [sc.r])
                R16 = range(16)
                for blk in R16:
                    dve(lambda: nc.vector.max(out=v16.t[:, blk, 0:8], in_=sc.t[:, blk, :]), [sc.r], [v16.res[blk]])
                for blk in R16:
                    dve(lambda: nc.vector.max_index(out=i16.t[:, blk, 0:8], in_max=v16.t[:, blk, 0:8], in_values=sc.t[:, blk, :]), [sc.r, v16.res[blk]], [i16.res[blk]])
                for blk in R16:
                    dve(lambda: nc.vector.match_replace(out=scw.t[:, blk, :], in_to_replace=v16.t[:, blk, 0:8], in_values=sc.t[:, blk, :], imm_value=-1e30), [sc.r, v16.res[blk]], [scw.res[blk]])
                for blk in R16:
                    dve(lambda: nc.vector.max(out=v16.t[:, blk, 8:16], in_=scw.t[:, blk, :]), [scw.res[blk]], [v16.res[blk]])
                for blk in R16:
                    dve(lambda: nc.vector.max_index(out=i16.t[:, blk, 8:16], in_max=v16.t[:, blk, 8:16], in_values=scw.t[:, blk, :]), [scw.res[blk], v16.res[blk]], [i16.res[blk]])
                dve(lambda: nc.vector.tensor_copy(out=i16f.t[:], in_=i16.t[:]), i16.res, [i16f.r])
                v16v = v16.t[:].rearrange("p (h two) k -> p h two k", two=2)
                i16v = i16f.t[:].rearrange("p (h two) k -> p h two k", two=2)
                dve(lambda: nc.vector.tensor_tensor(out=cand.t[:], in0=v16v[:, :, 0, :].unsqueeze(3).to_broadcast([128, 8, 16, 16]),
                                                    in1=v16v[:, :, 1, :].unsqueeze(2).to_broadcast([128, 8, 16, 16]), op=ALU.add), v16.res, [cand.r])
                candf = cand.t[:].rearrange("p h a b -> p h (a b)")
                R8 = range(8)
                for h in R8:
                    dve(lambda: nc.vector.max(out=c16.t[:, h, 0:8], in_=candf[:, h, :]), [cand.r], [c16.res[h]])
                for h in R8:
                    dve(lambda: nc.vector.max_index(out=ci16.t[:, h, 0:8], in_max=c16.t[:, h, 0:8], in_values=candf[:, h, :]), [cand.r, c16.res[h]], [ci16.res[h]])
                for h in R8:
                    dve(lambda: nc.vector.match_replace(out=candw.t[:, h, :], in_to_replace=c16.t[:, h, 0:8], in_values=candf[:, h, :], imm_value=-1e30), [cand.r, c16.res[h]], [candw.res[h]])
                for h in R8:
                    dve(lambda: nc.vector.max(out=c16.t[:, h, 8:16], in_=candw.t[:, h, :]), [candw.res[h]], [c16.res[h]])
                for h in R8:
                    dve(lambda: nc.vector.max_index(out=ci16.t[:, h, 8:16], in_max=c16.t[:, h, 8:16], in_values=candw.t[:, h, :]), [candw.res[h], c16.res[h]], [ci16.res[h]])
                dve(lambda: nc.vector.tensor_single_scalar(out=iab.t[:, 0, :, :], in_=ci16.t[:], scalar=4, op=ALU.arith_shift_right), ci16.res, [iab.r])
                dve(lambda: nc.vector.tensor_single_scalar(out=iab.t[:, 1, :, :], in_=ci16.t[:], scalar=15, op=ALU.bitwise_and), ci16.res, [iab.r])
                dve(lambda: nc.vector.tensor_copy(out=fab.t[:], in_=iab.t[:]), [iab.r], [fab.r])
                io16 = iof.t[:, 0:16].unsqueeze(1).unsqueeze(1).to_broadcast([128, 8, 16, 16])
                for w_ in range(2):
                    dve(lambda: nc.vector.tensor_tensor(out=oh.t[:], in0=io16, in1=fab.t[:, w_, :, :].unsqueeze(3).to_broadcast([128, 8, 16, 16]), op=ALU.is_equal), [fab.r, iof.r], [oh.r])
                    dve(lambda: nc.vector.tensor_tensor(out=oh.t[:], in0=oh.t[:], in1=i16v[:, :, w_, :].unsqueeze(2).to_broadcast([128, 8, 16, 16]), op=ALU.mult), [oh.r, i16f.r], [oh.r])
                    dve(lambda: nc.vector.tensor_reduce(out=sel.t[:, w_, :].rearrange("p (h k) -> p h k", k=16), in_=oh.t[:], axis=AX.X, op=ALU.add), [oh.r], [sel.r])
                g3 = sel.t[:, 2, :].rearrange("p (h k) -> p h k", k=16)
                dve(lambda: nc.vector.tensor_tensor(out=g3, in0=c16.t[:], in1=c16.t[:, :, 0:1].to_broadcast([128, 8, 16]), op=ALU.subtract), c16.res, [sel.r])
                act(lambda: nc.scalar.activation(out=g3, in_=g3, func=AF.Exp), [sel.r], [sel.r])
                dve(lambda: nc.vector.tensor_reduce(out=gs.t[:], in_=g3, axis=AX.X, op=ALU.add), [sel.r], [gs.r])
                dve(lambda: nc.vector.reciprocal(out=gs.t[:], in_=gs.t[:]), [gs.r], [gs.r])
                dve(lambda: nc.vector.tensor_tensor(out=g3, in0=g3, in1=gs.t[:].unsqueeze(2).to_broadcast([128, 8, 16]), op=ALU.mult), [sel.r, gs.r], [sel.r])
                ps = PS[ntile % 2]
                rt_ = rtile[ntile % 2]
                ntile += 1
                for w_ in range(3):
                    pe(lambda: nc.tensor.transpose(out=ps.t[:, w_ * 128:(w_ + 1) * 128], in_=sel.t[:, w_, :], identity=ident.t[:]), [sel.r, ident.r], [ps.r])
                act(lambda: nc.scalar.copy(out=rt_.t[:], in_=ps.t[:, 0:384].rearrange("p (w t) -> p w t", t=128)), [ps.r], [rt_.r])
                c0 = t0 + tt * 128
                sy.dma("sp", rt_d[:, :, c0:c0 + 128], rt_.t[:], reads=[rt_.r], writes=[rt_res])
        sy.barrier()
    tap(f"rt{l}", rt_d, [128, 3, T], F32, [rt_res])
    tap(f"h2T{l}", h2_d, [128, 8, T], BF16, [h2_res])
    if cfg.get("peer_pass1_only"):
        return

    UTs, Vs, tab = dram["UTs_d"][l], dram["Vs_d"][l], dram["tab_res"][l]
    if ctx_out:
        stiles = [(s_ * 384, 384) for s_ in range(6)]
    else:
        stiles = [(s_ * 384, 384) for s_ in range(5)] + [(1920, 128)]
    with contextlib.ExitStack() as s2:
        GW = mk(s2, "GW", [128, 128, 384], BF16)
        PH = mk(s2, "PH", [128, 32768], BF16)
        accO = mk(s2, "accO", [128, 8, 384], F32)
        h2s = mk(s2, "h2s", [128, 8, 384], BF16)
        xp = [mk(s2, "xp", [128, 384], F32) for _ in range(2)]
        UTc = [(PH.t[:, i * 4096:(i + 1) * 4096].rearrange("p (a k i) -> p a k i", a=4, k=8), Res()) for i in range(2)]
        Ab = [(PH.t[:, 8192 + i * 8192:8192 + i * 8192 + 4096].rearrange("p (i t) -> p i t", t=32), Res()) for i in range(3)]
        Bb = [(PH.t[:, 12288 + i * 8192:12288 + i * 8192 + 4096].rearrange("p (i t) -> p i t", t=32), Res()) for i in range(3)]
        Vc = [(PH.t[:, i * 16384:(i + 1) * 16384].rearrange("p (a d) -> p a d", d=1024), Res()) for i in range(2)]
        iotaT = Tile.__new__(Tile)
        iotaT.t = None
        iotaT_ap = PH.t[:, 0:4096].rearrange("p (i t) -> p i t", t=32)
        iotaT_r = UTc[0][1]
        RTb = mk(s2, "RTb", [128, 3, 384], BF16)
        tmpg = [mk(s2, "tmpg", [128, 384], BF16) for _ in range(2)]
        p2 = cfg.get("p2", "SWVR")
        wbanks = (PS[2], PS[3], PS[6], PS[7])
        for (c0, n) in stiles[:cfg.get("p2_ntiles", 99)]:
            sy.dma("sp", h2s.t[:, :, 0:n], h2_d[:, :, c0:c0 + n], reads=[h2_res], writes=[h2s.r])
            sy.dma("pq", RTb.t[:, :, 0:n], rt_d[:, :, c0:c0 + n], reads=[rt_res], writes=[RTb.r])
            dve(lambda: nc.vector.tensor_copy(out=iotaT_ap, in_=iof.t[:, :].unsqueeze(2).to_broadcast([128, 128, 32])), [iof.r], [iotaT_r])
            ngrp = n // 32 if "W" in p2 else 0
            wk = 0
            for gi in range(ngrp):
                (A_, ar), (B_, br) = Ab[gi % 3], Bb[gi % 3]
                tg = gi * 32
                dve(lambda: nc.vector.tensor_tensor(out=A_, in0=iotaT_ap, in1=RTb.t[:, 0, tg:tg + 32].unsqueeze(1).to_broadcast([128, 128, 32]), op=ALU.is_equal), [RTb.r, iotaT_r], [ar])
                dve(lambda: nc.vector.tensor_tensor(out=A_, in0=A_, in1=RTb.t[:, 2, tg:tg + 32].unsqueeze(1).to_broadcast([128, 128, 32]), op=ALU.mult), [RTb.r, ar], [ar])
                dve(lambda: nc.vector.tensor_tensor(out=B_, in0=iotaT_ap, in1=RTb.t[:, 1, tg:tg + 32].unsqueeze(1).to_broadcast([128, 128, 32]), op=ALU.is_equal), [RTb.r, iotaT_r], [br])
                for q_ in range(8):
                    ps = wbanks[wk % 4]
                    wk += 1
                    wdbg = cfg.get("wdbg", "")
                    for tl in range(4):
                        ti = q_ * 4 + tl
                        if "nope" not in wdbg:
                            pe(lambda: nc.tensor.matmul(ps.t[:, 0:512].rearrange("p (i t) -> p t i", t=4)[:, tl, :], A_[:, :, ti], B_[:, :, ti], start=True, stop=True), [ar, br], [ps.r])
                    ta = tg + q_ * 4
                    gwv = GW.t[:, :, ta:ta + 4]
                    if "noact" not in wdbg:
                        act(lambda: nc.scalar.copy(out=gwv, in_=ps.t[:, 0:512].rearrange("p (i t) -> p i t", t=4)), [ps.r], [GW.r])
            for g in (range(32) if "S" in p2 else ()):
                ut, ur = UTc[g % 2]
                sy.dma("sp", ut, UTs[:, g * 4:(g + 1) * 4, :, :], reads=[tab], writes=[ur])
                for a_ in range(4):
                    i2 = g * 4 + a_
                    ps = PS[i2 % 2]
                    tg_ = tmpg[i2 % 2]
                    for kc in range(8):
                        pe(lambda: nc.tensor.matmul(ps.t[:, 0:n], ut[:, a_, kc, :], h2s.t[:, kc, 0:n], start=(kc == 0), stop=(kc == 7)), [ur, h2s.r], [ps.r], final=(kc == 7))
                    act(lambda: nc.scalar.activation(out=tg_.t[:, 0:n], in_=ps.t[:, 0:n], func=AF.Gelu), [ps.r], [tg_.r])
                    dve(lambda: nc.vector.tensor_tensor(out=GW.t[:, i2, 0:n], in0=GW.t[:, i2, 0:n], in1=tg_.t[:, 0:n], op=ALU.mult), [GW.r, tg_.r], [GW.r])
            sy.barrier()
            for blk in (range(8) if "V" in p2 else ()):
                vt_, vr = Vc[blk % 2]
                for hv in range(2):
                    sy.dma("sp", vt_[:, hv * 8:(hv + 1) * 8, :], Vs[:, blk * 16 + hv * 8:blk * 16 + (hv + 1) * 8, :], reads=[tab], writes=[vr])
                for dc in range(8):
                    ps = PS[4 + dc % 2]
                    for ii in range(16):
                        pe(lambda: nc.tensor.matmul(ps.t[:, 0:n], vt_[:, ii, dc * 128:(dc + 1) * 128], GW.t[:, blk * 16 + ii, 0:n], start=(ii == 0), stop=(ii == 15)), [vr, GW.r], [ps.r], final=(ii == 15))
                    if blk == 0:
                        act(lambda: nc.scalar.copy(out=accO.t[:, dc, 0:n], in_=ps.t[:, 0:n]), [ps.r], [accO.r])
                    else:
                        dve(lambda: nc.vector.tensor_tensor(out=accO.t[:, dc, 0:n], in0=ps.t[:, 0:n], in1=accO.t[:, dc, 0:n], op=ALU.add), [ps.r, accO.r], [accO.r])
            xres = _overlap_res(xT_res[b], c0, n)
            if "R" in p2:
                segs = []
                if c0 < L:
                    segs.append((0, min(n, L - c0), b))
                if c0 + n > L:
                    segs.append((max(0, L - c0), n, 4))
                for (a0, a1, j) in segs:
                    dve(lambda: nc.vector.tensor_tensor(out=accO.t[:, :, a0:a1], in0=accO.t[:, :, a0:a1], in1=MOD.t[:, l, 5, :, j:j + 1].to_broadcast([128, 8, a1 - a0]), op=ALU.mult),
                        [accO.r, MOD.r], [accO.r])
                sy.dma("pq", xT_d[b, :, :, c0:c0 + n], accO.t[:, :, 0:n], reads=[accO.r], writes=xres, accum_op=ALU.add)
            sy.barrier()
    tap(f"x{l}", xT_d[b], [128, 8, T], F32, xT_res[b])


def final_phase(state, consts, dram, cfg, nb_run, norm_chunk):
    if cfg.get("skip_final"):
        return
    nc, sy, PS = state["nc"], state["sy"], state["PS"]
    ident = consts["ident"]
    xT_d, xT_res, out_d = dram["xT_d"], dram["xT_res"], dram["out_d"]
    with contextlib.ExitStack() as sf:
        def mk(name, shape, dt):
            _uid[0] += 1
            return Tile(sf.enter_context(nc.sbuf_tensor(f"{name}_{_uid[0]}", list(shape), dt)))
        xt = [mk("xt", [128, 8, 512], F32) for _ in range(2)]
        sq = mk("sq", [128, 8, 512], F32)
        tmp = [mk("tmp", [128, 8, 512], F32) for _ in range(2)]
        rs = [mk("rs", [128, 512], F32) for _ in range(2)]
        ot = [mk("ot", [128, 1024], F32) for _ in range(2)]
        k = 0
        for b in range(nb_run):
            for ci in range(4):
                t0, n = CHUNKS[ci]
                x_t, tm_ = xt[ci % 2], tmp[ci % 2]
                sy.dma("sp", x_t.t[:, :, 0:n], xT_d[b, :, :, t0:t0 + n], reads=[xT_res[b][ci]], writes=[x_t.r])
                norm_chunk(b, 0, ci, 2, x_t, sq, None, None, PS[ci % 2], rs[ci % 2], tm_)
                for tt in range(4):
                    o_ = ot[k % 2]
                    for half in range(2):
                        ps = PS[2 + (2 * k + half) % 4]
                        for c4 in range(4):
                            c = half * 4 + c4
                            sy.op("pe", lambda: nc.tensor.transpose(out=ps.t[:, c4 * 128:(c4 + 1) * 128], in_=tm_.t[:, c, tt * 128:(tt + 1) * 128], identity=ident.t[:]),
                                  reads=[tm_.r, ident.r], writes=[ps.r])
                        if half == 0:
                            sy.op("act", lambda: nc.scalar.copy(out=o_.t[:, 0:512], in_=ps.t[:, 0:512]), reads=[ps.r], writes=[o_.r])
                        else:
                            sy.op("dve", lambda: nc.vector.tensor_copy(out=o_.t[:, 512:1024], in_=ps.t[:, 0:512]), reads=[ps.r], writes=[o_.r])
                    k += 1
                    sy.dma("sp", out_d[b, t0 + tt * 128:t0 + (tt + 1) * 128, :], o_.t[:], reads=[o_.r], writes=[Res()])
        sy.barrier()


_W_NAMES = ["c_ctx", "w_mod", "b_mod", "norm1_g", "norm2_g", "w_in", "w_gate", "b_gate", "attn_sink", "lam_q1", "lam_k1", "lam_q2", "lam_k2",
            "diff_norm_g", "conv_w", "conv_b", "conv_ln_g", "conv_ln_b", "pool_w", "pool_scale", "w_branch", "w_out", "peer_wq", "peer_k1",
            "peer_k2", "peer_u", "peer_v", "final_g"]


def kernel(**inputs):
    n_cores = 8
    nc, _ = build_program({})
    shared = {k: np.ascontiguousarray(np.asarray(inputs[k], dtype=np.float32)) for k in _W_NAMES}
    in_maps = []
    for i in range(n_cores):
        m = dict(shared)
        for k in ("x", "c", "ctx"):
            m[k] = np.ascontiguousarray(np.asarray(inputs[k], dtype=np.float32)[i * NBC:(i + 1) * NBC])
        in_maps.append(m)
    res = run_bass_kernel_spmd(nc, in_maps, core_ids=list(range(n_cores)))
    return np.concatenate([np.asarray(r["out"], dtype=np.float32) for r in res.results], axis=0)
```

```python
import math
import contextlib
import numpy as np
import concourse.bass as bass
import concourse.mybir as mybir
from concourse.bass_utils import run_bass_kernel_spmd

F32 = mybir.dt.float32
BF16 = mybir.dt.bfloat16
I32 = mybir.dt.int32
U32 = mybir.dt.uint32
AF = mybir.ActivationFunctionType
ALU = mybir.AluOpType
AX = mybir.AxisListType

D = 1024
L = 2048
LC = 256
T = L + LC
NL = 4
NBC = 4
EPS = 1e-6
NE = 16384
CHUNKS = [(0, 512), (512, 512), (1024, 512), (1536, 512), (2048, 256)]
SEG = dict(a_q=0, a_k=512, a_v=640, c_q=768, c_k=1280, c_v=1792, b_in=2304, d_in=3328)
POOLW = (2, 4, 8, 16)


class Res:
    __slots__ = ("w", "r")

    def __init__(self):
        self.w = None
        self.r = {}


class SY:
    def __init__(self, nc, es):
        self.nc = nc
        self.E = dict(pe=nc.tensor, act=nc.scalar, dve=nc.vector, pool=nc.gpsimd, sp=nc.sync)
        self.csem = {e: es.enter_context(nc.semaphore("c_" + e)) for e in ("pe", "act", "dve", "pool")}
        self.ccnt = {e: 0 for e in self.csem}
        self.Q = dict(sp="sp", pq="pool", aq="act")
        self.R = dict(sp=8, pq=4, aq=2)
        self.dsem = {q: [es.enter_context(nc.semaphore(f"d_{q}{i}")) for i in range(self.R[q])] for q in self.Q}
        self.dval = {q: [0] * self.R[q] for q in self.Q}
        self.dn = {q: 0 for q in self.Q}
        self.seen = {e: {} for e in self.E}
        self.ninst = 0

    def _wait(self, eng, t):
        sem, val = t[0], t[1]
        k = id(sem)
        if self.seen[eng].get(k, 0) >= val:
            return
        self.seen[eng][k] = val
        self.E[eng].wait_ge(sem, val)

    def _deps(self, eng, is_dma, reads, writes):
        for r in reads:
            if r.w is not None:
                self._wait(eng, r.w)
        for w in writes:
            if w.w is not None and (is_dma or w.w[3] or w.w[2] != eng):
                self._wait(eng, w.w)
            for t in w.r.values():
                if is_dma or t[3] or t[2] != eng:
                    self._wait(eng, t)

    def _commit(self, t, reads, writes):
        for r in reads:
            r.r[id(t[0])] = t
        for w in writes:
            w.w = t
            w.r = {}

    def op(self, eng, fn, reads=(), writes=(), final=True):
        self._deps(eng, False, reads, writes)
        ins = fn()
        if final:
            self.ccnt[eng] += 1
            ins.then_inc(self.csem[eng], 1)
            t = (self.csem[eng], self.ccnt[eng], eng, False)
        else:
            t = (self.csem[eng], self.ccnt[eng] + 1, eng, False)
        self._commit(t, reads, writes)
        self.ninst += 1

    def dma(self, q, out, in_, reads=(), writes=(), **kw):
        eng = self.Q[q]
        self._deps(eng, True, reads, writes)
        i = self.dn[q] % self.R[q]
        self.dn[q] += 1
        sem = self.dsem[q][i]
        if self.dval[q][i] > 0:
            self._wait(eng, (sem, self.dval[q][i]))
        self.dval[q][i] += 16
        self.E[eng].dma_start(out=out, in_=in_, **kw).then_inc(sem, 16)
        self._commit((sem, self.dval[q][i], eng, True), reads, writes)
        self.ninst += 1

    def all_tickets(self):
        ts = [(self.csem[e], self.ccnt[e]) for e in self.csem if self.ccnt[e] > 0]
        for q in self.Q:
            for i in range(self.R[q]):
                if self.dval[q][i] > 0:
                    ts.append((self.dsem[q][i], self.dval[q][i]))
        return ts

    def barrier(self, engs=("pe", "act", "dve", "pool", "sp")):
        ts = self.all_tickets()
        for e in engs:
            for t in ts:
                self._wait(e, t)


class Tile:
    def __init__(self, t, nres=1):
        self.t = t
        self.res = [Res() for _ in range(nres)]

    @property
    def r(self):
        return self.res[0]


def build_program(cfg):
    nb_run = cfg.get("nb", NBC)
    nl_run = cfg.get("nl", NL)
    taps = cfg.get("taps", ())
    do_peer = cfg.get("peer", True)

    NLA = cfg.get("nl_alloc", NL)
    nc = bass.Bass("TRN2", target_bir_lowering=False)
    es = contextlib.ExitStack()
    es.__enter__()
    sy = SY(nc, es)

    def din(name, shape):
        return nc.dram_tensor(name, list(shape), F32, kind="ExternalInput").ap()

    x_in = din("x", [NBC, L, D])
    c_in = din("c", [NBC, D])
    ctx_in = din("ctx", [NBC, LC, D])
    c_ctx = din("c_ctx", [D])
    w_mod = din("w_mod", [NLA, D, 6 * D])
    b_mod = din("b_mod", [NLA, 6 * D])
    norm1_g = din("norm1_g", [NLA, D])
    norm2_g = din("norm2_g", [NLA, D])
    w_in = din("w_in", [NLA, D, 3840])
    w_gate = din("w_gate", [NLA, 4, D, D])
    b_gate = din("b_gate", [NLA, 4, D])
    attn_sink = din("attn_sink", [NLA, 8])
    lam_q1 = din("lam_q1", [NLA, 64])
    lam_k1 = din("lam_k1", [NLA, 64])
    lam_q2 = din("lam_q2", [NLA, 64])
    lam_k2 = din("lam_k2", [NLA, 64])
    diff_norm_g = din("diff_norm_g", [NLA, 128])
    conv_w = din("conv_w", [NLA, 31, 512])
    conv_b = din("conv_b", [NLA, 512])
    conv_ln_g = din("conv_ln_g", [NLA, 512])
    conv_ln_b = din("conv_ln_b", [NLA, 512])
    pool_w = din("pool_w", [NLA, 4, 128, 128])
    pool_scale = din("pool_scale", [NLA, 512])
    w_branch = din("w_branch", [NLA, 4, 512, D])
    w_out = din("w_out", [NLA, D, D])
    peer_wq = din("peer_wq", [NLA, D, 2048])
    peer_k1 = din("peer_k1", [NLA, 8, 128, 128])
    peer_k2 = din("peer_k2", [NLA, 8, 128, 128])
    peer_u = din("peer_u", [NLA, NE, D])
    peer_v = din("peer_v", [NLA, NE, D])
    final_g = din("final_g", [D])
    out_d = nc.dram_tensor("out", [NBC, L, D], F32, kind="ExternalOutput").ap()

    xT_d = nc.dram_tensor("xT_scr", [NBC, 128, 8, T], F32, kind="Internal").ap()
    xT_res = [[Res() for _ in CHUNKS] for _ in range(NBC)]
    yT_d = nc.dram_tensor("yT_scr", [4, 128, 4, T], BF16, kind="Internal").ap()
    yT_res = [[Res() for _ in CHUNKS] for _ in range(4)]
    tap_out = {}

    def sb(name, shape, dt, nres=1):
        return Tile(es.enter_context(nc.sbuf_tensor(name, list(shape), dt)), nres)

    def tap(name, ap, shape, dt, reads):
        if name not in taps:
            return
        d = nc.dram_tensor("tap_" + name, list(shape), dt, kind="ExternalOutput").ap()
        tap_out[name] = d
        r = Res()
        sy.dma("sp", d, ap, reads=reads, writes=[r])
        tap_res.append(r)

    tap_res = []

    PS = [Tile(es.enter_context(nc.psum_tensor(f"ps{i}", [128, 512], F32))) for i in range(8)]

    ident = sb("ident", [128, 128], F32)
    identb = sb("identb", [128, 128], BF16)
    maskGE = sb("maskGE", [128, 128], BF16)
    maskLE = sb("maskLE", [128, 128], BF16)
    onesD = sb("onesD", [128, 128], F32)
    ones5 = sb("ones5", [128, 128], F32)
    ones1 = sb("ones1", [128, 128], F32)
    onesb = sb("onesb", [128, 128], BF16)
    onesDb = sb("onesDb", [128, 128], BF16)
    ones1b = sb("ones1b", [128, 128], BF16)
    iof = sb("iof", [128, 128], F32)
    itmp = sb("itmp", [128, 128], I32)
    sy.op("pool", lambda: nc.gpsimd.iota(itmp.t[:], pattern=[[1, 128]], base=0, channel_multiplier=-1), writes=[itmp.r])
    sy.op("dve", lambda: nc.vector.tensor_single_scalar(out=ident.t[:], in_=itmp.t[:], scalar=0, op=ALU.is_equal), reads=[itmp.r], writes=[ident.r])
    sy.op("dve", lambda: nc.vector.tensor_single_scalar(out=identb.t[:], in_=itmp.t[:], scalar=0, op=ALU.is_equal), reads=[itmp.r], writes=[identb.r])
    sy.op("dve", lambda: nc.vector.tensor_single_scalar(out=maskGE.t[:], in_=itmp.t[:], scalar=0, op=ALU.is_le), reads=[itmp.r], writes=[maskGE.r])
    sy.op("dve", lambda: nc.vector.tensor_single_scalar(out=maskLE.t[:], in_=itmp.t[:], scalar=0, op=ALU.is_ge), reads=[itmp.r], writes=[maskLE.r])
    itmp2 = sb("itmp2", [128, 128], I32)
    sy.op("pool", lambda: nc.gpsimd.iota(itmp2.t[:], pattern=[[1, 128]], base=0, channel_multiplier=0), writes=[itmp2.r])
    sy.op("dve", lambda: nc.vector.tensor_copy(out=iof.t[:], in_=itmp2.t[:]), reads=[itmp2.r], writes=[iof.r])
    sy.op("pool", lambda: nc.gpsimd.memset(onesD.t[:], 1.0 / 1024), writes=[onesD.r])
    sy.op("pool", lambda: nc.gpsimd.memset(ones5.t[:], 1.0 / 512), writes=[ones5.r])
    sy.op("pool", lambda: nc.gpsimd.memset(ones1.t[:], 1.0 / 128), writes=[ones1.r])
    sy.op("pool", lambda: nc.gpsimd.memset(onesb.t[:], 1.0), writes=[onesb.r])
    sy.op("pool", lambda: nc.gpsimd.memset(onesDb.t[:], 1.0 / 1024), writes=[onesDb.r])
    sy.op("pool", lambda: nc.gpsimd.memset(ones1b.t[:], 1.0 / 128), writes=[ones1b.r])

    NVT = NL * 240 + 48
    VT = sb("VT", [128, NVT], F32)
    vt_off = {}

    def vt(name, l=0, i=0):
        return VT.t[:, vt_off[(name, l)] + i: vt_off[(name, l)] + i + 1]

    def vtr(name, l, i0, n):
        return VT.t[:, vt_off[(name, l)] + i0: vt_off[(name, l)] + i0 + n]

    stg = [sb(f"stg{i}", [128, 128], F32) for i in range(2)]
    col = 0
    nstage = 0

    def stage_transpose(items):
        nonlocal col, nstage
        st = stg[nstage % 2]
        ps = PS[nstage % 2]
        nstage += 1
        row = 0
        for (name, l, ap) in items:
            R_ = ap.shape[0]
            sy.dma("sp", st.t[row:row + R_, :], ap, writes=[st.r])
            vt_off[(name, l)] = col + row
            row += R_
        sy.op("pe", lambda: nc.tensor.transpose(out=ps.t[:, 0:row], in_=st.t[0:row, :], identity=ident.t[0:row, 0:row]),
              reads=[st.r, ident.r], writes=[ps.r])
        c0 = col
        sy.op("act", lambda: nc.scalar.copy(out=VT.t[:, c0:c0 + row], in_=ps.t[:, 0:row]), reads=[ps.r], writes=[VT.r])
        col += row

    def v2(ap1d):
        return ap1d.rearrange("(r p) -> r p", p=128)

    for l in range(NLA):
        stage_transpose([
            ("n1g", l, v2(norm1_g[l])), ("n2g", l, v2(norm2_g[l])),
            ("bg", l, b_gate[l].rearrange("n (r p) -> (n r) p", p=128)),
            ("cb", l, v2(conv_b[l])), ("lng", l, v2(conv_ln_g[l])), ("lnb", l, v2(conv_ln_b[l])),
            ("psc", l, v2(pool_scale[l])), ("dg", l, v2(diff_norm_g[l])), ("bmod", l, v2(b_mod[l])),
        ])
        stage_transpose([("cw", l, conv_w[l].rearrange("k (r p) -> (k r) p", p=128))])
    stage_transpose([("fg", 0, v2(final_g)), ("cctx", 0, v2(c_ctx)), ("cb4", 0, c_in.rearrange("b (r p) -> (b r) p", p=128))])
    assert col <= NVT, col

    scT = sb("scT", [128, 8, 5], F32)
    sy.op("act", lambda: nc.scalar.activation(out=scT.t[:, :, 0:4].rearrange("p k j -> p j k"), in_=vtr("cb4", 0, 0, 32).rearrange("p (j k) -> p j k", k=8), func=AF.Silu),
          reads=[VT.r], writes=[scT.r])
    sy.op("act", lambda: nc.scalar.activation(out=scT.t[:, :, 4], in_=vtr("cctx", 0, 0, 8), func=AF.Silu), reads=[VT.r], writes=[scT.r])

    MOD = sb("MOD", [128, NL, 6, 8, 5], F32)
    AM = sb("AM", [128, NL, 2, 8, 5], F32)
    with contextlib.ExitStack() as es0:
        wm = [Tile(es0.enter_context(nc.sbuf_tensor(f"wm{i}", [128, 8, 1024], F32))) for i in range(2)]
        k = 0
        for l in range(nl_run):
            for v in range(6):
                w = wm[k % 2]
                ps = PS[2 + k % 2]
                k += 1
                sy.dma("sp", w.t[:], w_mod[l].rearrange("(kc p) n -> p kc n", p=128)[:, :, v * 1024:(v + 1) * 1024], writes=[w.r])
                for oc in range(8):
                    for kc in range(8):
                        sy.op("pe", lambda: nc.tensor.matmul(ps.t[:, oc * 5:(oc + 1) * 5], w.t[:, kc, oc * 128:(oc + 1) * 128], scT.t[:, kc, :], start=(kc == 0), stop=(kc == 7)),
                              reads=[w.r, scT.r], writes=[ps.r])
                sy.op("dve", lambda: nc.vector.tensor_tensor(out=MOD.t[:, l, v, :, :], in0=ps.t[:, 0:40].rearrange("p (o j) -> p o j", j=5),
                                                             in1=vtr("bmod", l, v * 8, 8).unsqueeze(2).to_broadcast([128, 8, 5]), op=ALU.add),
                      reads=[ps.r, VT.r], writes=[MOD.r])
            for wi, (v, gname) in enumerate(((1, "n1g"), (4, "n2g"))):
                sy.op("dve", lambda: nc.vector.tensor_scalar(out=AM.t[:, l, wi, :, :], in0=MOD.t[:, l, v, :, :], scalar1=1.0, scalar2=None, op0=ALU.add),
                      reads=[MOD.r], writes=[AM.r])
                sy.op("dve", lambda: nc.vector.tensor_tensor(out=AM.t[:, l, wi, :, :], in0=AM.t[:, l, wi, :, :],
                                                             in1=vtr(gname, l, 0, 8).unsqueeze(2).to_broadcast([128, 8, 5]), op=ALU.mult),
                      reads=[AM.r, VT.r], writes=[AM.r])
        sy.barrier()
    tap("MOD", MOD.t[:], [128, NL, 6, 8, 5], F32, [MOD.r])
    tap("AM", AM.t[:], [128, NL, 2, 8, 5], F32, [AM.r])

    with contextlib.ExitStack() as es0:
        xin = [Tile(es0.enter_context(nc.sbuf_tensor(f"xin{i}", [128, D], F32))) for i in range(2)]
        xo = [Tile(es0.enter_context(nc.sbuf_tensor(f"xo{i}", [128, 8, 512], F32))) for i in range(2)]
        k = 0
        for b in range(nb_run):
            for ci, (t0, n) in enumerate(CHUNKS):
                o = xo[ci % 2]
                for tt in range(n // 128):
                    xi = xin[k % 2]
                    src = x_in[b, t0 + tt * 128:t0 + (tt + 1) * 128, :] if t0 < L else ctx_in[b, tt * 128:(tt + 1) * 128, :]
                    sy.dma("sp", xi.t[:], src, writes=[xi.r])
                    for half in range(2):
                        ps = PS[(2 * k + half) % 4]
                        for c4 in range(4):
                            c = half * 4 + c4
                            sy.op("pe", lambda: nc.tensor.transpose(out=ps.t[:, c4 * 128:(c4 + 1) * 128], in_=xi.t[:, c * 128:(c + 1) * 128], identity=ident.t[:]),
                                  reads=[xi.r, ident.r], writes=[ps.r])
                        eng = "act" if half == 0 else "dve"
                        dst = o.t[:, half * 4:(half + 1) * 4, tt * 128:(tt + 1) * 128]
                        srcp = ps.t[:, :].rearrange("p (c t) -> p c t", t=128)
                        if eng == "act":
                            sy.op("act", lambda: nc.scalar.copy(out=dst, in_=srcp), reads=[ps.r], writes=[o.r])
                        else:
                            sy.op("dve", lambda: nc.vector.tensor_copy(out=dst, in_=srcp), reads=[ps.r], writes=[o.r])
                    k += 1
                sy.dma("sp", xT_d[b, :, :, t0:t0 + n], o.t[:, :, 0:n], reads=[o.r], writes=[xT_res[b][ci]])
        sy.barrier()

    cs_d = nc.dram_tensor("cs_scr", [2, 128, T], F32, kind="Internal").ap()
    cs_res = Res()
    with contextlib.ExitStack() as es0:
        cosF = Tile(es0.enter_context(nc.sbuf_tensor("cosF", [128, T], F32)))
        sinS = Tile(es0.enter_context(nc.sbuf_tensor("sinS", [128, T], F32)))
        pidx = Tile(es0.enter_context(nc.sbuf_tensor("pidx", [128, 1], I32)))
        pf = Tile(es0.enter_context(nc.sbuf_tensor("pf", [128, 8], F32)))
        rowt = Tile(es0.enter_context(nc.sbuf_tensor("rowt", [128, L], I32)))
        colt = Tile(es0.enter_context(nc.sbuf_tensor("colt", [128, L], I32)))
        rowf = Tile(es0.enter_context(nc.sbuf_tensor("rowf", [128, L], F32)))
        colf = Tile(es0.enter_context(nc.sbuf_tensor("colf", [128, L], F32)))
        sy.op("pool", lambda: nc.gpsimd.iota(pidx.t[:], pattern=[[0, 1]], base=0, channel_multiplier=1), writes=[pidx.r])
        sy.op("pool", lambda: nc.gpsimd.iota(rowt.t[:], pattern=[[1, 32], [0, 64]], base=0, channel_multiplier=0), writes=[rowt.r])
        sy.op("pool", lambda: nc.gpsimd.iota(colt.t[:], pattern=[[0, 32], [1, 64]], base=0, channel_multiplier=0), writes=[colt.r])
        sy.op("dve", lambda: nc.vector.tensor_copy(out=rowf.t[:], in_=rowt.t[:]), reads=[rowt.r], writes=[rowf.r])
        sy.op("dve", lambda: nc.vector.tensor_copy(out=colf.t[:], in_=colt.t[:]), reads=[colt.r], writes=[colf.r])
        sy.op("dve", lambda: nc.vector.tensor_copy(out=pf.t[:, 0:1], in_=pidx.t[:]), reads=[pidx.r], writes=[pf.r])
        def dv(fn, reads, writes):
            sy.op("dve", fn, reads=reads, writes=writes)
        dv(lambda: nc.vector.tensor_single_scalar(out=pf.t[:, 7:8], in_=pf.t[:, 0:1], scalar=32.0, op=ALU.is_ge), [pf.r], [pf.r])
        dv(lambda: nc.vector.tensor_single_scalar(out=pf.t[:, 6:7], in_=pf.t[:, 0:1], scalar=64.0, op=ALU.is_ge), [pf.r], [pf.r])
        dv(lambda: nc.vector.tensor_tensor(out=pf.t[:, 7:8], in0=pf.t[:, 7:8], in1=pf.t[:, 6:7], op=ALU.add), [pf.r], [pf.r])
        dv(lambda: nc.vector.tensor_single_scalar(out=pf.t[:, 1:2], in_=pf.t[:, 0:1], scalar=96.0, op=ALU.is_ge), [pf.r], [pf.r])
        dv(lambda: nc.vector.tensor_tensor(out=pf.t[:, 7:8], in0=pf.t[:, 7:8], in1=pf.t[:, 1:2], op=ALU.add), [pf.r], [pf.r])
        dv(lambda: nc.vector.scalar_tensor_tensor(out=pf.t[:, 1:2], in0=pf.t[:, 7:8], scalar=-32.0, in1=pf.t[:, 0:1], op0=ALU.mult, op1=ALU.add), [pf.r], [pf.r])
        dv(lambda: nc.vector.tensor_single_scalar(out=pf.t[:, 3:4], in_=pf.t[:, 1:2], scalar=16.0, op=ALU.is_lt), [pf.r], [pf.r])
        dv(lambda: nc.vector.tensor_single_scalar(out=pf.t[:, 7:8], in_=pf.t[:, 1:2], scalar=16.0, op=ALU.is_ge), [pf.r], [pf.r])
        dv(lambda: nc.vector.scalar_tensor_tensor(out=pf.t[:, 2:3], in0=pf.t[:, 7:8], scalar=-16.0, in1=pf.t[:, 1:2], op0=ALU.mult, op1=ALU.add), [pf.r], [pf.r])
        sy.op("act", lambda: nc.scalar.activation(out=pf.t[:, 4:5], in_=pf.t[:, 2:3], func=AF.Exp, scale=-math.log(10000.0) / 16.0), reads=[pf.r], writes=[pf.r])
        dv(lambda: nc.vector.tensor_single_scalar(out=pf.t[:, 5:6], in_=pf.t[:, 0:1], scalar=32.0, op=ALU.is_ge), [pf.r], [pf.r])
        dv(lambda: nc.vector.tensor_single_scalar(out=pf.t[:, 7:8], in_=pf.t[:, 0:1], scalar=64.0, op=ALU.is_ge), [pf.r], [pf.r])
        dv(lambda: nc.vector.tensor_tensor(out=pf.t[:, 5:6], in0=pf.t[:, 5:6], in1=pf.t[:, 7:8], op=ALU.subtract), [pf.r], [pf.r])
        dv(lambda: nc.vector.tensor_single_scalar(out=pf.t[:, 7:8], in_=pf.t[:, 0:1], scalar=96.0, op=ALU.is_ge), [pf.r], [pf.r])
        dv(lambda: nc.vector.tensor_tensor(out=pf.t[:, 5:6], in0=pf.t[:, 5:6], in1=pf.t[:, 7:8], op=ALU.add), [pf.r], [pf.r])
        dv(lambda: nc.vector.tensor_scalar(out=pf.t[:, 5:6], in0=pf.t[:, 5:6], scalar1=2.0, scalar2=-1.0, op0=ALU.mult, op1=ALU.add), [pf.r], [pf.r])
        dv(lambda: nc.vector.tensor_tensor(out=rowf.t[:], in0=rowf.t[:], in1=colf.t[:], op=ALU.subtract), [rowf.r, colf.r], [rowf.r])
        dv(lambda: nc.vector.scalar_tensor_tensor(out=colf.t[:], in0=rowf.t[:], scalar=pf.t[:, 3:4], in1=colf.t[:], op0=ALU.mult, op1=ALU.add), [rowf.r, colf.r, pf.r], [colf.r])
        dv(lambda: nc.vector.tensor_scalar(out=colf.t[:], in0=colf.t[:], scalar1=pf.t[:, 4:5], scalar2=None, op0=ALU.mult), [colf.r, pf.r], [colf.r])

        def reduce_sin(dst, shift):
            dv(lambda: nc.vector.tensor_scalar(out=rowf.t[:], in0=colf.t[:], scalar1=shift, scalar2=1.0 / (2 * math.pi), op0=ALU.add, op1=ALU.mult), [colf.r], [rowf.r])
            dv(lambda: nc.vector.tensor_copy(out=rowt.t[:], in_=rowf.t[:]), [rowf.r], [rowt.r])
            dv(lambda: nc.vector.tensor_copy(out=rowf.t[:], in_=rowt.t[:]), [rowt.r], [rowf.r])
            dv(lambda: nc.vector.scalar_tensor_tensor(out=rowf.t[:], in0=rowf.t[:], scalar=-2 * math.pi, in1=colf.t[:], op0=ALU.mult, op1=ALU.add), [rowf.r, colf.r], [rowf.r])
            dv(lambda: nc.vector.tensor_single_scalar(out=rowf.t[:], in_=rowf.t[:], scalar=shift, op=ALU.add), [rowf.r], [rowf.r])
            dv(lambda: nc.vector.tensor_single_scalar(out=colt.t[:].bitcast(F32), in_=rowf.t[:], scalar=math.pi, op=ALU.is_gt), [rowf.r], [colt.r])
            dv(lambda: nc.vector.scalar_tensor_tensor(out=rowf.t[:], in0=colt.t[:].bitcast(F32), scalar=-2 * math.pi, in1=rowf.t[:], op0=ALU.mult, op1=ALU.add), [rowf.r, colt.r], [rowf.r])
            dv(lambda: nc.vector.tensor_scalar(out=rowf.t[:], in0=rowf.t[:], scalar1=math.pi, scalar2=-math.pi, op0=ALU.min, op1=ALU.max), [rowf.r], [rowf.r])
            sy.op("act", lambda: nc.scalar.activation(out=dst, in_=rowf.t[:], func=AF.Sin), reads=[rowf.r], writes=[sinS.r, cosF.r])

        reduce_sin(sinS.t[:, 0:L], 0.0)
        dv(lambda: nc.vector.tensor_scalar(out=sinS.t[:, 0:L], in0=sinS.t[:, 0:L], scalar1=pf.t[:, 5:6], scalar2=None, op0=ALU.mult), [sinS.r, pf.r], [sinS.r])
        reduce_sin(cosF.t[:, 0:L], 0.5 * math.pi)
        sy.op("pool", lambda: nc.gpsimd.memset(cosF.t[:, L:T], 1.0), writes=[cosF.r])
        sy.op("pool", lambda: nc.gpsimd.memset(sinS.t[:, L:T], 0.0), writes=[sinS.r])
        sy.dma("sp", cs_d[0], cosF.t[:], reads=[cosF.r], writes=[cs_res])
        sy.dma("sp", cs_d[1], sinS.t[:], reads=[sinS.r], writes=[cs_res])
        tap("cosF", cosF.t[:], [128, T], F32, [cosF.r])
        tap("sinS", sinS.t[:], [128, T], F32, [sinS.r])
        sy.barrier()

    skc = sb("skc", [1, NLA * 8], F32)
    sinkl = sb("sinkl", [128, 2, 128], BF16)
    nlam = sb("nlam", [128, NLA], F32)
    gsc = sb("gsc", [128, NLA], F32)
    with contextlib.ExitStack() as es0:
        sk = Tile(es0.enter_context(nc.sbuf_tensor("sk", [1, NLA * 8], F32)))
        lq = Tile(es0.enter_context(nc.sbuf_tensor("lq", [128, 4, NLA, 64], F32)))
        ls = Tile(es0.enter_context(nc.sbuf_tensor("ls", [128, 2, NLA], F32)))
        sy.dma("sp", sk.t[:], attn_sink.rearrange("l h -> (l h)").unsqueeze(0), writes=[sk.r])
        sy.op("act", lambda: nc.scalar.activation(out=skc.t[:], in_=sk.t[:], func=AF.Exp), reads=[sk.r], writes=[skc.r])
        sy.op("pool", lambda: nc.gpsimd.memset(sinkl.t[:], 0.0), writes=[sinkl.r])
        sy.op("pool", lambda: nc.gpsimd.memset(sinkl.t[0:1, 0, 64:128], 1.0), writes=[sinkl.r])
        sy.op("pool", lambda: nc.gpsimd.memset(sinkl.t[0:1, 1, 0:64], 1.0), writes=[sinkl.r])
        for i, a in enumerate((lam_q1, lam_k1, lam_q2, lam_k2)):
            sy.dma("sp", lq.t[:, i, :, :], a.unsqueeze(0).to_broadcast([128, NLA, 64]), writes=[lq.r])
        for i in range(2):
            sy.op("dve", lambda: nc.vector.tensor_tensor(out=lq.t[:, 2 * i, :, :], in0=lq.t[:, 2 * i, :, :], in1=lq.t[:, 2 * i + 1, :, :], op=ALU.mult), reads=[lq.r], writes=[lq.r])
            sy.op("dve", lambda: nc.vector.tensor_reduce(out=ls.t[:, i, :], in_=lq.t[:, 2 * i, :, :], axis=AX.X, op=ALU.add), reads=[lq.r], writes=[ls.r])
        sy.op("act", lambda: nc.scalar.activation(out=ls.t[:], in_=ls.t[:], func=AF.Exp), reads=[ls.r], writes=[ls.r])
        sy.op("dve", lambda: nc.vector.tensor_tensor(out=nlam.t[:], in0=ls.t[:, 1, :], in1=ls.t[:, 0, :], op=ALU.subtract), reads=[ls.r], writes=[nlam.r])
        for l in range(NLA):
            li = 0.8 - 0.6 * math.exp(-0.3 * l)
            sy.op("dve", lambda: nc.vector.tensor_single_scalar(out=nlam.t[:, l:l + 1], in_=nlam.t[:, l:l + 1], scalar=-li, op=ALU.add), reads=[nlam.r], writes=[nlam.r])
            sy.op("dve", lambda: nc.vector.tensor_single_scalar(out=gsc.t[:, l:l + 1], in_=vt("dg", l), scalar=1.0 - li, op=ALU.mult), reads=[VT.r], writes=[gsc.r])
        sy.barrier()
    tap("nlam", nlam.t[:], [128, NLA], F32, [nlam.r])

    UTs_d = nc.dram_tensor("UTs_scr", [NLA, 128, 128, 8, 128], BF16, kind="Internal").ap()
    Vs_d = nc.dram_tensor("Vs_scr", [NLA, 128, 128, 1024], BF16, kind="Internal").ap()
    h2_d = nc.dram_tensor("h2_scr", [128, 8, T], BF16, kind="Internal").ap()
    rt_d = nc.dram_tensor("rt_scr", [128, 3, T], F32, kind="Internal").ap()
    tab_res = [Res() for _ in range(NLA)]
    if do_peer:
        with contextlib.ExitStack() as es0:
            ub = [Tile(es0.enter_context(nc.sbuf_tensor(f"ub{i}", [128, 1024], BF16))) for i in range(3)]
            uo = [Tile(es0.enter_context(nc.sbuf_tensor(f"uo{i}", [128, 8, 128], BF16))) for i in range(3)]
            vb = [Tile(es0.enter_context(nc.sbuf_tensor(f"vb{i}", [128, 8, 1024], BF16))) for i in range(2)]
            for l in range(nl_run):
                uv = peer_u[l].rearrange("(i1 i2) d -> i2 i1 d", i2=128)
                vv = peer_v[l].rearrange("(i1 i2) d -> i1 i2 d", i2=128)
                for i2 in range(128):
                    u_, o_ = ub[i2 % 3], uo[i2 % 3]
                    ps = PS[i2 % 4]
                    psb = ps.t[:].bitcast(BF16)
                    sy.dma("pq", u_.t[:], uv[i2], writes=[u_.r])
                    for kc in range(8):
                        sy.op("pe", lambda: nc.tensor.transpose(out=psb[:, kc * 128:(kc + 1) * 128], in_=u_.t[:, kc * 128:(kc + 1) * 128], identity=identb.t[:]),
                              reads=[u_.r, identb.r], writes=[ps.r])
                    if i2 % 2 == 0:
                        sy.op("act", lambda: nc.scalar.copy(out=o_.t[:].rearrange("p k i -> p (k i)"), in_=psb[:, 0:1024]), reads=[ps.r], writes=[o_.r])
                    else:
                        sy.op("dve", lambda: nc.vector.tensor_copy(out=o_.t[:].rearrange("p k i -> p (k i)"), in_=psb[:, 0:1024]), reads=[ps.r], writes=[o_.r])
                    sy.dma("sp", UTs_d[l, :, i2, :, :], o_.t[:], reads=[o_.r], writes=[tab_res[l]])
                    if i2 % 8 == 7:
                        g = i2 // 8
                        v_ = vb[g % 2]
                        sy.dma("pq", v_.t[:], vv[:, g * 8:(g + 1) * 8, :], writes=[v_.r])
                        sy.dma("sp", Vs_d[l, :, g * 8:(g + 1) * 8, :], v_.t[:], reads=[v_.r], writes=[tab_res[l]])
            sy.barrier()

    wq_toggle = [0]

    def wload(dst_ap, src_ap, tile):
        sy.dma("pq", dst_ap, src_ap, writes=[tile.r])

    def norm_chunk(b, l, ci, which, x_t, sq_t, dst_fn, dst_res, ps, rs_t, tmp_t):
        t0, n = CHUNKS[ci]
        j = b if t0 < L else 4
        sy.op("act", lambda: nc.scalar.activation(out=sq_t.t[:, :, 0:n], in_=x_t.t[:, :, 0:n], func=AF.Square), reads=[x_t.r], writes=[sq_t.r])
        for c in range(8):
            sy.op("pe", lambda: nc.tensor.matmul(ps.t[:, 0:n], onesDb.t[:], sq_t.t[:, c, 0:n], start=(c == 0), stop=(c == 7)), reads=[sq_t.r, onesDb.r], writes=[ps.r], final=(c == 7))
        sy.op("act", lambda: nc.scalar.activation(out=rs_t.t[:, 0:n], in_=ps.t[:, 0:n], func=AF.Sqrt, bias=EPS, scale=1.0), reads=[ps.r], writes=[rs_t.r])
        sy.op("dve", lambda: nc.vector.reciprocal(out=rs_t.t[:, 0:n], in_=rs_t.t[:, 0:n]), reads=[rs_t.r], writes=[rs_t.r])
        for c in range(8):
            if which < 2:
                a_ap = AM.t[:, l, which, c, j:j + 1]
                s_ap = MOD.t[:, l, 0 if which == 0 else 3, c, j:j + 1]
            else:
                a_ap = vt("fg", 0, c)
                s_ap = None
            sy.op("dve", lambda: nc.vector.scalar_tensor_tensor(out=tmp_t.t[:, c, 0:n], in0=x_t.t[:, c, 0:n], scalar=a_ap, in1=rs_t.t[:, 0:n], op0=ALU.mult, op1=ALU.mult),
                  reads=[x_t.r, rs_t.r, AM.r, VT.r], writes=[tmp_t.r])
            if s_ap is not None:
                sy.op("act", lambda: nc.scalar.activation(out=dst_fn(c), in_=tmp_t.t[:, c, 0:n], func=AF.Identity, bias=s_ap, scale=1.0), reads=[tmp_t.r, MOD.r], writes=[dst_res])

    state = dict(nc=nc, sy=sy, es=es, PS=PS, sb=sb, tap=tap, tap_out=tap_out, tap_res=tap_res)
    consts = dict(ident=ident, identb=identb, maskGE=maskGE, maskLE=maskLE, onesD=onesD, ones5=ones5, ones1=ones1, onesb=onesb, ones1b=ones1b, iof=iof,
                  VT=VT, vt=vt, vtr=vtr, MOD=MOD, AM=AM, cs_d=cs_d, cs_res=cs_res, skc=skc, sinkl=sinkl, nlam=nlam, gsc=gsc)
    dram = dict(w_in=w_in, w_gate=w_gate, w_branch=w_branch, w_out=w_out, pool_w=pool_w, peer_wq=peer_wq, peer_k1=peer_k1, peer_k2=peer_k2,
                peer_u=peer_u, peer_v=peer_v, xT_d=xT_d, xT_res=xT_res, out_d=out_d, yT_d=yT_d, yT_res=yT_res,
                UTs_d=UTs_d, Vs_d=Vs_d, h2_d=h2_d, rt_d=rt_d, tab_res=tab_res)

    from_mixer = mixer_phase
    for l in range(nl_run):
        for b in range(nb_run):
            from_mixer(state, consts, dram, cfg, b, l, norm_chunk, wload)
            if do_peer and not cfg.get("skip_peer_phase"):
                peer_phase(state, consts, dram, cfg, b, l, norm_chunk, wload)

    final_phase(state, consts, dram, cfg, nb_run, norm_chunk)
    sy.barrier(engs=("sp",))
    es.close()
    return nc, tap_out


_uid = [0]


def mixer_phase(state, consts, dram, cfg, b, l, norm_chunk, wload):
    if cfg.get("skip_mixer"):
        return
    nc, sy, PS, tap = state["nc"], state["sy"], state["PS"], state["tap"]
    C = consts
    vt, vtr, MOD = C["vt"], C["vtr"], C["MOD"]
    xT_d, xT_res = dram["xT_d"], dram["xT_res"]
    ctx_out = l < NL - 1
    qchunks = list(range(5)) if ctx_out else list(range(4))
    wv = dram["w_in"][l].rearrange("(kc p) n -> p kc n", p=128)
    branches = cfg.get("branches", "ACBD")

    def mk(scope, name, shape, dt, nres=1):
        _uid[0] += 1
        return Tile(scope.enter_context(nc.sbuf_tensor(f"{name}_{_uid[0]}", list(shape), dt)), nres)

    def dve(fn, reads, writes):
        sy.op("dve", fn, reads=reads, writes=writes)

    def act(fn, reads, writes):
        sy.op("act", fn, reads=reads, writes=writes)

    def pool(fn, reads, writes):
        sy.op("pool", fn, reads=reads, writes=writes)

    def pe(fn, reads, writes, final=True):
        sy.op("pe", fn, reads=reads, writes=writes, final=final)

    with contextlib.ExitStack() as ms:
        hT = mk(ms, "hT", [128, 8, T], BF16, nres=5)
        with contextlib.ExitStack() as s1:
            xt = [mk(s1, "xt", [128, 8, 512], F32) for _ in range(2)]
            sq = mk(s1, "sq", [128, 8, 512], BF16)
            tmp = mk(s1, "tmp", [128, 8, 512], F32)
            rs = [mk(s1, "rs", [128, 512], F32) for _ in range(2)]
            for ci, (t0, n) in enumerate(CHUNKS):
                x_t = xt[ci % 2]
                sy.dma("sp", x_t.t[:, :, 0:n], xT_d[b, :, :, t0:t0 + n], reads=[xT_res[b][ci]], writes=[x_t.r])
                norm_chunk(b, l, ci, 0, x_t, sq, (lambda c, t0=t0, n=n: hT.t[:, c, t0:t0 + n]), hT.res[ci], PS[ci % 2], rs[ci % 2], tmp)
            sy.barrier()
        tap(f"hT{l}", hT.t[:], [128, 8, T], BF16, hT.res)

        yT_d, yT_res = dram["yT_d"], dram["yT_res"]
        ystg = [mk(ms, "ystg", [128, 512], BF16) for _ in range(3)]
        acs = contextlib.ExitStack()
        cosF = mk(acs, "cosF", [128, T], F32)
        sinS = mk(acs, "sinS", [128, T], F32)
        sy.dma("sp", cosF.t[:], C["cs_d"][0], reads=[C["cs_res"]], writes=[cosF.r])
        sy.dma("sp", sinS.t[:], C["cs_d"][1], reads=[C["cs_res"]], writes=[sinS.r])
        ycnt = [0]

        def y_out(nbr, kc, ci, p0, p1, fn_write):
            t0, n = CHUNKS[ci]
            st = ystg[ycnt[0] % 3]
            ycnt[0] += 1
            fn_write(st.t[p0:p1, 0:n], st.r)
            sy.dma("sp", yT_d[nbr, p0:p1, kc, t0:t0 + n], st.t[p0:p1, 0:n], reads=[st.r], writes=[yT_res[nbr][ci]])

        def proj(ps, Wt, c0, ci, ncols=128):
            t0, n = CHUNKS[ci]
            for kc in range(8):
                pe(lambda: nc.tensor.matmul(ps.t[0:ncols, 0:n], Wt.t[:, kc, c0:c0 + ncols], hT.t[:, kc, t0:t0 + n], start=(kc == 0), stop=(kc == 7)),
                   [Wt.r, hT.res[ci]], [ps.r], final=(kc == 7))

        def load_rot(Wr, seg0, ncols):
            dv_ = Wr.t[:, :, 0:ncols].rearrange("p k (h two j) -> p k h two j", two=2, j=32)
            sv_ = wv[:, :, seg0:seg0 + ncols].rearrange("p k (h two j) -> p k h two j", two=2, j=32)
            if cfg.get("fake_rot"):
                sy.dma("pq", Wr.t[:, :, 0:ncols], wv[:, :, seg0:seg0 + ncols], writes=[Wr.r])
                return
            for kc in range(8):
                sy.dma("pq", dv_[:, kc, :, 0, :], sv_[:, kc, :, 1, :], writes=[Wr.r])
                sy.dma("pq", dv_[:, kc, :, 1, :], sv_[:, kc, :, 0, :], writes=[Wr.r])

        def rope_evac(psA, psB, ci, dst_ap, dst_res, t1, t2, splits=None):
            t0, n = CHUNKS[ci]
            dve(lambda: nc.vector.tensor_tensor(out=t1.t[:, 0:n], in0=psA.t[:, 0:n], in1=cosF.t[:, t0:t0 + n], op=ALU.mult), [psA.r, cosF.r], [t1.r])
            dve(lambda: nc.vector.tensor_tensor(out=t2.t[:, 0:n], in0=psB.t[:, 0:n], in1=sinS.t[:, t0:t0 + n], op=ALU.mult), [psB.r, sinS.r], [t2.r])
            if splits is None:
                dve(lambda: nc.vector.tensor_tensor(out=dst_ap, in0=t1.t[:, 0:n], in1=t2.t[:, 0:n], op=ALU.add), [t1.r, t2.r], [dst_res])
            else:
                for (q0_, q1_, dap) in splits:
                    dve(lambda: nc.vector.tensor_tensor(out=dap, in0=t1.t[q0_:q1_, 0:n], in1=t2.t[q0_:q1_, 0:n], op=ALU.add), [t1.r, t2.r], [dst_res])

        if "A" in branches:
            with contextlib.ExitStack() as sa:
                qaT = mk(sa, "qaT", [128, 4, T], BF16)
                kaz = mk(sa, "kaz", [128, 2, 2, T], BF16)
                va = mk(sa, "va", [128, 18, 2, 2, 128], BF16)
                sap = contextlib.ExitStack()
                Waq = mk(sap, "Waq", [128, 8, 512], BF16)
                WaqR = mk(sap, "WaqR", [128, 8, 512], BF16)
                Wak = mk(sap, "Wak", [128, 8, 256], BF16)
                WakR = mk(sap, "WakR", [128, 8, 256], BF16)
                Wav = mk(sap, "Wav", [128, 8, 128], BF16)
                t1 = [mk(sap, "t1", [128, 512], F32) for _ in range(2)]
                t2 = [mk(sap, "t2", [128, 512], F32) for _ in range(2)]
                pool(lambda: nc.gpsimd.memset(kaz.t[64:128, :, 0, :], 0.0), [], [kaz.r])
                pool(lambda: nc.gpsimd.memset(kaz.t[0:64, :, 1, :], 0.0), [], [kaz.r])
                wload(Waq.t[:], wv[:, :, 0:512], Waq)
                load_rot(WaqR, 0, 512)
                for kvh in range(2):
                    for dup in range(2):
                        c0 = (kvh * 2 + dup) * 64
                        sy.dma("pq", Wak.t[:, :, c0:c0 + 64], wv[:, :, 512 + kvh * 64:512 + (kvh + 1) * 64], writes=[Wak.r])
                        for hf in range(2):
                            sy.dma("pq", WakR.t[:, :, c0 + hf * 32:c0 + (hf + 1) * 32],
                                   wv[:, :, 512 + kvh * 64 + (1 - hf) * 32:512 + kvh * 64 + (2 - hf) * 32], writes=[WakR.r])
                wload(Wav.t[:], wv[:, :, 640:768], Wav)
                pool(lambda: nc.gpsimd.memset(va.t[:, :, :, 0, 64:128], 1.0), [], [va.r])
                pool(lambda: nc.gpsimd.memset(va.t[:, :, :, 1, 0:64], 1.0), [], [va.r])
                k = 0
                for ci in range(5):
                    t0, n = CHUNKS[ci]
                    for hc in range(4):
                        if ci == 4 and not ctx_out:
                            continue
                        pa, pb = PS[(2 * k) % 4], PS[(2 * k + 1) % 4]
                        proj(pa, Waq, hc * 128, ci)
                        proj(pb, WaqR, hc * 128, ci)
                        rope_evac(pa, pb, ci, qaT.t[:, hc, t0:t0 + n], qaT.r, t1[k % 2], t2[k % 2])
                        k += 1
                    for kc2 in range(2):
                        pa, pb = PS[(2 * k) % 4], PS[(2 * k + 1) % 4]
                        proj(pa, Wak, kc2 * 128, ci)
                        proj(pb, WakR, kc2 * 128, ci)
                        rope_evac(pa, pb, ci, None, kaz.r, t1[k % 2], t2[k % 2],
                                  splits=[(0, 64, kaz.t[0:64, kc2, 0, t0:t0 + n]), (64, 128, kaz.t[64:128, kc2, 1, t0:t0 + n])])
                        k += 1
                for tt in range(18):
                    ps = PS[4 + tt % 2]
                    ci = min(tt // 4, 4)
                    for kc in range(8):
                        pe(lambda: nc.tensor.matmul(ps.t[:, 0:128], hT.t[:, kc, tt * 128:(tt + 1) * 128], Wav.t[:, kc, :], start=(kc == 0), stop=(kc == 7)),
                           [Wav.r, hT.res[ci]], [ps.r], final=(kc == 7))
                    src = ps.t[:, 0:128].rearrange("p (k d) -> p k d", d=64)
                    act(lambda: nc.scalar.copy(out=va.t[:, tt, :, 0, 0:64], in_=src), [ps.r], [va.r])
                    dve(lambda: nc.vector.tensor_copy(out=va.t[:, tt, :, 1, 64:128], in_=src), [ps.r], [va.r])
                sy.barrier()
                sap.close()
                E = [mk(sa, "E", [128, 16, 384], BF16) for _ in range(2)]
                Ec = [mk(sa, "Ec", [128, 2, T], BF16) for _ in range(2)]
                rden = [mk(sa, "rden", [128, 512], F32) for _ in range(2)]
                maskGE, maskLE = C["maskGE"], C["maskLE"]
                sinkl, skc = C["sinkl"], C["skc"]
                esrow = mk(sa, "esrow", [128, 8, 512], BF16)
                pool(lambda: nc.gpsimd.memset(esrow.t[:], 0.0), [], [esrow.r])
                dve(lambda: nc.vector.tensor_copy(out=esrow.t[0:1, :, :], in_=skc.t[:, l * 8:(l + 1) * 8].unsqueeze(2).to_broadcast([1, 8, 512])), [skc.r], [esrow.r])
                kk = 0
                for h in range(8):
                    kvh, par, hc = h // 4, h % 2, h // 2
                    p0 = 64 * par
                    q0 = 64 - p0
                    Eh, Ech = E[h % 2], Ec[h % 2]
                    for j in range(16):
                        qlo, qhi = max(0, j - 1) * 128, min(16, j + 2) * 128
                        n = qhi - qlo
                        ps = PS[kk % 2]
                        kk += 1
                        pe(lambda: nc.tensor.matmul(ps.t[:, 0:n], kaz.t[:, kvh, par, j * 128:(j + 1) * 128], qaT.t[:, hc, qlo:qhi], start=True, stop=True),
                           [kaz.r, qaT.r], [ps.r])
                        act(lambda: nc.scalar.activation(out=Eh.t[:, j, 0:n], in_=ps.t[:, 0:n], func=AF.Exp, scale=0.125), [ps.r], [Eh.r])
                        if j >= 1:
                            dve(lambda: nc.vector.tensor_tensor(out=Eh.t[:, j, 0:128], in0=Eh.t[:, j, 0:128], in1=maskLE.t[:], op=ALU.mult), [Eh.r, maskLE.r], [Eh.r])
                        if j <= 14:
                            off = (j + 1) * 128 - qlo
                            dve(lambda: nc.vector.tensor_tensor(out=Eh.t[:, j, off:off + 128], in0=Eh.t[:, j, off:off + 128], in1=maskGE.t[:], op=ALU.mult), [Eh.r, maskGE.r], [Eh.r])
                    for jc in range(2):
                        for ci in qchunks:
                            t0, n = CHUNKS[ci]
                            ps = PS[kk % 2]
                            kk += 1
                            pe(lambda: nc.tensor.matmul(ps.t[:, 0:n], kaz.t[:, kvh, par, L + jc * 128:L + (jc + 1) * 128], qaT.t[:, hc, t0:t0 + n], start=True, stop=True),
                               [kaz.r, qaT.r], [ps.r])
                            act(lambda: nc.scalar.activation(out=Ech.t[:, jc, t0:t0 + n], in_=ps.t[:, 0:n], func=AF.Exp, scale=0.125), [ps.r], [Ech.r])
                    for ci in qchunks:
                        t0, n = CHUNKS[ci]
                        pso = PS[2 + kk % 2]
                        rd = rden[kk % 2]
                        kk += 1
                        mm = [(slice(0, n), sinkl.t[:, par, :], esrow.t[:, h, 0:n], [sinkl.r, esrow.r])]
                        for jc in range(2):
                            mm.append((slice(0, n), va.t[:, 16 + jc, kvh, par, :], Ech.t[:, jc, t0:t0 + n], [va.r, Ech.r]))
                        if ci < 4:
                            for j in range(4 * ci - 1, 4 * ci + 5):
                                if 0 <= j < 16:
                                    bl_lo, bl_hi = max(4 * ci, j - 1), min(4 * ci + 3, j + 1)
                                    sub0, nsub = (bl_lo - 4 * ci) * 128, (bl_hi - bl_lo + 1) * 128
                                    eoff = bl_lo * 128 - max(0, j - 1) * 128
                                    mm.append((slice(sub0, sub0 + nsub), va.t[:, j, kvh, par, :], Eh.t[:, j, eoff:eoff + nsub], [va.r, Eh.r]))
                        for i, (sl, lt, rh, rd_) in enumerate(mm):
                            pe(lambda: nc.tensor.matmul(pso.t[:, sl], lt, rh, start=(i == 0), stop=(i == len(mm) - 1)), rd_, [pso.r], final=(i == len(mm) - 1))
                        dve(lambda: nc.vector.reciprocal(out=rd.t[p0:p0 + 64, 0:n], in_=pso.t[q0:q0 + 64, 0:n]), [pso.r], [rd.r])
                        y_out(0, hc, ci, p0, p0 + 64, lambda ap, r_: dve(lambda: nc.vector.tensor_tensor(out=ap, in0=pso.t[p0:p0 + 64, 0:n], in1=rd.t[p0:p0 + 64, 0:n], op=ALU.mult),
                                                                        [pso.r, rd.r], [r_]))
                sy.barrier()
            tap(f"yA{l}", yT_d[0], [128, 4, T], BF16, yT_res[0])

        if "C" in branches:
            with contextlib.ExitStack() as sc:
                qcT = mk(sc, "qcT", [128, 4, T], BF16)
                kcz = mk(sc, "kcz", [128, 2, 4, T], BF16)
                vc = mk(sc, "vc", [128, 18, 512], BF16)
                scp = contextlib.ExitStack()
                Wcq = mk(scp, "Wcq", [128, 8, 512], BF16)
                WcqR = mk(scp, "WcqR", [128, 8, 512], BF16)
                Wck = mk(scp, "Wck", [128, 8, 512], BF16)
                WckR = mk(scp, "WckR", [128, 8, 512], BF16)
                Wcv = mk(scp, "Wcv", [128, 8, 512], BF16)
                t1 = [mk(scp, "t1", [128, 512], F32) for _ in range(2)]
                t2 = [mk(scp, "t2", [128, 512], F32) for _ in range(2)]
                pool(lambda: nc.gpsimd.memset(kcz.t[64:128, 0, :, :], 0.0), [], [kcz.r])
                pool(lambda: nc.gpsimd.memset(kcz.t[0:64, 1, :, :], 0.0), [], [kcz.r])
                wload(Wcq.t[:], wv[:, :, 768:1280], Wcq)
                load_rot(WcqR, 768, 512)
                wload(Wck.t[:], wv[:, :, 1280:1792], Wck)
                load_rot(WckR, 1280, 512)
                wload(Wcv.t[:], wv[:, :, 1792:2304], Wcv)
                k = 0
                for ci in range(5):
                    t0, n = CHUNKS[ci]
                    for hc in range(4):
                        for (W_, WR_, dstT) in ((Wcq, WcqR, qcT), (Wck, WckR, kcz)):
                            if dstT is qcT and ci == 4 and not ctx_out:
                                continue
                            pa, pb = PS[(2 * k) % 4], PS[(2 * k + 1) % 4]
                            proj(pa, W_, hc * 128, ci)
                            proj(pb, WR_, hc * 128, ci)
                            if dstT is qcT:
                                rope_evac(pa, pb, ci, qcT.t[:, hc, t0:t0 + n], qcT.r, t1[k % 2], t2[k % 2])
                            else:
                                rope_evac(pa, pb, ci, None, kcz.r, t1[k % 2], t2[k % 2],
                                          splits=[(0, 64, kcz.t[0:64, 0, hc, t0:t0 + n]), (64, 128, kcz.t[64:128, 1, hc, t0:t0 + n])])
                            k += 1
                for tt in range(18):
                    ps = PS[4 + tt % 2]
                    ci = min(tt // 4, 4)
                    for kc in range(8):
                        pe(lambda: nc.tensor.matmul(ps.t[:, 0:512], hT.t[:, kc, tt * 128:(tt + 1) * 128], Wcv.t[:, kc, :], start=(kc == 0), stop=(kc == 7)),
                           [Wcv.r, hT.res[ci]], [ps.r], final=(kc == 7))
                    if tt % 2 == 0:
                        act(lambda: nc.scalar.copy(out=vc.t[:, tt, :], in_=ps.t[:, 0:512]), [ps.r], [vc.r])
                    else:
                        dve(lambda: nc.vector.tensor_copy(out=vc.t[:, tt, :], in_=ps.t[:, 0:512]), [ps.r], [vc.r])
                sy.barrier()
                scp.close()
                Et = [mk(sc, "Et", [128, 512], BF16) for _ in range(3)]
                rd = [mk(sc, "rdc", [128, 512], F32) for _ in range(2)]
                tu = [mk(sc, "tu", [128, 512], F32) for _ in range(2)]
                ot = mk(sc, "ot", [128, 512], F32)
                sqo = mk(sc, "sqo", [128, 512], BF16)
                rso = mk(sc, "rso", [128, 512], F32)
                onesb, ones1, nlam, gsc = C["onesb"], C["ones1b"], C["nlam"], C["gsc"]
                kk = 0
                for h in range(4):
                    for ci in qchunks:
                        t0, n = CHUNKS[ci]
                        keyt = list(range(18)) if ci < 4 else [16, 17]
                        items = [(c, ji, j) for c in range(2) for ji, j in enumerate(keyt)]
                        SK = 2
                        scb = (PS[0], PS[1], PS[6])
                        pend = []

                        def emit_score(idx_):
                            c, ji, j = items[idx_]
                            p0 = 64 * c
                            ps = scb[(kk0 + idx_) % 3]
                            et = Et[(kk0 + idx_) % 3]
                            pe(lambda: nc.tensor.matmul(ps.t[:, 0:n], kcz.t[:, c, h, j * 128:(j + 1) * 128], qcT.t[:, h, t0:t0 + n], start=True, stop=True),
                               [kcz.r, qcT.r], [ps.r])
                            act(lambda: nc.scalar.activation(out=et.t[:, 0:n], in_=ps.t[:, 0:n], func=AF.Exp, scale=0.125), [ps.r], [et.r])

                        def emit_pv(idx_):
                            c, ji, j = items[idx_]
                            et = Et[(kk0 + idx_) % 3]
                            psU, psD = PS[2 + c], PS[4 + c]
                            pe(lambda: nc.tensor.matmul(psU.t[:, 0:n], vc.t[:, j, h * 128:(h + 1) * 128], et.t[:, 0:n], start=(ji == 0), stop=(ji == len(keyt) - 1)),
                               [vc.r, et.r], [psU.r], final=(ji == len(keyt) - 1))
                            pe(lambda: nc.tensor.matmul(psD.t[:, 0:n], onesb.t[:], et.t[:, 0:n], start=(ji == 0), stop=(ji == len(keyt) - 1)),
                               [onesb.r, et.r], [psD.r], final=(ji == len(keyt) - 1))

                        kk0 = kk
                        for idx_ in range(len(items) + SK):
                            if idx_ < len(items):
                                emit_score(idx_)
                            if idx_ >= SK:
                                emit_pv(idx_ - SK)
                        kk += len(items)
                        for c in range(2):
                            dve(lambda: nc.vector.reciprocal(out=rd[c].t[:, 0:n], in_=PS[4 + c].t[:, 0:n]), [PS[4 + c].r], [rd[c].r])
                            dve(lambda: nc.vector.tensor_tensor(out=tu[c].t[:, 0:n], in0=PS[2 + c].t[:, 0:n], in1=rd[c].t[:, 0:n], op=ALU.mult), [PS[2 + c].r, rd[c].r], [tu[c].r])
                        dve(lambda: nc.vector.scalar_tensor_tensor(out=ot.t[:, 0:n], in0=tu[1].t[:, 0:n], scalar=nlam.t[:, l:l + 1], in1=tu[0].t[:, 0:n], op0=ALU.mult, op1=ALU.add),
                            [tu[0].r, tu[1].r, nlam.r], [ot.r])
                        act(lambda: nc.scalar.activation(out=sqo.t[:, 0:n], in_=ot.t[:, 0:n], func=AF.Square), [ot.r], [sqo.r])
                        psn = PS[7]
                        pe(lambda: nc.tensor.matmul(psn.t[:, 0:n], ones1.t[:], sqo.t[:, 0:n], start=True, stop=True), [ones1.r, sqo.r], [psn.r])
                        act(lambda: nc.scalar.activation(out=rso.t[:, 0:n], in_=psn.t[:, 0:n], func=AF.Sqrt, bias=EPS, scale=1.0), [psn.r], [rso.r])
                        dve(lambda: nc.vector.reciprocal(out=rso.t[:, 0:n], in_=rso.t[:, 0:n]), [rso.r], [rso.r])
                        y_out(1, h, ci, 0, 128, lambda ap, r_: dve(lambda: nc.vector.scalar_tensor_tensor(out=ap, in0=ot.t[:, 0:n], scalar=gsc.t[:, l:l + 1], in1=rso.t[:, 0:n], op0=ALU.mult, op1=ALU.mult),
                                                                  [ot.r, rso.r, gsc.r], [r_]))
                sy.barrier()
            tap(f"yC{l}", yT_d[1], [128, 4, T], BF16, yT_res[1])

        sy.barrier()
        acs.close()
        if "B" in branches:
            with contextlib.ExitStack() as sb_:
                YW = 15 + L + 15 + 15 + LC + 15
                offs = (15, 15 + L + 15 + 15)
                Wb = mk(sb_, "Wb", [128, 8, 1024], BF16)
                ypad = mk(sb_, "ypad", [128, 4, YW], BF16)
                Dm = mk(sb_, "Dm", [128, 124, 128], BF16)
                z = mk(sb_, "z", [128, 4, T], F32)
                sg = [mk(sb_, "sg", [128, 512], F32) for _ in range(2)]
                wload(Wb.t[:], wv[:, :, 2304:3328], Wb)
                pool(lambda: nc.gpsimd.memset(ypad.t[:], 0.0), [], [ypad.r])
                identb = C["identb"]
                for i in range(124):
                    fn = (lambda: nc.vector.tensor_scalar(out=Dm.t[:, i, :], in0=identb.t[:], scalar1=vt("cw", l, i), scalar2=None, op0=ALU.mult))
                    dve(fn, [identb.r, C["VT"].r], [Dm.r])
                bchunks = qchunks
                k = 0
                for cc in range(4):
                    for ci in bchunks:
                        t0, n = CHUNKS[ci]
                        pa, pg = PS[(2 * k) % 4], PS[(2 * k + 1) % 4]
                        s_ = sg[k % 2]
                        k += 1
                        proj(pa, Wb, cc * 128, ci)
                        proj(pg, Wb, 512 + cc * 128, ci)
                        act(lambda: nc.scalar.activation(out=s_.t[:, 0:n], in_=pg.t[:, 0:n], func=AF.Sigmoid), [pg.r], [s_.r])
                        yo = offs[0] + t0 if ci < 4 else offs[1]
                        dve(lambda: nc.vector.tensor_tensor(out=ypad.t[:, cc, yo:yo + n], in0=pa.t[:, 0:n], in1=s_.t[:, 0:n], op=ALU.mult), [pa.r, s_.r], [ypad.r])
                k = 0
                for cc in range(4):
                    for ci in bchunks:
                        t0, n = CHUNKS[ci]
                        ps = PS[4 + k % 2]
                        k += 1
                        base = (t0 if ci < 4 else offs[1] - 15)
                        for kk_ in range(31):
                            pe(lambda: nc.tensor.matmul(ps.t[:, 0:n], Dm.t[:, kk_ * 4 + cc, :], ypad.t[:, cc, base + kk_:base + kk_ + n], start=(kk_ == 0), stop=(kk_ == 30)),
                               [Dm.r, ypad.r], [ps.r], final=(kk_ == 30))
                        act(lambda: nc.scalar.activation(out=z.t[:, cc, t0:t0 + n], in_=ps.t[:, 0:n], func=AF.Identity, bias=vt("cb", l, cc), scale=1.0), [ps.r, C["VT"].r], [z.r])
                ones5 = C["ones5"]
                zsq = mk(sb_, "zsq", [128, 4, 512], F32)
                m2 = mk(sb_, "m2", [128, 512], F32)
                var = mk(sb_, "var", [128, 512], F32)
                tz = [mk(sb_, "tz", [128, 512], F32) for _ in range(2)]
                for ci in bchunks:
                    t0, n = CHUNKS[ci]
                    psm, psq = PS[6], PS[7]
                    act(lambda: nc.scalar.activation(out=zsq.t[:, :, 0:n], in_=z.t[:, :, t0:t0 + n], func=AF.Square), [z.r], [zsq.r])
                    for cc in range(4):
                        pe(lambda: nc.tensor.matmul(psm.t[:, 0:n], ones5.t[:], z.t[:, cc, t0:t0 + n], start=(cc == 0), stop=(cc == 3)), [ones5.r, z.r], [psm.r], final=(cc == 3))
                    for cc in range(4):
                        pe(lambda: nc.tensor.matmul(psq.t[:, 0:n], ones5.t[:], zsq.t[:, cc, 0:n], start=(cc == 0), stop=(cc == 3)), [ones5.r, zsq.r], [psq.r], final=(cc == 3))
                    act(lambda: nc.scalar.activation(out=m2.t[:, 0:n], in_=psm.t[:, 0:n], func=AF.Square), [psm.r], [m2.r])
                    dve(lambda: nc.vector.tensor_tensor(out=var.t[:, 0:n], in0=psq.t[:, 0:n], in1=m2.t[:, 0:n], op=ALU.subtract), [psq.r, m2.r], [var.r])
                    dve(lambda: nc.vector.tensor_scalar_max(out=var.t[:, 0:n], in0=var.t[:, 0:n], scalar1=0.0), [var.r], [var.r])
                    act(lambda: nc.scalar.activation(out=var.t[:, 0:n], in_=var.t[:, 0:n], func=AF.Sqrt, bias=EPS, scale=1.0), [var.r], [var.r])
                    dve(lambda: nc.vector.reciprocal(out=var.t[:, 0:n], in_=var.t[:, 0:n]), [var.r], [var.r])
                    for cc in range(4):
                        tz_ = tz[cc % 2]
                        dve(lambda: nc.vector.tensor_tensor(out=tz_.t[:, 0:n], in0=z.t[:, cc, t0:t0 + n], in1=psm.t[:, 0:n], op=ALU.subtract), [z.r, psm.r], [tz_.r])
                        dve(lambda: nc.vector.tensor_tensor(out=tz_.t[:, 0:n], in0=tz_.t[:, 0:n], in1=var.t[:, 0:n], op=ALU.mult), [tz_.r, var.r], [tz_.r])
                        y_out(2, cc, ci, 0, 128, lambda ap, r_: act(lambda: nc.scalar.activation(out=ap, in_=tz_.t[:, 0:n], func=AF.Silu, bias=vt("lnb", l, cc), scale=vt("lng", l, cc)),
                                                                   [tz_.r, C["VT"].r], [r_]))
                sy.barrier()
            tap(f"yB{l}", yT_d[2], [128, 4, T], BF16, yT_res[2])

        if "D" in branches:
            with contextlib.ExitStack() as sd:
                PW = 24 + L + 24 + LC + 24
                poff = (24, 24 + L + 24)
                Wd = mk(sd, "Wd", [128, 8, 512], BF16)
                Wp = mk(sd, "Wp", [128, 4, 128], BF16)
                bufs = [mk(sd, f"pb{i}", [128, PW], F32) for i in range(5)]
                yq = mk(sd, "yq", [128, T], BF16)
                edge = mk(sd, "edge", [128, 64], F32)
                etmp = mk(sd, "etmp", [128, 16], F32)
                wload(Wd.t[:], wv[:, :, 3328:3840], Wd)
                wload(Wp.t[:], dram["pool_w"][l].rearrange("g c e -> c g e"), Wp)
                for bf in bufs:
                    pool(lambda: nc.gpsimd.memset(bf.t[:], 0.0), [], [bf.r])
                bchunks = qchunks
                k = 0
                for g in range(4):
                    win = POOLW[g]
                    lo, hi = win // 2, win - 1 - win // 2
                    xb = bufs[0]
                    for ci in bchunks:
                        t0, n = CHUNKS[ci]
                        ps = PS[k % 2]
                        k += 1
                        proj(ps, Wd, g * 128, ci)
                        xo_ = poff[0] + t0 if ci < 4 else poff[1]
                        act(lambda: nc.scalar.copy(out=xb.t[:, xo_:xo_ + n], in_=ps.t[:, 0:n]), [ps.r], [xb.r])
                    nlev = g + 1
                    for lv in range(nlev):
                        w_ = 1 << lv
                        src, dst = bufs[lv], bufs[lv + 1]
                        pool(lambda: nc.gpsimd.tensor_tensor(out=dst.t[:, w_:PW], in0=src.t[:, w_:PW], in1=src.t[:, 0:PW - w_], op=ALU.add), [src.r], [dst.r])
                    ws = bufs[nlev]
                    segs = [(poff[0], L, 0)] + ([(poff[1], LC, L)] if ctx_out else [])
                    for (so, sl, yo) in segs:
                        dve(lambda: nc.vector.scalar_tensor_tensor(out=yq.t[:, yo:yo + sl], in0=ws.t[:, so + hi:so + hi + sl], scalar=1.0 / win, in1=xb.t[:, so:so + sl], op0=ALU.mult, op1=ALU.subtract),
                            [ws.r, xb.r], [yq.r])
                        ecols = [(t, t + hi + 1) for t in range(lo)] + [(sl - 1 - i, lo + 1 + i) for i in range(hi)]
                        for (t, cnt) in ecols:
                            dve(lambda: nc.vector.scalar_tensor_tensor(out=yq.t[:, yo + t:yo + t + 1], in0=ws.t[:, so + hi + t:so + hi + t + 1], scalar=1.0 / cnt, in1=xb.t[:, so + t:so + t + 1], op0=ALU.mult, op1=ALU.subtract),
                                [ws.r, xb.r], [yq.r])
                    for ci in bchunks:
                        t0, n = CHUNKS[ci]
                        ps = PS[2 + k % 2]
                        k += 1
                        pe(lambda: nc.tensor.matmul(ps.t[:, 0:n], Wp.t[:, g, :], yq.t[:, t0:t0 + n], start=True, stop=True), [Wp.r, yq.r], [ps.r])
                        y_out(3, g, ci, 0, 128, lambda ap, r_: act(lambda: nc.scalar.activation(out=ap, in_=ps.t[:, 0:n], func=AF.Copy, scale=vt("psc", l, g)), [ps.r, C["VT"].r], [r_]))
                sy.barrier()
            tap(f"yD{l}", yT_d[3], [128, 4, T], BF16, yT_res[3])

        if cfg.get("merge", True):
            with contextlib.ExitStack() as sm:
                accT = mk(sm, "accT", [128, 8, T], BF16)
                with contextlib.ExitStack() as sm1:
                    Wg = [mk(sm1, "Wg", [128, 4, 8, 256], BF16) for _ in range(2)]
                    Wbr = [mk(sm1, "Wbr", [128, 4, 4, 256], BF16) for _ in range(2)]
                    sgm = [mk(sm1, "sgm", [128, 512], F32) for _ in range(2)]
                    tm = [mk(sm1, "tm", [128, 512], F32) for _ in range(2)]
                    acc = mk(sm1, "acc", [128, 512], F32)
                    ych = [mk(sm1, "ych", [128, 4, 4, 512], BF16) for _ in range(2)]
                    wgv = dram["w_gate"][l].rearrange("n (kc p) e -> p n kc e", p=128)
                    wbv = dram["w_branch"][l].rearrange("n (kc p) e -> p n kc e", p=128)
                    border = (0, 2, 1, 3)
                    border = (0, 1, 2, 3)
                    k = 0
                    for ecp in range(4):
                        wg_, wb_ = Wg[ecp % 2], Wbr[ecp % 2]
                        for n_ in range(4):
                            sy.dma("pq", wg_.t[:, n_, :, :], wgv[:, n_, :, ecp * 256:(ecp + 1) * 256], writes=[wg_.r])
                            sy.dma("pq", wb_.t[:, n_, :, :], wbv[:, n_, :, ecp * 256:(ecp + 1) * 256], writes=[wb_.r])
                        for ci in qchunks:
                            t0, n = CHUNKS[ci]
                            yc_ = ych[(ecp * 5 + ci) % 2]
                            for i_ in range(4):
                                sy.dma("sp", yc_.t[:, i_, :, 0:n], yT_d[i_, :, :, t0:t0 + n], reads=[yT_res[i_][ci]], writes=[yc_.r])
                            for e2 in range(2):
                                ec = ecp * 2 + e2
                                es_ = slice(e2 * 128, (e2 + 1) * 128)
                                for n_ in range(4):
                                    pg, pb_ = PS[(2 * k) % 4], PS[(2 * k + 1) % 4]
                                    s_ = sgm[k % 2]
                                    t_ = tm[k % 2]
                                    k += 1
                                    for kc in range(8):
                                        pe(lambda: nc.tensor.matmul(pg.t[:, 0:n], wg_.t[:, n_, kc, es_], hT.t[:, kc, t0:t0 + n], start=(kc == 0), stop=(kc == 7)), [wg_.r, hT.res[ci]], [pg.r], final=(kc == 7))
                                    for kc in range(4):
                                        pe(lambda: nc.tensor.matmul(pb_.t[:, 0:n], wb_.t[:, n_, kc, es_], yc_.t[:, n_, kc, 0:n], start=(kc == 0), stop=(kc == 3)), [wb_.r, yc_.r], [pb_.r], final=(kc == 3))
                                    act(lambda: nc.scalar.activation(out=s_.t[:, 0:n], in_=pg.t[:, 0:n], func=AF.Sigmoid, bias=vt("bg", l, n_ * 8 + ec), scale=1.0), [pg.r, C["VT"].r], [s_.r])
                                    if n_ == 0:
                                        dve(lambda: nc.vector.tensor_tensor(out=acc.t[:, 0:n], in0=pb_.t[:, 0:n], in1=s_.t[:, 0:n], op=ALU.mult), [pb_.r, s_.r], [acc.r])
                                    else:
                                        dve(lambda: nc.vector.tensor_tensor(out=t_.t[:, 0:n], in0=pb_.t[:, 0:n], in1=s_.t[:, 0:n], op=ALU.mult), [pb_.r, s_.r], [t_.r])
                                        if n_ < 3:
                                            dve(lambda: nc.vector.tensor_tensor(out=acc.t[:, 0:n], in0=acc.t[:, 0:n], in1=t_.t[:, 0:n], op=ALU.add), [acc.r, t_.r], [acc.r])
                                        else:
                                            dve(lambda: nc.vector.tensor_tensor(out=accT.t[:, ec, t0:t0 + n], in0=acc.t[:, 0:n], in1=t_.t[:, 0:n], op=ALU.add), [acc.r, t_.r], [accT.r])
                    sy.barrier()
                tap(f"accT{l}", accT.t[:], [128, 8, T], BF16, [accT.r])
                Wo = mk(sm, "Wo", [128, 8, 8, 128], BF16)
                xt = [mk(sm, "xtm", [128, 8, 512], F32) for _ in range(2)]
                wov = dram["w_out"][l].rearrange("(kc p) (oc e) -> p oc kc e", p=128, e=128)
                for oc in range(8):
                    sy.dma("pq", Wo.t[:, oc, :, :], wov[:, oc, :, :], writes=[Wo.r])
                for ci in qchunks:
                    t0, n = CHUNKS[ci]
                    j = b if ci < 4 else 4
                    x_t = xt[ci % 2]
                    sy.dma("sp", x_t.t[:, :, 0:n], xT_d[b, :, :, t0:t0 + n], reads=[xT_res[b][ci]], writes=[x_t.r])
                    for oc in range(8):
                        ps = PS[4 + oc % 4]
                        for kc in range(8):
                            pe(lambda: nc.tensor.matmul(ps.t[:, 0:n], Wo.t[:, oc, kc, :], accT.t[:, kc, t0:t0 + n], start=(kc == 0), stop=(kc == 7)), [Wo.r, accT.r], [ps.r], final=(kc == 7))
                        dve(lambda: nc.vector.scalar_tensor_tensor(out=x_t.t[:, oc, 0:n], in0=ps.t[:, 0:n], scalar=MOD.t[:, l, 2, oc, j:j + 1], in1=x_t.t[:, oc, 0:n], op0=ALU.mult, op1=ALU.add),
                            [ps.r, x_t.r, MOD.r], [x_t.r])
                    sy.dma("sp", xT_d[b, :, :, t0:t0 + n], x_t.t[:, :, 0:n], reads=[x_t.r], writes=[xT_res[b][ci]])
                sy.barrier()
        tap(f"xmix{l}", xT_d[b], [128, 8, T], F32, xT_res[b])
        sy.barrier()


def _overlap_res(xT_res_b, c0, n):
    out = []
    for ci, (t0, nn) in enumerate(CHUNKS):
        if t0 < c0 + n and c0 < t0 + nn:
            out.append(xT_res_b[ci])
    return out


def peer_phase(state, consts, dram, cfg, b, l, norm_chunk, wload):
    nc, sy, PS, tap = state["nc"], state["sy"], state["PS"], state["tap"]
    C = consts
    vt, MOD, iof, ident, identb = C["vt"], C["MOD"], C["iof"], C["ident"], C["identb"]
    xT_d, xT_res = dram["xT_d"], dram["xT_res"]
    h2_d, rt_d = dram["h2_d"], dram["rt_d"]
    ctx_out = l < NL - 1
    chunks = list(range(5)) if ctx_out else list(range(4))
    h2_res, rt_res = Res(), Res()

    def mk(scope, name, shape, dt, nres=1):
        _uid[0] += 1
        return Tile(scope.enter_context(nc.sbuf_tensor(f"{name}_{_uid[0]}", list(shape), dt)), nres)

    def dve(fn, reads, writes):
        sy.op("dve", fn, reads=reads, writes=writes)

    def act(fn, reads, writes):
        sy.op("act", fn, reads=reads, writes=writes)

    def pool(fn, reads, writes):
        sy.op("pool", fn, reads=reads, writes=writes)

    def pe(fn, reads, writes, final=True):
        sy.op("pe", fn, reads=reads, writes=writes, final=final)

    with contextlib.ExitStack() as s1:
        Wq = mk(s1, "Wq", [128, 8, 2048], BF16)
        kst = mk(s1, "kst", [128, 16, 128], BF16)
        kT = mk(s1, "kT", [128, 16, 128], BF16)
        xt = [mk(s1, "xt", [128, 8, 512], F32) for _ in range(2)]
        sq = mk(s1, "sq", [128, 8, 512], BF16)
        tmp = mk(s1, "tmp", [128, 8, 512], F32)
        rs = [mk(s1, "rs", [128, 512], F32) for _ in range(2)]
        h2c = [mk(s1, "h2c", [128, 8, 512], BF16) for _ in range(2)]
        qT = mk(s1, "qT", [128, 16, 512], BF16)
        sc = mk(s1, "sc", [128, 16, 128], F32)
        scw = mk(s1, "scw", [128, 16, 128], F32, nres=16)
        v16 = mk(s1, "v16", [128, 16, 16], F32, nres=16)
        i16 = mk(s1, "i16", [128, 16, 16], U32, nres=16)
        i16f = mk(s1, "i16f", [128, 16, 16], F32)
        cand = mk(s1, "cand", [128, 8, 16, 16], F32)
        candw = mk(s1, "candw", [128, 8, 256], F32, nres=8)
        c16 = mk(s1, "c16", [128, 8, 16], F32, nres=8)
        ci16 = mk(s1, "ci16", [128, 8, 16], U32, nres=8)
        iab = mk(s1, "iab", [128, 2, 8, 16], U32)
        fab = mk(s1, "fab", [128, 2, 8, 16], F32)
        oh = mk(s1, "oh", [128, 8, 16, 16], F32)
        sel = mk(s1, "sel", [128, 3, 128], F32)
        gs = mk(s1, "gs", [128, 8], F32)
        rtile = [mk(s1, "rtile", [128, 3, 128], F32) for _ in range(2)]
        wload(Wq.t[:], dram["peer_wq"][l].rearrange("(kc p) n -> p kc n", p=128), Wq)
        for half, kd in enumerate((dram["peer_k1"], dram["peer_k2"])):
            sy.dma("pq", kst.t[:].rearrange("n (h two) d -> n h two d", two=2)[:, :, half, :], kd[l].rearrange("h n d -> n h d"), writes=[kst.r])
        for blk in range(16):
            ps = PS[blk % 2]
            psb = ps.t[:].bitcast(BF16)
            pe(lambda: nc.tensor.transpose(out=psb[:, 0:128], in_=kst.t[:, blk, :], identity=identb.t[:]), [kst.r, identb.r], [ps.r])
            act(lambda: nc.scalar.copy(out=kT.t[:, blk, :], in_=psb[:, 0:128]), [ps.r], [kT.r])
        ntile = 0
        for ci in chunks:
            t0, n = CHUNKS[ci]
            x_t = xt[ci % 2]
            h2 = h2c[ci % 2]
            sy.dma("sp", x_t.t[:, :, 0:n], xT_d[b, :, :, t0:t0 + n], reads=[xT_res[b][ci]], writes=[x_t.r])
            norm_chunk(b, l, ci, 1, x_t, sq, (lambda c, h2=h2, n=n: h2.t[:, c, 0:n]), h2.r, PS[ci % 2], rs[ci % 2], tmp)
            sy.dma("sp", h2_d[:, :, t0:t0 + n], h2.t[:, :, 0:n], reads=[h2.r], writes=[h2_res])
            for blk in range(16):
                ps = PS[2 + blk % 2]
                for kc in range(8):
                    pe(lambda: nc.tensor.matmul(ps.t[:, 0:n], Wq.t[:, kc, blk * 128:(blk + 1) * 128], h2.t[:, kc, 0:n], start=(kc == 0), stop=(kc == 7)), [Wq.r, h2.r], [ps.r], final=(kc == 7))
                if blk % 2 == 0:
                    act(lambda: nc.scalar.copy(out=qT.t[:, blk, 0:n], in_=ps.t[:, 0:n]), [ps.r], [qT.r])
                else:
                    dve(lambda: nc.vector.tensor_copy(out=qT.t[:, blk, 0:n], in_=ps.t[:, 0:n]), [ps.r], [qT.r])
            for tt in range(n // 128):
                for q4 in range(4):
                    ps = PS[4 + q4]
                    for bi in range(4):
                        blk = q4 * 4 + bi
                        pe(lambda: nc.tensor.matmul(ps.t[:, bi * 128:(bi + 1) * 128], qT.t[:, blk, tt * 128:(tt + 1) * 128], kT.t[:, blk, :], start=True, stop=True), [qT.r, kT.r], [ps.r])
                    src = ps.t[:, :].rearrange("p (k n) -> p k n", n=128)
                    if q4 % 2 == 0:
                        act(lambda: nc.scalar.copy(out=sc.t[:, q4 * 4:(q4 + 1) * 4, :], in_=src), [ps.r], [sc.r])
                    else:
                        dve(lambda: nc.vector.tensor_copy(out=sc.t[:, q4 * 4:(q4 + 1) * 4, :], in_=src), [ps.r], [sc.r])
                R16 = range(16)
                for blk in R16:
                    dve(lambda: nc.vector.max(out=v16.t[:, blk, 0:8], in_=sc.t[:, blk, :]), [sc.r], [v16.res[blk]])
                for blk in R16:
                    dve(lambda: nc.vector.max_index(out=i16.t[:, blk, 0:8], in_max=v16.t[:, blk, 0:8], in_values=sc.t[:, blk, :]), [sc.r, v16.res[blk]], [i16.res[blk]])
                for blk in R16:
                    dve(lambda: nc.vector.match_replace(out=scw.t[:, blk, :], in_to_replace=v16.t[:, blk, 0:8], in_values=sc.t[:, blk, :], imm_value=-1e30), [sc.r, v16.res[blk]], [scw.res[blk]])
                for blk in R16:
                    dve(lambda: nc.vector.max(out=v16.t[:, blk, 8:16], in_=scw.t[:, blk, :]), [scw.res[blk]], [v16.res[blk]])
                for blk in R16:
                    dve(lambda: nc.vector.max_index(out=i16.t[:, blk, 8:16], in_max=v16.t[:, blk, 8:16], in_values=scw.t[:, blk, :]), [scw.res[blk], v16.res[blk]], [i16.res[blk]])
                dve(lambda: nc.vector.tensor_copy(out=i16f.t[:], in_=i16.t[:]), i16.res, [i16f.r])
                v16v = v16.t[:].rearrange("p (h two) k -> p h two k", two=2)
                i16v = i16f.t[:].rearrange("p (h two) k -> p h two k", two=2)
                dve(lambda: nc.vector.tensor_tensor(out=cand.t[:], in0=v16v[:, :, 0, :].unsqueeze(3).to_broadcast([128, 8, 16, 16]),
                                                    in1=v16v[:, :, 1, :].unsqueeze(2).to_broadcast([128, 8, 16, 16]), op=ALU.add), v16.res, [cand.r])
                candf = cand.t[:].rearrange("p h a b -> p h (a b)")
                R8 = range(8)
                for h in R8:
                    dve(lambda: nc.vector.max(out=c16.t[:, h, 0:8], in_=candf[:, h, :]), [cand.r], [c16.res[h]])
                for h in R8:
                    dve(lambda: nc.vector.max_index(out=ci16.t[:, h, 0:8], in_max=c16.t[:, h, 0:8], in_values=candf[:, h, :]), [cand.r, c16.res[h]], [ci16.res[h]])
                for h in R8:
                    dve(lambda: nc.vector.match_replace(out=candw.t[:, h, :], in_to_replace=c16.t[:, h, 0:8], in_values=candf[:, h, :], imm_value=-1e30), [cand.r, c16.res[h]], [candw.res[h]])
                for h in R8:
                    dve(lambda: nc.vector.max(out=c16.t[:, h, 8:16], in_=candw.t[:, h, :]), [candw.res[h]], [c16.res[h]])
                for h in R8:
                    dve(lambda: nc.vector.max_index(out=ci16.t[:, h, 8:16], in_max=c16.t[:, h, 8:16], in_values=candw.t[:, h, :]), [candw.res[h], c16.res[h]], [ci16.res[h]])
                dve(lambda: nc.vector.tensor_single_scalar(out=iab.t[:, 0, :, :], in_=ci16.t[:], scalar=4, op=ALU.arith_shift_right), ci16.res, [iab.r])
                dve(lambda: nc.vector.tensor_single_scalar(out=iab.t[:, 1, :, :], in_=ci16.t[:], scalar=15, op=ALU.bitwise_and), ci16.res, [iab.r])
                dve(lambda: nc.vector.tensor_copy(out=fab.t[:], in_=iab.t[:]), [iab.r], [fab.r])
                io16 = iof.t[:, 0:16].unsqueeze(1).unsqueeze(1).to_broadcast([128, 8, 16, 16])
                for w_ in range(2):
                    dve(lambda: nc.vector.tensor_tensor(out=oh.t[:], in0=io16, in1=fab.t[:, w_, :, :].unsqueeze(3).to_broadcast([128, 8, 16, 16]), op=ALU.is_equal), [fab.r, iof.r], [oh.r])
                    dve(lambda: nc.vector.tensor_tensor(out=oh.t[:], in0=oh.t[:], in1=i16v[:, :, w_, :].unsqueeze(2).to_broadcast([128, 8, 16, 16]), op=ALU.mult), [oh.r, i16f.r], [oh.r])
                    dve(lambda: nc.vector.tensor_reduce(out=sel.t[:, w_, :].rearrange("p (h k) -> p h k", k=16), in_=oh.t[:], axis=AX.X, op=ALU.add), [oh.r], [sel.r])
                g3 = sel.t[:, 2, :].rearrange("p (h k) -> p h k", k=16)
                dve(lambda: nc.vector.tensor_tensor(out=g3, in0=c16.t[:], in1=c16.t[:, :, 0:1].to_broadcast([128, 8, 16]), op=ALU.subtract), c16.res, [sel.r])
                act(lambda: nc.scalar.activation(out=g3, in_=g3, func=AF.Exp), [sel.r], [sel.r])
                dve(lambda: nc.vector.tensor_reduce(out=gs.t[:], in_=g3, axis=AX.X, op=ALU.add), [sel.r], [gs.r])
                dve(lambda: nc.vector.reciprocal(out=gs.t[:], in_=gs.t[:]), [gs.r], [gs.r])
                dve(lambda: nc.vector.tensor_tensor(out=g3, in0=g3, in1=gs.t[:].unsqueeze(2).to_broadcast([128, 8, 16]), op=ALU.mult), [sel.r, gs.r], [sel.r])
                ps = PS[ntile % 2]
                rt_ = rtile[ntile % 2]
                ntile += 1
                for w_ in range(3):
                    pe(lambda: nc.tensor.transpose(out=ps.t[:, w_ * 128:(w_ + 1) * 128], in_=sel.t[:, w_, :], identity=ident.t[:]), [sel.r, ident.r], [ps.r])
                act(lambda: nc.scalar.copy(out=rt_.t[:], in_=ps.t[:, 0:384].rearrange("p (w t) -> p w t", t=128)), [ps.r], [rt_.r])
                c0 = t0 + tt * 128
                sy.dma("sp", rt_d[:, :, c0:c0 + 128], rt_.t[:], reads=[rt_.r], writes=[rt_res])
        sy.barrier()
    tap(f"rt{l}", rt_d, [128, 3, T], F32, [rt_res])
    tap(f"h2T{l}", h2_d, [128, 8, T], BF16, [h2_res])
    if cfg.get("peer_pass1_only"):
        return

    UTs, Vs, tab = dram["UTs_d"][l], dram["Vs_d"][l], dram["tab_res"][l]
    if ctx_out:
        stiles = [(s_ * 384, 384) for s_ in range(6)]
    else:
        stiles = [(s_ * 384, 384) for s_ in range(5)] + [(1920, 128)]
    with contextlib.ExitStack() as s2:
        GW = mk(s2, "GW", [128, 128, 384], BF16)
        PH = mk(s2, "PH", [128, 32768], BF16)
        accO = mk(s2, "accO", [128, 8, 384], F32)
        h2s = mk(s2, "h2s", [128, 8, 384], BF16)
        xp = [mk(s2, "xp", [128, 384], F32) for _ in range(2)]
        UTc = [(PH.t[:, i * 4096:(i + 1) * 4096].rearrange("p (a k i) -> p a k i", a=4, k=8), Res()) for i in range(2)]
        Ab = [(PH.t[:, 8192 + i * 8192:8192 + i * 8192 + 4096].rearrange("p (i t) -> p i t", t=32), Res()) for i in range(3)]
        Bb = [(PH.t[:, 12288 + i * 8192:12288 + i * 8192 + 4096].rearrange("p (i t) -> p i t", t=32), Res()) for i in range(3)]
        Vc = [(PH.t[:, i * 16384:(i + 1) * 16384].rearrange("p (a d) -> p a d", d=1024), Res()) for i in range(2)]
        iotaT = Tile.__new__(Tile)
        iotaT.t = None
        iotaT_ap = PH.t[:, 0:4096].rearrange("p (i t) -> p i t", t=32)
        iotaT_r = UTc[0][1]
        RTb = mk(s2, "RTb", [128, 3, 384], BF16)
        tmpg = [mk(s2, "tmpg", [128, 384], BF16) for _ in range(2)]
        p2 = cfg.get("p2", "SWVR")
        wbanks = (PS[2], PS[3], PS[6], PS[7])
        for (c0, n) in stiles[:cfg.get("p2_ntiles", 99)]:
            sy.dma("sp", h2s.t[:, :, 0:n], h2_d[:, :, c0:c0 + n], reads=[h2_res], writes=[h2s.r])
            sy.dma("pq", RTb.t[:, :, 0:n], rt_d[:, :, c0:c0 + n], reads=[rt_res], writes=[RTb.r])
            dve(lambda: nc.vector.tensor_copy(out=iotaT_ap, in_=iof.t[:, :].unsqueeze(2).to_broadcast([128, 128, 32])), [iof.r], [iotaT_r])
            ngrp = n // 32 if "W" in p2 else 0
            wk = 0
            for gi in range(ngrp):
                (A_, ar), (B_, br) = Ab[gi % 3], Bb[gi % 3]
                tg = gi * 32
                dve(lambda: nc.vector.tensor_tensor(out=A_, in0=iotaT_ap, in1=RTb.t[:, 0, tg:tg + 32].unsqueeze(1).to_broadcast([128, 128, 32]), op=ALU.is_equal), [RTb.r, iotaT_r], [ar])
                dve(lambda: nc.vector.tensor_tensor(out=A_, in0=A_, in1=RTb.t[:, 2, tg:tg + 32].unsqueeze(1).to_broadcast([128, 128, 32]), op=ALU.mult), [RTb.r, ar], [ar])
                dve(lambda: nc.vector.tensor_tensor(out=B_, in0=iotaT_ap, in1=RTb.t[:, 1, tg:tg + 32].unsqueeze(1).to_broadcast([128, 128, 32]), op=ALU.is_equal), [RTb.r, iotaT_r], [br])
                for q_ in range(8):
                    ps = wbanks[wk % 4]
                    wk += 1
                    wdbg = cfg.get("wdbg", "")
                    for tl in range(4):
                        ti = q_ * 4 + tl
                        if "nope" not in wdbg:
                            pe(lambda: nc.tensor.matmul(ps.t[:, 0:512].rearrange("p (i t) -> p t i", t=4)[:, tl, :], A_[:, :, ti], B_[:, :, ti], start=True, stop=True), [ar, br], [ps.r])
                    ta = tg + q_ * 4
                    gwv = GW.t[:, :, ta:ta + 4]
                    if "noact" not in wdbg:
                        act(lambda: nc.scalar.copy(out=gwv, in_=ps.t[:, 0:512].rearrange("p (i t) -> p i t", t=4)), [ps.r], [GW.r])
            for g in (range(32) if "S" in p2 else ()):
                ut, ur = UTc[g % 2]
                sy.dma("sp", ut, UTs[:, g * 4:(g + 1) * 4, :, :], reads=[tab], writes=[ur])
                for a_ in range(4):
                    i2 = g * 4 + a_
                    ps = PS[i2 % 2]
                    tg_ = tmpg[i2 % 2]
                    for kc in range(8):
                        pe(lambda: nc.tensor.matmul(ps.t[:, 0:n], ut[:, a_, kc, :], h2s.t[:, kc, 0:n], start=(kc == 0), stop=(kc == 7)), [ur, h2s.r], [ps.r], final=(kc == 7))
                    act(lambda: nc.scalar.activation(out=tg_.t[:, 0:n], in_=ps.t[:, 0:n], func=AF.Gelu), [ps.r], [tg_.r])
                    dve(lambda: nc.vector.tensor_tensor(out=GW.t[:, i2, 0:n], in0=GW.t[:, i2, 0:n], in1=tg_.t[:, 0:n], op=ALU.mult), [GW.r, tg_.r], [GW.r])
            sy.barrier()
            for blk in (range(8) if "V" in p2 else ()):
                vt_, vr = Vc[blk % 2]
                for hv in range(2):
                    sy.dma("sp", vt_[:, hv * 8:(hv + 1) * 8, :], Vs[:, blk * 16 + hv * 8:blk * 16 + (hv + 1) * 8, :], reads=[tab], writes=[vr])
                for dc in range(8):
                    ps = PS[4 + dc % 2]
                    for ii in range(16):
                        pe(lambda: nc.tensor.matmul(ps.t[:, 0:n], vt_[:, ii, dc * 128:(dc + 1) * 128], GW.t[:, blk * 16 + ii, 0:n], start=(ii == 0), stop=(ii == 15)), [vr, GW.r], [ps.r], final=(ii == 15))
                    if blk == 0:
                        act(lambda: nc.scalar.copy(out=accO.t[:, dc, 0:n], in_=ps.t[:, 0:n]), [ps.r], [accO.r])
                    else:
                        dve(lambda: nc.vector.tensor_tensor(out=accO.t[:, dc, 0:n], in0=ps.t[:, 0:n], in1=accO.t[:, dc, 0:n], op=ALU.add), [ps.r, accO.r], [accO.r])
            xres = _overlap_res(xT_res[b], c0, n)
            if "R" in p2:
                segs = []
                if c0 < L:
                    segs.append((0, min(n, L - c0), b))
                if c0 + n > L:
                    segs.append((max(0, L - c0), n, 4))
                for (a0, a1, j) in segs:
                    dve(lambda: nc.vector.tensor_tensor(out=accO.t[:, :, a0:a1], in0=accO.t[:, :, a0:a1], in1=MOD.t[:, l, 5, :, j:j + 1].to_broadcast([128, 8, a1 - a0]), op=ALU.mult),
                        [accO.r, MOD.r], [accO.r])
                sy.dma("pq", xT_d[b, :, :, c0:c0 + n], accO.t[:, :, 0:n], reads=[accO.r], writes=xres, accum_op=ALU.add)
            sy.barrier()
    tap(f"x{l}", xT_d[b], [128, 8, T], F32, xT_res[b])


def final_phase(state, consts, dram, cfg, nb_run, norm_chunk):
    if cfg.get("skip_final"):
        return
    nc, sy, PS = state["nc"], state["sy"], state["PS"]
    ident = consts["ident"]
    xT_d, xT_res, out_d = dram["xT_d"], dram["xT_res"], dram["out_d"]
    with contextlib.ExitStack() as sf:
        def mk(name, shape, dt):
            _uid[0] += 1
            return Tile(sf.enter_context(nc.sbuf_tensor(f"{name}_{_uid[0]}", list(shape), dt)))
        xt = [mk("xt", [128, 8, 512], F32) for _ in range(2)]
        sq = mk("sq", [128, 8, 512], BF16)
        tmp = [mk("tmp", [128, 8, 512], F32) for _ in range(2)]
        rs = [mk("rs", [128, 512], F32) for _ in range(2)]
        ot = [mk("ot", [128, 1024], F32) for _ in range(2)]
        k = 0
        for b in range(nb_run):
            for ci in range(4):
                t0, n = CHUNKS[ci]
                x_t, tm_ = xt[ci % 2], tmp[ci % 2]
                sy.dma("sp", x_t.t[:, :, 0:n], xT_d[b, :, :, t0:t0 + n], reads=[xT_res[b][ci]], writes=[x_t.r])
                norm_chunk(b, 0, ci, 2, x_t, sq, None, None, PS[ci % 2], rs[ci % 2], tm_)
                for tt in range(4):
                    o_ = ot[k % 2]
                    for half in range(2):
                        ps = PS[2 + (2 * k + half) % 4]
                        for c4 in range(4):
                            c = half * 4 + c4
                            sy.op("pe", lambda: nc.tensor.transpose(out=ps.t[:, c4 * 128:(c4 + 1) * 128], in_=tm_.t[:, c, tt * 128:(tt + 1) * 128], identity=ident.t[:]),
                                  reads=[tm_.r, ident.r], writes=[ps.r])
                        if half == 0:
                            sy.op("act", lambda: nc.scalar.copy(out=o_.t[:, 0:512], in_=ps.t[:, 0:512]), reads=[ps.r], writes=[o_.r])
                        else:
                            sy.op("dve", lambda: nc.vector.tensor_copy(out=o_.t[:, 512:1024], in_=ps.t[:, 0:512]), reads=[ps.r], writes=[o_.r])
                    k += 1
                    sy.dma("sp", out_d[b, t0 + tt * 128:t0 + (tt + 1) * 128, :], o_.t[:], reads=[o_.r], writes=[Res()])
        sy.barrier()


_W_NAMES = ["c_ctx", "w_mod", "b_mod", "norm1_g", "norm2_g", "w_in", "w_gate", "b_gate", "attn_sink", "lam_q1", "lam_k1", "lam_q2", "lam_k2",
            "diff_norm_g", "conv_w", "conv_b", "conv_ln_g", "conv_ln_b", "pool_w", "pool_scale", "w_branch", "w_out", "peer_wq", "peer_k1",
            "peer_k2", "peer_u", "peer_v", "final_g"]


def kernel(**inputs):
    n_cores = 8
    nc, _ = build_program({})
    shared = {k: np.ascontiguousarray(np.asarray(inputs[k], dtype=np.float32)) for k in _W_NAMES}
    in_maps = []
    for i in range(n_cores):
        m = dict(shared)
        for k in ("x", "c", "ctx"):
            m[k] = np.ascontiguousarray(np.asarray(inputs[k], dtype=np.float32)[i * NBC:(i + 1) * NBC])
        in_maps.append(m)
    res = run_bass_kernel_spmd(nc, in_maps, core_ids=list(range(n_cores)))
    return np.concatenate([np.asarray(r["out"], dtype=np.float32) for r in res.results], axis=0)
```
